# Optimizing a Trainium2 kernel written in Bass

```python
import math
import jax, jax.numpy as jnp
from jax import lax
import numpy as np

D_MODEL = 1024
BATCH = 1
SEQ = 16384
DEPTH = 2

CTX_LEN = 256
GRID_W = 64
EPS = 1e-6
ROPE_THETA = 10000.0
Q_BLOCK = 128

GLA_HEADS = 4
GLA_DK = 64
GLA_DV = 128
GLA_GATE_RANK = 16
GLA_TAU = 16.0
GLA_CHUNK = 64
GLA_QK = GLA_HEADS * GLA_DK
GLA_VW = GLA_HEADS * GLA_DV

MLA_HEADS = 4
MLA_Q_LORA = 256
MLA_KV_LORA = 128
MLA_NOPE = 128
MLA_ROPE = 64
MLA_V = 128
MLA_VW = MLA_HEADS * MLA_V

MIX_WIDTH = GLA_VW + MLA_VW
AB_SIZES = (GLA_QK, GLA_QK, GLA_VW, GLA_VW, GLA_GATE_RANK, GLA_GATE_RANK, MLA_Q_LORA, MLA_KV_LORA, MLA_ROPE)
IN_AB = GLA_QK + GLA_QK + GLA_VW + GLA_VW + GLA_GATE_RANK + GLA_GATE_RANK + MLA_Q_LORA + MLA_KV_LORA + MLA_ROPE

NA_HEADS = 16
NA_DH = D_MODEL // NA_HEADS
NA_KH_MAX = 8
NA_KW = 16

N_EXPERTS = 32
TOP_K = 4
D_FF_EXPERT = D_MODEL
SWIGLU_LIMIT = 7.0
SWIGLU_ALPHA = 1.702
MOE_BLOCK = 128

N_EVEN = (DEPTH + 1) // 2
N_ODD = DEPTH // 2

kernel_name = "hybrid_gla_mla_natten_moe_dit"


def rmsnorm(x, g):
    xf = x.astype(jnp.float32)
    y = xf * lax.rsqrt(jnp.mean(xf * xf, axis=-1, keepdims=True) + EPS)
    return (y * g.astype(jnp.float32)).astype(x.dtype)


def axial_rope(n_tok, rot_dim, dtype):
    t = jnp.arange(n_tok)
    row = (t // GRID_W).astype(jnp.float32)
    col = (t % GRID_W).astype(jnp.float32)
    n_freq = rot_dim // 4
    inv = ROPE_THETA ** (-jnp.arange(n_freq, dtype=jnp.float32) / n_freq)
    ang = jnp.concatenate([row[:, None] * inv, col[:, None] * inv], axis=-1)
    return jnp.cos(ang).astype(dtype), jnp.sin(ang).astype(dtype)


def apply_rope(x, cos, sin):
    x1, x2 = jnp.split(x, 2, axis=-1)
    return jnp.concatenate([x1 * cos - x2 * sin, x1 * sin + x2 * cos], axis=-1)


def dense_attention_blocks(q, k, v, scale):
    B, S, H, dq = q.shape
    nb = S // Q_BLOCK
    qb = jnp.moveaxis(q.reshape(B, nb, Q_BLOCK, H, dq), 1, 0)

    def one(qi):
        s = jnp.einsum('bqhd,bkhd->bhqk', qi, k).astype(jnp.float32) * scale
        p = jax.nn.softmax(s, axis=-1).astype(v.dtype)
        return jnp.einsum('bhqk,bkhv->bqhv', p, v)

    o = lax.map(one, qb)
    return jnp.moveaxis(o, 0, 1).reshape(B, S, H, v.shape[-1])


def gla_scan(q, k, v, log_a, s0):
    B, T, H, DK = q.shape
    DV = v.shape[-1]
    nc = T // GLA_CHUNK

    def to_chunks(a):
        return jnp.moveaxis(a.astype(jnp.float32).reshape(B, nc, GLA_CHUNK, H, a.shape[-1]), 1, 0)

    causal = jnp.tril(jnp.ones((GLA_CHUNK, GLA_CHUNK), dtype=bool))

    def step(s, inp):
        qc, kc, vc, gc = inp
        b = jnp.cumsum(gc, axis=1)
        diff = b[:, :, None] - b[:, None, :]
        decay = jnp.exp(jnp.where(causal[None, :, :, None, None], diff, -jnp.inf))
        a = jnp.sum(qc[:, :, None] * kc[:, None] * decay, axis=-1)
        o_intra = jnp.einsum('btsh,bshv->bthv', a, vc)
        o_inter = jnp.einsum('bthd,bhdv->bthv', qc * jnp.exp(b), s)
        b_last = b[:, -1]
        k_dec = kc * jnp.exp(b_last[:, None] - b)
        s_new = jnp.exp(b_last)[..., None] * s + jnp.einsum('bshd,bshv->bhdv', k_dec, vc)
        return s_new, o_intra + o_inter

    s_fin, o = lax.scan(step, s0, (to_chunks(q), to_chunks(k), to_chunks(v), to_chunks(log_a)))
    o = jnp.moveaxis(o, 0, 1).reshape(B, T, H, DV)
    return o.astype(v.dtype), s_fin


def mixer_gla_mla(h_l, h_c, in_w, wa_f, ba_f, wa_b, ba_b, onorm, qnorm, wuq, kvnorm, wukv, out_w, cos, sin, need_ctx):
    split_pts = np.cumsum(AB_SIZES)[:-1].tolist()

    def project(h, rotary):
        B, T, _ = h.shape
        q, k, v, r, a_f, a_b, cq, ckv, kr = jnp.split(h @ in_w, split_pts, axis=-1)
        q = q.reshape(B, T, GLA_HEADS, GLA_DK) * (GLA_DK ** -0.5)
        k = k.reshape(B, T, GLA_HEADS, GLA_DK)
        v = v.reshape(B, T, GLA_HEADS, GLA_DV)
        la_f = jax.nn.log_sigmoid((a_f @ wa_f + ba_f).astype(jnp.float32)).reshape(B, T, GLA_HEADS, GLA_DK) / GLA_TAU
        la_b = jax.nn.log_sigmoid((a_b @ wa_b + ba_b).astype(jnp.float32)).reshape(B, T, GLA_HEADS, GLA_DK) / GLA_TAU
        qm = (rmsnorm(cq, qnorm) @ wuq).reshape(B, T, MLA_HEADS, MLA_NOPE + MLA_ROPE)
        kvm = (rmsnorm(ckv, kvnorm) @ wukv).reshape(B, T, MLA_HEADS, MLA_NOPE + MLA_V)
        q_nope, q_rope = jnp.split(qm, [MLA_NOPE], axis=-1)
        k_nope, vm = jnp.split(kvm, [MLA_NOPE], axis=-1)
        if rotary:
            q_rope = apply_rope(q_rope, cos[:, None, :], sin[:, None, :])
            kr = apply_rope(kr, cos, sin)
        qm = jnp.concatenate([q_nope, q_rope], axis=-1)
        km = jnp.concatenate([k_nope, jnp.broadcast_to(kr[:, :, None, :], (B, T, MLA_HEADS, MLA_ROPE))], axis=-1)
        return (q, k, v, la_f, la_b, r), (qm, km, vm)

    (q_l, k_l, v_l, fl, bl, r_l), (qm_l, km_l, vm_l) = project(h_l, True)
    (q_c, k_c, v_c, fc, bc, r_c), (qm_c, km_c, vm_c) = project(h_c, False)
    B, S, _ = h_l.shape
    flip = lambda a: a[:, ::-1]
    s0 = jnp.zeros((q_l.shape[0], GLA_HEADS, GLA_DK, GLA_DV), jnp.float32)
    o_cf, s_cf = gla_scan(q_c, k_c, v_c, fc, s0)
    o_cb, s_cb = gla_scan(flip(q_c), flip(k_c), flip(v_c), flip(bc), s0)
    o_lf, _ = gla_scan(q_l, k_l, v_l, fl, s_cf)
    o_lb, _ = gla_scan(flip(q_l), flip(k_l), flip(v_l), flip(bl), s_cb)

    def gla_out(o, r):
        return rmsnorm(o, onorm).reshape(o.shape[0], o.shape[1], GLA_VW) * jax.nn.silu(r)

    scale = (MLA_NOPE + MLA_ROPE) ** -0.5
    k_all = jnp.concatenate([km_c, km_l], axis=1)
    v_all = jnp.concatenate([vm_c, vm_l], axis=1)
    o_mla_l = dense_attention_blocks(qm_l, k_all, v_all, scale)
    y_l = jnp.concatenate([gla_out(o_lf + flip(o_lb), r_l), o_mla_l.reshape(B, S, MLA_VW)], axis=-1) @ out_w
    if not need_ctx:
        return y_l, None
    o_mla_c = dense_attention_blocks(qm_c, km_c, vm_c, scale)
    y_c = jnp.concatenate([gla_out(o_cf + flip(o_cb), r_c), o_mla_c.reshape(B, CTX_LEN, MLA_VW)], axis=-1) @ out_w
    return y_l, y_c


def neighbourhood_attention(q, k, v, k_ctx, v_ctx, rpb):
    B, S, H, dh = q.shape
    rows = S // GRID_W
    kh = min(NA_KH_MAX, rows)
    kw = NA_KW
    scale = dh ** -0.5
    qg = q.reshape(B, rows, GRID_W, H, dh)
    kg = k.reshape(B, rows, GRID_W, H, dh)
    vg = v.reshape(B, rows, GRID_W, H, dh)
    col = jnp.arange(GRID_W)
    c0 = jnp.clip(col - kw // 2, 0, GRID_W - kw)
    col_idx = c0[:, None] + jnp.arange(kw)[None, :]
    col_bias_idx = col_idx - col[:, None] + NA_KW - 1
    rpb_f = rpb.astype(jnp.float32)

    def one_row(r):
        r0 = jnp.clip(r - kh // 2, 0, rows - kh)
        q_r = lax.dynamic_index_in_dim(qg, r, axis=1, keepdims=False)
        k_rows = lax.dynamic_slice_in_dim(kg, r0, kh, axis=1)
        v_rows = lax.dynamic_slice_in_dim(vg, r0, kh, axis=1)
        k_nb = k_rows[:, :, col_idx]
        v_nb = v_rows[:, :, col_idx]
        s_nb = jnp.einsum('bqhd,bkqwhd->bhqkw', q_r, k_nb).astype(jnp.float32) * scale
        row_bias_idx = r0 + jnp.arange(kh) - r + NA_KH_MAX - 1
        bias = jnp.transpose(rpb_f[:, row_bias_idx][:, :, col_bias_idx], (0, 2, 1, 3))
        s_nb = (s_nb + bias[None]).reshape(B, H, GRID_W, kh * kw)
        s_ctx = jnp.einsum('bqhd,bkhd->bhqk', q_r, k_ctx).astype(jnp.float32) * scale
        p = jax.nn.softmax(jnp.concatenate([s_nb, s_ctx], axis=-1), axis=-1).astype(v.dtype)
        p_nb = p[..., :kh * kw].reshape(B, H, GRID_W, kh, kw)
        p_ctx = p[..., kh * kw:]
        return (jnp.einsum('bhqkw,bkqwhd->bqhd', p_nb, v_nb)
                + jnp.einsum('bhqk,bkhd->bqhd', p_ctx, v_ctx))

    o = lax.map(one_row, jnp.arange(rows))
    return jnp.moveaxis(o, 0, 1).reshape(B, S, H, dh)


def mixer_na(h_l, h_c, qkv_w, rpb, out_w, need_ctx):
    def proj(h):
        B, T, _ = h.shape
        p = (h @ qkv_w).reshape(B, T, 3, NA_HEADS, NA_DH)
        return p[:, :, 0], p[:, :, 1], p[:, :, 2]

    q_l, k_l, v_l = proj(h_l)
    q_c, k_c, v_c = proj(h_c)
    B, S, _ = h_l.shape
    y_l = neighbourhood_attention(q_l, k_l, v_l, k_c, v_c, rpb).reshape(B, S, D_MODEL) @ out_w
    if not need_ctx:
        return y_l, None
    y_c = dense_attention_blocks(q_c, k_c, v_c, NA_DH ** -0.5).reshape(B, CTX_LEN, D_MODEL) @ out_w
    return y_l, y_c


def moe_ffn(h, router_w, router_b, w1, b1, w2, b2):
    T, D = h.shape
    logits = (h @ router_w + router_b).astype(jnp.float32)
    top_val, top_idx = lax.top_k(logits, TOP_K)
    gates = jax.nn.softmax(top_val, axis=-1)
    n_assign = T * TOP_K
    flat_e = top_idx.reshape(-1)
    flat_tok = jnp.arange(n_assign, dtype=jnp.int32) // TOP_K
    order = jnp.argsort(flat_e)
    e_sorted = flat_e[order]
    counts = jnp.bincount(flat_e, length=N_EXPERTS)
    padded = ((counts + MOE_BLOCK - 1) // MOE_BLOCK) * MOE_BLOCK
    pad_end = jnp.cumsum(padded)
    pad_start = pad_end - padded
    start = jnp.cumsum(counts) - counts
    slot = pad_start[e_sorted] + jnp.arange(n_assign) - start[e_sorted]
    n_blocks = -(-n_assign // MOE_BLOCK) + N_EXPERTS
    n_slots = n_blocks * MOE_BLOCK
    slot_tok = jnp.full((n_slots,), T, jnp.int32).at[slot].set(flat_tok[order])
    slot_gate = jnp.zeros((n_slots,), jnp.float32).at[slot].set(gates.reshape(-1)[order])
    block_e = jnp.minimum(jnp.searchsorted(pad_end, jnp.arange(n_blocks) * MOE_BLOCK, side='right'), N_EXPERTS - 1)
    h_pad = jnp.concatenate([h, jnp.zeros((1, D), h.dtype)], axis=0)
    xb = h_pad[slot_tok].reshape(n_blocks, MOE_BLOCK, D)

    def expert_block(args):
        x_blk, e = args
        u = x_blk @ w1[e] + b1[e]
        glu = jnp.minimum(u[:, ::2], SWIGLU_LIMIT)
        lin = jnp.clip(u[:, 1::2], -SWIGLU_LIMIT, SWIGLU_LIMIT)
        act = glu * jax.nn.sigmoid(SWIGLU_ALPHA * glu) * (lin + 1)
        return act @ w2[e] + b2[e]

    yb = lax.map(expert_block, (xb, block_e)).reshape(n_slots, D)
    y = jnp.zeros((T + 1, D), h.dtype).at[slot_tok].add(yb * slot_gate[:, None].astype(h.dtype))
    return y[:T]


def setup_inputs(seed: int = 0) -> dict:
    key = jax.random.key(seed)
    ks = iter(jax.random.split(key, 40))
    f32 = jnp.float32

    def nrm(shape, fan_in, mult=1.0):
        return jax.random.normal(next(ks), shape, f32) * (mult * fan_in ** -0.5)

    def gain(shape):
        return 1.0 + 0.05 * jax.random.normal(next(ks), shape, f32)

    def small(shape, s=0.02):
        return s * jax.random.normal(next(ks), shape, f32)

    D = D_MODEL
    F = D_FF_EXPERT
    return {
        "x": jax.random.normal(next(ks), (BATCH, SEQ, D), f32),
        "c": jax.random.normal(next(ks), (BATCH, D), f32),
        "ctx": jax.random.normal(next(ks), (BATCH, CTX_LEN, D), f32),
        "c_ctx": jax.random.normal(next(ks), (D,), f32),
        "ada_w": nrm((DEPTH, D, 6 * D), D, 0.2),
        "ada_b": small((DEPTH, 6 * D)),
        "norm_g": gain((DEPTH, 4, D)),
        "router_w": nrm((DEPTH, D, N_EXPERTS), D),
        "router_b": small((DEPTH, N_EXPERTS), 0.01),
        "moe_w1": nrm((DEPTH, N_EXPERTS, D, 2 * F), D),
        "moe_b1": small((DEPTH, N_EXPERTS, 2 * F)),
        "moe_w2": nrm((DEPTH, N_EXPERTS, F, D), F),
        "moe_b2": small((DEPTH, N_EXPERTS, D)),
        "ab_in_w": nrm((N_EVEN, D, IN_AB), D),
        "gla_wa_f": nrm((N_EVEN, GLA_GATE_RANK, GLA_QK), GLA_GATE_RANK),
        "gla_ba_f": small((N_EVEN, GLA_QK), 0.1),
        "gla_wa_b": nrm((N_EVEN, GLA_GATE_RANK, GLA_QK), GLA_GATE_RANK),
        "gla_ba_b": small((N_EVEN, GLA_QK), 0.1),
        "gla_onorm": gain((N_EVEN, GLA_DV)),
        "mla_qnorm": gain((N_EVEN, MLA_Q_LORA)),
        "mla_wuq": nrm((N_EVEN, MLA_Q_LORA, MLA_HEADS * (MLA_NOPE + MLA_ROPE)), MLA_Q_LORA),
        "mla_kvnorm": gain((N_EVEN, MLA_KV_LORA)),
        "mla_wukv": nrm((N_EVEN, MLA_KV_LORA, MLA_HEADS * (MLA_NOPE + MLA_V)), MLA_KV_LORA),
        "ab_out_w": nrm((N_EVEN, MIX_WIDTH, D), MIX_WIDTH),
        "na_qkv_w": nrm((N_ODD, D, 3 * D), D),
        "na_rpb": small((N_ODD, NA_HEADS, 2 * NA_KH_MAX - 1, 2 * NA_KW - 1), 0.1),
        "na_out_w": nrm((N_ODD, D, D), D),
    }


def reference(x, c, ctx, c_ctx, ada_w, ada_b, norm_g, router_w, router_b, moe_w1, moe_b1, moe_w2, moe_b2,
              ab_in_w, gla_wa_f, gla_ba_f, gla_wa_b, gla_ba_b, gla_onorm, mla_qnorm, mla_wuq, mla_kvnorm, mla_wukv,
              ab_out_w, na_qkv_w, na_rpb, na_out_w):
    B, S, D = x.shape
    cos, sin = axial_rope(S, MLA_ROPE, x.dtype)
    silu_c = jax.nn.silu(c)
    silu_cc = jax.nn.silu(c_ctx)[None]
    xc = ctx
    for l in range(DEPTH):
        last = l == DEPTH - 1
        sh1_l, sc1_l, g1_l, sh2_l, sc2_l, g2_l = jnp.split((silu_c @ ada_w[l] + ada_b[l])[:, None, :], 6, axis=-1)
        sh1_c, sc1_c, g1_c, sh2_c, sc2_c, g2_c = jnp.split((silu_cc @ ada_w[l] + ada_b[l])[:, None, :], 6, axis=-1)
        h_l = rmsnorm(x, norm_g[l, 0]) * (1 + sc1_l) + sh1_l
        h_c = rmsnorm(xc, norm_g[l, 0]) * (1 + sc1_c) + sh1_c
        i = l // 2
        if l % 2 == 0:
            o_l, o_c = mixer_gla_mla(h_l, h_c, ab_in_w[i], gla_wa_f[i], gla_ba_f[i], gla_wa_b[i], gla_ba_b[i],
                                     gla_onorm[i], mla_qnorm[i], mla_wuq[i], mla_kvnorm[i], mla_wukv[i],
                                     ab_out_w[i], cos, sin, not last)
        else:
            o_l, o_c = mixer_na(h_l, h_c, na_qkv_w[i], na_rpb[i], na_out_w[i], not last)
        x = x + rmsnorm(o_l, norm_g[l, 1]) * g1_l
        h_l = rmsnorm(x, norm_g[l, 2]) * (1 + sc2_l) + sh2_l
        if not last:
            xc = xc + rmsnorm(o_c, norm_g[l, 1]) * g1_c
            h_c = rmsnorm(xc, norm_g[l, 2]) * (1 + sc2_c) + sh2_c
            tokens = jnp.concatenate([h_c.reshape(-1, D), h_l.reshape(-1, D)], axis=0)
            y = moe_ffn(tokens, router_w[l], router_b[l], moe_w1[l], moe_b1[l], moe_w2[l], moe_b2[l])
            y_c = y[:B * CTX_LEN].reshape(B, CTX_LEN, D)
            y_l = y[B * CTX_LEN:].reshape(B, S, D)
            xc = xc + rmsnorm(y_c, norm_g[l, 3]) * g2_c
        else:
            y_l = moe_ffn(h_l.reshape(-1, D), router_w[l], router_b[l], moe_w1[l], moe_b1[l], moe_w2[l],
                          moe_b2[l]).reshape(B, S, D)
        x = x + rmsnorm(y_l, norm_g[l, 3]) * g2_l
    return x
```

```python
import numpy as np
import ml_dtypes
import concourse.bass as bass
import concourse.mybir as mybir
from concourse.bass_utils import run_bass_kernel_spmd

F32 = mybir.dt.float32
BF16 = mybir.dt.bfloat16
I32 = mybir.dt.int32
ALU = mybir.AluOpType
AF = mybir.ActivationFunctionType
AX = mybir.AxisListType

COMPUTE = ("pe", "act", "dve", "pool")


class View:
    __slots__ = ("b", "ap")

    def __init__(self, b, ap):
        self.b = b
        self.ap = ap

    def __getitem__(self, k):
        return View(self.b, self.ap[k])

    def bitcast(self, dt):
        return View(self.b, self.ap.bitcast(dt))

    def rearrange(self, s, **kw):
        return View(self.b, self.ap.rearrange(s, **kw))

    def to_broadcast(self, shape):
        return View(self.b, self.ap.to_broadcast(shape))


class Buf:
    def __init__(self, prog, t, name):
        self.p = prog
        self.t = t
        self.name = name
        self.w = None
        self.r = {}
        self.wsem = None
        self.wcnt = 0
        self.rsem = None
        self.rcnt = 0

    def __getitem__(self, k):
        return View(self, self.t[k])

    @property
    def v(self):
        return View(self, self.t.ap())


class Prog:
    def __init__(self, name="k"):
        self.nc = bass.Bass("TRN2", target_bir_lowering=False, name=name)
        nc = self.nc
        self.E = dict(pe=nc.tensor, act=nc.scalar, dve=nc.vector, pool=nc.gpsimd, sp=nc.sync)
        self.sem = {e: nc.alloc_semaphore("sem_" + e) for e in COMPUTE}
        self.cnt = {e: 0 for e in COMPUTE}
        self.pending = {e: False for e in COMPUTE}
        self.seen = {e: {} for e in self.E}
        self.bufs = []
        self.nsem = 4
        self.ninst = 0
        self.sempool = []
        self.semid = {}
        self.stack = None
        self.scope_bufs = None
        self.nscope = 0

    def sb(self, name, shape, dt=F32):
        if self.stack is not None:
            t = self.stack.enter_context(self.nc.sbuf_tensor("sb%d_%s" % (self.nscope, name), list(shape), dt))
        else:
            t = self.nc.alloc_sbuf_tensor("sb_" + name, list(shape), dt)
        b = Buf(self, t, name)
        self.bufs.append(b)
        if self.scope_bufs is not None:
            self.scope_bufs.append(b)
        return b

    def ps(self, name, shape, dt=F32):
        if self.stack is not None:
            t = self.stack.enter_context(self.nc.psum_tensor("ps%d_%s" % (self.nscope, name), list(shape), dt))
        else:
            t = self.nc.alloc_psum_tensor("ps_" + name, list(shape), dt)
        b = Buf(self, t, name)
        self.bufs.append(b)
        if self.scope_bufs is not None:
            self.scope_bufs.append(b)
        return b

    def scratch(self, name, shape, dt=F32, dbg=False):
        return self.nc.dram_tensor(name, list(shape), dt, kind="ExternalOutput" if dbg else "Internal").ap()

    def begin(self):
        from contextlib import ExitStack
        self.nscope += 1
        self.stack = ExitStack()
        self.scope_bufs = []

    def barrier(self):
        for e in self.E:
            for f in COMPUTE:
                if self.cnt[f]:
                    self._wait(e, f, self.sem[f], self.cnt[f])
            for b in self.bufs:
                if b.wsem is not None and b.wcnt:
                    self._wait(e, ("s", id(b.wsem)), b.wsem, 16 * b.wcnt)
                if b.rsem is not None and b.rcnt:
                    self._wait(e, ("s", id(b.rsem)), b.rsem, 16 * b.rcnt)

    def end(self):
        self.barrier()
        for b in self.scope_bufs:
            if b.wsem is not None:
                self.sempool.append([b.wsem, b.wcnt]); b.wsem = None
            if b.rsem is not None:
                self.sempool.append([b.rsem, b.rcnt]); b.rsem = None
            self.bufs.remove(b)
        self.stack.close()
        self.stack = None
        self.scope_bufs = None

    def _getsem(self, name):
        if self.sempool:
            s, c = self.sempool.pop()
            return s, c
        return self._newsem(name), 0

    def dram(self, name, shape, dt=F32, out=False):
        if not out:
            self.__dict__.setdefault("in_names", []).append(name)
        return self.nc.dram_tensor(name, list(shape), dt, kind="ExternalOutput" if out else "ExternalInput").ap()

    def _newsem(self, name):
        self.nsem += 1
        return self.nc.alloc_semaphore(name)

    def _wait(self, e, key, sem, val):
        if self.seen[e].get(key, 0) >= val:
            return
        self.E[e].wait_ge(sem, val)
        self.seen[e][key] = val

    def _w_done(self, e, b):
        if b.w is not None:
            f, n = b.w
            if not (f == "pe" and e == "pe"):
                self._wait(e, f, self.sem[f], n)
        if b.wcnt:
            self._wait(e, ("s", id(b.wsem)), b.wsem, 16 * b.wcnt)

    def _r_done(self, e, b):
        for f, n in b.r.items():
            if f == "pe" and e == "pe":
                continue
            self._wait(e, f, self.sem[f], n)
        if b.rcnt:
            self._wait(e, ("s", id(b.rsem)), b.rsem, 16 * b.rcnt)

    def op(self, e, fn, track=True, **kw):
        wkeys = ("out", "accum_out", "ap")
        reads = [v.b for k, v in kw.items() if isinstance(v, View) and k not in wkeys]
        writes = [kw[k].b for k in wkeys if k in kw and isinstance(kw[k], View)]
        for b in reads:
            self._w_done(e, b)
        for b in writes:
            self._w_done(e, b)
            self._r_done(e, b)
        args = {k: (v.ap if isinstance(v, View) else v) for k, v in kw.items()}
        ins = getattr(self.E[e], fn)(**args)
        self.ninst += 1
        seq = self.cnt[e] + 1
        if track:
            ins.then_inc(self.sem[e], 1)
            self.cnt[e] = seq
            self.pending[e] = False
        else:
            self.pending[e] = True
        for b in reads:
            b.r[e] = seq
        for b in writes:
            b.w = (e, seq)
            b.r = {}
        return ins

    def dma(self, q, out, in_, **kw):
        if isinstance(out, View):
            b = out.b
            self._w_done(q, b)
            self._r_done(q, b)
            if b.wsem is None:
                b.wsem, b.wcnt = self._getsem("dw%d_%s" % (self.nscope, b.name))
            ins = self.E[q].dma_start(out=out.ap, in_=in_, **kw)
            ins.then_inc(b.wsem, 16)
            b.wcnt += 1
            b.w = None
            b.r = {}
        else:
            b = in_.b
            self._w_done(q, b)
            if b.rsem is None:
                b.rsem, b.rcnt = self._getsem("dr%d_%s" % (self.nscope, b.name))
            ins = self.E[q].dma_start(out=out, in_=in_.ap, **kw)
            ins.then_inc(b.rsem, 16)
            b.rcnt += 1
        self.ninst += 1
        return ins

    def gather(self, out, src_ap, idx):
        q = "pool"
        b = out.b
        self._w_done(q, b); self._r_done(q, b); self._w_done(q, idx.b)
        if b.wsem is None:
            b.wsem, b.wcnt = self._getsem("dw%d_%s" % (self.nscope, b.name))
        ins = self.nc.gpsimd.indirect_dma_start(out=out.ap, out_offset=None, in_=src_ap,
                                                in_offset=bass.IndirectOffsetOnAxis(ap=idx.ap, axis=0))
        ins.then_inc(b.wsem, 16)
        b.wcnt += 1; b.w = None; b.r = {}
        idx.b.r["pool"] = self.cnt["pool"] + 1
        self.ninst += 1
        return ins

    def finish(self, q="sp"):
        for b in self.bufs:
            if b.rcnt:
                self._wait(q, ("s", id(b.rsem)), b.rsem, 16 * b.rcnt)
        for e in COMPUTE:
            if self.cnt[e]:
                self._wait(q, e, self.sem[e], self.cnt[e])
        return self.nc


def run(prog, in_maps, ncores=8, trace=False):
    res = run_bass_kernel_spmd(prog.nc, in_maps, core_ids=list(range(ncores)), trace=trace)
    return res


EPS = 1e-6


def ident(P):
    idf = P.sb("idf", [128, 128]); idb = P.sb("idb", [128, 128], BF16)
    P.op("pool", "memset", ap=idf.v, constant=1.0)
    P.op("pool", "affine_select", out=idf.v, in_=idf.v, pattern=[[-1, 128]], compare_op=ALU.is_equal, fill=0.0, base=0, channel_multiplier=1)
    P.op("pool", "tensor_copy", out=idb.v, in_=idf.v)
    return idf, idb


def load_w_bf16(P, w_d, K, N, name, q="sp", cast_eng="pool", stage=None):
    KC = (K + 127) // 128
    wb = P.sb(name, [128, KC, N], BF16)
    for k in range(KC):
        rows = min(128, K - k * 128)
        st = stage[k % len(stage)]
        P.dma(q, st[0:rows, 0:N], w_d[k * 128:k * 128 + rows, :])
        P.op(cast_eng, "tensor_copy", out=wb[0:rows, k, :], in_=st[0:rows, 0:N])
    return wb


def rstd_of(P, ss_view, out_view, tmp_view, D):
    P.op("dve", "tensor_scalar", out=tmp_view, in0=ss_view, scalar1=1.0 / D, scalar2=EPS, op0=ALU.mult, op1=ALU.add)
    P.op("act", "activation", out=tmp_view, in_=tmp_view, func=AF.Sqrt)
    P.op("dve", "reciprocal", out=out_view, in_=tmp_view)


def build_proj(D, N, variants, rope=None, name="proj"):
    P = Prog(name)
    nt = len(variants); nv = max(variants) + 1
    KC = D // 128
    x_d = P.dram("x", [nt * 128, D]); w_d = P.dram("w", [D, N]); y_d = P.dram("y", [nt * 128, N], out=True)
    g_d = P.dram("gcol", [128, KC]); sc_d = P.dram("sc", [128, nv, KC]); sh_d = P.dram("sh", [128, nv, KC])
    if rope:
        cs_d = P.dram("cs", [nt * 128, 2, rope[1], 32])
    idf, idb = ident(P)
    stage = [P.sb("wst%d" % i, [128, N]) for i in range(2)]
    wb = load_w_bf16(P, w_d, D, N, "wb", stage=stage)
    g = P.sb("g", [128, KC]); sc = P.sb("scm", [128, nv, KC]); sh = P.sb("shm", [128, nv, KC])
    P.dma("sp", g.v, g_d); P.dma("sp", sc.v, sc_d); P.dma("sp", sh.v, sh_d)
    for v in range(nv):
        P.op("dve", "scalar_tensor_tensor", out=sc[:, v, :], in0=sc[:, v, :], scalar=1.0, in1=g.v, op0=ALU.add, op1=ALU.mult)
    xs = [P.sb("x%d" % i, [128, D]) for i in range(2)]
    junk = P.sb("junk", [128, D]); st = [P.sb("st%d" % i, [128, 4]) for i in range(2)]
    xn = [P.sb("xn%d" % i, [128, D], BF16) for i in range(2)]
    hT = [P.sb("hT%d" % i, [128, KC, 128], BF16) for i in range(2)]
    ys = [P.sb("y%d" % i, [128, N]) for i in range(2)]
    pT = [P.ps("pT%d" % i, [128, KC, 128], BF16) for i in range(2)]
    py = [P.ps("py%d" % i, [128, 512]) for i in range(4)]
    if rope:
        cs = [P.sb("cs%d" % i, [128, 2, rope[1], 32]) for i in range(2)]
        rt = [P.sb("rt%d" % i, [128, rope[1], 32]) for i in range(4)]
    nchunks = [(c, min(512, N - c)) for c in range(0, N, 512)]
    ci = 0
    for t in range(nt):
        b = t % 2; v = variants[t]
        P.dma("sp", xs[b].v, x_d[t * 128:(t + 1) * 128, :])
        if rope and v == 0:
            P.dma("sp", cs[b].v, cs_d[t * 128:(t + 1) * 128])
        P.op("act", "activation", out=junk.v, in_=xs[b].v, func=AF.Square, accum_out=st[b][:, 0:1])
        rstd_of(P, st[b][:, 0:1], st[b][:, 2:3], st[b][:, 1:2], D)
        P.op("dve", "tensor_scalar", out=xn[b].v, in0=xs[b].v, scalar1=st[b][:, 2:3], scalar2=None, op0=ALU.mult)
        for k in range(KC):
            P.op("pe", "transpose", out=pT[b][:, k, :], in_=xn[b][:, k * 128:(k + 1) * 128], identity=idb.v)
        for k in range(KC):
            P.op("dve", "tensor_scalar", out=hT[b][:, k, :], in0=pT[b][:, k, :], scalar1=sc[:, v, k:k + 1], scalar2=sh[:, v, k:k + 1], op0=ALU.mult, op1=ALU.add)
        for (c0, cn) in nchunks:
            pb = py[ci % 4]; ci += 1
            for k in range(KC):
                P.op("pe", "matmul", track=(k == KC - 1), out=pb[:, 0:cn], lhsT=hT[b][:, k, :], rhs=wb[:, k, c0:c0 + cn], start=(k == 0), stop=(k == KC - 1))
            P.op("act", "activation", out=ys[b][:, c0:c0 + cn], in_=pb[:, 0:cn], func=AF.Copy)
        if rope and v == 0:
            col0, ns, stride, off = rope
            seg = ys[b][:, col0:col0 + ns * stride].rearrange("p (h d) -> p h d", d=stride)
            x1 = seg[:, :, off:off + 32]; x2 = seg[:, :, off + 32:off + 64]
            co = cs[b][:, 0]; si = cs[b][:, 1]
            P.op("pool", "tensor_tensor", out=rt[0].v, in0=x1, in1=co, op=ALU.mult)
            P.op("pool", "tensor_tensor", out=rt[1].v, in0=x2, in1=si, op=ALU.mult)
            P.op("pool", "tensor_tensor", out=rt[2].v, in0=x1, in1=si, op=ALU.mult)
            P.op("pool", "tensor_tensor", out=rt[3].v, in0=x2, in1=co, op=ALU.mult)
            P.op("pool", "tensor_tensor", out=x1, in0=rt[0].v, in1=rt[1].v, op=ALU.subtract)
            P.op("pool", "tensor_tensor", out=x2, in0=rt[2].v, in1=rt[3].v, op=ALU.add)
        P.dma("sp", y_d[t * 128:(t + 1) * 128, :], ys[b].v)
    P.finish()
    return P


def build_post(mode, variants, name="post"):
    P = Prog(name)
    nt = len(variants); nv = max(variants) + 1; T = nt * 128; D = 1024; KC = 8
    x_d = P.dram("x", [T, D]); ow_d = P.dram("ow", [D, D])
    g1_d = P.dram("g1g", [128, D]); gate_d = P.dram("gate", [nv, 128, D])
    g2_d = P.dram("gcol", [128, KC]); sc_d = P.dram("sc", [128, nv, KC]); sh_d = P.dram("sh", [128, nv, KC])
    rw_d = P.dram("rw", [D, 32]); rb_d = P.dram("rb", [128, 32])
    x1_d = P.dram("x1", [T, D], out=True); hT_d = P.dram("hT", [D, T], BF16, out=True); G_d = P.dram("G", [T, 32], out=True)
    if mode == "gla":
        of_d = P.dram("of", [T, 512]); ob_d = P.dram("ob", [T, 512]); r_d = P.dram("r", [T, 512]); om_d = P.dram("om", [T, 512])
        on_d = P.dram("oncol", [128, 1])
    else:
        aT_d = P.dram("aT", [D, T])
    idf, idb = ident(P)
    stage = [P.sb("wst%d" % i, [128, D]) for i in range(2)]
    wb = load_w_bf16(P, ow_d, D, D, "owb", stage=stage)
    rw = P.sb("rw", [128, KC, 32]); P.dma("sp", rw.v, rw_d.rearrange("(k p) n -> p k n", p=128))
    rb = P.sb("rb", [128, 32]); P.dma("sp", rb.v, rb_d)
    g2 = P.sb("g2", [128, KC]); sc = P.sb("scm", [128, nv, KC]); sh = P.sb("shm", [128, nv, KC])
    P.dma("sp", g2.v, g2_d); P.dma("sp", sc.v, sc_d); P.dma("sp", sh.v, sh_d)
    for v in range(nv):
        P.op("dve", "scalar_tensor_tensor", out=sc[:, v, :], in0=sc[:, v, :], scalar=1.0, in1=g2.v, op0=ALU.add, op1=ALU.mult)
    g1 = P.sb("g1", [128, D]); P.dma("sp", g1.v, g1_d)
    GG = []
    for v in range(nv):
        gg = P.sb("GG%d" % v, [128, D]); P.dma("sp", gg.v, gate_d[v])
        P.op("dve", "tensor_tensor", out=gg.v, in0=gg.v, in1=g1.v, op=ALU.mult)
        GG.append(gg)
    if mode == "gla":
        onc = P.sb("onc", [128, 1]); P.dma("sp", onc.v, on_d)
        tof = P.sb("tof", [128, 512]); tob = P.sb("tob", [128, 512]); tr = P.sb("tr", [128, 512]); tom = P.sb("tom", [128, 512])
        cat = P.sb("cat", [128, D], BF16)
        pT = P.ps("pT", [128, KC, 128], BF16)
    else:
        a32 = P.sb("a32", [128, KC, 128])
    aT = P.sb("aT", [128, KC, 128], BF16)
    xt = P.sb("xt", [128, D]); x1 = P.sb("x1", [128, D]); junk = P.sb("junk", [128, D]); st = P.sb("st", [128, 16])
    xn2 = P.sb("xn2", [128, D]); h2T = P.sb("h2T", [128, KC, 128]); h2Tb = P.sb("h2Tb", [128, KC, 128], BF16)
    lg = P.sb("lg", [128, 32]); t8 = P.sb("t8", [128, 8]); msk = P.sb("msk", [128, 32]); ex = P.sb("ex", [128, 32]); Gt = P.sb("Gt", [128, 32])
    py = [P.ps("py%d" % i, [128, 512]) for i in range(2)]
    pT32 = P.ps("pT32", [128, KC, 128])
    plg = P.ps("plg", [128, 32])
    for t in range(nt):
        v = variants[t]; sl = slice(t * 128, (t + 1) * 128)
        P.dma("sp", xt.v, x_d[sl, :])
        if mode == "gla":
            P.dma("sp", tof.v, of_d[sl, :]); P.dma("sp", tob.v, ob_d[sl, :]); P.dma("sp", tr.v, r_d[sl, :]); P.dma("sp", tom.v, om_d[sl, :])
            P.op("dve", "tensor_tensor", out=tof.v, in0=tof.v, in1=tob.v, op=ALU.add)
            for h in range(4):
                P.op("act", "activation", out=junk[:, 0:128], in_=tof[:, h * 128:(h + 1) * 128], func=AF.Square, accum_out=st[:, h:h + 1])
            rstd_of(P, st[:, 0:4], st[:, 8:12], st[:, 4:8], 128)
            P.op("act", "activation", out=tr.v, in_=tr.v, func=AF.Silu)
            for h in range(4):
                hs = slice(h * 128, (h + 1) * 128)
                P.op("dve", "scalar_tensor_tensor", out=cat[:, hs], in0=tof[:, hs], scalar=st[:, 8 + h:9 + h], in1=tr[:, hs], op0=ALU.mult, op1=ALU.mult)
            P.op("pool", "tensor_copy", out=cat[:, 512:1024], in_=tom.v)
            for k in range(KC):
                P.op("pe", "transpose", out=pT[:, k, :], in_=cat[:, k * 128:(k + 1) * 128], identity=idb.v)
            P.op("dve", "tensor_scalar", out=aT[:, 0:4, :], in0=pT[:, 0:4, :], scalar1=onc[:, 0:1], scalar2=None, op0=ALU.mult)
            P.op("act", "activation", out=aT[:, 4:8, :], in_=pT[:, 4:8, :], func=AF.Copy)
        else:
            P.dma("sp", a32.v, aT_d[:, sl].rearrange("(k p) t -> p k t", p=128))
            P.op("pool", "tensor_copy", out=aT.v, in_=a32.v)
        for n in range(2):
            for k in range(KC):
                P.op("pe", "matmul", track=(k == KC - 1), out=py[n].v, lhsT=aT[:, k, :], rhs=wb[:, k, n * 512:(n + 1) * 512], start=(k == 0), stop=(k == KC - 1))
            P.op("act", "activation", out=junk[:, 0:512], in_=py[n].v, func=AF.Square, accum_out=st[:, 12 + n:13 + n])
        P.op("dve", "tensor_tensor", out=st[:, 12:13], in0=st[:, 12:13], in1=st[:, 13:14], op=ALU.add)
        rstd_of(P, st[:, 12:13], st[:, 14:15], st[:, 13:14], D)
        for n in range(2):
            ns = slice(n * 512, (n + 1) * 512)
            P.op("dve", "scalar_tensor_tensor", out=x1[:, ns], in0=py[n].v, scalar=st[:, 14:15], in1=GG[v][:, ns], op0=ALU.mult, op1=ALU.mult)
        P.op("pool", "tensor_tensor", out=x1.v, in0=x1.v, in1=xt.v, op=ALU.add)
        P.dma("sp", x1_d[sl, :], x1.v)
        P.op("act", "activation", out=junk.v, in_=x1.v, func=AF.Square, accum_out=st[:, 15:16])
        rstd_of(P, st[:, 15:16], st[:, 4:5], st[:, 5:6], D)
        P.op("dve", "tensor_scalar", out=xn2.v, in0=x1.v, scalar1=st[:, 4:5], scalar2=None, op0=ALU.mult)
        for k in range(KC):
            P.op("pe", "transpose", out=pT32[:, k, :], in_=xn2[:, k * 128:(k + 1) * 128], identity=idf.v)
        for k in range(KC):
            P.op("dve", "tensor_scalar", out=h2T[:, k, :], in0=pT32[:, k, :], scalar1=sc[:, v, k:k + 1], scalar2=sh[:, v, k:k + 1], op0=ALU.mult, op1=ALU.add)
        P.op("pool", "tensor_copy", out=h2Tb.v, in_=h2T.v)
        P.dma("sp", hT_d[:, sl].rearrange("(k p) t -> p k t", p=128), h2Tb.v)
        for k in range(KC):
            P.op("pe", "matmul", track=(k == KC - 1), out=plg.v, lhsT=h2T[:, k, :], rhs=rw[:, k, :], start=(k == 0), stop=(k == KC - 1))
        P.op("dve", "tensor_tensor", out=lg.v, in0=plg.v, in1=rb.v, op=ALU.add)
        P.op("dve", "max", out=t8.v, in_=lg.v)
        P.op("dve", "tensor_scalar", out=msk.v, in0=lg.v, scalar1=t8[:, 3:4], scalar2=None, op0=ALU.is_ge)
        P.op("dve", "tensor_scalar", out=t8[:, 7:8], in0=t8[:, 0:1], scalar1=-1.0, scalar2=None, op0=ALU.mult)
        P.op("act", "activation", out=ex.v, in_=lg.v, func=AF.Exp, bias=t8[:, 7:8])
        P.op("dve", "tensor_tensor", out=ex.v, in0=ex.v, in1=msk.v, op=ALU.mult)
        P.op("dve", "tensor_reduce", out=t8[:, 6:7], in_=ex.v, axis=AX.X, op=ALU.add)
        P.op("dve", "reciprocal", out=t8[:, 5:6], in_=t8[:, 6:7])
        P.op("dve", "tensor_scalar", out=Gt.v, in0=ex.v, scalar1=t8[:, 5:6], scalar2=None, op0=ALU.mult)
        P.dma("sp", G_d[sl, :], Gt.v)
    P.finish()
    return P


def build_moe(variants, groups, NE=32, name="moe"):
    P = Prog(name)
    nt = len(variants); nv = max(variants) + 1; T = nt * 128; D = 1024; KC = 8; F = 1024
    hT_d = P.dram("hT", [D, T], BF16); G_d = P.dram("G", [T, 32]); GT_d = P.dram("GT", [32, T]); x1_d = P.dram("x1", [T, D])
    w1_d = P.dram("w1", [32, D, 2 * F]); w2_d = P.dram("w2", [32, F, D])
    b1g_d = P.dram("b1g", [128, 32, KC]); b1l_d = P.dram("b1l", [128, 32, KC]); b2_d = P.dram("b2", [32, D])
    g3_d = P.dram("g3g", [128, D]); gate_d = P.dram("gate", [nv, 128, D])
    x2_d = P.dram("x2", [T, D], out=True)
    mg = max(len(g) for g in groups)
    g3 = P.sb("g3", [128, D]); P.dma("sp", g3.v, g3_d)
    GG = []
    for v in range(nv):
        gg = P.sb("GG%d" % v, [128, D]); P.dma("sp", gg.v, gate_d[v])
        P.op("dve", "tensor_tensor", out=gg.v, in0=gg.v, in1=g3.v, op=ALU.mult)
        GG.append(gg)
    b1g = P.sb("b1g", [128, 32, KC]); b1l = P.sb("b1l", [128, 32, KC]); P.dma("sp", b1g.v, b1g_d); P.dma("sp", b1l.v, b1l_d)
    b2 = P.sb("b2", [32, D]); P.dma("sp", b2.v, b2_d)
    stg = [P.sb("stg%d" % i, [128, 2 * F]) for i in range(2)]
    w1g = [P.sb("w1g%d" % i, [128, KC, F], BF16) for i in range(2)]
    w1l = [P.sb("w1l%d" % i, [128, KC, F], BF16) for i in range(2)]
    w2b = [P.sb("w2b%d" % i, [128, KC, D], BF16) for i in range(2)]
    hT = P.sb("hT", [128, KC, mg * 128], BF16)
    Gs = P.sb("Gs", [128, mg, 32]); GTs = P.sb("GTs", [32, mg * 128])
    yacc = P.sb("yacc", [128, mg, D])
    actT = P.sb("actT", [128, KC, 512], BF16)
    tg = [P.sb("tg%d" % i, [128, 512]) for i in range(2)]; tsg = [P.sb("tsg%d" % i, [128, 512]) for i in range(2)]
    tl = [P.sb("tl%d" % i, [128, 512]) for i in range(2)]
    xt = P.sb("xt", [128, D]); junk = P.sb("junk", [128, D]); st = P.sb("st", [128, 4])
    pg = [P.ps("pg%d" % i, [128, 512]) for i in range(2)]; pl = [P.ps("pl%d" % i, [128, 512]) for i in range(2)]
    py = [P.ps("py%d" % i, [128, 512]) for i in range(2)]
    sti = [0]

    def load_expert_steps(e, buf):
        steps = []
        for k in range(KC):
            def step(k=k):
                s = stg[sti[0] % 2]; sti[0] += 1
                P.dma("sp", s.v, w1_d[e, k * 128:(k + 1) * 128, :])
                sv = s.v.rearrange("p (f two) -> p f two", two=2)
                P.op("act", "activation", out=w1g[buf][:, k, :], in_=sv[:, :, 0], func=AF.Copy)
                P.op("act", "activation", out=w1l[buf][:, k, :], in_=sv[:, :, 1], func=AF.Copy)
                s = stg[sti[0] % 2]; sti[0] += 1
                P.dma("sp", s[:, 0:D], w2_d[e, k * 128:(k + 1) * 128, :])
                P.op("act", "activation", out=w2b[buf][:, k, :], in_=s[:, 0:D], func=AF.Copy)
            steps.append(step)
        return steps

    for grp in groups:
        ng = len(grp)
        t0 = grp[0]; tok0 = t0 * 128; ntok = ng * 128
        P.dma("sp", hT[:, :, 0:ntok], hT_d[:, tok0:tok0 + ntok].rearrange("(k p) t -> p k t", p=128))
        P.dma("sp", Gs[:, 0:ng, :], G_d[tok0:tok0 + ntok, :].rearrange("(g p) e -> p g e", p=128))
        P.dma("sp", GTs[:, 0:ntok], GT_d[:, tok0:tok0 + ntok])
        for i in range(ng):
            for n in range(2):
                P.op("pe", "matmul", out=py[n].v, lhsT=GTs[:, i * 128:(i + 1) * 128], rhs=b2[:, n * 512:(n + 1) * 512], start=True, stop=True)
                P.op("act", "activation", out=yacc[:, i, n * 512:(n + 1) * 512], in_=py[n].v, func=AF.Copy)
        for s in load_expert_steps(0, 0):
            s()
        blocks = [(b0, min(512, ntok - b0)) for b0 in range(0, ntok, 512)]
        ci = 0
        for e in range(NE):
            buf = e % 2
            nxt = load_expert_steps(e + 1, 1 - buf) if e + 1 < NE else []
            for bi, (b0, bn) in enumerate(blocks):
                for j in range(KC):
                    if bi == 0 and nxt:
                        nxt[j]()
                    c = ci % 2; ci += 1
                    for k in range(KC):
                        P.op("pe", "matmul", track=(k == KC - 1), out=pg[c][:, 0:bn], lhsT=w1g[buf][:, k, j * 128:(j + 1) * 128], rhs=hT[:, k, b0:b0 + bn], start=(k == 0), stop=(k == KC - 1))
                    for k in range(KC):
                        P.op("pe", "matmul", track=(k == KC - 1), out=pl[c][:, 0:bn], lhsT=w1l[buf][:, k, j * 128:(j + 1) * 128], rhs=hT[:, k, b0:b0 + bn], start=(k == 0), stop=(k == KC - 1))
                    P.op("dve", "tensor_scalar", out=tg[c][:, 0:bn], in0=pg[c][:, 0:bn], scalar1=b1g[:, e, j:j + 1], scalar2=7.0, op0=ALU.add, op1=ALU.min)
                    P.op("act", "activation", out=tsg[c][:, 0:bn], in_=tg[c][:, 0:bn], func=AF.Sigmoid, scale=1.702)
                    P.op("dve", "tensor_scalar", out=tl[c][:, 0:bn], in0=pl[c][:, 0:bn], scalar1=b1l[:, e, j:j + 1], scalar2=-7.0, op0=ALU.add, op1=ALU.max)
                    P.op("dve", "tensor_scalar", out=tl[c][:, 0:bn], in0=tl[c][:, 0:bn], scalar1=7.0, scalar2=1.0, op0=ALU.min, op1=ALU.add)
                    P.op("pool", "tensor_tensor", out=tg[c][:, 0:bn], in0=tg[c][:, 0:bn], in1=tsg[c][:, 0:bn], op=ALU.mult)
                    P.op("pool", "tensor_tensor", out=actT[:, j, 0:bn], in0=tg[c][:, 0:bn], in1=tl[c][:, 0:bn], op=ALU.mult)
                for i in range(b0 // 128, (b0 + bn) // 128):
                    for n in range(2):
                        ns = slice(n * 512, (n + 1) * 512)
                        for j in range(KC):
                            P.op("pe", "matmul", track=(j == KC - 1), out=py[n].v, lhsT=actT[:, j, i * 128 - b0:(i + 1) * 128 - b0], rhs=w2b[buf][:, j, ns], start=(j == 0), stop=(j == KC - 1))
                        P.op("dve", "scalar_tensor_tensor", out=yacc[:, i, ns], in0=py[n].v, scalar=Gs[:, i, e:e + 1], in1=yacc[:, i, ns], op0=ALU.mult, op1=ALU.add)
        for i in range(ng):
            t = grp[i]; v = variants[t]; sl = slice(t * 128, (t + 1) * 128)
            P.dma("sp", xt.v, x1_d[sl, :])
            P.op("act", "activation", out=junk.v, in_=yacc[:, i, :], func=AF.Square, accum_out=st[:, 0:1])
            rstd_of(P, st[:, 0:1], st[:, 2:3], st[:, 1:2], D)
            P.op("dve", "scalar_tensor_tensor", out=junk.v, in0=yacc[:, i, :], scalar=st[:, 2:3], in1=GG[v].v, op0=ALU.mult, op1=ALU.mult)
            P.op("pool", "tensor_tensor", out=xt.v, in0=junk.v, in1=xt.v, op=ALU.add)
            P.dma("sp", x2_d[sl, :], xt.v)
    P.finish()
    return P


def build_ada(name="ada"):
    P = Prog(name)
    NC_ = 1536
    w_d = P.dram("w", [1024, NC_]); b_d = P.dram("b", [2, NC_]); c_d = P.dram("cT", [128, 8, 2]); o_d = P.dram("mod", [2, NC_], out=True)
    w = P.sb("w", [128, 8, NC_]); P.dma("sp", w.v, w_d.rearrange("(k p) n -> p k n", p=128))
    b = P.sb("b", [2, NC_]); P.dma("sp", b.v, b_d)
    c = P.sb("c", [128, 8, 2]); P.dma("sp", c.v, c_d)
    o = P.sb("o", [2, NC_])
    P.op("act", "activation", out=c.v, in_=c.v, func=AF.Silu)
    pp = [P.ps("pp%d" % i, [2, 512]) for i in range(3)]
    for n in range(3):
        for k in range(8):
            P.op("pe", "matmul", track=(k == 7), out=pp[n].v, lhsT=c[:, k, :], rhs=w[:, k, n * 512:(n + 1) * 512], start=(k == 0), stop=(k == 7))
        P.op("dve", "tensor_tensor", out=o[:, n * 512:(n + 1) * 512], in0=pp[n].v, in1=b[:, n * 512:(n + 1) * 512], op=ALU.add)
    P.dma("sp", o_d, o.v)
    P.finish()
    return P


def build_gla(nchunks, name="gla"):
    P = Prog(name)
    Tt = nchunks * 64; CB = 8
    qT_d = P.dram("qT", [64, Tt]); kT_d = P.dram("kT", [64, Tt]); k_d = P.dram("k", [Tt, 64]); v_d = P.dram("v", [Tt, 128])
    aT_d = P.dram("aT", [17, Tt]); wa_d = P.dram("wa", [17, 64]); tri_d = P.dram("tri", [64, 64]); mu_d = P.dram("mu", [64, CB, 64])
    o_d = P.dram("o", [Tt, 128], out=True)
    wa = P.sb("wa", [17, 64]); P.dma("sp", wa.v, wa_d)
    tri = P.sb("tri", [64, 64]); P.dma("sp", tri.v, tri_d)
    mu = P.sb("mu", [64, CB, 64]); P.dma("sp", mu.v, mu_d)
    one = P.sb("one", [64, 1]); P.op("dve", "memset", ap=one.v, constant=1.0)
    S = [P.sb("S%d" % i, [64, 128]) for i in range(2)]
    P.op("dve", "memset", ap=S[0].v, constant=0.0)
    tmp = [P.sb("tmp%d" % i, [64, 128]) for i in range(2)]
    L = lambda nm, shp: [P.sb("%s%d" % (nm, i), shp) for i in range(2)]
    qTb = L("qTb", [64, CB * 64]); kTb = L("kTb", [64, CB * 64]); aTb = L("aTb", [17, CB * 64]); kb = L("kb", [64, CB, 64]); vb = L("vb", [64, CB, 128])
    le = L("le", [64, CB, 64]); E1 = L("E1", [64, CB * 64]); E2 = L("E2", [64, CB * 64]); E3 = L("E3", [64, CB, 64])
    qs = L("qs", [64, CB * 64]); ks = L("ks", [64, CB * 64]); kd = L("kd", [64, CB, 64]); ATm = L("ATm", [64, CB, 64]); ob = L("ob", [64, CB, 128])
    pXA = P.ps("pXA", [64, CB, 64]); pb = P.ps("pb", [64, CB, 64]); pbT = P.ps("pbT", [64, CB, 64]); pA = P.ps("pA", [64, CB, 64])
    pKV = P.ps("pKV", [64, CB, 128]); po = P.ps("po", [64, CB, 128])
    cur = 0; ti = 0
    blocks = [(c0, min(CB, nchunks - c0)) for c0 in range(0, nchunks, CB)]
    for bi, (c0, n) in enumerate(blocks):
        u = bi % 2; t0 = c0 * 64; W = n * 64
        P.dma("sp", qTb[u][:, 0:W], qT_d[:, t0:t0 + W]); P.dma("sp", kTb[u][:, 0:W], kT_d[:, t0:t0 + W]); P.dma("sp", aTb[u][:, 0:W], aT_d[:, t0:t0 + W])
        P.dma("sp", kb[u][:, 0:n, :], k_d[t0:t0 + W, :].rearrange("(c p) d -> p c d", p=64))
        P.dma("sp", vb[u][:, 0:n, :], v_d[t0:t0 + W, :].rearrange("(c p) d -> p c d", p=64))
        for c in range(n):
            P.op("pe", "matmul", out=pXA[:, c, :], lhsT=aTb[u][:, c * 64:(c + 1) * 64], rhs=wa.v, start=True, stop=True, track=(c == n - 1))
        P.op("act", "activation", out=le[u][:, 0:n, :], in_=pXA[:, 0:n, :], func=AF.Exp, scale=-1.0)
        P.op("act", "activation", out=le[u][:, 0:n, :], in_=le[u][:, 0:n, :], func=AF.Ln, bias=one[:, 0:1])
        for c in range(n):
            P.op("pe", "matmul", out=pb[:, c, :], lhsT=tri.v, rhs=le[u][:, c, :], start=True, stop=True, track=False)
            P.op("pe", "matmul", out=pbT[:, c, :], lhsT=le[u][:, c, :], rhs=tri.v, start=True, stop=True, track=(c == n - 1))
        pbTf = pbT.v.rearrange("p c t -> p (c t)")
        P.op("act", "activation", out=E1[u][:, 0:W], in_=pbTf[:, 0:W], func=AF.Exp)
        P.op("act", "activation", out=E2[u][:, 0:W], in_=pbTf[:, 0:W], func=AF.Exp, scale=-1.0)
        P.op("act", "activation", out=E3[u][:, 0:n, :], in_=pb[:, 0:n, :], func=AF.Exp, scale=-1.0)
        P.op("dve", "scalar_tensor_tensor", out=qs[u][:, 0:W], in0=qTb[u][:, 0:W], scalar=0.125, in1=E1[u][:, 0:W], op0=ALU.mult, op1=ALU.mult)
        P.op("dve", "tensor_tensor", out=ks[u][:, 0:W], in0=kTb[u][:, 0:W], in1=E2[u][:, 0:W], op=ALU.mult)
        P.op("pool", "tensor_tensor", out=kd[u][:, 0:n, :], in0=kb[u][:, 0:n, :], in1=E3[u][:, 0:n, :], op=ALU.mult)
        for c in range(n):
            cs_ = slice(c * 64, (c + 1) * 64)
            P.op("pe", "matmul", out=pA[:, c, :], lhsT=ks[u][:, cs_], rhs=qs[u][:, cs_], start=True, stop=True, track=(c == n - 1))
        P.op("dve", "tensor_tensor", out=ATm[u][:, 0:n, :], in0=pA[:, 0:n, :], in1=mu[:, 0:n, :], op=ALU.mult)
        for c in range(n):
            P.op("pe", "matmul", out=pKV[:, c, :], lhsT=kd[u][:, c, :], rhs=vb[u][:, c, :], start=True, stop=True, track=(c == n - 1))
        for c in range(n):
            cs_ = slice(c * 64, (c + 1) * 64)
            P.op("pe", "matmul", out=po[:, c, :], lhsT=qs[u][:, cs_], rhs=S[cur].v, start=True, stop=False, track=False)
            P.op("pe", "matmul", out=po[:, c, :], lhsT=ATm[u][:, c, :], rhs=vb[u][:, c, :], start=False, stop=True)
            tb = tmp[ti % 2]; ti += 1
            P.op("dve", "tensor_tensor", out=tb.v, in0=S[cur].v, in1=pKV[:, c, :], op=ALU.add)
            P.op("dve", "tensor_scalar", out=S[1 - cur].v, in0=tb.v, scalar1=E1[u][:, c * 64 + 63:c * 64 + 64], scalar2=None, op0=ALU.mult)
            cur = 1 - cur
        P.op("act", "activation", out=ob[u][:, 0:n, :], in_=po[:, 0:n, :], func=AF.Copy)
        P.dma("sp", o_d[t0:t0 + W, :].rearrange("(c p) d -> p c d", p=64), ob[u][:, 0:n, :])
    P.finish()
    return P


def build_mla(groups, NK, name="mla"):
    P = Prog(name)
    NQ = max(q0 + nq for q0, nq, _ in groups)
    NB = NK // 128
    qT_d = P.dram("qT", [193, NQ]); kT_d = P.dram("kT", [193, NK]); v_d = P.dram("V", [NK, 129]); o_d = P.dram("O", [NQ, 128], out=True)
    idf, idb = ident(P)
    kTa = P.sb("kTa", [128, NK], BF16); kTb = P.sb("kTb", [65, NK], BF16); Vb = P.sb("Vb", [128, NB, 129], BF16)
    stg = [P.sb("stg%d" % i, [128, 2048]) for i in range(2)]
    si = 0
    for c0 in range(0, NK, 2048):
        cn = min(2048, NK - c0)
        for (r0, rn, dst) in ((0, 128, kTa), (128, 65, kTb)):
            s = stg[si % 2]; si += 1
            P.dma("sp", s[0:rn, 0:cn], kT_d[r0:r0 + rn, c0:c0 + cn])
            P.op("pool" if si % 2 else "act", "tensor_copy" if si % 2 else "copy", out=dst[0:rn, c0:c0 + cn], in_=s[0:rn, 0:cn])
    VB = 15
    for b0 in range(0, NB, VB):
        bn = min(VB, NB - b0)
        s = stg[si % 2]; si += 1
        sv = s[:, 0:bn * 129].rearrange("p (b f) -> p b f", f=129)
        P.dma("sp", sv, v_d[b0 * 128:(b0 + bn) * 128, :].rearrange("(b p) f -> p b f", p=128))
        P.op("pool" if si % 2 else "act", "tensor_copy" if si % 2 else "copy", out=Vb[:, b0:b0 + bn, :], in_=sv)
    qst = [P.sb("qst%d" % i, [128, 512]) for i in range(2)]
    qa = [P.sb("qa%d" % i, [128, 512], BF16) for i in range(2)]; qb = [P.sb("qb%d" % i, [65, 512], BF16) for i in range(2)]
    mx = P.sb("mx", [128, 4, 40]); negm = P.sb("negm", [128, 4]); Z = P.sb("Z", [128, 65])
    P.op("dve", "memset", ap=Z.v, constant=0.0)
    pt = [P.sb("pt%d" % i, [128, 512], BF16) for i in range(2)]
    rl = P.sb("rl", [128, 4]); ot = [P.sb("ot%d" % i, [128, 4, 128]) for i in range(2)]
    ps1 = [P.ps("ps1_%d" % i, [128, 512]) for i in range(2)]; pst = [P.ps("pst%d" % i, [128, 512]) for i in range(2)]
    pO = [P.ps("pO%d" % i, [128, 129]) for i in range(4)]
    scale = 192.0 ** -0.5
    c1 = 0; c2 = 0
    for gi, (q0, nq, nkeys) in enumerate(groups):
        u = gi % 2; nqt = nq // 128
        P.dma("sp", qst[0][:, 0:nq], qT_d[0:128, q0:q0 + nq]); P.dma("sp", qst[1][0:64, 0:nq], qT_d[128:192, q0:q0 + nq])
        P.op("pool", "tensor_scalar", out=qa[u][:, 0:nq], in0=qst[0][:, 0:nq], scalar1=scale, scalar2=None, op0=ALU.mult)
        P.op("pool", "tensor_scalar", out=qb[u][0:64, 0:nq], in0=qst[1][0:64, 0:nq], scalar1=scale, scalar2=None, op0=ALU.mult)
        kblocks = [(k0, min(512, nkeys - k0)) for k0 in range(0, nkeys, 512)]
        for qt in range(nqt):
            qs_ = slice(qt * 128, (qt + 1) * 128)
            for bi, (k0, kn) in enumerate(kblocks):
                pb = ps1[c1 % 2]; c1 += 1
                P.op("pe", "matmul", track=False, out=pb[:, 0:kn], lhsT=qa[u][:, qs_], rhs=kTa[:, k0:k0 + kn], start=True, stop=False)
                P.op("pe", "matmul", out=pb[:, 0:kn], lhsT=qb[u][0:64, qs_], rhs=kTb[0:64, k0:k0 + kn], start=False, stop=True)
                P.op("dve", "tensor_reduce", out=mx[:, qt, bi:bi + 1], in_=pb[:, 0:kn], axis=AX.X, op=ALU.max)
            P.op("dve", "tensor_reduce", out=negm[:, qt:qt + 1], in_=mx[:, qt, 0:len(kblocks)], axis=AX.X, op=ALU.max)
            P.op("dve", "tensor_scalar", out=Z[:, 64:65], in0=negm[:, qt:qt + 1], scalar1=-1.0, scalar2=None, op0=ALU.mult)
            pz = ps1[c1 % 2]; c1 += 1
            P.op("pe", "matmul", out=pz[0:65, 0:128], lhsT=Z.v, rhs=idf.v, start=True, stop=True)
            P.op("dve", "tensor_copy", out=qb[u][64:65, qs_], in_=pz[64:65, 0:128])
        nkb = nkeys // 128
        for kb_ in range(nkb):
            ks_ = slice(kb_ * 128, (kb_ + 1) * 128)
            pb = pst[c2 % 2]; ptb = pt[c2 % 2]; c2 += 1
            P.op("pe", "matmul", track=False, out=pb[:, 0:nq], lhsT=kTa[:, ks_], rhs=qa[u][:, 0:nq], start=True, stop=False)
            P.op("pe", "matmul", out=pb[:, 0:nq], lhsT=kTb[0:65, ks_], rhs=qb[u][0:65, 0:nq], start=False, stop=True)
            P.op("act", "activation", out=ptb[:, 0:nq], in_=pb[:, 0:nq], func=AF.Exp)
            for qt in range(nqt):
                P.op("pe", "matmul", track=(kb_ == nkb - 1), out=pO[qt].v, lhsT=ptb[:, qt * 128:(qt + 1) * 128], rhs=Vb[:, kb_, :], start=(kb_ == 0), stop=(kb_ == nkb - 1))
        for qt in range(nqt):
            P.op("dve", "reciprocal", out=rl[:, qt:qt + 1], in_=pO[qt][:, 128:129])
            P.op("dve", "tensor_scalar", out=ot[u][:, qt, :], in0=pO[qt][:, 0:128], scalar1=rl[:, qt:qt + 1], scalar2=None, op0=ALU.mult)
        P.dma("sp", o_d[q0:q0 + nq, :].rearrange("(t p) d -> p t d", p=128), ot[u][:, 0:nqt, :])
    P.finish()
    return P


def build_na(nrows, edge_rows, name="na"):
    P = Prog(name)
    T = nrows * 64; ne = max(1, len(edge_rows))
    qT_d = P.dram("qT", [1024, T]); kw_d = P.dram("kwin", [nrows, 1024, 512]); vw_d = P.dram("vwin", [nrows, 512, 1024])
    kc_d = P.dram("kcT", [1024, 256]); vc_d = P.dram("vc", [256, 1024])
    bi_d = P.dram("Bint", [128, 8, 512]); be_d = P.dram("Bedge", [ne, 128, 8, 512])
    oT_d = P.dram("oT", [1024, T], out=True)
    idf, idb = ident(P)
    ones = P.sb("ones", [128, 128], BF16); P.op("pool", "memset", ap=ones.v, constant=1.0)
    kst = P.sb("kst", [128, 8, 512]); vst = P.sb("vst", [128, 4, 1024])
    kb = [P.sb("kb%d" % i, [128, 8, 512], BF16) for i in range(2)]; vb = [P.sb("vb%d" % i, [128, 4, 1024], BF16) for i in range(2)]
    kcb = P.sb("kcb", [128, 8, 256], BF16); vcb = P.sb("vcb", [128, 2, 1024], BF16)
    P.dma("sp", kst[:, :, 0:256], kc_d.rearrange("(k p) n -> p k n", p=128)); P.op("pool", "tensor_copy", out=kcb.v, in_=kst[:, :, 0:256])
    P.dma("sp", vst[:, 0:2, :], vc_d.rearrange("(b p) f -> p b f", p=128)); P.op("pool", "tensor_copy", out=vcb.v, in_=vst[:, 0:2, :])
    Bi = P.sb("Bi", [128, 8, 512]); P.dma("sp", Bi.v, bi_d)
    Be = [P.sb("Be%d" % i, [128, 8, 512]) for i in range(2)]
    qrow = [P.sb("qrow%d" % i, [128, 8, 64]) for i in range(2)]
    QBD = [P.sb("QBD%d" % i, [128, 128], BF16) for i in range(2)]
    for z in QBD:
        P.op("pool", "memset", ap=z.v, constant=0.0)
    Sb = [P.sb("Sb%d" % i, [128, 768]) for i in range(2)]; Pm = [P.sb("Pm%d" % i, [128, 768], BF16) for i in range(2)]
    PT = [P.sb("PT%d" % i, [128, 6, 128], BF16) for i in range(2)]
    sm = P.sb("sm", [128, 4]); rl = P.sb("rl", [128, 128]); orow = [P.sb("orow%d" % i, [128, 8, 64]) for i in range(2)]
    pS = [P.ps("pS%d" % i, [128, 1024]) for i in range(2)]
    pPT = P.ps("pPT", [128, 6, 128], BF16); pO = P.ps("pO", [128, 128]); pL = P.ps("pL", [128, 128])
    it = 0; nbe = 0
    for i in range(nrows):
        u = i % 2
        P.dma("sp", kst.v, kw_d[i].rearrange("(k p) n -> p k n", p=128)); P.op("pool", "tensor_copy", out=kb[u].v, in_=kst.v)
        P.dma("sp", vst.v, vw_d[i].rearrange("(b p) f -> p b f", p=128)); P.op("pool", "tensor_copy", out=vb[u].v, in_=vst.v)
        P.dma("sp", qrow[u].v, qT_d[:, i * 64:(i + 1) * 64].rearrange("(k p) t -> p k t", p=128))
        if i in edge_rows:
            B = Be[nbe % 2]; nbe += 1
            P.dma("sp", B.v, be_d[edge_rows[i]])
        else:
            B = Bi
        for hp in range(8):
            w = it % 2; it += 1
            P.op("pool", "tensor_scalar", out=QBD[w][0:64, 0:64], in0=qrow[u][0:64, hp, :], scalar1=0.125, scalar2=None, op0=ALU.mult)
            P.op("pool", "tensor_scalar", out=QBD[w][64:128, 64:128], in0=qrow[u][64:128, hp, :], scalar1=0.125, scalar2=None, op0=ALU.mult)
            P.op("pe", "matmul", out=pS[w][:, 0:512], lhsT=QBD[w].v, rhs=kb[u][:, hp, :], start=True, stop=True, track=False)
            P.op("pe", "matmul", out=pS[w][:, 512:768], lhsT=QBD[w].v, rhs=kcb[:, hp, :], start=True, stop=True)
            P.op("dve", "tensor_tensor", out=Sb[w][:, 0:512], in0=pS[w][:, 0:512], in1=B[:, hp, :], op=ALU.add)
            P.op("act", "activation", out=Sb[w][:, 512:768], in_=pS[w][:, 512:768], func=AF.Copy)
            P.op("dve", "tensor_reduce", out=sm[:, 0:1], in_=Sb[w].v, axis=AX.X, op=ALU.max)
            P.op("dve", "tensor_scalar", out=sm[:, 1:2], in0=sm[:, 0:1], scalar1=-1.0, scalar2=None, op0=ALU.mult)
            P.op("act", "activation", out=Pm[w].v, in_=Sb[w].v, func=AF.Exp, bias=sm[:, 1:2])
            for b in range(6):
                P.op("pe", "transpose", out=pPT[:, b, :], in_=Pm[w][:, b * 128:(b + 1) * 128], identity=idb.v, track=(b == 5))
            P.op("dve", "tensor_copy", out=PT[w].v, in_=pPT.v)
            hs = slice(hp * 128, (hp + 1) * 128)
            for b in range(6):
                vblk = vb[u][:, b, hs] if b < 4 else vcb[:, b - 4, hs]
                P.op("pe", "matmul", out=pO.v, lhsT=vblk, rhs=PT[w][:, b, :], start=(b == 0), stop=(b == 5), track=(b == 5))
            for b in range(6):
                P.op("pe", "matmul", out=pL.v, lhsT=ones.v, rhs=PT[w][:, b, :], start=(b == 0), stop=(b == 5), track=(b == 5))
            P.op("dve", "reciprocal", out=rl.v, in_=pL.v)
            P.op("dve", "tensor_tensor", out=orow[u][0:64, hp, :], in0=pO[0:64, 0:64], in1=rl[0:64, 0:64], op=ALU.mult)
            P.op("dve", "tensor_tensor", out=orow[u][64:128, hp, :], in0=pO[64:128, 64:128], in1=rl[64:128, 64:128], op=ALU.mult)
        P.dma("sp", oT_d[:, i * 64:(i + 1) * 64].rearrange("(k p) t -> p k t", p=128), orow[u].v)
    P.finish()
    return P


def na_bias_table(rpb, d):
    col = np.arange(64)
    c0 = np.clip(col - 8, 0, 48)
    kcol = np.arange(64)
    inwin = (kcol[None, :] >= c0[:, None]) & (kcol[None, :] < c0[:, None] + 16)
    cidx = np.clip(kcol[None, :] - col[:, None] + 15, 0, 30)
    ridx = np.arange(8) - d + 7
    t = rpb[:, ridx][:, :, cidx]
    t = np.where(inwin[None, None], t, np.float32(-30000.0)).astype(np.float32)
    t = t.transpose(0, 2, 1, 3).reshape(8, 2, 64, 512)
    return np.ascontiguousarray(t.transpose(1, 2, 0, 3).reshape(128, 8, 512))


TALL = 16640
NOWN = 22
VOWN = [0] * 20 + [1] * 2
TOWN = NOWN * 128


def modcols(P, modcol, l, i, g_d_view, name):
    t = P.sb(name, [128, 2, 8])
    for s in range(2):
        P.op("dve", "scalar_tensor_tensor", out=t[:, s, :], in0=modcol[:, l, i * 8:(i + 1) * 8, s], scalar=1.0, in1=g_d_view, op0=ALU.add, op1=ALU.mult)
    return t


def stage_ada(P, adaw_d, bcol_d, brow_d, cT_d, modcol, gates_s):
    P.begin()
    c = P.sb("c", [128, 8, 2]); P.dma("sp", c.v, cT_d)
    P.op("act", "activation", out=c.v, in_=c.v, func=AF.Silu)
    ones = P.sb("ones", [128, 128]); P.op("dve", "memset", ap=ones.v, constant=1.0)
    crep = P.sb("crep", [128, 2, 8, 128])
    for s in range(2):
        for k in range(8):
            P.op("dve", "tensor_scalar", out=crep[:, s, k, :], in0=ones.v, scalar1=c[:, k, s:s + 1], scalar2=None, op0=ALU.mult)
    bcol = P.sb("bcol", [128, 2, 48]); P.dma("sp", bcol.v, bcol_d)
    wp = [P.sb("wp%d" % i, [128, 8, 512]) for i in range(2)]
    brow = P.sb("brow", [128, 512]); grow = [P.sb("grow%d" % i, [128, 512]) for i in range(2)]
    pc = [P.ps("pc%d" % i, [128, 2]) for i in range(2)]; pr = [P.ps("pr%d" % i, [128, 512]) for i in range(2)]
    ci = 0; ri = 0
    for l in range(2):
        for piece in range(12):
            w = wp[piece % 2]
            P.dma("sp", w.v, adaw_d[l][:, piece * 512:(piece + 1) * 512].rearrange("(k p) n -> p k n", p=128))
            for jj in range(4):
                j = piece * 4 + jj
                p_ = pc[ci % 2]; ci += 1
                for k in range(8):
                    P.op("pe", "matmul", track=(k == 7), out=p_.v, lhsT=w[:, k, jj * 128:(jj + 1) * 128], rhs=c[:, k, :], start=(k == 0), stop=(k == 7))
                P.op("dve", "tensor_scalar", out=modcol[:, l, j, :], in0=p_.v, scalar1=bcol[:, l, j:j + 1], scalar2=None, op0=ALU.add)
            if piece in (4, 5, 10, 11):
                g = 0 if piece < 6 else 1; half = piece % 2
                P.dma("sp", brow.v, brow_d[l, :, piece * 512:(piece + 1) * 512])
                for s in range(2):
                    p_ = pr[ri % 2]; gr = grow[ri % 2]; ri += 1
                    for k in range(8):
                        P.op("pe", "matmul", track=(k == 7), out=p_.v, lhsT=crep[:, s, k, :], rhs=w[:, k, :], start=(k == 0), stop=(k == 7))
                    P.op("dve", "tensor_tensor", out=gr.v, in0=p_.v, in1=brow.v, op=ALU.add)
                    P.dma("sp", gates_s[l, g, s, :, half * 512:(half + 1) * 512], gr.v)
    P.end()


def stage_pro(P, idb, x_d, groups, gvar, gsc, gsh, D, wtm_d, ntm, tm_outs, wfm_d, nfm, fm_specs, rope=None, mla=None, tag="pro"):
    P.begin()
    KC = D // 128
    stg = [P.sb("wst%d" % i, [128, max(ntm, nfm, 1024)]) for i in range(2)]
    wtm = load_w_bf16(P, wtm_d, D, ntm, "wtm", stage=stg) if ntm else None
    wfm = load_w_bf16(P, wfm_d, D, nfm, "wfm", stage=stg) if nfm else None
    if mla:
        wuk = load_w_bf16(P, mla["wukv_d"], 128, 1024, "wuk", stage=stg)
        kvn = P.sb("kvn", [128, 1]); P.dma("sp", kvn.v, mla["kvn_d"])
        ckn = [P.sb("ckn%d" % i, [128, 128], BF16) for i in range(2)]
        ckT = [P.sb("ckT%d" % i, [128, 512], BF16) for i in range(2)]
        kst = [P.sb("kst%d" % i, [128, 512], BF16) for i in range(2)]; vst = [P.sb("vst%d" % i, [128, 512], BF16) for i in range(2)]
        pck = P.ps("pck", [128, 128], BF16)
    xs = [P.sb("x%d" % i, [128, D]) for i in range(2)]; junk = P.sb("junk", [128, D]); st = [P.sb("st%d" % i, [128, 8]) for i in range(2)]
    xn = [P.sb("xn%d" % i, [128, D], BF16) for i in range(2)]
    hT = [P.sb("hT%d" % i, [128, KC, 512], BF16) for i in range(2)]
    yt = [P.sb("yt%d" % i, [128, max(ntm, 1)]) for i in range(2)]
    ytb = [P.sb("ytb%d" % i, [128, max(ntm, 1)], BF16) for i in range(2)]
    fo = [P.sb("fo%d" % i, [128, 512]) for i in range(2)]; fob = [P.sb("fob%d" % i, [128, 512], BF16) for i in range(2)]
    pT = [P.ps("pT%d" % i, [128, KC, 128], BF16) for i in range(2)]
    py = [P.ps("py%d" % i, [128, 512]) for i in range(2)]; pf = [P.ps("pf%d" % i, [128, 512]) for i in range(2)]
    if rope:
        Ct = [P.sb("Ct%d" % i, [64, 512]) for i in range(2)]; St = [P.sb("St%d" % i, [64, 512]) for i in range(2)]
        r1 = P.sb("r1", [64, 512]); r2 = P.sb("r2", [64, 512]); rb_ = [P.sb("rb%d" % i, [64, 512], BF16) for i in range(2)]
    ti = 0; yi = 0; fi = 0
    for gi, (tok0, gt) in enumerate(groups):
        v = gvar[gi]; W = gt * 128; hg = hT[gi % 2]
        for t in range(gt):
            b = ti % 2; ti += 1
            r0 = tok0 + t * 128
            P.dma("sp", xs[b].v, x_d[r0:r0 + 128, :])
            P.op("act", "activation", out=junk.v, in_=xs[b].v, func=AF.Square, accum_out=st[b][:, 0:1])
            rstd_of(P, st[b][:, 0:1], st[b][:, 2:3], st[b][:, 1:2], D)
            P.op("dve", "tensor_scalar", out=xn[b].v, in0=xs[b].v, scalar1=st[b][:, 2:3], scalar2=None, op0=ALU.mult)
            for k in range(KC):
                P.op("pe", "transpose", out=pT[b][:, k, :], in_=xn[b][:, k * 128:(k + 1) * 128], identity=idb.v)
            for k in range(KC):
                P.op("dve", "tensor_scalar", out=hg[:, k, t * 128:(t + 1) * 128], in0=pT[b][:, k, :], scalar1=gsc[:, v, k:k + 1], scalar2=gsh[:, v, k:k + 1], op0=ALU.mult, op1=ALU.add)
            if ntm:
                for c0 in range(0, ntm, 512):
                    cn = min(512, ntm - c0); pb = py[yi % 2]; yi += 1
                    for k in range(KC):
                        P.op("pe", "matmul", track=(k == KC - 1), out=pb[:, 0:cn], lhsT=hg[:, k, t * 128:(t + 1) * 128], rhs=wtm[:, k, c0:c0 + cn], start=(k == 0), stop=(k == KC - 1))
                    P.op("act", "activation", out=yt[b][:, c0:c0 + cn], in_=pb[:, 0:cn], func=AF.Copy)
                for (c0, n, ap, dt) in tm_outs:
                    if dt == BF16:
                        P.op("pool", "tensor_copy", out=ytb[b][:, c0:c0 + n], in_=yt[b][:, c0:c0 + n])
                        P.dma("sp", ap[r0:r0 + 128, :], ytb[b][:, c0:c0 + n])
                    else:
                        P.dma("sp", ap[r0:r0 + 128, :], yt[b][:, c0:c0 + n])
            if mla:
                cc = mla["col"]
                P.op("act", "activation", out=junk[:, 0:128], in_=yt[b][:, cc:cc + 128], func=AF.Square, accum_out=st[b][:, 4:5])
                rstd_of(P, st[b][:, 4:5], st[b][:, 6:7], st[b][:, 5:6], 128)
                P.op("dve", "tensor_scalar", out=ckn[b].v, in0=yt[b][:, cc:cc + 128], scalar1=st[b][:, 6:7], scalar2=None, op0=ALU.mult)
                P.op("pe", "transpose", out=pck.v, in_=ckn[b].v, identity=idb.v)
                P.op("dve", "tensor_scalar", out=ckT[gi % 2][:, t * 128:(t + 1) * 128], in0=pck.v, scalar1=kvn[:, 0:1], scalar2=None, op0=ALU.mult)
                pb = py[yi % 2]; yi += 1
                P.op("pe", "matmul", out=pb.v, lhsT=ckT[gi % 2][:, t * 128:(t + 1) * 128], rhs=wuk[:, 0, 512:1024], start=True, stop=True)
                vb_ = vst[ti % 2]
                P.op("act", "activation", out=vb_.v, in_=pb.v, func=AF.Copy)
                P.dma("sp", mla["V_s"][r0:r0 + 128, :], vb_.v)
        for (c0, m, ap, dt, scale) in fm_specs:
            pb = pf[fi % 2]; f32t = fo[fi % 2]; bft = fob[fi % 2]; fi += 1
            for k in range(KC):
                P.op("pe", "matmul", track=(k == KC - 1), out=pb[0:m, 0:W], lhsT=wfm[:, k, c0:c0 + m], rhs=hg[:, k, 0:W], start=(k == 0), stop=(k == KC - 1))
            dst = bft if dt == BF16 else f32t
            P.op("act", "activation", out=dst[0:m, 0:W], in_=pb[0:m, 0:W], func=AF.Copy, scale=float(scale))
            P.dma("sp", ap[:, tok0:tok0 + W], dst[0:m, 0:W])
        if rope:
            ckr, ckrs, ap, C_d, S_d = rope
            u = gi % 2
            P.dma("sp", Ct[u][:, 0:W], C_d[:, tok0:tok0 + W]); P.dma("sp", St[u][:, 0:W], S_d[:, tok0:tok0 + W])
            pa = pf[fi % 2]; fi += 1; pb = pf[fi % 2]; fi += 1
            for k in range(KC):
                P.op("pe", "matmul", track=(k == KC - 1), out=pa[0:64, 0:W], lhsT=wfm[:, k, ckr:ckr + 64], rhs=hg[:, k, 0:W], start=(k == 0), stop=(k == KC - 1))
            for k in range(KC):
                P.op("pe", "matmul", track=(k == KC - 1), out=pb[0:64, 0:W], lhsT=wfm[:, k, ckrs:ckrs + 64], rhs=hg[:, k, 0:W], start=(k == 0), stop=(k == KC - 1))
            P.op("dve", "tensor_tensor", out=r1[:, 0:W], in0=pa[0:64, 0:W], in1=Ct[u][:, 0:W], op=ALU.mult)
            P.op("dve", "tensor_tensor", out=r2[:, 0:W], in0=pb[0:64, 0:W], in1=St[u][:, 0:W], op=ALU.mult)
            P.op("pool", "tensor_tensor", out=rb_[u][:, 0:W], in0=r1[:, 0:W], in1=r2[:, 0:W], op=ALU.add)
            P.dma("sp", ap[:, tok0:tok0 + W], rb_[u][:, 0:W])
        if mla:
            for h in range(4):
                pb = pf[fi % 2]; kb_ = kst[fi % 2]; fi += 1
                P.op("pe", "matmul", out=pb[:, 0:W], lhsT=wuk[:, 0, h * 128:(h + 1) * 128], rhs=ckT[gi % 2][:, 0:W], start=True, stop=True)
                P.op("act", "activation", out=kb_[:, 0:W], in_=pb[:, 0:W], func=AF.Copy)
                P.dma("sp", mla["kT_s"][h, :, tok0:tok0 + W], kb_[:, 0:W])
    P.end()


def conv_items(w1_d, w2_d, b1_d, w1bf, w2bf, b1bf, l):
    items = [(b1_d[l], b1bf[l], 32, 2048)]
    for e in range(32):
        for k in range(8):
            ks = slice(k * 128, (k + 1) * 128)
            items.append((w1_d[l, e, ks, :], w1bf[l, e, ks, :], 128, 2048))
            items.append((w2_d[l, e, ks, :], w2bf[l, e, ks, :], 128, 1024))
    return items


def conv_steps(P, items):
    R = 6
    pools = {2048: ([P.sb("cstA%d" % i, [128, 2048]) for i in range(R)], [P.sb("cbfA%d" % i, [128, 2048], BF16) for i in range(R)]),
             1024: ([P.sb("cstB%d" % i, [128, 1024]) for i in range(R)], [P.sb("cbfB%d" % i, [128, 1024], BF16) for i in range(R)])}
    pcnt = {2048: 0, 1024: 0}
    state = dict(nxt=0, loaded=[], cast=[])

    def tick(k):
        for (i, dst, rows, n) in state["cast"]:
            P.dma("sp", dst, pools[n][1][i][0:rows, :])
        state["cast"] = []
        for (i, dst, rows, n) in state["loaded"]:
            P.op("act", "activation", out=pools[n][1][i][0:rows, :], in_=pools[n][0][i][0:rows, :], func=AF.Copy)
            state["cast"].append((i, dst, rows, n))
        state["loaded"] = []
        for _ in range(k):
            if state["nxt"] < len(items):
                src, dst, rows, n = items[state["nxt"]]; state["nxt"] += 1
                i = pcnt[n] % R; pcnt[n] += 1
                P.dma("sp", pools[n][0][i][0:rows, :], src)
                state["loaded"].append((i, dst, rows, n))
        return state["nxt"] < len(items) or state["loaded"] or state["cast"]
    return tick


def stage_gla(P, scans, wa_d, tri_d, mu_d, extra=None, per_block=4):
    P.begin()
    CB = 8
    tri = P.sb("tri", [64, 64]); P.dma("sp", tri.v, tri_d)
    mu = P.sb("mu", [64, CB, 64]); P.dma("sp", mu.v, mu_d)
    one = P.sb("one", [64, 1]); P.op("dve", "memset", ap=one.v, constant=1.0)
    wa = P.sb("wa", [17, 8, 64]); P.dma("sp", wa.v, wa_d)
    S = [P.sb("S%d" % i, [64, 128]) for i in range(2)]
    tmp = [P.sb("tmp%d" % i, [64, 128]) for i in range(2)]
    L = lambda nm, shp: [P.sb("%s%d" % (nm, i), shp) for i in range(2)]
    qTb = L("qTb", [64, CB * 64]); kTb = L("kTb", [64, CB * 64]); aTb = L("aTb", [17, CB * 64]); kb = L("kb", [64, CB, 64]); vb = L("vb", [64, CB, 128])
    for a in aTb:
        P.op("dve", "memset", ap=a.v, constant=1.0)
    le = L("le", [64, CB, 64]); E1 = L("E1", [64, CB * 64]); E2 = L("E2", [64, CB * 64]); E3 = L("E3", [64, CB, 64])
    qs = L("qs", [64, CB * 64]); ks = L("ks", [64, CB * 64]); kd = L("kd", [64, CB, 64]); ATm = L("ATm", [64, CB, 64]); ob = L("ob", [64, CB, 128])
    pXA = P.ps("pXA", [64, CB, 64]); pb = P.ps("pb", [64, CB, 64]); pbT = P.ps("pbT", [64, CB, 64]); pA = P.ps("pA", [64, CB, 64])
    pKV = P.ps("pKV", [64, CB, 128]); po = P.ps("po", [64, CB, 128])
    blocks = [(0, 4)] + [(4 + 8 * i, 8) for i in range(32)]
    bi = 0; ti = 0
    tick = extra(P) if extra else None
    for (qT_d, kT_d, k_d, v_d, aT_d, wi, o_d) in scans:
        cur = 0
        P.op("dve", "memset", ap=S[0].v, constant=0.0)
        for (c0, n) in blocks:
            u = bi % 2; bi += 1; t0 = c0 * 64; W = n * 64
            if tick:
                tick(per_block)
            P.dma("sp", qTb[u][:, 0:W], qT_d[:, t0:t0 + W]); P.dma("sp", kTb[u][:, 0:W], kT_d[:, t0:t0 + W]); P.dma("sp", aTb[u][0:16, 0:W], aT_d[:, t0:t0 + W])
            P.dma("sp", kb[u][:, 0:n, :], k_d[t0:t0 + W, :].rearrange("(c p) d -> p c d", p=64))
            P.dma("sp", vb[u][:, 0:n, :], v_d[t0:t0 + W, :].rearrange("(c p) d -> p c d", p=64))
            for c in range(n):
                P.op("pe", "matmul", out=pXA[:, c, :], lhsT=aTb[u][:, c * 64:(c + 1) * 64], rhs=wa[:, wi, :], start=True, stop=True, track=(c == n - 1))
            P.op("act", "activation", out=le[u][:, 0:n, :], in_=pXA[:, 0:n, :], func=AF.Exp, scale=-1.0)
            P.op("act", "activation", out=le[u][:, 0:n, :], in_=le[u][:, 0:n, :], func=AF.Ln, bias=one[:, 0:1])
            for c in range(n):
                P.op("pe", "matmul", out=pb[:, c, :], lhsT=tri.v, rhs=le[u][:, c, :], start=True, stop=True, track=False)
                P.op("pe", "matmul", out=pbT[:, c, :], lhsT=le[u][:, c, :], rhs=tri.v, start=True, stop=True, track=(c == n - 1))
            pbTf = pbT.v.rearrange("p c t -> p (c t)")
            P.op("act", "activation", out=E1[u][:, 0:W], in_=pbTf[:, 0:W], func=AF.Exp)
            P.op("act", "activation", out=E2[u][:, 0:W], in_=pbTf[:, 0:W], func=AF.Exp, scale=-1.0)
            P.op("act", "activation", out=E3[u][:, 0:n, :], in_=pb[:, 0:n, :], func=AF.Exp, scale=-1.0)
            P.op("dve", "scalar_tensor_tensor", out=qs[u][:, 0:W], in0=qTb[u][:, 0:W], scalar=0.125, in1=E1[u][:, 0:W], op0=ALU.mult, op1=ALU.mult)
            P.op("dve", "tensor_tensor", out=ks[u][:, 0:W], in0=kTb[u][:, 0:W], in1=E2[u][:, 0:W], op=ALU.mult)
            P.op("dve", "tensor_tensor", out=kd[u][:, 0:n, :], in0=kb[u][:, 0:n, :], in1=E3[u][:, 0:n, :], op=ALU.mult)
            for c in range(n):
                cs_ = slice(c * 64, (c + 1) * 64)
                P.op("pe", "matmul", out=pA[:, c, :], lhsT=ks[u][:, cs_], rhs=qs[u][:, cs_], start=True, stop=True, track=(c == n - 1))
            P.op("dve", "tensor_tensor", out=ATm[u][:, 0:n, :], in0=pA[:, 0:n, :], in1=mu[:, 0:n, :], op=ALU.mult)
            for c in range(n):
                P.op("pe", "matmul", out=pKV[:, c, :], lhsT=kd[u][:, c, :], rhs=vb[u][:, c, :], start=True, stop=True, track=(c == n - 1))
            for c in range(n):
                cs_ = slice(c * 64, (c + 1) * 64)
                P.op("pe", "matmul", out=po[:, c, :], lhsT=qs[u][:, cs_], rhs=S[cur].v, start=True, stop=False, track=False)
                P.op("pe", "matmul", out=po[:, c, :], lhsT=ATm[u][:, c, :], rhs=vb[u][:, c, :], start=False, stop=True)
                tb = tmp[ti % 2]; ti += 1
                P.op("dve", "tensor_tensor", out=tb.v, in0=S[cur].v, in1=pKV[:, c, :], op=ALU.add)
                P.op("dve", "tensor_scalar", out=S[1 - cur].v, in0=tb.v, scalar1=E1[u][:, c * 64 + 63:c * 64 + 64], scalar2=None, op0=ALU.mult)
                cur = 1 - cur
            P.op("act", "activation", out=ob[u][:, 0:n, :], in_=po[:, 0:n, :], func=AF.Copy)
            P.dma("sp", o_d[t0:t0 + W, :].rearrange("(c p) d -> p c d", p=64), ob[u][:, 0:n, :])
    while tick and tick(per_block):
        pass
    P.end()


def stage_mla_full(P, idf, idb, cq_s, qn_d, wuq_d, wuqs_d, Cq_d, Sq_d, kT_s, krT_s, V_s, om_s):
    P.begin()
    NK = TALL; NB = NK // 128; NQ = TOWN
    scale = 192.0 ** -0.5
    qa = P.sb("qa", [128, 4, NQ], BF16); qb = P.sb("qb", [65, 4, NQ], BF16)
    stg = [P.sb("wst%d" % i, [128, 768]) for i in range(2)]
    wuq = load_w_bf16(P, wuq_d, 256, 768, "wuq", stage=stg)
    wuqs = load_w_bf16(P, wuqs_d, 256, 256, "wuqs", stage=stg)
    qn = P.sb("qn", [128, 2]); P.dma("sp", qn.v, qn_d)
    cqT = P.sb("cqT", [128, 2, NQ], BF16)
    xs = [P.sb("x%d" % i, [128, 256]) for i in range(2)]; junk = P.sb("junk", [128, 256]); st = [P.sb("st%d" % i, [128, 4]) for i in range(2)]
    xn = [P.sb("xn%d" % i, [128, 256], BF16) for i in range(2)]
    ps1 = [P.ps("ps1_%d" % i, [128, 512]) for i in range(2)]; pst = [P.ps("pst%d" % i, [128, 512]) for i in range(2)]
    pO = [P.ps("pO%d" % i, [128, 512]) for i in range(4)]
    for t in range(NOWN):
        b = t % 2
        pT = pO[b].v.bitcast(BF16)
        P.dma("sp", xs[b].v, cq_s[t * 128:(t + 1) * 128, :])
        P.op("act", "activation", out=junk.v, in_=xs[b].v, func=AF.Square, accum_out=st[b][:, 0:1])
        rstd_of(P, st[b][:, 0:1], st[b][:, 2:3], st[b][:, 1:2], 256)
        P.op("dve", "tensor_scalar", out=xn[b].v, in0=xs[b].v, scalar1=st[b][:, 2:3], scalar2=None, op0=ALU.mult)
        for k in range(2):
            P.op("pe", "transpose", out=pT[:, k * 128:(k + 1) * 128], in_=xn[b][:, k * 128:(k + 1) * 128], identity=idb.v)
        for k in range(2):
            P.op("dve", "tensor_scalar", out=cqT[:, k, t * 128:(t + 1) * 128], in0=pT[:, k * 128:(k + 1) * 128], scalar1=qn[:, k:k + 1], scalar2=None, op0=ALU.mult)
    Ct = P.sb("Ct", [64, 512]); St = P.sb("St", [64, 512]); r1 = P.sb("r1", [64, 512]); r2 = P.sb("r2", [64, 512])
    for c0 in range(0, NQ, 512):
        cn = min(512, NQ - c0)
        P.dma("sp", Ct[:, 0:cn], Cq_d[:, c0:c0 + cn]); P.dma("sp", St[:, 0:cn], Sq_d[:, c0:c0 + cn])
        for h in range(4):
            for k in range(2):
                P.op("pe", "matmul", track=(k == 1), out=ps1[0][:, 0:cn], lhsT=wuq[:, k, h * 192:h * 192 + 128], rhs=cqT[:, k, c0:c0 + cn], start=(k == 0), stop=(k == 1))
            P.op("act", "activation", out=qa[:, h, c0:c0 + cn], in_=ps1[0][:, 0:cn], func=AF.Copy, scale=scale)
            for k in range(2):
                P.op("pe", "matmul", track=(k == 1), out=ps1[1][0:64, 0:cn], lhsT=wuq[:, k, h * 192 + 128:h * 192 + 192], rhs=cqT[:, k, c0:c0 + cn], start=(k == 0), stop=(k == 1))
            for k in range(2):
                P.op("pe", "matmul", track=(k == 1), out=pst[0][0:64, 0:cn], lhsT=wuqs[:, k, h * 64:(h + 1) * 64], rhs=cqT[:, k, c0:c0 + cn], start=(k == 0), stop=(k == 1))
            P.op("dve", "tensor_tensor", out=r1[:, 0:cn], in0=ps1[1][0:64, 0:cn], in1=Ct[:, 0:cn], op=ALU.mult)
            P.op("dve", "tensor_tensor", out=r2[:, 0:cn], in0=pst[0][0:64, 0:cn], in1=St[:, 0:cn], op=ALU.mult)
            P.op("dve", "tensor_tensor", out=r1[:, 0:cn], in0=r1[:, 0:cn], in1=r2[:, 0:cn], op=ALU.add)
            P.op("act", "activation", out=qb[0:64, h, c0:c0 + cn], in_=r1[:, 0:cn], func=AF.Copy, scale=scale)
    kTa = P.sb("kTa", [128, NK], BF16); kTb = P.sb("kTb", [65, NK], BF16); Vb = P.sb("Vb", [128, NB, 129], BF16)
    P.op("pool", "memset", ap=kTb.v, constant=1.0)
    P.op("pool", "memset", ap=Vb.v, constant=1.0)
    P.dma("sp", kTb[0:64, :], krT_s)
    mx = P.sb("mx", [128, 4, 40]); negm = P.sb("negm", [128, 4]); Z = P.sb("Z", [128, 65])
    P.op("dve", "memset", ap=Z.v, constant=0.0)
    pt = [P.sb("pt%d" % i, [128, 512], BF16) for i in range(2)]
    rl = P.sb("rl", [128, 4]); ot = [P.sb("ot%d" % i, [128, 4, 128]) for i in range(2)]
    groups = [(g * 512, 512, NK) for g in range(5)] + [(2560, 128, 256), (2688, 128, 256)]
    c1 = 0; c2 = 0; gi = 0
    for h in range(4):
        P.dma("sp", kTa.v, kT_s[h])
        for b0 in range(0, NB, 26):
            bn = min(26, NB - b0)
            P.dma("sp", Vb[:, b0:b0 + bn, 0:128], V_s[b0 * 128:(b0 + bn) * 128, h * 128:(h + 1) * 128].rearrange("(b p) f -> p b f", p=128))
        for (q0, nq, nkeys) in groups:
            u = gi % 2; gi += 1; nqt = nq // 128
            kblocks = [(k0, min(512, nkeys - k0)) for k0 in range(0, nkeys, 512)]
            for qt in range(nqt):
                qs_ = slice(q0 + qt * 128, q0 + (qt + 1) * 128)
                for bi, (k0, kn) in enumerate(kblocks):
                    pb = ps1[c1 % 2]; c1 += 1
                    P.op("pe", "matmul", track=False, out=pb[:, 0:kn], lhsT=qa[:, h, qs_], rhs=kTa[:, k0:k0 + kn], start=True, stop=False)
                    P.op("pe", "matmul", out=pb[:, 0:kn], lhsT=qb[0:64, h, qs_], rhs=kTb[0:64, k0:k0 + kn], start=False, stop=True)
                    P.op("dve", "tensor_reduce", out=mx[:, qt, bi:bi + 1], in_=pb[:, 0:kn], axis=AX.X, op=ALU.max)
                P.op("dve", "tensor_reduce", out=negm[:, qt:qt + 1], in_=mx[:, qt, 0:len(kblocks)], axis=AX.X, op=ALU.max)
                P.op("dve", "tensor_scalar", out=Z[:, 64:65], in0=negm[:, qt:qt + 1], scalar1=-1.0, scalar2=None, op0=ALU.mult)
                pz = ps1[c1 % 2]; c1 += 1
                P.op("pe", "matmul", out=pz[0:65, 0:128], lhsT=Z.v, rhs=idf.v, start=True, stop=True)
                P.op("dve", "tensor_copy", out=qb[64:65, h, qs_], in_=pz[64:65, 0:128])
            nkb = nkeys // 128

            def qk(kb_, slot):
                ks_ = slice(kb_ * 128, (kb_ + 1) * 128)
                pb = pst[slot % 2]
                P.op("pe", "matmul", track=False, out=pb[:, 0:nq], lhsT=kTa[:, ks_], rhs=qa[:, h, q0:q0 + nq], start=True, stop=False)
                P.op("pe", "matmul", out=pb[:, 0:nq], lhsT=kTb[0:65, ks_], rhs=qb[0:65, h, q0:q0 + nq], start=False, stop=True)
            qk(0, c2)
            for kb_ in range(nkb):
                pb = pst[c2 % 2]; ptb = pt[c2 % 2]
                P.op("act", "activation", out=ptb[:, 0:nq], in_=pb[:, 0:nq], func=AF.Exp)
                if kb_ + 1 < nkb:
                    qk(kb_ + 1, c2 + 1)
                c2 += 1
                for qt in range(nqt):
                    P.op("pe", "matmul", track=(kb_ == nkb - 1), out=pO[qt][:, 0:129], lhsT=ptb[:, qt * 128:(qt + 1) * 128], rhs=Vb[:, kb_, :], start=(kb_ == 0), stop=(kb_ == nkb - 1))
            for qt in range(nqt):
                P.op("dve", "reciprocal", out=rl[:, qt:qt + 1], in_=pO[qt][:, 128:129])
                P.op("dve", "tensor_scalar", out=ot[u][:, qt, :], in0=pO[qt][:, 0:128], scalar1=rl[:, qt:qt + 1], scalar2=None, op0=ALU.mult)
            P.dma("sp", om_s[q0:q0 + nq, h * 128:(h + 1) * 128].rearrange("(t p) d -> p t d", p=128), ot[u][:, 0:nqt, :])
    P.end()


def stage_post(P, idf, idb, mode, variants, x_ap, ow_d, g1_d, gates_s, l, modcol, g2col_d, rw_d, rb_d, x1_s, hT_s, G_s, GT_s, gla=None, aT_ap=None, hTok_s=None):
    P.begin()
    nt = len(variants); nv = 2; T = nt * 128; D = 1024; KC = 8
    R2 = lambda nm, shp, dt=F32: [P.sb("%s_%d" % (nm, i), shp, dt) for i in range(2)]
    stage = [P.sb("wst%d" % i, [128, D]) for i in range(2)]
    wb = load_w_bf16(P, ow_d, D, D, "owb", stage=stage)
    rw = P.sb("rw", [128, KC, 32]); P.dma("sp", rw.v, rw_d.rearrange("(k p) n -> p k n", p=128))
    rb = P.sb("rb", [128, 32]); P.dma("sp", rb.v, rb_d)
    g2 = P.sb("g2", [128, KC]); P.dma("sp", g2.v, g2col_d)
    sc = modcols(P, modcol, l, 4, g2.v, "scm")
    sh = lambda v, k: modcol[:, l, 24 + k, v:v + 1]
    g1 = P.sb("g1", [128, D]); P.dma("sp", g1.v, g1_d)
    GG = []
    for v in range(nv):
        gg = P.sb("GG%d" % v, [128, D]); P.dma("sp", gg.v, gates_s[l, 0, v])
        P.op("dve", "tensor_tensor", out=gg.v, in0=gg.v, in1=g1.v, op=ALU.mult)
        GG.append(gg)
    if mode == "gla":
        onc = P.sb("onc", [128, 1]); P.dma("sp", onc.v, gla["on_d"])
        idxf = P.sb("idxf", [128, nt], I32); idxb = P.sb("idxb", [128, nt], I32)
        P.dma("sp", idxf.v, gla["idxf_d"]); P.dma("sp", idxb.v, gla["idxb_d"])
        tof_ = R2("tof", [128, 512]); tob_ = R2("tob", [128, 512]); tr_ = R2("tr", [128, 512]); tom_ = R2("tom", [128, 512])
        cat_ = R2("cat", [128, D], BF16)
        pT = P.ps("pT", [128, KC, 128], BF16)
    else:
        a32_ = R2("a32", [128, KC, 128])
    aT_ = R2("aT", [128, KC, 128], BF16)
    xt_ = R2("xt", [128, D]); x1_ = R2("x1", [128, D]); junk = P.sb("junk", [128, D]); st_ = R2("st", [128, 16])
    xn2_ = R2("xn2", [128, D]); h2T_ = R2("h2T", [128, KC, 128]); h2Tb_ = R2("h2Tb", [128, KC, 128], BF16)
    lg_ = R2("lg", [128, 32]); t8_ = R2("t8", [128, 8]); msk_ = R2("msk", [128, 32]); ex_ = R2("ex", [128, 32]); Gt_ = R2("Gt", [128, 32]); GTt_ = R2("GTt", [32, 128])
    py = [P.ps("py%d" % i, [128, 512]) for i in range(2)]
    pT32 = P.ps("pT32", [128, KC, 128])
    plg = P.ps("plg", [128, 128])
    pTt = P.ps("pTt", [128, D], BF16); htok_ = R2("htok", [128, D], BF16)
    for t in range(nt):
        v = variants[t]; sl = slice(t * 128, (t + 1) * 128); u_ = t % 2
        if mode == "gla":
            tof = tof_[u_]; tob = tob_[u_]; tr = tr_[u_]; tom = tom_[u_]; cat = cat_[u_]
        else:
            a32 = a32_[u_]
        aT = aT_[u_]; xt = xt_[u_]; x1 = x1_[u_]; st = st_[u_]; xn2 = xn2_[u_]; h2T = h2T_[u_]; h2Tb = h2Tb_[u_]
        lg = lg_[u_]; t8 = t8_[u_]; msk = msk_[u_]; ex = ex_[u_]; Gt = Gt_[u_]; GTt = GTt_[u_]
        P.dma("sp", xt.v, x_ap[sl, :])
        if mode == "gla":
            P.gather(tof.v, gla["of_s"], idxf[:, t:t + 1]); P.gather(tob.v, gla["ob_s"], idxb[:, t:t + 1])
            P.dma("sp", tr.v, gla["r_s"][sl, :]); P.dma("sp", tom.v, gla["om_s"][sl, :])
            P.op("dve", "tensor_tensor", out=tof.v, in0=tof.v, in1=tob.v, op=ALU.add)
            for h in range(4):
                P.op("act", "activation", out=junk[:, 0:128], in_=tof[:, h * 128:(h + 1) * 128], func=AF.Square, accum_out=st[:, h:h + 1])
            rstd_of(P, st[:, 0:4], st[:, 8:12], st[:, 4:8], 128)
            P.op("act", "activation", out=tr.v, in_=tr.v, func=AF.Silu)
            for h in range(4):
                hs = slice(h * 128, (h + 1) * 128)
                P.op("dve", "scalar_tensor_tensor", out=cat[:, hs], in0=tof[:, hs], scalar=st[:, 8 + h:9 + h], in1=tr[:, hs], op0=ALU.mult, op1=ALU.mult)
            P.op("pool", "tensor_copy", out=cat[:, 512:1024], in_=tom.v)
            for k in range(KC):
                P.op("pe", "transpose", out=pT[:, k, :], in_=cat[:, k * 128:(k + 1) * 128], identity=idb.v)
            P.op("dve", "tensor_scalar", out=aT[:, 0:4, :], in0=pT[:, 0:4, :], scalar1=onc[:, 0:1], scalar2=None, op0=ALU.mult)
            P.op("act", "activation", out=aT[:, 4:8, :], in_=pT[:, 4:8, :], func=AF.Copy)
        else:
            P.dma("sp", a32.v, aT_ap[:, sl].rearrange("(k p) t -> p k t", p=128))
            P.op("pool", "tensor_copy", out=aT.v, in_=a32.v)
        for n in range(2):
            for k in range(KC):
                P.op("pe", "matmul", track=(k == KC - 1), out=py[n].v, lhsT=aT[:, k, :], rhs=wb[:, k, n * 512:(n + 1) * 512], start=(k == 0), stop=(k == KC - 1))
            P.op("act", "activation", out=junk[:, 0:512], in_=py[n].v, func=AF.Square, accum_out=st[:, 12 + n:13 + n])
        P.op("dve", "tensor_tensor", out=st[:, 12:13], in0=st[:, 12:13], in1=st[:, 13:14], op=ALU.add)
        rstd_of(P, st[:, 12:13], st[:, 14:15], st[:, 13:14], D)
        for n in range(2):
            ns = slice(n * 512, (n + 1) * 512)
            P.op("dve", "scalar_tensor_tensor", out=x1[:, ns], in0=py[n].v, scalar=st[:, 14:15], in1=GG[v][:, ns], op0=ALU.mult, op1=ALU.mult)
        P.op("pool", "tensor_tensor", out=x1.v, in0=x1.v, in1=xt.v, op=ALU.add)
        P.dma("sp", x1_s[sl, :], x1.v)
        P.op("act", "activation", out=junk.v, in_=x1.v, func=AF.Square, accum_out=st[:, 15:16])
        rstd_of(P, st[:, 15:16], st[:, 4:5], st[:, 5:6], D)
        P.op("dve", "tensor_scalar", out=xn2.v, in0=x1.v, scalar1=st[:, 4:5], scalar2=None, op0=ALU.mult)
        for k in range(KC):
            P.op("pe", "transpose", out=pT32[:, k, :], in_=xn2[:, k * 128:(k + 1) * 128], identity=idf.v)
        for k in range(KC):
            P.op("dve", "tensor_scalar", out=h2T[:, k, :], in0=pT32[:, k, :], scalar1=sc[:, v, k:k + 1], scalar2=sh(v, k), op0=ALU.mult, op1=ALU.add)
        P.op("pool", "tensor_copy", out=h2Tb.v, in_=h2T.v)
        P.dma("sp", hT_s[:, sl].rearrange("(k p) t -> p k t", p=128), h2Tb.v)
        if hTok_s is not None:
            for k in range(KC):
                P.op("pe", "transpose", out=pTt[:, k * 128:(k + 1) * 128], in_=h2Tb[:, k, :], identity=idb.v, track=(k == KC - 1))
            P.op("act", "activation", out=htok_[u_].v, in_=pTt.v, func=AF.Copy)
            P.dma("sp", hTok_s[sl, :], htok_[u_].v)
        for k in range(KC):
            P.op("pe", "matmul", track=(k == KC - 1), out=plg[:, 0:32], lhsT=h2T[:, k, :], rhs=rw[:, k, :], start=(k == 0), stop=(k == KC - 1))
        P.op("dve", "tensor_tensor", out=lg.v, in0=plg[:, 0:32], in1=rb.v, op=ALU.add)
        P.op("dve", "max", out=t8.v, in_=lg.v)
        P.op("dve", "tensor_scalar", out=msk.v, in0=lg.v, scalar1=t8[:, 3:4], scalar2=None, op0=ALU.is_ge)
        P.op("dve", "tensor_scalar", out=t8[:, 7:8], in0=t8[:, 0:1], scalar1=-1.0, scalar2=None, op0=ALU.mult)
        P.op("act", "activation", out=ex.v, in_=lg.v, func=AF.Exp, bias=t8[:, 7:8])
        P.op("dve", "tensor_tensor", out=ex.v, in0=ex.v, in1=msk.v, op=ALU.mult)
        P.op("dve", "tensor_reduce", out=t8[:, 6:7], in_=ex.v, axis=AX.X, op=ALU.add)
        P.op("dve", "reciprocal", out=t8[:, 5:6], in_=t8[:, 6:7])
        P.op("dve", "tensor_scalar", out=Gt.v, in0=ex.v, scalar1=t8[:, 5:6], scalar2=None, op0=ALU.mult)
        P.dma("sp", G_s[sl, :], Gt.v)
        P.op("pe", "transpose", out=plg[0:32, :], in_=Gt.v, identity=idf.v)
        P.op("act", "activation", out=GTt.v, in_=plg[0:32, :], func=AF.Copy)
        P.dma("sp", GT_s[:, sl], GTt.v)
    P.end()


def stage_moe(P, variants, groups, hT_s, G_s, GT_s, x1_s, w1_d, w2_d, b1g_d, b1l_d, b2_d, g3_d, gates_s, l, x2_ap, NE=32):
    P.begin()
    nt = len(variants); nv = 2; T = nt * 128; D = 1024; KC = 8; F = 1024
    mg = max(len(g) for g in groups)
    g3 = P.sb("g3", [128, D]); P.dma("sp", g3.v, g3_d)
    GG = []
    for v in range(nv):
        gg = P.sb("GG%d" % v, [128, D]); P.dma("sp", gg.v, gates_s[l, 1, v])
        P.op("dve", "tensor_tensor", out=gg.v, in0=gg.v, in1=g3.v, op=ALU.mult)
        GG.append(gg)
    b1g = P.sb("b1g", [128, 32, KC]); b1l = P.sb("b1l", [128, 32, KC]); P.dma("sp", b1g.v, b1g_d); P.dma("sp", b1l.v, b1l_d)
    b2 = P.sb("b2", [32, D]); P.dma("sp", b2.v, b2_d)
    stg = [P.sb("stg%d" % i, [128, 2 * F]) for i in range(2)]
    w1g = [P.sb("w1g%d" % i, [128, KC, F], BF16) for i in range(2)]
    w1l = [P.sb("w1l%d" % i, [128, KC, F], BF16) for i in range(2)]
    w2b = [P.sb("w2b%d" % i, [128, KC, D], BF16) for i in range(2)]
    hT = P.sb("hT", [128, KC, mg * 128], BF16)
    Gs = P.sb("Gs", [128, mg, 32]); GTs = P.sb("GTs", [32, mg * 128])
    yacc = P.sb("yacc", [128, mg, D])
    actT = P.sb("actT", [128, KC, 512], BF16)
    tg = [P.sb("tg%d" % i, [128, 512]) for i in range(2)]; tsg = [P.sb("tsg%d" % i, [128, 512]) for i in range(2)]
    tl = [P.sb("tl%d" % i, [128, 512]) for i in range(2)]
    xt = g3; junk = P.sb("junk", [128, D]); st = P.sb("st", [128, 4])
    pg = [P.ps("pg%d" % i, [128, 512]) for i in range(2)]; pl = [P.ps("pl%d" % i, [128, 512]) for i in range(2)]
    py = [P.ps("py%d" % i, [128, 512]) for i in range(2)]
    sti = [0]

    def load_expert_steps(e, buf):
        steps = []
        for k in range(KC):
            def step(k=k):
                s = stg[sti[0] % 2]; sti[0] += 1
                P.dma("sp", s.v, w1_d[e, k * 128:(k + 1) * 128, :])
                sv = s.v.rearrange("p (f two) -> p f two", two=2)
                P.op("act", "activation", out=w1g[buf][:, k, :], in_=sv[:, :, 0], func=AF.Copy)
                P.op("act", "activation", out=w1l[buf][:, k, :], in_=sv[:, :, 1], func=AF.Copy)
                s = stg[sti[0] % 2]; sti[0] += 1
                P.dma("sp", s[:, 0:D], w2_d[e, k * 128:(k + 1) * 128, :])
                P.op("act", "activation", out=w2b[buf][:, k, :], in_=s[:, 0:D], func=AF.Copy)
            steps.append(step)
        return steps

    for grp in groups:
        ng = len(grp)
        t0 = grp[0]; tok0 = t0 * 128; ntok = ng * 128
        P.dma("sp", hT[:, :, 0:ntok], hT_s[:, tok0:tok0 + ntok].rearrange("(k p) t -> p k t", p=128))
        P.dma("sp", Gs[:, 0:ng, :], G_s[tok0:tok0 + ntok, :].rearrange("(g p) e -> p g e", p=128))
        P.dma("sp", GTs[:, 0:ntok], GT_s[:, tok0:tok0 + ntok])
        for i in range(ng):
            for n in range(2):
                P.op("pe", "matmul", out=py[n].v, lhsT=GTs[:, i * 128:(i + 1) * 128], rhs=b2[:, n * 512:(n + 1) * 512], start=True, stop=True)
                P.op("act", "activation", out=yacc[:, i, n * 512:(n + 1) * 512], in_=py[n].v, func=AF.Copy)
        for s in load_expert_steps(0, 0):
            s()
        blocks = [(b0, min(512, ntok - b0)) for b0 in range(0, ntok, 512)]
        ci = 0
        for e in range(NE):
            buf = e % 2
            nxt = load_expert_steps(e + 1, 1 - buf) if e + 1 < NE else []
            for bi, (b0, bn) in enumerate(blocks):
                for j in range(KC):
                    if bi == 0 and nxt:
                        nxt[j]()
                    c = ci % 2; ci += 1
                    for k in range(KC):
                        P.op("pe", "matmul", track=(k == KC - 1), out=pg[c][:, 0:bn], lhsT=w1g[buf][:, k, j * 128:(j + 1) * 128], rhs=hT[:, k, b0:b0 + bn], start=(k == 0), stop=(k == KC - 1))
                    for k in range(KC):
                        P.op("pe", "matmul", track=(k == KC - 1), out=pl[c][:, 0:bn], lhsT=w1l[buf][:, k, j * 128:(j + 1) * 128], rhs=hT[:, k, b0:b0 + bn], start=(k == 0), stop=(k == KC - 1))
                    P.op("dve", "tensor_scalar", out=tg[c][:, 0:bn], in0=pg[c][:, 0:bn], scalar1=b1g[:, e, j:j + 1], scalar2=7.0, op0=ALU.add, op1=ALU.min)
                    P.op("act", "activation", out=tsg[c][:, 0:bn], in_=tg[c][:, 0:bn], func=AF.Sigmoid, scale=1.702)
                    P.op("dve", "tensor_scalar", out=tl[c][:, 0:bn], in0=pl[c][:, 0:bn], scalar1=b1l[:, e, j:j + 1], scalar2=-7.0, op0=ALU.add, op1=ALU.max)
                    P.op("dve", "tensor_scalar", out=tl[c][:, 0:bn], in0=tl[c][:, 0:bn], scalar1=7.0, scalar2=1.0, op0=ALU.min, op1=ALU.add)
                    P.op("pool", "tensor_tensor", out=tg[c][:, 0:bn], in0=tg[c][:, 0:bn], in1=tsg[c][:, 0:bn], op=ALU.mult)
                    P.op("pool", "tensor_tensor", out=actT[:, j, 0:bn], in0=tg[c][:, 0:bn], in1=tl[c][:, 0:bn], op=ALU.mult)
                for i in range(b0 // 128, (b0 + bn) // 128):
                    for n in range(2):
                        ns = slice(n * 512, (n + 1) * 512)
                        for j in range(KC):
                            P.op("pe", "matmul", track=(j == KC - 1), out=py[n].v, lhsT=actT[:, j, i * 128 - b0:(i + 1) * 128 - b0], rhs=w2b[buf][:, j, ns], start=(j == 0), stop=(j == KC - 1))
                        P.op("dve", "scalar_tensor_tensor", out=yacc[:, i, ns], in0=py[n].v, scalar=Gs[:, i, e:e + 1], in1=yacc[:, i, ns], op0=ALU.mult, op1=ALU.add)
        for i in range(ng):
            t = grp[i]; v = variants[t]; sl = slice(t * 128, (t + 1) * 128)
            P.dma("sp", xt.v, x1_s[sl, :])
            P.op("act", "activation", out=junk.v, in_=yacc[:, i, :], func=AF.Square, accum_out=st[:, 0:1])
            rstd_of(P, st[:, 0:1], st[:, 2:3], st[:, 1:2], D)
            P.op("dve", "scalar_tensor_tensor", out=junk.v, in0=yacc[:, i, :], scalar=st[:, 2:3], in1=GG[v].v, op0=ALU.mult, op1=ALU.mult)
            P.op("pool", "tensor_tensor", out=xt.v, in0=junk.v, in1=xt.v, op=ALU.add)
            P.dma("sp", x2_ap[sl, :], xt.v)
    P.end()


def stage_na(P, idb, qT_s, kT_s, V_s, bi_d, be_d, oT_s):
    P.begin()
    ones = P.sb("ones", [128, 128], BF16); P.op("pool", "memset", ap=ones.v, constant=1.0)
    kb = [P.sb("kb%d" % i, [128, 8, 768], BF16) for i in range(2)]; vb = [P.sb("vb%d" % i, [128, 6, 1024], BF16) for i in range(2)]
    kcb = P.sb("kcb", [128, 8, 256], BF16); vcb = P.sb("vcb", [128, 2, 1024], BF16)
    P.dma("sp", kcb.v, kT_s[:, 2560:2816].rearrange("(k p) n -> p k n", p=128))
    P.dma("sp", vcb.v, V_s[2560:2816, :].rearrange("(b p) f -> p b f", p=128))
    Bi = P.sb("Bi", [128, 8, 512]); P.dma("sp", Bi.v, bi_d)
    Be = [P.sb("Be%d" % i, [128, 8, 768]) for i in range(2)]
    qrow = [P.sb("qrow%d" % i, [128, 8, 64], BF16) for i in range(2)]
    QBD = [P.sb("QBD%d" % i, [128, 128], BF16) for i in range(2)]
    for z in QBD:
        P.op("pool", "memset", ap=z.v, constant=0.0)
    Sb = [P.sb("Sb%d" % i, [128, 1024]) for i in range(2)]; Pm = [P.sb("Pm%d" % i, [128, 1024], BF16) for i in range(2)]
    PT = [P.sb("PT%d" % i, [128, 8, 128], BF16) for i in range(2)]
    sm = P.sb("sm", [128, 4]); rl = P.sb("rl", [128, 128]); orow = [P.sb("orow%d" % i, [128, 8, 64]) for i in range(2)]
    pS = [P.ps("pS%d" % i, [128, 1024]) for i in range(2)]
    pPT = P.ps("pPT", [128, 8, 128], BF16); pO = P.ps("pO", [128, 128]); pL = P.ps("pL", [128, 128])
    it = 0; nbe = 0
    for i in range(32):
        u = i % 2
        if i < 4:
            e0, nr, edge = 0, 12, i
        elif i >= 28:
            e0, nr, edge = 28, 12, i - 24
        else:
            e0, nr, edge = i, 8, None
        nw = nr * 64; nbk = nw // 128
        P.dma("sp", kb[u][:, :, 0:nw], kT_s[:, e0 * 64:e0 * 64 + nw].rearrange("(k p) n -> p k n", p=128))
        P.dma("sp", vb[u][:, 0:nbk, :], V_s[e0 * 64:e0 * 64 + nw, :].rearrange("(b p) f -> p b f", p=128))
        P.dma("sp", qrow[u].v, qT_s[:, (i + 4) * 64:(i + 5) * 64].rearrange("(k p) t -> p k t", p=128))
        if edge is not None:
            B = Be[nbe % 2]; nbe += 1
            P.dma("sp", B.v, be_d[edge])
        else:
            B = Bi
        ntot = nw + 256; nblk = nbk + 2
        def scores(hp, w):
            P.op("pool", "tensor_copy", out=QBD[w][0:64, 0:64], in_=qrow[u][0:64, hp, :])
            P.op("pool", "tensor_copy", out=QBD[w][64:128, 64:128], in_=qrow[u][64:128, hp, :])
            for c0 in range(0, nw, 512):
                cn = min(512, nw - c0)
                P.op("pe", "matmul", out=pS[w][:, c0:c0 + cn], lhsT=QBD[w].v, rhs=kb[u][:, hp, c0:c0 + cn], start=True, stop=True, track=False)
            P.op("pe", "matmul", out=pS[w][:, nw:ntot], lhsT=QBD[w].v, rhs=kcb[:, hp, :], start=True, stop=True)
        scores(0, it % 2)
        for hp in range(8):
            w = it % 2; it += 1
            if hp + 1 < 8:
                scores(hp + 1, it % 2)
            P.op("dve", "tensor_tensor", out=Sb[w][:, 0:nw], in0=pS[w][:, 0:nw], in1=B[:, hp, 0:nw], op=ALU.add)
            P.op("act", "activation", out=Sb[w][:, nw:ntot], in_=pS[w][:, nw:ntot], func=AF.Copy)
            P.op("dve", "tensor_reduce", out=sm[:, 0:1], in_=Sb[w][:, 0:ntot], axis=AX.X, op=ALU.max)
            P.op("dve", "tensor_scalar", out=sm[:, 1:2], in0=sm[:, 0:1], scalar1=-1.0, scalar2=None, op0=ALU.mult)
            P.op("act", "activation", out=Pm[w][:, 0:ntot], in_=Sb[w][:, 0:ntot], func=AF.Exp, bias=sm[:, 1:2])
            for b in range(nblk):
                P.op("pe", "transpose", out=pPT[:, b, :], in_=Pm[w][:, b * 128:(b + 1) * 128], identity=idb.v, track=(b == nblk - 1))
            P.op("dve", "tensor_copy", out=PT[w][:, 0:nblk, :], in_=pPT[:, 0:nblk, :])
            hs = slice(hp * 128, (hp + 1) * 128)
            for b in range(nblk):
                vblk = vb[u][:, b, hs] if b < nbk else vcb[:, b - nbk, hs]
                P.op("pe", "matmul", out=pO.v, lhsT=vblk, rhs=PT[w][:, b, :], start=(b == 0), stop=(b == nblk - 1), track=(b == nblk - 1))
            for b in range(nblk):
                P.op("pe", "matmul", out=pL.v, lhsT=ones.v, rhs=PT[w][:, b, :], start=(b == 0), stop=(b == nblk - 1), track=(b == nblk - 1))
            P.op("dve", "reciprocal", out=rl.v, in_=pL.v)
            P.op("dve", "tensor_tensor", out=orow[u][0:64, hp, :], in0=pO[0:64, 0:64], in1=rl[0:64, 0:64], op=ALU.mult)
            P.op("dve", "tensor_tensor", out=orow[u][64:128, hp, :], in0=pO[64:128, 64:128], in1=rl[64:128, 64:128], op=ALU.mult)
        P.dma("sp", oT_s[:, i * 64:(i + 1) * 64].rearrange("(k p) t -> p k t", p=128), orow[u].v)
    P.end()


MOE_PASSES_F0 = [[[0, 1, 2, 3], [4, 5, 6, 7]], [[8, 9, 10, 11], [12, 13, 14, 15]], [[16, 17, 18, 19], [20, 21]]]
MOE_PASSES_F1 = [[[0, 1, 2, 3], [4, 5, 6, 7]], [[8, 9, 10, 11], [12, 13, 14, 15]]]
MOE_GROUPS_F0 = [list(range(0, 6)), list(range(6, 12)), list(range(12, 17)), list(range(17, 22))]
MOE_GROUPS_F1 = [list(range(0, 6)), list(range(6, 11)), list(range(11, 16))]


def build_fused(dbg=False, upto=99):
    P = Prog("fused")
    D = 1024
    dr = P.dram
    x_all = dr("x_all", [TALL, D]); x_rev = dr("x_rev", [TALL, D]); x_own = dr("x_own", [TOWN, D])
    adaw = dr("adaw", [2, D, 6144]); bcol = dr("bcol", [128, 2, 48]); brow = dr("brow", [2, 128, 6144]); cT = dr("cT", [128, 8, 2])
    ng = dr("ng", [2, 4, 128, 8]); ngrow = dr("ngrow", [2, 4, 128, D])
    w_tmA = dr("w_tmA", [D, 896]); w_fmA = dr("w_fmA", [D, 672]); w_tmB = dr("w_tmB", [D, 768]); w_fmB = dr("w_fmB", [D, 528]); w_own = dr("w_own", [D, 768])
    Ca = dr("Ca", [64, TALL]); Sa = dr("Sa", [64, TALL]); Cq = dr("Cq", [64, TOWN]); Sq = dr("Sq", [64, TOWN])
    wukv = dr("wukv", [128, 1024]); kvn = dr("kvn", [128, 1]); qn = dr("qn", [128, 2]); wuq = dr("wuq", [256, 768]); wuqs = dr("wuqs", [256, 256])
    wa = dr("wa", [17, 8, 64]); tri = dr("tri", [64, 64]); mu = dr("mu", [64, 8, 64])
    idxf = dr("idxf", [128, NOWN], I32); idxb = dr("idxb", [128, NOWN], I32)
    ow0 = dr("ow0", [D, D]); ow1 = dr("ow1", [D, D]); onc = dr("onc", [128, 1])
    rw = dr("rw", [2, D, 32]); rb = dr("rb", [2, 128, 32])
    if upto >= 7:
        w1 = dr("w1", [2, 32, D, 2048]); w2 = dr("w2", [2, 32, D, D])
    b1 = dr("b1", [2, 32, 2048]); b2 = dr("b2", [2, 32, D]); iota_d = dr("iota", [128, 128]); lst_d = dr("lst", [128, 128])
    wqk = dr("wqk", [D, 2048]); wv = dr("wv", [D, D]); Bint = dr("Bint", [128, 8, 512]); Bedge = dr("Bedge", [8, 128, 8, 768])
    out = dr("out", [2048, D], out=True)
    S = lambda n, s, dt=F32: P.scratch(n, s, dt, dbg=(dbg is True or (dbg and n in dbg)))
    gates = S("gates", [2, 2, 2, 128, D])
    kA = S("kA", [TALL, 256]); vA = S("vA", [TALL, 512]); qTA = S("qTA", [256, TALL]); kTA = S("kTA", [256, TALL]); aTA = S("aTA", [16, TALL])
    kB = S("kB", [TALL, 256]); vB = S("vB", [TALL, 512]); qTB = S("qTB", [256, TALL]); kTB = S("kTB", [256, TALL]); aTB = S("aTB", [16, TALL])
    kTm = S("kTm", [4, 128, TALL], BF16); krT = S("krT", [64, TALL], BF16); Vm = S("Vm", [TALL, 512], BF16)
    r_own = S("r_own", [TOWN, 512]); cq_own = S("cq_own", [TOWN, 256])
    oF = S("oF", [TALL, 512]); oB = S("oB", [TALL, 512]); om = S("om", [TOWN, 512])
    w1bf = S("w1bf", [2, 32, D, 2048], BF16); w2bf = S("w2bf", [2, 32, D, D], BF16); b1bf = S("b1bf", [2, 32, 2048], BF16)
    hka = S("hka", [TOWN, D], BF16); hkb = S("hkb", [2048, D], BF16)
    x1a = S("x1a", [TOWN, D]); hTa = S("hTa", [D, TOWN], BF16); Ga = S("Ga", [TOWN, 32]); GTa = S("GTa", [32, TOWN]); x2a = S("x2a", [TOWN, D])
    qT1 = S("qT1", [D, TOWN], BF16); kT1 = S("kT1", [D, TOWN], BF16); V1 = S("V1", [TOWN, D], BF16); oT1 = S("oT1", [D, 2048])
    x1b = S("x1b", [2048, D]); hTb = S("hTb", [D, 2048], BF16); Gb = S("Gb", [2048, 32]); GTb = S("GTb", [32, 2048])
    idf, idb = ident(P)
    modcol = P.sb("modcol", [128, 2, 48, 2])
    stage_ada(P, adaw, bcol, brow, cT, modcol, gates)
    if upto < 1:
        P.finish(); return P
    gall = [(0, 2)] + [(256 + 512 * i, 4) for i in range(32)]; gv = [1] + [0] * 32
    gown = [(512 * i, 4) for i in range(5)] + [(2560, 2)]; gvo = [0] * 5 + [1]

    def n1(l):
        g = P.sb("gn%d" % P.nscope, [128, 8]); P.dma("sp", g.v, ng[l, 0])
        sc = modcols(P, modcol, l, 1, g.v, "sc%d" % P.nscope)
        sh = P.sb("sh%d" % P.nscope, [128, 2, 8])
        for s in range(2):
            P.op("dve", "tensor_copy", out=sh[:, s, :], in_=modcol[:, l, 0:8, s])
        return sc, sh
    sc0, sh0 = n1(0)
    if upto >= 7:
        it0 = conv_items(w1, w2, b1, w1bf, w2bf, b1bf, 0); it1 = conv_items(w1, w2, b1, w1bf, w2bf, b1bf, 1)
        xA = xB = None
    else:
        xA = xB = None
    stage_pro2(P, idb, x_all, gall, gv, sc0, sh0, D, w_tmA, 896, [(0, 256, kA, F32), (256, 512, vA, F32)], w_fmA, 672,
              [(0, 128, qTA[0:128], F32, 1.0), (128, 128, qTA[128:256], F32, 1.0), (256, 128, kTA[0:128], F32, 1.0), (384, 128, kTA[128:256], F32, 1.0), (512, 16, aTA, F32, 1.0)],
              rope=(544, 608, krT, Ca, Sa), mla=dict(col=768, wukv_d=wukv, kvn_d=kvn, kT_s=kTm, V_s=Vm), tag="proA", extra=xA)
    stage_pro2(P, idb, x_rev, gall, gv, sc0, sh0, D, w_tmB, 768, [(0, 256, kB, F32), (256, 512, vB, F32)], w_fmB, 528,
              [(0, 128, qTB[0:128], F32, 1.0), (128, 128, qTB[128:256], F32, 1.0), (256, 128, kTB[0:128], F32, 1.0), (384, 128, kTB[128:256], F32, 1.0), (512, 16, aTB, F32, 1.0)], tag="proB", extra=xB)
    if upto < 2:
        P.finish(); return P
    stage_pro2(P, idb, x_own, gown, gvo, sc0, sh0, D, w_own, 768, [(0, 512, r_own, F32), (512, 256, cq_own, F32)], None, 0, [], tag="proO")
    if upto < 3:
        P.finish(); return P
    scans = []
    for h in range(4):
        hs = slice(h * 64, (h + 1) * 64)
        scans.append((qTA[hs], kTA[hs], kA[:, hs], vA[:, h * 128:(h + 1) * 128], aTA, h, oF[:, h * 128:(h + 1) * 128]))
    for h in range(4):
        hs = slice(h * 64, (h + 1) * 64)
        scans.append((qTB[hs], kTB[hs], kB[:, hs], vB[:, h * 128:(h + 1) * 128], aTB, 4 + h, oB[:, h * 128:(h + 1) * 128]))
    stage_gla(P, scans, wa, tri, mu, extra=(lambda P_: conv_steps(P_, it0 + it1)) if upto >= 7 else None, per_block=4)
    if upto < 4:
        P.finish(); return P
    stage_mla_full(P, idf, idb, cq_own, qn, wuq, wuqs, Cq, Sq, kTm, krT, Vm, om)
    if upto < 5:
        P.finish(); return P
    stage_post(P, idf, idb, "gla", VOWN, x_own, ow0, ngrow[0, 1], gates, 0, modcol, ng[0, 2], rw[0], rb[0], x1a, hTa, Ga, GTa,
               gla=dict(on_d=onc, idxf_d=idxf, idxb_d=idxb, of_s=oF, ob_s=oB, r_s=r_own, om_s=om), hTok_s=hka)
    if upto < 7:
        P.finish(); return P
    stage_moe_sp(P, idb, VOWN, MOE_PASSES_F0, hka, Ga, GTa, x1a, w1bf[0], w2bf[0], b1bf[0], b2[0], ngrow[0, 3], gates, 0, x2a, iota_d, lst_d)
    sc1, sh1 = n1(1)
    stage_pro2(P, idb, x2a, gown, gvo, sc1, sh1, D, wv, 1024, [(0, 1024, V1, BF16)], wqk, 2048,
              [(j * 128, 128, qT1[j * 128:(j + 1) * 128], BF16, 0.125) for j in range(8)] + [(1024 + j * 128, 128, kT1[j * 128:(j + 1) * 128], BF16, 1.0) for j in range(8)], tag="proQ")
    stage_na(P, idb, qT1, kT1, V1, Bint, Bedge, oT1)
    stage_post(P, idf, idb, "fm", [0] * 16, x2a[256:2304], ow1, ngrow[1, 1], gates, 1, modcol, ng[1, 2], rw[1], rb[1], x1b, hTb, Gb, GTb, aT_ap=oT1, hTok_s=hkb)
    stage_moe_sp(P, idb, [0] * 16, MOE_PASSES_F1, hkb, Gb, GTb, x1b, w1bf[1], w2bf[1], b1bf[1], b2[1], ngrow[1, 3], gates, 1, out, iota_d, lst_d)
    P.finish()
    return P


def _c(a):
    return np.ascontiguousarray(a, dtype=np.float32)


def _col(a):
    return _c(a.reshape(-1, 128).T)


def _bc(a):
    return _c(np.broadcast_to(a, (128,) + a.shape))


def _rope_tables():
    t = np.arange(16384)
    row = (t // 64).astype(np.float32); colp = (t % 64).astype(np.float32)
    inv = (np.float32(10000.0) ** (-np.arange(16, dtype=np.float32) / np.float32(16))).astype(np.float32)
    ang = np.concatenate([row[:, None] * inv, colp[:, None] * inv], -1).astype(np.float32)
    return np.cos(ang).astype(np.float32), np.sin(ang).astype(np.float32)


def na_bias_edge(rpb, r, grows):
    r0 = int(np.clip(r - 4, 0, 248))
    col = np.arange(64); c0 = np.clip(col - 8, 0, 48); kcol = np.arange(64)
    inwin = (kcol[None, :] >= c0[:, None]) & (kcol[None, :] < c0[:, None] + 16)
    cidx = np.clip(kcol[None, :] - col[:, None] + 15, 0, 30)
    out = np.full((16, 64, len(grows), 64), -30000.0, np.float32)
    for w, g in enumerate(grows):
        if 0 <= g <= 255 and r0 <= g < r0 + 8:
            t = rpb[:, g - r + 7][:, cidx]
            out[:, :, w, :] = np.where(inwin[None], t, np.float32(-30000.0))
    t = out.reshape(8, 2, 64, len(grows) * 64)
    return np.ascontiguousarray(t.transpose(1, 2, 0, 3).reshape(128, 8, len(grows) * 64))


_FP = {}


def fused_inputs(x, c, ctx, c_ctx, ada_w, ada_b, norm_g, router_w, router_b, moe_w1, moe_b1, moe_w2, moe_b2,
                 ab_in_w, gla_wa_f, gla_ba_f, gla_wa_b, gla_ba_b, gla_onorm, mla_qnorm, mla_wuq, mla_kvnorm, mla_wukv,
                 ab_out_w, na_qkv_w, na_rpb, na_out_w):
    f32 = np.float32
    A = lambda a: np.asarray(a, dtype=f32)
    x = A(x)[0]; ctx = A(ctx)[0]; ada_w = A(ada_w); ada_b = A(ada_b); norm_g = A(norm_g); inw = A(ab_in_w)[0]
    cos, sin = _rope_tables()
    com = {}
    com["x_all"] = _c(np.concatenate([ctx, x], 0)); com["x_rev"] = _c(np.concatenate([ctx[::-1], x[::-1]], 0))
    com["adaw"] = ada_w; com["bcol"] = _c(ada_b.reshape(2, 48, 128).transpose(2, 0, 1)); com["brow"] = _c(np.stack([_bc(ada_b[0]), _bc(ada_b[1])]))
    com["cT"] = _c(np.stack([A(c)[0], A(c_ctx)], 0).reshape(2, 8, 128).transpose(2, 1, 0))
    com["ng"] = _c(np.stack([np.stack([_col(norm_g[l, i]) for i in range(4)]) for l in range(2)]))
    com["ngrow"] = _c(np.stack([np.stack([_bc(norm_g[l, i]) for i in range(4)]) for l in range(2)]))
    kr = inw[:, 1952:2016]; krs = np.concatenate([kr[:, 32:], kr[:, :32]], 1)
    z16 = np.zeros((1024, 16), f32)
    com["w_tmA"] = _c(np.concatenate([inw[:, 256:512], inw[:, 512:1024], inw[:, 1824:1952]], 1))
    com["w_fmA"] = _c(np.concatenate([inw[:, 0:256], inw[:, 256:512], inw[:, 1536:1552], z16, kr, krs], 1))
    com["w_tmB"] = _c(np.concatenate([inw[:, 256:512], inw[:, 512:1024]], 1))
    com["w_fmB"] = _c(np.concatenate([inw[:, 0:256], inw[:, 256:512], inw[:, 1552:1568]], 1))
    com["w_own"] = _c(np.concatenate([inw[:, 1024:1536], inw[:, 1568:1824]], 1))
    Ca = np.ones((64, TALL), f32); Sa = np.zeros((64, TALL), f32)
    Ca[:, 256:] = np.concatenate([cos.T, cos.T], 0); Sa[:, 256:] = np.concatenate([-sin.T, sin.T], 0)
    com["Ca"] = Ca; com["Sa"] = Sa
    wk = A(mla_wukv)[0].reshape(128, 4, 256)
    com["wukv"] = _c(np.concatenate([wk[:, :, :128].reshape(128, 512), wk[:, :, 128:].reshape(128, 512)], 1))
    com["kvn"] = _c(A(mla_kvnorm)[0].reshape(128, 1)); com["qn"] = _col(A(mla_qnorm)[0])
    wq = A(mla_wuq)[0]; com["wuq"] = _c(wq)
    com["wuqs"] = _c(np.concatenate([np.concatenate([wq[:, h * 192 + 160:h * 192 + 192], wq[:, h * 192 + 128:h * 192 + 160]], 1) for h in range(4)], 1))
    wa = np.zeros((17, 8, 64), f32)
    for d, (w_, b_) in enumerate(((A(gla_wa_f)[0], A(gla_ba_f)[0]), (A(gla_wa_b)[0], A(gla_ba_b)[0]))):
        for h in range(4):
            wa[:16, d * 4 + h] = w_[:, h * 64:(h + 1) * 64]; wa[16, d * 4 + h] = b_[h * 64:(h + 1) * 64]
    com["wa"] = wa
    com["tri"] = np.where(np.arange(64)[:, None] <= np.arange(64)[None, :], -1.0 / 16, 0.0).astype(f32)
    com["mu"] = _c(np.repeat((np.arange(64)[:, None] <= np.arange(64)[None, :]).astype(f32)[:, None, :], 8, 1))
    com["ow0"] = _c(A(ab_out_w)[0]); com["ow1"] = _c(A(na_out_w)[0]); com["onc"] = _c(A(gla_onorm)[0].reshape(128, 1))
    com["rw"] = _c(A(router_w)); com["rb"] = _c(np.stack([_bc(A(router_b)[0]), _bc(A(router_b)[1])]))
    com["w1"] = A(moe_w1); com["w2"] = A(moe_w2)
    b1 = A(moe_b1)
    colE = lambda a: _c(a.reshape(32, 8, 128).transpose(2, 0, 1))
    com["b1"] = _c(b1); com["iota"] = _bc(np.arange(128, dtype=f32)); com["lst"] = (np.arange(128)[:, None] < np.arange(128)[None, :]).astype(f32)
    com["b2"] = _c(A(moe_b2))
    qkv = A(na_qkv_w)[0]; com["wqk"] = _c(qkv[:, :2048]); com["wv"] = _c(qkv[:, 2048:])
    rpb = A(na_rpb)[0]
    com["Bint"] = na_bias_table(rpb, 4)
    maps = []
    for cc in range(8):
        m = dict(com)
        er = np.arange(32 * cc - 4, 32 * cc + 36)
        erc = np.clip(er, 0, 255)
        tok = (erc[:, None] * 64 + np.arange(64)[None, :]).ravel()
        m["x_own"] = _c(np.concatenate([x[tok], ctx], 0))
        Cq = np.ones((64, TOWN), f32); Sq = np.zeros((64, TOWN), f32)
        Cq[:, :2560] = np.concatenate([cos[tok].T, cos[tok].T], 0); Sq[:, :2560] = np.concatenate([-sin[tok].T, sin[tok].T], 0)
        m["Cq"] = Cq; m["Sq"] = Sq
        posf = np.concatenate([256 + tok, np.arange(256)]); posb = np.concatenate([256 + (16383 - tok), 255 - np.arange(256)])
        m["idxf"] = np.ascontiguousarray(posf.reshape(NOWN, 128).T.astype(np.int32)); m["idxb"] = np.ascontiguousarray(posb.reshape(NOWN, 128).T.astype(np.int32))
        be = []
        for e in range(8):
            i = e if e < 4 else 24 + e
            e0 = 0 if e < 4 else 28
            be.append(na_bias_edge(rpb, 32 * cc + i, [int(er[e0 + w]) for w in range(12)]))
        m["Bedge"] = _c(np.stack(be))
        maps.append(m)
    return maps


def kernel(**inputs):
    if "P" not in _FP:
        _FP["P"] = build_fused()
    P = _FP["P"]
    maps = fused_inputs(**inputs)
    maps = [{k: m[k] for k in P.in_names} for m in maps]
    res = run_bass_kernel_spmd(P.nc, maps, core_ids=list(range(8)))
    return np.concatenate([r["out"] for r in res.results], 0)[None].astype(np.float32)


def stage_pro2(P, idb, x_d, groups, gvar, gsc, gsh, D, wtm_d, ntm, tm_outs, wfm_d, nfm, fm_specs, rope=None, mla=None, tag="pro", extra=None, per_tile=2):
    P.begin()
    KC = D // 128
    tiles = []
    for gi, (tok0, gt) in enumerate(groups):
        for t in range(gt):
            tiles.append((tok0 + t * 128, gvar[gi]))
    nt = len(tiles)
    stg = [P.sb("wst%d" % i, [128, max(ntm, nfm, 1024)]) for i in range(2)]
    wtm = load_w_bf16(P, wtm_d, D, ntm, "wtm", stage=stg) if ntm else None
    wfm = load_w_bf16(P, wfm_d, D, nfm, "wfm", stage=stg) if nfm else None
    R = 3
    ring = lambda nm, shp, dt=F32, n=R: [P.sb("%s%d" % (nm, i), shp, dt) for i in range(n)]
    if mla:
        wuk = load_w_bf16(P, mla["wukv_d"], 128, 1024, "wuk", stage=stg)
        kvn = P.sb("kvn", [128, 1]); P.dma("sp", kvn.v, mla["kvn_d"])
        ckn = ring("ckn", [128, 128], BF16); ckT = ring("ckT", [128, 128], BF16)
        kst = ring("kst", [128, 4, 128], BF16); vst = ring("vst", [128, 512], BF16)
        pck = P.ps("pck", [128, 128], BF16)
    xs = ring("x", [128, D]); junk = P.sb("junk", [128, D]); st = ring("st", [128, 8])
    xn = ring("xn", [128, D], BF16); hT = ring("hT", [128, KC, 128], BF16)
    yt = ring("yt", [128, max(ntm, 1)]); ytb = ring("ytb", [128, max(ntm, 1)], BF16)
    nspec = max(len(fm_specs), 1)
    fo = [[P.sb("fo%d_%d" % (i, j), [128, 128], (BF16 if fm_specs[j][3] == BF16 else F32)) for j in range(len(fm_specs))] for i in range(R)]
    pT = [P.ps("pT%d" % i, [128, KC, 128], BF16) for i in range(2)]
    py = [P.ps("py%d" % i, [128, 512]) for i in range(2)]; pf = [P.ps("pf%d" % i, [128, 4, 128]) for i in range(2)]
    if rope:
        Ct = ring("Ct", [64, 128]); St = ring("St", [64, 128]); r1 = ring("r1", [64, 128]); r2 = ring("r2", [64, 128]); rb_ = ring("rb", [64, 128], BF16)
    cnt = dict(y=0, f=0)

    def A(t):
        r0, v = tiles[t]; a = t % R; b = t % 2
        P.dma("sp", xs[a].v, x_d[r0:r0 + 128, :])
        P.op("act", "activation", out=junk.v, in_=xs[a].v, func=AF.Square, accum_out=st[a][:, 0:1])
        rstd_of(P, st[a][:, 0:1], st[a][:, 2:3], st[a][:, 1:2], D)
        P.op("dve", "tensor_scalar", out=xn[a].v, in0=xs[a].v, scalar1=st[a][:, 2:3], scalar2=None, op0=ALU.mult)
        for k in range(KC):
            P.op("pe", "transpose", out=pT[b][:, k, :], in_=xn[a][:, k * 128:(k + 1) * 128], identity=idb.v, track=(k == KC - 1))
        for k in range(KC):
            P.op("dve", "tensor_scalar", out=hT[a][:, k, :], in0=pT[b][:, k, :], scalar1=gsc[:, v, k:k + 1], scalar2=gsh[:, v, k:k + 1], op0=ALU.mult, op1=ALU.add)

    def B1(t):
        r0, v = tiles[t]; a = t % R
        if ntm:
            for c0 in range(0, ntm, 512):
                cn = min(512, ntm - c0); pb = py[cnt["y"] % 2]; cnt["y"] += 1
                for k in range(KC):
                    P.op("pe", "matmul", track=(k == KC - 1), out=pb[:, 0:cn], lhsT=hT[a][:, k, :], rhs=wtm[:, k, c0:c0 + cn], start=(k == 0), stop=(k == KC - 1))
                P.op("act", "activation", out=yt[a][:, c0:c0 + cn], in_=pb[:, 0:cn], func=AF.Copy)
            for (c0, n, ap, dt) in tm_outs:
                if dt == BF16:
                    P.op("pool", "tensor_copy", out=ytb[a][:, c0:c0 + n], in_=yt[a][:, c0:c0 + n])
                    P.dma("sp", ap[r0:r0 + 128, :], ytb[a][:, c0:c0 + n])
                else:
                    P.dma("sp", ap[r0:r0 + 128, :], yt[a][:, c0:c0 + n])
        if mla:
            cc = mla["col"]
            P.op("act", "activation", out=junk[:, 0:128], in_=yt[a][:, cc:cc + 128], func=AF.Square, accum_out=st[a][:, 4:5])
            rstd_of(P, st[a][:, 4:5], st[a][:, 6:7], st[a][:, 5:6], 128)
            P.op("dve", "tensor_scalar", out=ckn[a].v, in0=yt[a][:, cc:cc + 128], scalar1=st[a][:, 6:7], scalar2=None, op0=ALU.mult)
            P.op("pe", "transpose", out=pck.v, in_=ckn[a].v, identity=idb.v)
            P.op("dve", "tensor_scalar", out=ckT[a].v, in0=pck.v, scalar1=kvn[:, 0:1], scalar2=None, op0=ALU.mult)

    def B2(t):
        r0, v = tiles[t]; a = t % R
        for si in range(0, len(fm_specs), 4):
            chunk = fm_specs[si:si + 4]
            pb = pf[cnt["f"] % 2]; cnt["f"] += 1
            for j, (c0, m, ap, dt, scale) in enumerate(chunk):
                for k in range(KC):
                    P.op("pe", "matmul", track=(k == KC - 1), out=pb[0:m, j, :], lhsT=wfm[:, k, c0:c0 + m], rhs=hT[a][:, k, :], start=(k == 0), stop=(k == KC - 1))
            for j, (c0, m, ap, dt, scale) in enumerate(chunk):
                dst = fo[a][si + j]
                P.op("act", "activation", out=dst[0:m, :], in_=pb[0:m, j, :], func=AF.Copy, scale=float(scale))
                P.dma("sp", ap[:, r0:r0 + 128], dst[0:m, :])
        if rope:
            ckr, ckrs, ap, C_d, S_d = rope
            P.dma("sp", Ct[a].v, C_d[:, r0:r0 + 128]); P.dma("sp", St[a].v, S_d[:, r0:r0 + 128])
            pb = pf[cnt["f"] % 2]; cnt["f"] += 1
            for k in range(KC):
                P.op("pe", "matmul", track=(k == KC - 1), out=pb[0:64, 0, :], lhsT=wfm[:, k, ckr:ckr + 64], rhs=hT[a][:, k, :], start=(k == 0), stop=(k == KC - 1))
            for k in range(KC):
                P.op("pe", "matmul", track=(k == KC - 1), out=pb[0:64, 1, :], lhsT=wfm[:, k, ckrs:ckrs + 64], rhs=hT[a][:, k, :], start=(k == 0), stop=(k == KC - 1))
            P.op("dve", "tensor_tensor", out=r1[a].v, in0=pb[0:64, 0, :], in1=Ct[a].v, op=ALU.mult)
            P.op("dve", "tensor_tensor", out=r2[a].v, in0=pb[0:64, 1, :], in1=St[a].v, op=ALU.mult)
            P.op("pool", "tensor_tensor", out=rb_[a].v, in0=r1[a].v, in1=r2[a].v, op=ALU.add)
            P.dma("sp", ap[:, r0:r0 + 128], rb_[a].v)
        if mla:
            pb = py[cnt["y"] % 2]; cnt["y"] += 1
            P.op("pe", "matmul", out=pb.v, lhsT=ckT[a].v, rhs=wuk[:, 0, 512:1024], start=True, stop=True)
            P.op("act", "activation", out=vst[a].v, in_=pb.v, func=AF.Copy)
            P.dma("sp", mla["V_s"][r0:r0 + 128, :], vst[a].v)
            pb = pf[cnt["f"] % 2]; cnt["f"] += 1
            for h in range(4):
                P.op("pe", "matmul", out=pb[:, h, :], lhsT=wuk[:, 0, h * 128:(h + 1) * 128], rhs=ckT[a].v, start=True, stop=True, track=(h == 3))
            P.op("act", "activation", out=kst[a].v, in_=pb.v, func=AF.Copy)
            P.dma("sp", mla["kT_s"][:, :, r0:r0 + 128].rearrange("h p t -> p h t"), kst[a].v)

    xsteps = extra(P) if extra else []
    xi = 0
    for step in range(nt + 2):
        for _ in range(per_tile):
            if xi < len(xsteps):
                xsteps[xi](); xi += 1
        if step < nt:
            A(step)
        if 0 <= step - 1 < nt:
            B1(step - 1)
        if 0 <= step - 2 < nt:
            B2(step - 2)
    while xi < len(xsteps):
        xsteps[xi](); xi += 1
    P.end()


def stage_moe_sp(P, idb, variants, passes, hTok_s, G_s, GT_s, x1_s, w1_d, w2_d, b1_d, b2_d, g3_d, gates_s, l, x2_ap, iota_d, lst_d, NE=32):
    P.begin()
    nt = len(variants); nv = 2; D = 1024; KC = 8; F = 1024
    mp = max(sum(len(g) for g in ps_) for ps_ in passes)
    b2 = P.sb("b2", [32, D]); P.dma("sp", b2.v, b2_d)
    iota = P.sb("iota", [128, 128]); P.dma("sp", iota.v, iota_d)
    lst32 = P.sb("lst32", [128, 128]); P.dma("sp", lst32.v, lst_d)
    lst = P.sb("lst", [128, 128], BF16); P.op("dve", "tensor_copy", out=lst.v, in_=lst32.v)
    onesb = P.sb("onesb", [128, 128], BF16); P.op("dve", "memset", ap=onesb.v, constant=1.0)
    stg = [P.sb("stg%d" % i, [128, 2 * F]) for i in range(2)]
    w1b = [P.sb("w1b%d" % i, [128, KC, 2 * F], BF16) for i in range(2)]
    w2b = [P.sb("w2b%d" % i, [128, KC, D], BF16) for i in range(2)]
    b1b = [P.sb("b1b%d" % i, [1, 2 * F], BF16) for i in range(2)]
    Gs = P.sb("Gs", [128, mp, 32]); GTs = P.sb("GTs", [32, mp * 128]); msk = P.sb("msk", [128, mp, 32]); mskb = P.sb("mskb", [128, mp, 32], BF16)
    pos = P.sb("pos", [128, mp, 32])
    yacc = P.sb("yacc", [128, mp, D])
    htm = [P.sb("htm%d" % i, [128, 4, D], BF16) for i in range(2)]
    Sel = [[P.sb("Sel%d_%d" % (j, i), [128, 128], BF16) for i in range(4)] for j in range(2)]
    SelG = [[P.sb("SelG%d_%d" % (j, i), [128, 128], BF16) for i in range(4)] for j in range(2)]
    XeT = [P.sb("XeT%d" % i, [128, KC, 128], BF16) for i in range(2)]
    tgh = [P.sb("tg%d" % i, [128, 512]) for i in range(2)]; tlh = [P.sb("tl%d" % i, [128, 512], BF16) for i in range(2)]
    tsgh = [P.sb("tsg%d" % i, [128, 512], BF16) for i in range(2)]; actbh = [P.sb("actb%d" % i, [128, 512], BF16) for i in range(2)]
    actT = P.sb("actT", [128, KC, 128], BF16); Ye = P.sb("Ye", [128, D], BF16); SGT = P.sb("SGT", [128, 4, 128], BF16)
    st = P.sb("st", [128, 4])
    pX = P.ps("pX", [128, 1024]); pUa = P.ps("pUa", [128, 1024]); pUb = P.ps("pUb", [128, 1024])
    pT = P.ps("pT", [128, KC, 128], BF16); pST = P.ps("pST", [128, 4, 128], BF16)
    pXv = pX.v.rearrange("p (f q) -> p f q", q=128)

    def load_expert_steps(e, buf):
        steps = []
        for k in range(KC):
            def step(k=k):
                P.dma("sp", w1b[buf][:, k, :], w1_d[e, k * 128:(k + 1) * 128, :])
                P.dma("sp", w2b[buf][:, k, :], w2_d[e, k * 128:(k + 1) * 128, :])
                if k == KC - 1:
                    P.dma("sp", b1b[buf].v, b1_d[e:e + 1, :])
            steps.append(step)
        return steps

    for groups in passes:
        ptiles = [t for g in groups for t in g]
        t0 = ptiles[0]; npt = len(ptiles); tok0 = t0 * 128; ntok = npt * 128
        P.dma("sp", Gs[:, 0:npt, :], G_s[tok0:tok0 + ntok, :].rearrange("(g p) e -> p g e", p=128))
        P.dma("sp", GTs[:, 0:ntok], GT_s[:, tok0:tok0 + ntok])
        P.op("dve", "tensor_scalar", out=msk[:, 0:npt, :], in0=Gs[:, 0:npt, :], scalar1=0.0, scalar2=None, op0=ALU.is_gt)
        P.op("dve", "tensor_copy", out=mskb[:, 0:npt, :], in_=msk[:, 0:npt, :])
        for i in range(npt):
            for n in range(2):
                P.op("pe", "matmul", out=pX[:, n * 512:(n + 1) * 512], lhsT=GTs[:, i * 128:(i + 1) * 128], rhs=b2[:, n * 512:(n + 1) * 512], start=True, stop=True)
            P.op("act", "activation", out=yacc[:, i, :], in_=pX.v, func=AF.Copy)
        for g in groups:
            for gi_, t in enumerate(g):
                i = t - t0
                for j_ in range(gi_):
                    P.op("pe", "matmul", track=False, out=pUa[:, 0:32], lhsT=onesb.v, rhs=mskb[:, g[j_] - t0, :], start=(j_ == 0), stop=False)
                P.op("pe", "matmul", out=pUa[:, 0:32], lhsT=lst.v, rhs=mskb[:, i, :], start=(gi_ == 0), stop=True)
                P.op("dve", "tensor_copy", out=pos[:, i, :], in_=pUa[:, 0:32])
        for s_ in load_expert_steps(0, 0):
            s_()
        its = [(e, gidx) for e in range(NE) for gidx in range(len(groups))]
        nxt_cache = {}

        def prep(n):
            e, gidx = its[n]; g = groups[gidx]; ng = len(g); i0 = g[0] - t0; s2 = n % 2
            hb = htm[s2]
            P.dma("sp", hb[:, 0:ng, :], hTok_s[g[0] * 128:(g[0] + ng) * 128, :].rearrange("(g p) f -> p g f", p=128))
            for ii in range(ng):
                P.op("dve", "tensor_scalar", out=Sel[s2][ii].v, in0=iota.v, scalar1=pos[:, i0 + ii, e:e + 1], scalar2=msk[:, i0 + ii, e:e + 1], op0=ALU.is_equal, op1=ALU.mult)
                P.op("dve", "tensor_scalar", out=SelG[s2][ii].v, in0=iota.v, scalar1=pos[:, i0 + ii, e:e + 1], scalar2=Gs[:, i0 + ii, e:e + 1], op0=ALU.is_equal, op1=ALU.mult)
            for f in range(KC):
                for ii in range(ng):
                    P.op("pe", "matmul", track=(f == KC - 1 and ii == ng - 1), out=pXv[:, f, :], lhsT=hb[:, ii, f * 128:(f + 1) * 128], rhs=Sel[s2][ii].v, start=(ii == 0), stop=(ii == ng - 1))
            P.op("act", "activation", out=XeT[s2].v, in_=pXv, func=AF.Copy)

        def ffn1(n):
            e, gidx = its[n]; buf = e % 2; s2 = n % 2
            for h in range(2):
                pu = (pUa, pUb)[h]
                for c in range(2):
                    col = h * 1024 + c * 512
                    for k in range(KC):
                        P.op("pe", "matmul", track=False, out=pu[:, c * 512:(c + 1) * 512], lhsT=XeT[s2][:, k, :], rhs=w1b[buf][:, k, col:col + 512], start=(k == 0), stop=False)
                    P.op("pe", "matmul", track=(c == 1), out=pu[:, c * 512:(c + 1) * 512], lhsT=onesb[0:1, :], rhs=b1b[buf][0:1, col:col + 512], start=False, stop=True)
                uv = pu.v.rearrange("p (f two) -> p f two", two=2)
                P.op("dve", "tensor_scalar", out=tgh[h].v, in0=uv[:, :, 0], scalar1=7.0, scalar2=None, op0=ALU.min)
                P.op("act", "activation", out=tsgh[h].v, in_=tgh[h].v, func=AF.Sigmoid, scale=1.702)
                P.op("dve", "tensor_scalar", out=tlh[h].v, in0=uv[:, :, 1], scalar1=-7.0, scalar2=7.0, op0=ALU.max, op1=ALU.min)
                P.op("pool", "tensor_tensor", out=tgh[h].v, in0=tgh[h].v, in1=tsgh[h].v, op=ALU.mult)
                P.op("pool", "tensor_scalar", out=tlh[h].v, in0=tlh[h].v, scalar1=1.0, scalar2=None, op0=ALU.add)
                P.op("pool", "tensor_tensor", out=actbh[h].v, in0=tlh[h].v, in1=tgh[h].v, op=ALU.mult)

        def stage3(n):
            e, gidx = its[n]; g = groups[gidx]; ng = len(g); i0 = g[0] - t0; buf = e % 2; s2 = n % 2
            for f in range(KC):
                P.op("pe", "transpose", out=pT[:, f, :], in_=actbh[f // 4][:, (f % 4) * 128:(f % 4 + 1) * 128], identity=idb.v, track=(f == KC - 1))
            P.op("act", "activation", out=actT.v, in_=pT.v, func=AF.Copy)
            for ii in range(ng):
                P.op("pe", "transpose", out=pST[:, ii, :], in_=SelG[s2][ii].v, identity=idb.v, track=(ii == ng - 1))
            P.op("dve", "tensor_copy", out=SGT[:, 0:ng, :], in_=pST[:, 0:ng, :])
            for half in range(2):
                hs = slice(half * 512, (half + 1) * 512)
                for k in range(KC):
                    P.op("pe", "matmul", track=(k == KC - 1), out=pUa[:, hs], lhsT=actT[:, k, :], rhs=w2b[buf][:, k, hs], start=(k == 0), stop=(k == KC - 1))
            P.op("act", "activation", out=Ye.v, in_=pUa.v, func=AF.Copy)
            ci = 0
            for ii in range(ng):
                for half in range(2):
                    hs = slice(half * 512, (half + 1) * 512); pc = pUb[:, (ci % 2) * 512:(ci % 2 + 1) * 512]; ci += 1
                    P.op("pe", "matmul", out=pc, lhsT=SGT[:, ii, :], rhs=Ye[:, hs], start=True, stop=True)
                    P.op("dve", "tensor_tensor", out=yacc[:, i0 + ii, hs], in0=pc, in1=yacc[:, i0 + ii, hs], op=ALU.add)
            if e + 1 < NE:
                if e not in nxt_cache:
                    nxt_cache[e] = load_expert_steps(e + 1, 1 - buf)
                nxt = nxt_cache[e]
                per = (len(nxt) + len(groups) - 1) // len(groups)
                for s_ in nxt[gidx * per:(gidx + 1) * per]:
                    s_()

        prep(0)
        for n in range(len(its)):
            ffn1(n)
            if n + 1 < len(its):
                prep(n + 1)
            stage3(n)
        P.dma("sp", stg[1][:, 0:D], g3_d)
        for v in range(nv):
            P.dma("sp", stg[0][:, v * D:(v + 1) * D], gates_s[l, 1, v])
            P.op("dve", "tensor_tensor", out=stg[0][:, v * D:(v + 1) * D], in0=stg[0][:, v * D:(v + 1) * D], in1=stg[1][:, 0:D], op=ALU.mult)
        hf = htm[0].v.rearrange("p a b -> p (a b)").bitcast(F32)
        for i in range(npt):
            t = ptiles[i]; v = variants[t]; sl = slice(t * 128, (t + 1) * 128)
            xt = hf[:, 0:D]; junk = hf[:, D:2 * D]; tmp = stg[1][:, D:2 * D]
            P.dma("sp", xt, x1_s[sl, :])
            P.op("act", "activation", out=junk, in_=yacc[:, i, :], func=AF.Square, accum_out=st[:, 0:1])
            rstd_of(P, st[:, 0:1], st[:, 2:3], st[:, 1:2], D)
            P.op("dve", "scalar_tensor_tensor", out=tmp, in0=yacc[:, i, :], scalar=st[:, 2:3], in1=stg[0][:, v * D:(v + 1) * D], op0=ALU.mult, op1=ALU.mult)
            P.op("pool", "tensor_tensor", out=xt, in0=tmp, in1=xt, op=ALU.add)
            P.dma("sp", x2_ap[sl, :], xt)
    P.end()
```

```python
import numpy as np
import ml_dtypes
import concourse.bass as bass
import concourse.mybir as mybir
from concourse.bass_utils import run_bass_kernel_spmd

F32 = mybir.dt.float32
BF16 = mybir.dt.bfloat16
I32 = mybir.dt.int32
ALU = mybir.AluOpType
AF = mybir.ActivationFunctionType
AX = mybir.AxisListType

COMPUTE = ("pe", "act", "dve", "pool")


class View:
    __slots__ = ("b", "ap")

    def __init__(self, b, ap):
        self.b = b
        self.ap = ap

    def __getitem__(self, k):
        return View(self.b, self.ap[k])

    def bitcast(self, dt):
        return View(self.b, self.ap.bitcast(dt))

    def rearrange(self, s, **kw):
        return View(self.b, self.ap.rearrange(s, **kw))

    def to_broadcast(self, shape):
        return View(self.b, self.ap.to_broadcast(shape))


class Buf:
    def __init__(self, prog, t, name):
        self.p = prog
        self.t = t
        self.name = name
        self.w = None
        self.r = {}
        self.wsem = None
        self.wcnt = 0
        self.rsem = None
        self.rcnt = 0

    def __getitem__(self, k):
        return View(self, self.t[k])

    @property
    def v(self):
        return View(self, self.t.ap())


class Prog:
    def __init__(self, name="k"):
        self.nc = bass.Bass("TRN2", target_bir_lowering=False, name=name)
        nc = self.nc
        self.E = dict(pe=nc.tensor, act=nc.scalar, dve=nc.vector, pool=nc.gpsimd, sp=nc.sync)
        self.sem = {e: nc.alloc_semaphore("sem_" + e) for e in COMPUTE}
        self.cnt = {e: 0 for e in COMPUTE}
        self.pending = {e: False for e in COMPUTE}
        self.seen = {e: {} for e in self.E}
        self.bufs = []
        self.nsem = 4
        self.ninst = 0
        self.sempool = []
        self.semid = {}
        self.stack = None
        self.scope_bufs = None
        self.nscope = 0

    def sb(self, name, shape, dt=F32):
        if self.stack is not None:
            t = self.stack.enter_context(self.nc.sbuf_tensor("sb%d_%s" % (self.nscope, name), list(shape), dt))
        else:
            t = self.nc.alloc_sbuf_tensor("sb_" + name, list(shape), dt)
        b = Buf(self, t, name)
        self.bufs.append(b)
        if self.scope_bufs is not None:
            self.scope_bufs.append(b)
        return b

    def ps(self, name, shape, dt=F32):
        if self.stack is not None:
            t = self.stack.enter_context(self.nc.psum_tensor("ps%d_%s" % (self.nscope, name), list(shape), dt))
        else:
            t = self.nc.alloc_psum_tensor("ps_" + name, list(shape), dt)
        b = Buf(self, t, name)
        self.bufs.append(b)
        if self.scope_bufs is not None:
            self.scope_bufs.append(b)
        return b

    def scratch(self, name, shape, dt=F32, dbg=False):
        return self.nc.dram_tensor(name, list(shape), dt, kind="ExternalOutput" if dbg else "Internal").ap()

    def begin(self):
        from contextlib import ExitStack
        self.nscope += 1
        self.stack = ExitStack()
        self.scope_bufs = []

    def barrier(self):
        for e in self.E:
            for f in COMPUTE:
                if self.cnt[f]:
                    self._wait(e, f, self.sem[f], self.cnt[f])
            for b in self.bufs:
                if b.wsem is not None and b.wcnt:
                    self._wait(e, ("s", id(b.wsem)), b.wsem, 16 * b.wcnt)
                if b.rsem is not None and b.rcnt:
                    self._wait(e, ("s", id(b.rsem)), b.rsem, 16 * b.rcnt)

    def end(self):
        self.barrier()
        for b in self.scope_bufs:
            if b.wsem is not None:
                self.sempool.append([b.wsem, b.wcnt]); b.wsem = None
            if b.rsem is not None:
                self.sempool.append([b.rsem, b.rcnt]); b.rsem = None
            self.bufs.remove(b)
        self.stack.close()
        self.stack = None
        self.scope_bufs = None

    def _getsem(self, name):
        if self.sempool:
            s, c = self.sempool.pop()
            return s, c
        return self._newsem(name), 0

    def dram(self, name, shape, dt=F32, out=False):
        if not out:
            self.__dict__.setdefault("in_names", []).append(name)
        return self.nc.dram_tensor(name, list(shape), dt, kind="ExternalOutput" if out else "ExternalInput").ap()

    def _newsem(self, name):
        self.nsem += 1
        return self.nc.alloc_semaphore(name)

    def _wait(self, e, key, sem, val):
        if self.seen[e].get(key, 0) >= val:
            return
        self.E[e].wait_ge(sem, val)
        self.seen[e][key] = val

    def _w_done(self, e, b):
        if b.w is not None:
            f, n = b.w
            if not (f == "pe" and e == "pe"):
                self._wait(e, f, self.sem[f], n)
        if b.wcnt:
            self._wait(e, ("s", id(b.wsem)), b.wsem, 16 * b.wcnt)

    def _r_done(self, e, b):
        for f, n in b.r.items():
            if f == "pe" and e == "pe":
                continue
            self._wait(e, f, self.sem[f], n)
        if b.rcnt:
            self._wait(e, ("s", id(b.rsem)), b.rsem, 16 * b.rcnt)

    def op(self, e, fn, track=True, **kw):
        wkeys = ("out", "accum_out", "ap")
        reads = [v.b for k, v in kw.items() if isinstance(v, View) and k not in wkeys]
        writes = [kw[k].b for k in wkeys if k in kw and isinstance(kw[k], View)]
        for b in reads:
            self._w_done(e, b)
        for b in writes:
            self._w_done(e, b)
            self._r_done(e, b)
        args = {k: (v.ap if isinstance(v, View) else v) for k, v in kw.items()}
        ins = getattr(self.E[e], fn)(**args)
        self.ninst += 1
        seq = self.cnt[e] + 1
        if track:
            ins.then_inc(self.sem[e], 1)
            self.cnt[e] = seq
            self.pending[e] = False
        else:
            self.pending[e] = True
        for b in reads:
            b.r[e] = seq
        for b in writes:
            b.w = (e, seq)
            b.r = {}
        return ins

    def dma(self, q, out, in_, **kw):
        if isinstance(out, View):
            b = out.b
            self._w_done(q, b)
            self._r_done(q, b)
            if b.wsem is None:
                b.wsem, b.wcnt = self._getsem("dw%d_%s" % (self.nscope, b.name))
            ins = self.E[q].dma_start(out=out.ap, in_=in_, **kw)
            ins.then_inc(b.wsem, 16)
            b.wcnt += 1
            b.w = None
            b.r = {}
        else:
            b = in_.b
            self._w_done(q, b)
            if b.rsem is None:
                b.rsem, b.rcnt = self._getsem("dr%d_%s" % (self.nscope, b.name))
            ins = self.E[q].dma_start(out=out, in_=in_.ap, **kw)
            ins.then_inc(b.rsem, 16)
            b.rcnt += 1
        self.ninst += 1
        return ins

    def gather(self, out, src_ap, idx):
        q = "pool"
        b = out.b
        self._w_done(q, b); self._r_done(q, b); self._w_done(q, idx.b)
        if b.wsem is None:
            b.wsem, b.wcnt = self._getsem("dw%d_%s" % (self.nscope, b.name))
        ins = self.nc.gpsimd.indirect_dma_start(out=out.ap, out_offset=None, in_=src_ap,
                                                in_offset=bass.IndirectOffsetOnAxis(ap=idx.ap, axis=0))
        ins.then_inc(b.wsem, 16)
        b.wcnt += 1; b.w = None; b.r = {}
        idx.b.r["pool"] = self.cnt["pool"] + 1
        self.ninst += 1
        return ins

    def finish(self, q="sp"):
        for b in self.bufs:
            if b.rcnt:
                self._wait(q, ("s", id(b.rsem)), b.rsem, 16 * b.rcnt)
        for e in COMPUTE:
            if self.cnt[e]:
                self._wait(q, e, self.sem[e], self.cnt[e])
        return self.nc


def run(prog, in_maps, ncores=8, trace=False):
    res = run_bass_kernel_spmd(prog.nc, in_maps, core_ids=list(range(ncores)), trace=trace)
    return res


EPS = 1e-6


def ident(P):
    idf = P.sb("idf", [128, 128]); idb = P.sb("idb", [128, 128], BF16)
    P.op("pool", "memset", ap=idf.v, constant=1.0)
    P.op("pool", "affine_select", out=idf.v, in_=idf.v, pattern=[[-1, 128]], compare_op=ALU.is_equal, fill=0.0, base=0, channel_multiplier=1)
    P.op("pool", "tensor_copy", out=idb.v, in_=idf.v)
    return idf, idb


def load_w_bf16(P, w_d, K, N, name, q="sp", cast_eng="pool", stage=None):
    KC = (K + 127) // 128
    wb = P.sb(name, [128, KC, N], BF16)
    for k in range(KC):
        rows = min(128, K - k * 128)
        st = stage[k % len(stage)]
        P.dma(q, st[0:rows, 0:N], w_d[k * 128:k * 128 + rows, :])
        P.op(cast_eng, "tensor_copy", out=wb[0:rows, k, :], in_=st[0:rows, 0:N])
    return wb


def rstd_of(P, ss_view, out_view, tmp_view, D):
    P.op("dve", "tensor_scalar", out=tmp_view, in0=ss_view, scalar1=1.0 / D, scalar2=EPS, op0=ALU.mult, op1=ALU.add)
    P.op("act", "activation", out=tmp_view, in_=tmp_view, func=AF.Sqrt)
    P.op("dve", "reciprocal", out=out_view, in_=tmp_view)


def build_proj(D, N, variants, rope=None, name="proj"):
    P = Prog(name)
    nt = len(variants); nv = max(variants) + 1
    KC = D // 128
    x_d = P.dram("x", [nt * 128, D]); w_d = P.dram("w", [D, N]); y_d = P.dram("y", [nt * 128, N], out=True)
    g_d = P.dram("gcol", [128, KC]); sc_d = P.dram("sc", [128, nv, KC]); sh_d = P.dram("sh", [128, nv, KC])
    if rope:
        cs_d = P.dram("cs", [nt * 128, 2, rope[1], 32])
    idf, idb = ident(P)
    stage = [P.sb("wst%d" % i, [128, N]) for i in range(2)]
    wb = load_w_bf16(P, w_d, D, N, "wb", stage=stage)
    g = P.sb("g", [128, KC]); sc = P.sb("scm", [128, nv, KC]); sh = P.sb("shm", [128, nv, KC])
    P.dma("sp", g.v, g_d); P.dma("sp", sc.v, sc_d); P.dma("sp", sh.v, sh_d)
    for v in range(nv):
        P.op("dve", "scalar_tensor_tensor", out=sc[:, v, :], in0=sc[:, v, :], scalar=1.0, in1=g.v, op0=ALU.add, op1=ALU.mult)
    xs = [P.sb("x%d" % i, [128, D]) for i in range(2)]
    junk = P.sb("junk", [128, D]); st = [P.sb("st%d" % i, [128, 4]) for i in range(2)]
    xn = [P.sb("xn%d" % i, [128, D], BF16) for i in range(2)]
    hT = [P.sb("hT%d" % i, [128, KC, 128], BF16) for i in range(2)]
    ys = [P.sb("y%d" % i, [128, N]) for i in range(2)]
    pT = [P.ps("pT%d" % i, [128, KC, 128], BF16) for i in range(2)]
    py = [P.ps("py%d" % i, [128, 512]) for i in range(4)]
    if rope:
        cs = [P.sb("cs%d" % i, [128, 2, rope[1], 32]) for i in range(2)]
        rt = [P.sb("rt%d" % i, [128, rope[1], 32]) for i in range(4)]
    nchunks = [(c, min(512, N - c)) for c in range(0, N, 512)]
    ci = 0
    for t in range(nt):
        b = t % 2; v = variants[t]
        P.dma("sp", xs[b].v, x_d[t * 128:(t + 1) * 128, :])
        if rope and v == 0:
            P.dma("sp", cs[b].v, cs_d[t * 128:(t + 1) * 128])
        P.op("act", "activation", out=junk.v, in_=xs[b].v, func=AF.Square, accum_out=st[b][:, 0:1])
        rstd_of(P, st[b][:, 0:1], st[b][:, 2:3], st[b][:, 1:2], D)
        P.op("dve", "tensor_scalar", out=xn[b].v, in0=xs[b].v, scalar1=st[b][:, 2:3], scalar2=None, op0=ALU.mult)
        for k in range(KC):
            P.op("pe", "transpose", out=pT[b][:, k, :], in_=xn[b][:, k * 128:(k + 1) * 128], identity=idb.v)
        for k in range(KC):
            P.op("dve", "tensor_scalar", out=hT[b][:, k, :], in0=pT[b][:, k, :], scalar1=sc[:, v, k:k + 1], scalar2=sh[:, v, k:k + 1], op0=ALU.mult, op1=ALU.add)
        for (c0, cn) in nchunks:
            pb = py[ci % 4]; ci += 1
            for k in range(KC):
                P.op("pe", "matmul", track=(k == KC - 1), out=pb[:, 0:cn], lhsT=hT[b][:, k, :], rhs=wb[:, k, c0:c0 + cn], start=(k == 0), stop=(k == KC - 1))
            P.op("act", "activation", out=ys[b][:, c0:c0 + cn], in_=pb[:, 0:cn], func=AF.Copy)
        if rope and v == 0:
            col0, ns, stride, off = rope
            seg = ys[b][:, col0:col0 + ns * stride].rearrange("p (h d) -> p h d", d=stride)
            x1 = seg[:, :, off:off + 32]; x2 = seg[:, :, off + 32:off + 64]
            co = cs[b][:, 0]; si = cs[b][:, 1]
            P.op("pool", "tensor_tensor", out=rt[0].v, in0=x1, in1=co, op=ALU.mult)
            P.op("pool", "tensor_tensor", out=rt[1].v, in0=x2, in1=si, op=ALU.mult)
            P.op("pool", "tensor_tensor", out=rt[2].v, in0=x1, in1=si, op=ALU.mult)
            P.op("pool", "tensor_tensor", out=rt[3].v, in0=x2, in1=co, op=ALU.mult)
            P.op("pool", "tensor_tensor", out=x1, in0=rt[0].v, in1=rt[1].v, op=ALU.subtract)
            P.op("pool", "tensor_tensor", out=x2, in0=rt[2].v, in1=rt[3].v, op=ALU.add)
        P.dma("sp", y_d[t * 128:(t + 1) * 128, :], ys[b].v)
    P.finish()
    return P


def build_post(mode, variants, name="post"):
    P = Prog(name)
    nt = len(variants); nv = max(variants) + 1; T = nt * 128; D = 1024; KC = 8
    x_d = P.dram("x", [T, D]); ow_d = P.dram("ow", [D, D])
    g1_d = P.dram("g1g", [128, D]); gate_d = P.dram("gate", [nv, 128, D])
    g2_d = P.dram("gcol", [128, KC]); sc_d = P.dram("sc", [128, nv, KC]); sh_d = P.dram("sh", [128, nv, KC])
    rw_d = P.dram("rw", [D, 32]); rb_d = P.dram("rb", [128, 32])
    x1_d = P.dram("x1", [T, D], out=True); hT_d = P.dram("hT", [D, T], BF16, out=True); G_d = P.dram("G", [T, 32], out=True)
    if mode == "gla":
        of_d = P.dram("of", [T, 512]); ob_d = P.dram("ob", [T, 512]); r_d = P.dram("r", [T, 512]); om_d = P.dram("om", [T, 512])
        on_d = P.dram("oncol", [128, 1])
    else:
        aT_d = P.dram("aT", [D, T])
    idf, idb = ident(P)
    stage = [P.sb("wst%d" % i, [128, D]) for i in range(2)]
    wb = load_w_bf16(P, ow_d, D, D, "owb", stage=stage)
    rw = P.sb("rw", [128, KC, 32]); P.dma("sp", rw.v, rw_d.rearrange("(k p) n -> p k n", p=128))
    rb = P.sb("rb", [128, 32]); P.dma("sp", rb.v, rb_d)
    g2 = P.sb("g2", [128, KC]); sc = P.sb("scm", [128, nv, KC]); sh = P.sb("shm", [128, nv, KC])
    P.dma("sp", g2.v, g2_d); P.dma("sp", sc.v, sc_d); P.dma("sp", sh.v, sh_d)
    for v in range(nv):
        P.op("dve", "scalar_tensor_tensor", out=sc[:, v, :], in0=sc[:, v, :], scalar=1.0, in1=g2.v, op0=ALU.add, op1=ALU.mult)
    g1 = P.sb("g1", [128, D]); P.dma("sp", g1.v, g1_d)
    GG = []
    for v in range(nv):
        gg = P.sb("GG%d" % v, [128, D]); P.dma("sp", gg.v, gate_d[v])
        P.op("dve", "tensor_tensor", out=gg.v, in0=gg.v, in1=g1.v, op=ALU.mult)
        GG.append(gg)
    if mode == "gla":
        onc = P.sb("onc", [128, 1]); P.dma("sp", onc.v, on_d)
        tof = P.sb("tof", [128, 512]); tob = P.sb("tob", [128, 512]); tr = P.sb("tr", [128, 512]); tom = P.sb("tom", [128, 512])
        cat = P.sb("cat", [128, D], BF16)
        pT = P.ps("pT", [128, KC, 128], BF16)
    else:
        a32 = P.sb("a32", [128, KC, 128])
    aT = P.sb("aT", [128, KC, 128], BF16)
    xt = P.sb("xt", [128, D]); x1 = P.sb("x1", [128, D]); junk = P.sb("junk", [128, D]); st = P.sb("st", [128, 16])
    xn2 = P.sb("xn2", [128, D]); h2T = P.sb("h2T", [128, KC, 128]); h2Tb = P.sb("h2Tb", [128, KC, 128], BF16)
    lg = P.sb("lg", [128, 32]); t8 = P.sb("t8", [128, 8]); msk = P.sb("msk", [128, 32]); ex = P.sb("ex", [128, 32]); Gt = P.sb("Gt", [128, 32])
    py = [P.ps("py%d" % i, [128, 512]) for i in range(2)]
    pT32 = P.ps("pT32", [128, KC, 128])
    plg = P.ps("plg", [128, 32])
    for t in range(nt):
        v = variants[t]; sl = slice(t * 128, (t + 1) * 128)
        P.dma("sp", xt.v, x_d[sl, :])
        if mode == "gla":
            P.dma("sp", tof.v, of_d[sl, :]); P.dma("sp", tob.v, ob_d[sl, :]); P.dma("sp", tr.v, r_d[sl, :]); P.dma("sp", tom.v, om_d[sl, :])
            P.op("dve", "tensor_tensor", out=tof.v, in0=tof.v, in1=tob.v, op=ALU.add)
            for h in range(4):
                P.op("act", "activation", out=junk[:, 0:128], in_=tof[:, h * 128:(h + 1) * 128], func=AF.Square, accum_out=st[:, h:h + 1])
            rstd_of(P, st[:, 0:4], st[:, 8:12], st[:, 4:8], 128)
            P.op("act", "activation", out=tr.v, in_=tr.v, func=AF.Silu)
            for h in range(4):
                hs = slice(h * 128, (h + 1) * 128)
                P.op("dve", "scalar_tensor_tensor", out=cat[:, hs], in0=tof[:, hs], scalar=st[:, 8 + h:9 + h], in1=tr[:, hs], op0=ALU.mult, op1=ALU.mult)
            P.op("pool", "tensor_copy", out=cat[:, 512:1024], in_=tom.v)
            for k in range(KC):
                P.op("pe", "transpose", out=pT[:, k, :], in_=cat[:, k * 128:(k + 1) * 128], identity=idb.v)
            P.op("dve", "tensor_scalar", out=aT[:, 0:4, :], in0=pT[:, 0:4, :], scalar1=onc[:, 0:1], scalar2=None, op0=ALU.mult)
            P.op("act", "activation", out=aT[:, 4:8, :], in_=pT[:, 4:8, :], func=AF.Copy)
        else:
            P.dma("sp", a32.v, aT_d[:, sl].rearrange("(k p) t -> p k t", p=128))
            P.op("pool", "tensor_copy", out=aT.v, in_=a32.v)
        for n in range(2):
            for k in range(KC):
                P.op("pe", "matmul", track=(k == KC - 1), out=py[n].v, lhsT=aT[:, k, :], rhs=wb[:, k, n * 512:(n + 1) * 512], start=(k == 0), stop=(k == KC - 1))
            P.op("act", "activation", out=junk[:, 0:512], in_=py[n].v, func=AF.Square, accum_out=st[:, 12 + n:13 + n])
        P.op("dve", "tensor_tensor", out=st[:, 12:13], in0=st[:, 12:13], in1=st[:, 13:14], op=ALU.add)
        rstd_of(P, st[:, 12:13], st[:, 14:15], st[:, 13:14], D)
        for n in range(2):
            ns = slice(n * 512, (n + 1) * 512)
            P.op("dve", "scalar_tensor_tensor", out=x1[:, ns], in0=py[n].v, scalar=st[:, 14:15], in1=GG[v][:, ns], op0=ALU.mult, op1=ALU.mult)
        P.op("pool", "tensor_tensor", out=x1.v, in0=x1.v, in1=xt.v, op=ALU.add)
        P.dma("sp", x1_d[sl, :], x1.v)
        P.op("act", "activation", out=junk.v, in_=x1.v, func=AF.Square, accum_out=st[:, 15:16])
        rstd_of(P, st[:, 15:16], st[:, 4:5], st[:, 5:6], D)
        P.op("dve", "tensor_scalar", out=xn2.v, in0=x1.v, scalar1=st[:, 4:5], scalar2=None, op0=ALU.mult)
        for k in range(KC):
            P.op("pe", "transpose", out=pT32[:, k, :], in_=xn2[:, k * 128:(k + 1) * 128], identity=idf.v)
        for k in range(KC):
            P.op("dve", "tensor_scalar", out=h2T[:, k, :], in0=pT32[:, k, :], scalar1=sc[:, v, k:k + 1], scalar2=sh[:, v, k:k + 1], op0=ALU.mult, op1=ALU.add)
        P.op("pool", "tensor_copy", out=h2Tb.v, in_=h2T.v)
        P.dma("sp", hT_d[:, sl].rearrange("(k p) t -> p k t", p=128), h2Tb.v)
        for k in range(KC):
            P.op("pe", "matmul", track=(k == KC - 1), out=plg.v, lhsT=h2T[:, k, :], rhs=rw[:, k, :], start=(k == 0), stop=(k == KC - 1))
        P.op("dve", "tensor_tensor", out=lg.v, in0=plg.v, in1=rb.v, op=ALU.add)
        P.op("dve", "max", out=t8.v, in_=lg.v)
        P.op("dve", "tensor_scalar", out=msk.v, in0=lg.v, scalar1=t8[:, 3:4], scalar2=None, op0=ALU.is_ge)
        P.op("dve", "tensor_scalar", out=t8[:, 7:8], in0=t8[:, 0:1], scalar1=-1.0, scalar2=None, op0=ALU.mult)
        P.op("act", "activation", out=ex.v, in_=lg.v, func=AF.Exp, bias=t8[:, 7:8])
        P.op("dve", "tensor_tensor", out=ex.v, in0=ex.v, in1=msk.v, op=ALU.mult)
        P.op("dve", "tensor_reduce", out=t8[:, 6:7], in_=ex.v, axis=AX.X, op=ALU.add)
        P.op("dve", "reciprocal", out=t8[:, 5:6], in_=t8[:, 6:7])
        P.op("dve", "tensor_scalar", out=Gt.v, in0=ex.v, scalar1=t8[:, 5:6], scalar2=None, op0=ALU.mult)
        P.dma("sp", G_d[sl, :], Gt.v)
    P.finish()
    return P


def build_moe(variants, groups, NE=32, name="moe"):
    P = Prog(name)
    nt = len(variants); nv = max(variants) + 1; T = nt * 128; D = 1024; KC = 8; F = 1024
    hT_d = P.dram("hT", [D, T], BF16); G_d = P.dram("G", [T, 32]); GT_d = P.dram("GT", [32, T]); x1_d = P.dram("x1", [T, D])
    w1_d = P.dram("w1", [32, D, 2 * F]); w2_d = P.dram("w2", [32, F, D])
    b1g_d = P.dram("b1g", [128, 32, KC]); b1l_d = P.dram("b1l", [128, 32, KC]); b2_d = P.dram("b2", [32, D])
    g3_d = P.dram("g3g", [128, D]); gate_d = P.dram("gate", [nv, 128, D])
    x2_d = P.dram("x2", [T, D], out=True)
    mg = max(len(g) for g in groups)
    g3 = P.sb("g3", [128, D]); P.dma("sp", g3.v, g3_d)
    GG = []
    for v in range(nv):
        gg = P.sb("GG%d" % v, [128, D]); P.dma("sp", gg.v, gate_d[v])
        P.op("dve", "tensor_tensor", out=gg.v, in0=gg.v, in1=g3.v, op=ALU.mult)
        GG.append(gg)
    b1g = P.sb("b1g", [128, 32, KC]); b1l = P.sb("b1l", [128, 32, KC]); P.dma("sp", b1g.v, b1g_d); P.dma("sp", b1l.v, b1l_d)
    b2 = P.sb("b2", [32, D]); P.dma("sp", b2.v, b2_d)
    stg = [P.sb("stg%d" % i, [128, 2 * F]) for i in range(2)]
    w1g = [P.sb("w1g%d" % i, [128, KC, F], BF16) for i in range(2)]
    w1l = [P.sb("w1l%d" % i, [128, KC, F], BF16) for i in range(2)]
    w2b = [P.sb("w2b%d" % i, [128, KC, D], BF16) for i in range(2)]
    hT = P.sb("hT", [128, KC, mg * 128], BF16)
    Gs = P.sb("Gs", [128, mg, 32]); GTs = P.sb("GTs", [32, mg * 128])
    yacc = P.sb("yacc", [128, mg, D])
    actT = P.sb("actT", [128, KC, 512], BF16)
    tg = [P.sb("tg%d" % i, [128, 512]) for i in range(2)]; tsg = [P.sb("tsg%d" % i, [128, 512]) for i in range(2)]
    tl = [P.sb("tl%d" % i, [128, 512]) for i in range(2)]
    xt = P.sb("xt", [128, D]); junk = P.sb("junk", [128, D]); st = P.sb("st", [128, 4])
    pg = [P.ps("pg%d" % i, [128, 512]) for i in range(2)]; pl = [P.ps("pl%d" % i, [128, 512]) for i in range(2)]
    py = [P.ps("py%d" % i, [128, 512]) for i in range(2)]
    sti = [0]

    def load_expert_steps(e, buf):
        steps = []
        for k in range(KC):
            def step(k=k):
                s = stg[sti[0] % 2]; sti[0] += 1
                P.dma("sp", s.v, w1_d[e, k * 128:(k + 1) * 128, :])
                sv = s.v.rearrange("p (f two) -> p f two", two=2)
                P.op("act", "activation", out=w1g[buf][:, k, :], in_=sv[:, :, 0], func=AF.Copy)
                P.op("act", "activation", out=w1l[buf][:, k, :], in_=sv[:, :, 1], func=AF.Copy)
                s = stg[sti[0] % 2]; sti[0] += 1
                P.dma("sp", s[:, 0:D], w2_d[e, k * 128:(k + 1) * 128, :])
                P.op("act", "activation", out=w2b[buf][:, k, :], in_=s[:, 0:D], func=AF.Copy)
            steps.append(step)
        return steps

    for grp in groups:
        ng = len(grp)
        t0 = grp[0]; tok0 = t0 * 128; ntok = ng * 128
        P.dma("sp", hT[:, :, 0:ntok], hT_d[:, tok0:tok0 + ntok].rearrange("(k p) t -> p k t", p=128))
        P.dma("sp", Gs[:, 0:ng, :], G_d[tok0:tok0 + ntok, :].rearrange("(g p) e -> p g e", p=128))
        P.dma("sp", GTs[:, 0:ntok], GT_d[:, tok0:tok0 + ntok])
        for i in range(ng):
            for n in range(2):
                P.op("pe", "matmul", out=py[n].v, lhsT=GTs[:, i * 128:(i + 1) * 128], rhs=b2[:, n * 512:(n + 1) * 512], start=True, stop=True)
                P.op("act", "activation", out=yacc[:, i, n * 512:(n + 1) * 512], in_=py[n].v, func=AF.Copy)
        for s in load_expert_steps(0, 0):
            s()
        blocks = [(b0, min(512, ntok - b0)) for b0 in range(0, ntok, 512)]
        ci = 0
        for e in range(NE):
            buf = e % 2
            nxt = load_expert_steps(e + 1, 1 - buf) if e + 1 < NE else []
            for bi, (b0, bn) in enumerate(blocks):
                for j in range(KC):
                    if bi == 0 and nxt:
                        nxt[j]()
                    c = ci % 2; ci += 1
                    for k in range(KC):
                        P.op("pe", "matmul", track=(k == KC - 1), out=pg[c][:, 0:bn], lhsT=w1g[buf][:, k, j * 128:(j + 1) * 128], rhs=hT[:, k, b0:b0 + bn], start=(k == 0), stop=(k == KC - 1))
                    for k in range(KC):
                        P.op("pe", "matmul", track=(k == KC - 1), out=pl[c][:, 0:bn], lhsT=w1l[buf][:, k, j * 128:(j + 1) * 128], rhs=hT[:, k, b0:b0 + bn], start=(k == 0), stop=(k == KC - 1))
                    P.op("dve", "tensor_scalar", out=tg[c][:, 0:bn], in0=pg[c][:, 0:bn], scalar1=b1g[:, e, j:j + 1], scalar2=7.0, op0=ALU.add, op1=ALU.min)
                    P.op("act", "activation", out=tsg[c][:, 0:bn], in_=tg[c][:, 0:bn], func=AF.Sigmoid, scale=1.702)
                    P.op("dve", "tensor_scalar", out=tl[c][:, 0:bn], in0=pl[c][:, 0:bn], scalar1=b1l[:, e, j:j + 1], scalar2=-7.0, op0=ALU.add, op1=ALU.max)
                    P.op("dve", "tensor_scalar", out=tl[c][:, 0:bn], in0=tl[c][:, 0:bn], scalar1=7.0, scalar2=1.0, op0=ALU.min, op1=ALU.add)
                    P.op("pool", "tensor_tensor", out=tg[c][:, 0:bn], in0=tg[c][:, 0:bn], in1=tsg[c][:, 0:bn], op=ALU.mult)
                    P.op("pool", "tensor_tensor", out=actT[:, j, 0:bn], in0=tg[c][:, 0:bn], in1=tl[c][:, 0:bn], op=ALU.mult)
                for i in range(b0 // 128, (b0 + bn) // 128):
                    for n in range(2):
                        ns = slice(n * 512, (n + 1) * 512)
                        for j in range(KC):
                            P.op("pe", "matmul", track=(j == KC - 1), out=py[n].v, lhsT=actT[:, j, i * 128 - b0:(i + 1) * 128 - b0], rhs=w2b[buf][:, j, ns], start=(j == 0), stop=(j == KC - 1))
                        P.op("dve", "scalar_tensor_tensor", out=yacc[:, i, ns], in0=py[n].v, scalar=Gs[:, i, e:e + 1], in1=yacc[:, i, ns], op0=ALU.mult, op1=ALU.add)
        for i in range(ng):
            t = grp[i]; v = variants[t]; sl = slice(t * 128, (t + 1) * 128)
            P.dma("sp", xt.v, x1_d[sl, :])
            P.op("act", "activation", out=junk.v, in_=yacc[:, i, :], func=AF.Square, accum_out=st[:, 0:1])
            rstd_of(P, st[:, 0:1], st[:, 2:3], st[:, 1:2], D)
            P.op("dve", "scalar_tensor_tensor", out=junk.v, in0=yacc[:, i, :], scalar=st[:, 2:3], in1=GG[v].v, op0=ALU.mult, op1=ALU.mult)
            P.op("pool", "tensor_tensor", out=xt.v, in0=junk.v, in1=xt.v, op=ALU.add)
            P.dma("sp", x2_d[sl, :], xt.v)
    P.finish()
    return P


def build_ada(name="ada"):
    P = Prog(name)
    NC_ = 1536
    w_d = P.dram("w", [1024, NC_]); b_d = P.dram("b", [2, NC_]); c_d = P.dram("cT", [128, 8, 2]); o_d = P.dram("mod", [2, NC_], out=True)
    w = P.sb("w", [128, 8, NC_]); P.dma("sp", w.v, w_d.rearrange("(k p) n -> p k n", p=128))
    b = P.sb("b", [2, NC_]); P.dma("sp", b.v, b_d)
    c = P.sb("c", [128, 8, 2]); P.dma("sp", c.v, c_d)
    o = P.sb("o", [2, NC_])
    P.op("act", "activation", out=c.v, in_=c.v, func=AF.Silu)
    pp = [P.ps("pp%d" % i, [2, 512]) for i in range(3)]
    for n in range(3):
        for k in range(8):
            P.op("pe", "matmul", track=(k == 7), out=pp[n].v, lhsT=c[:, k, :], rhs=w[:, k, n * 512:(n + 1) * 512], start=(k == 0), stop=(k == 7))
        P.op("dve", "tensor_tensor", out=o[:, n * 512:(n + 1) * 512], in0=pp[n].v, in1=b[:, n * 512:(n + 1) * 512], op=ALU.add)
    P.dma("sp", o_d, o.v)
    P.finish()
    return P


def build_gla(nchunks, name="gla"):
    P = Prog(name)
    Tt = nchunks * 64; CB = 8
    qT_d = P.dram("qT", [64, Tt]); kT_d = P.dram("kT", [64, Tt]); k_d = P.dram("k", [Tt, 64]); v_d = P.dram("v", [Tt, 128])
    aT_d = P.dram("aT", [17, Tt]); wa_d = P.dram("wa", [17, 64]); tri_d = P.dram("tri", [64, 64]); mu_d = P.dram("mu", [64, CB, 64])
    o_d = P.dram("o", [Tt, 128], out=True)
    wa = P.sb("wa", [17, 64]); P.dma("sp", wa.v, wa_d)
    tri = P.sb("tri", [64, 64]); P.dma("sp", tri.v, tri_d)
    mu = P.sb("mu", [64, CB, 64]); P.dma("sp", mu.v, mu_d)
    one = P.sb("one", [64, 1]); P.op("dve", "memset", ap=one.v, constant=1.0)
    S = [P.sb("S%d" % i, [64, 128]) for i in range(2)]
    P.op("dve", "memset", ap=S[0].v, constant=0.0)
    tmp = [P.sb("tmp%d" % i, [64, 128]) for i in range(2)]
    L = lambda nm, shp: [P.sb("%s%d" % (nm, i), shp) for i in range(2)]
    qTb = L("qTb", [64, CB * 64]); kTb = L("kTb", [64, CB * 64]); aTb = L("aTb", [17, CB * 64]); kb = L("kb", [64, CB, 64]); vb = L("vb", [64, CB, 128])
    le = L("le", [64, CB, 64]); E1 = L("E1", [64, CB * 64]); E2 = L("E2", [64, CB * 64]); E3 = L("E3", [64, CB, 64])
    qs = L("qs", [64, CB * 64]); ks = L("ks", [64, CB * 64]); kd = L("kd", [64, CB, 64]); ATm = L("ATm", [64, CB, 64]); ob = L("ob", [64, CB, 128])
    pXA = P.ps("pXA", [64, CB, 64]); pb = P.ps("pb", [64, CB, 64]); pbT = P.ps("pbT", [64, CB, 64]); pA = P.ps("pA", [64, CB, 64])
    pKV = P.ps("pKV", [64, CB, 128]); po = P.ps("po", [64, CB, 128])
    cur = 0; ti = 0
    blocks = [(c0, min(CB, nchunks - c0)) for c0 in range(0, nchunks, CB)]
    for bi, (c0, n) in enumerate(blocks):
        u = bi % 2; t0 = c0 * 64; W = n * 64
        P.dma("sp", qTb[u][:, 0:W], qT_d[:, t0:t0 + W]); P.dma("sp", kTb[u][:, 0:W], kT_d[:, t0:t0 + W]); P.dma("sp", aTb[u][:, 0:W], aT_d[:, t0:t0 + W])
        P.dma("sp", kb[u][:, 0:n, :], k_d[t0:t0 + W, :].rearrange("(c p) d -> p c d", p=64))
        P.dma("sp", vb[u][:, 0:n, :], v_d[t0:t0 + W, :].rearrange("(c p) d -> p c d", p=64))
        for c in range(n):
            P.op("pe", "matmul", out=pXA[:, c, :], lhsT=aTb[u][:, c * 64:(c + 1) * 64], rhs=wa.v, start=True, stop=True, track=(c == n - 1))
        P.op("act", "activation", out=le[u][:, 0:n, :], in_=pXA[:, 0:n, :], func=AF.Exp, scale=-1.0)
        P.op("act", "activation", out=le[u][:, 0:n, :], in_=le[u][:, 0:n, :], func=AF.Ln, bias=one[:, 0:1])
        for c in range(n):
            P.op("pe", "matmul", out=pb[:, c, :], lhsT=tri.v, rhs=le[u][:, c, :], start=True, stop=True, track=False)
            P.op("pe", "matmul", out=pbT[:, c, :], lhsT=le[u][:, c, :], rhs=tri.v, start=True, stop=True, track=(c == n - 1))
        pbTf = pbT.v.rearrange("p c t -> p (c t)")
        P.op("act", "activation", out=E1[u][:, 0:W], in_=pbTf[:, 0:W], func=AF.Exp)
        P.op("act", "activation", out=E2[u][:, 0:W], in_=pbTf[:, 0:W], func=AF.Exp, scale=-1.0)
        P.op("act", "activation", out=E3[u][:, 0:n, :], in_=pb[:, 0:n, :], func=AF.Exp, scale=-1.0)
        P.op("dve", "scalar_tensor_tensor", out=qs[u][:, 0:W], in0=qTb[u][:, 0:W], scalar=0.125, in1=E1[u][:, 0:W], op0=ALU.mult, op1=ALU.mult)
        P.op("dve", "tensor_tensor", out=ks[u][:, 0:W], in0=kTb[u][:, 0:W], in1=E2[u][:, 0:W], op=ALU.mult)
        P.op("pool", "tensor_tensor", out=kd[u][:, 0:n, :], in0=kb[u][:, 0:n, :], in1=E3[u][:, 0:n, :], op=ALU.mult)
        for c in range(n):
            cs_ = slice(c * 64, (c + 1) * 64)
            P.op("pe", "matmul", out=pA[:, c, :], lhsT=ks[u][:, cs_], rhs=qs[u][:, cs_], start=True, stop=True, track=(c == n - 1))
        P.op("dve", "tensor_tensor", out=ATm[u][:, 0:n, :], in0=pA[:, 0:n, :], in1=mu[:, 0:n, :], op=ALU.mult)
        for c in range(n):
            P.op("pe", "matmul", out=pKV[:, c, :], lhsT=kd[u][:, c, :], rhs=vb[u][:, c, :], start=True, stop=True, track=(c == n - 1))
        for c in range(n):
            cs_ = slice(c * 64, (c + 1) * 64)
            P.op("pe", "matmul", out=po[:, c, :], lhsT=qs[u][:, cs_], rhs=S[cur].v, start=True, stop=False, track=False)
            P.op("pe", "matmul", out=po[:, c, :], lhsT=ATm[u][:, c, :], rhs=vb[u][:, c, :], start=False, stop=True)
            tb = tmp[ti % 2]; ti += 1
            P.op("dve", "tensor_tensor", out=tb.v, in0=S[cur].v, in1=pKV[:, c, :], op=ALU.add)
            P.op("dve", "tensor_scalar", out=S[1 - cur].v, in0=tb.v, scalar1=E1[u][:, c * 64 + 63:c * 64 + 64], scalar2=None, op0=ALU.mult)
            cur = 1 - cur
        P.op("act", "activation", out=ob[u][:, 0:n, :], in_=po[:, 0:n, :], func=AF.Copy)
        P.dma("sp", o_d[t0:t0 + W, :].rearrange("(c p) d -> p c d", p=64), ob[u][:, 0:n, :])
    P.finish()
    return P


def build_mla(groups, NK, name="mla"):
    P = Prog(name)
    NQ = max(q0 + nq for q0, nq, _ in groups)
    NB = NK // 128
    qT_d = P.dram("qT", [193, NQ]); kT_d = P.dram("kT", [193, NK]); v_d = P.dram("V", [NK, 129]); o_d = P.dram("O", [NQ, 128], out=True)
    idf, idb = ident(P)
    kTa = P.sb("kTa", [128, NK], BF16); kTb = P.sb("kTb", [65, NK], BF16); Vb = P.sb("Vb", [128, NB, 129], BF16)
    stg = [P.sb("stg%d" % i, [128, 2048]) for i in range(2)]
    si = 0
    for c0 in range(0, NK, 2048):
        cn = min(2048, NK - c0)
        for (r0, rn, dst) in ((0, 128, kTa), (128, 65, kTb)):
            s = stg[si % 2]; si += 1
            P.dma("sp", s[0:rn, 0:cn], kT_d[r0:r0 + rn, c0:c0 + cn])
            P.op("pool" if si % 2 else "act", "tensor_copy" if si % 2 else "copy", out=dst[0:rn, c0:c0 + cn], in_=s[0:rn, 0:cn])
    VB = 15
    for b0 in range(0, NB, VB):
        bn = min(VB, NB - b0)
        s = stg[si % 2]; si += 1
        sv = s[:, 0:bn * 129].rearrange("p (b f) -> p b f", f=129)
        P.dma("sp", sv, v_d[b0 * 128:(b0 + bn) * 128, :].rearrange("(b p) f -> p b f", p=128))
        P.op("pool" if si % 2 else "act", "tensor_copy" if si % 2 else "copy", out=Vb[:, b0:b0 + bn, :], in_=sv)
    qst = [P.sb("qst%d" % i, [128, 512]) for i in range(2)]
    qa = [P.sb("qa%d" % i, [128, 512], BF16) for i in range(2)]; qb = [P.sb("qb%d" % i, [65, 512], BF16) for i in range(2)]
    mx = P.sb("mx", [128, 4, 40]); negm = P.sb("negm", [128, 4]); Z = P.sb("Z", [128, 65])
    P.op("dve", "memset", ap=Z.v, constant=0.0)
    pt = [P.sb("pt%d" % i, [128, 512], BF16) for i in range(2)]
    rl = P.sb("rl", [128, 4]); ot = [P.sb("ot%d" % i, [128, 4, 128]) for i in range(2)]
    ps1 = [P.ps("ps1_%d" % i, [128, 512]) for i in range(2)]; pst = [P.ps("pst%d" % i, [128, 512]) for i in range(2)]
    pO = [P.ps("pO%d" % i, [128, 129]) for i in range(4)]
    scale = 192.0 ** -0.5
    c1 = 0; c2 = 0
    for gi, (q0, nq, nkeys) in enumerate(groups):
        u = gi % 2; nqt = nq // 128
        P.dma("sp", qst[0][:, 0:nq], qT_d[0:128, q0:q0 + nq]); P.dma("sp", qst[1][0:64, 0:nq], qT_d[128:192, q0:q0 + nq])
        P.op("pool", "tensor_scalar", out=qa[u][:, 0:nq], in0=qst[0][:, 0:nq], scalar1=scale, scalar2=None, op0=ALU.mult)
        P.op("pool", "tensor_scalar", out=qb[u][0:64, 0:nq], in0=qst[1][0:64, 0:nq], scalar1=scale, scalar2=None, op0=ALU.mult)
        kblocks = [(k0, min(512, nkeys - k0)) for k0 in range(0, nkeys, 512)]
        for qt in range(nqt):
            qs_ = slice(qt * 128, (qt + 1) * 128)
            for bi, (k0, kn) in enumerate(kblocks):
                pb = ps1[c1 % 2]; c1 += 1
                P.op("pe", "matmul", track=False, out=pb[:, 0:kn], lhsT=qa[u][:, qs_], rhs=kTa[:, k0:k0 + kn], start=True, stop=False)
                P.op("pe", "matmul", out=pb[:, 0:kn], lhsT=qb[u][0:64, qs_], rhs=kTb[0:64, k0:k0 + kn], start=False, stop=True)
                P.op("dve", "tensor_reduce", out=mx[:, qt, bi:bi + 1], in_=pb[:, 0:kn], axis=AX.X, op=ALU.max)
            P.op("dve", "tensor_reduce", out=negm[:, qt:qt + 1], in_=mx[:, qt, 0:len(kblocks)], axis=AX.X, op=ALU.max)
            P.op("dve", "tensor_scalar", out=Z[:, 64:65], in0=negm[:, qt:qt + 1], scalar1=-1.0, scalar2=None, op0=ALU.mult)
            pz = ps1[c1 % 2]; c1 += 1
            P.op("pe", "matmul", out=pz[0:65, 0:128], lhsT=Z.v, rhs=idf.v, start=True, stop=True)
            P.op("dve", "tensor_copy", out=qb[u][64:65, qs_], in_=pz[64:65, 0:128])
        nkb = nkeys // 128
        for kb_ in range(nkb):
            ks_ = slice(kb_ * 128, (kb_ + 1) * 128)
            pb = pst[c2 % 2]; ptb = pt[c2 % 2]; c2 += 1
            P.op("pe", "matmul", track=False, out=pb[:, 0:nq], lhsT=kTa[:, ks_], rhs=qa[u][:, 0:nq], start=True, stop=False)
            P.op("pe", "matmul", out=pb[:, 0:nq], lhsT=kTb[0:65, ks_], rhs=qb[u][0:65, 0:nq], start=False, stop=True)
            P.op("act", "activation", out=ptb[:, 0:nq], in_=pb[:, 0:nq], func=AF.Exp)
            for qt in range(nqt):
                P.op("pe", "matmul", track=(kb_ == nkb - 1), out=pO[qt].v, lhsT=ptb[:, qt * 128:(qt + 1) * 128], rhs=Vb[:, kb_, :], start=(kb_ == 0), stop=(kb_ == nkb - 1))
        for qt in range(nqt):
            P.op("dve", "reciprocal", out=rl[:, qt:qt + 1], in_=pO[qt][:, 128:129])
            P.op("dve", "tensor_scalar", out=ot[u][:, qt, :], in0=pO[qt][:, 0:128], scalar1=rl[:, qt:qt + 1], scalar2=None, op0=ALU.mult)
        P.dma("sp", o_d[q0:q0 + nq, :].rearrange("(t p) d -> p t d", p=128), ot[u][:, 0:nqt, :])
    P.finish()
    return P


def build_na(nrows, edge_rows, name="na"):
    P = Prog(name)
    T = nrows * 64; ne = max(1, len(edge_rows))
    qT_d = P.dram("qT", [1024, T]); kw_d = P.dram("kwin", [nrows, 1024, 512]); vw_d = P.dram("vwin", [nrows, 512, 1024])
    kc_d = P.dram("kcT", [1024, 256]); vc_d = P.dram("vc", [256, 1024])
    bi_d = P.dram("Bint", [128, 8, 512]); be_d = P.dram("Bedge", [ne, 128, 8, 512])
    oT_d = P.dram("oT", [1024, T], out=True)
    idf, idb = ident(P)
    ones = P.sb("ones", [128, 128], BF16); P.op("pool", "memset", ap=ones.v, constant=1.0)
    kst = P.sb("kst", [128, 8, 512]); vst = P.sb("vst", [128, 4, 1024])
    kb = [P.sb("kb%d" % i, [128, 8, 512], BF16) for i in range(2)]; vb = [P.sb("vb%d" % i, [128, 4, 1024], BF16) for i in range(2)]
    kcb = P.sb("kcb", [128, 8, 256], BF16); vcb = P.sb("vcb", [128, 2, 1024], BF16)
    P.dma("sp", kst[:, :, 0:256], kc_d.rearrange("(k p) n -> p k n", p=128)); P.op("pool", "tensor_copy", out=kcb.v, in_=kst[:, :, 0:256])
    P.dma("sp", vst[:, 0:2, :], vc_d.rearrange("(b p) f -> p b f", p=128)); P.op("pool", "tensor_copy", out=vcb.v, in_=vst[:, 0:2, :])
    Bi = P.sb("Bi", [128, 8, 512]); P.dma("sp", Bi.v, bi_d)
    Be = [P.sb("Be%d" % i, [128, 8, 512]) for i in range(2)]
    qrow = [P.sb("qrow%d" % i, [128, 8, 64]) for i in range(2)]
    QBD = [P.sb("QBD%d" % i, [128, 128], BF16) for i in range(2)]
    for z in QBD:
        P.op("pool", "memset", ap=z.v, constant=0.0)
    Sb = [P.sb("Sb%d" % i, [128, 768]) for i in range(2)]; Pm = [P.sb("Pm%d" % i, [128, 768], BF16) for i in range(2)]
    PT = [P.sb("PT%d" % i, [128, 6, 128], BF16) for i in range(2)]
    sm = P.sb("sm", [128, 4]); rl = P.sb("rl", [128, 128]); orow = [P.sb("orow%d" % i, [128, 8, 64]) for i in range(2)]
    pS = [P.ps("pS%d" % i, [128, 1024]) for i in range(2)]
    pPT = P.ps("pPT", [128, 6, 128], BF16); pO = P.ps("pO", [128, 128]); pL = P.ps("pL", [128, 128])
    it = 0; nbe = 0
    for i in range(nrows):
        u = i % 2
        P.dma("sp", kst.v, kw_d[i].rearrange("(k p) n -> p k n", p=128)); P.op("pool", "tensor_copy", out=kb[u].v, in_=kst.v)
        P.dma("sp", vst.v, vw_d[i].rearrange("(b p) f -> p b f", p=128)); P.op("pool", "tensor_copy", out=vb[u].v, in_=vst.v)
        P.dma("sp", qrow[u].v, qT_d[:, i * 64:(i + 1) * 64].rearrange("(k p) t -> p k t", p=128))
        if i in edge_rows:
            B = Be[nbe % 2]; nbe += 1
            P.dma("sp", B.v, be_d[edge_rows[i]])
        else:
            B = Bi
        for hp in range(8):
            w = it % 2; it += 1
            P.op("pool", "tensor_scalar", out=QBD[w][0:64, 0:64], in0=qrow[u][0:64, hp, :], scalar1=0.125, scalar2=None, op0=ALU.mult)
            P.op("pool", "tensor_scalar", out=QBD[w][64:128, 64:128], in0=qrow[u][64:128, hp, :], scalar1=0.125, scalar2=None, op0=ALU.mult)
            P.op("pe", "matmul", out=pS[w][:, 0:512], lhsT=QBD[w].v, rhs=kb[u][:, hp, :], start=True, stop=True, track=False)
            P.op("pe", "matmul", out=pS[w][:, 512:768], lhsT=QBD[w].v, rhs=kcb[:, hp, :], start=True, stop=True)
            P.op("dve", "tensor_tensor", out=Sb[w][:, 0:512], in0=pS[w][:, 0:512], in1=B[:, hp, :], op=ALU.add)
            P.op("act", "activation", out=Sb[w][:, 512:768], in_=pS[w][:, 512:768], func=AF.Copy)
            P.op("dve", "tensor_reduce", out=sm[:, 0:1], in_=Sb[w].v, axis=AX.X, op=ALU.max)
            P.op("dve", "tensor_scalar", out=sm[:, 1:2], in0=sm[:, 0:1], scalar1=-1.0, scalar2=None, op0=ALU.mult)
            P.op("act", "activation", out=Pm[w].v, in_=Sb[w].v, func=AF.Exp, bias=sm[:, 1:2])
            for b in range(6):
                P.op("pe", "transpose", out=pPT[:, b, :], in_=Pm[w][:, b * 128:(b + 1) * 128], identity=idb.v, track=(b == 5))
            P.op("dve", "tensor_copy", out=PT[w].v, in_=pPT.v)
            hs = slice(hp * 128, (hp + 1) * 128)
            for b in range(6):
                vblk = vb[u][:, b, hs] if b < 4 else vcb[:, b - 4, hs]
                P.op("pe", "matmul", out=pO.v, lhsT=vblk, rhs=PT[w][:, b, :], start=(b == 0), stop=(b == 5), track=(b == 5))
            for b in range(6):
                P.op("pe", "matmul", out=pL.v, lhsT=ones.v, rhs=PT[w][:, b, :], start=(b == 0), stop=(b == 5), track=(b == 5))
            P.op("dve", "reciprocal", out=rl.v, in_=pL.v)
            P.op("dve", "tensor_tensor", out=orow[u][0:64, hp, :], in0=pO[0:64, 0:64], in1=rl[0:64, 0:64], op=ALU.mult)
            P.op("dve", "tensor_tensor", out=orow[u][64:128, hp, :], in0=pO[64:128, 64:128], in1=rl[64:128, 64:128], op=ALU.mult)
        P.dma("sp", oT_d[:, i * 64:(i + 1) * 64].rearrange("(k p) t -> p k t", p=128), orow[u].v)
    P.finish()
    return P


def na_bias_table(rpb, d):
    col = np.arange(64)
    c0 = np.clip(col - 8, 0, 48)
    kcol = np.arange(64)
    inwin = (kcol[None, :] >= c0[:, None]) & (kcol[None, :] < c0[:, None] + 16)
    cidx = np.clip(kcol[None, :] - col[:, None] + 15, 0, 30)
    ridx = np.arange(8) - d + 7
    t = rpb[:, ridx][:, :, cidx]
    t = np.where(inwin[None, None], t, np.float32(-30000.0)).astype(np.float32)
    t = t.transpose(0, 2, 1, 3).reshape(8, 2, 64, 512)
    return np.ascontiguousarray(t.transpose(1, 2, 0, 3).reshape(128, 8, 512))


TALL = 16640
NOWN = 22
VOWN = [0] * 20 + [1] * 2
TOWN = NOWN * 128


def modcols(P, modcol, l, i, g_d_view, name):
    t = P.sb(name, [128, 2, 8])
    for s in range(2):
        P.op("dve", "scalar_tensor_tensor", out=t[:, s, :], in0=modcol[:, l, i * 8:(i + 1) * 8, s], scalar=1.0, in1=g_d_view, op0=ALU.add, op1=ALU.mult)
    return t


def stage_ada(P, adaw_d, bcol_d, brow_d, cT_d, modcol, gates_s):
    P.begin()
    c = P.sb("c", [128, 8, 2]); P.dma("sp", c.v, cT_d)
    P.op("act", "activation", out=c.v, in_=c.v, func=AF.Silu)
    ones = P.sb("ones", [128, 128]); P.op("dve", "memset", ap=ones.v, constant=1.0)
    crep = P.sb("crep", [128, 2, 8, 128])
    for s in range(2):
        for k in range(8):
            P.op("dve", "tensor_scalar", out=crep[:, s, k, :], in0=ones.v, scalar1=c[:, k, s:s + 1], scalar2=None, op0=ALU.mult)
    bcol = P.sb("bcol", [128, 2, 48]); P.dma("sp", bcol.v, bcol_d)
    wp = [P.sb("wp%d" % i, [128, 8, 512]) for i in range(2)]
    brow = P.sb("brow", [128, 512]); grow = [P.sb("grow%d" % i, [128, 512]) for i in range(2)]
    pc = [P.ps("pc%d" % i, [128, 2]) for i in range(2)]; pr = [P.ps("pr%d" % i, [128, 512]) for i in range(2)]
    ci = 0; ri = 0
    for l in range(2):
        for piece in range(12):
            w = wp[piece % 2]
            P.dma("sp", w.v, adaw_d[l][:, piece * 512:(piece + 1) * 512].rearrange("(k p) n -> p k n", p=128))
            for jj in range(4):
                j = piece * 4 + jj
                p_ = pc[ci % 2]; ci += 1
                for k in range(8):
                    P.op("pe", "matmul", track=(k == 7), out=p_.v, lhsT=w[:, k, jj * 128:(jj + 1) * 128], rhs=c[:, k, :], start=(k == 0), stop=(k == 7))
                P.op("dve", "tensor_scalar", out=modcol[:, l, j, :], in0=p_.v, scalar1=bcol[:, l, j:j + 1], scalar2=None, op0=ALU.add)
            if piece in (4, 5, 10, 11):
                g = 0 if piece < 6 else 1; half = piece % 2
                P.dma("sp", brow.v, brow_d[l, :, piece * 512:(piece + 1) * 512])
                for s in range(2):
                    p_ = pr[ri % 2]; gr = grow[ri % 2]; ri += 1
                    for k in range(8):
                        P.op("pe", "matmul", track=(k == 7), out=p_.v, lhsT=crep[:, s, k, :], rhs=w[:, k, :], start=(k == 0), stop=(k == 7))
                    P.op("dve", "tensor_tensor", out=gr.v, in0=p_.v, in1=brow.v, op=ALU.add)
                    P.dma("sp", gates_s[l, g, s, :, half * 512:(half + 1) * 512], gr.v)
    P.end()


def stage_pro(P, idb, x_d, groups, gvar, gsc, gsh, D, wtm_d, ntm, tm_outs, wfm_d, nfm, fm_specs, rope=None, mla=None, tag="pro"):
    P.begin()
    KC = D // 128
    stg = [P.sb("wst%d" % i, [128, max(ntm, nfm, 1024)]) for i in range(2)]
    wtm = load_w_bf16(P, wtm_d, D, ntm, "wtm", stage=stg) if ntm else None
    wfm = load_w_bf16(P, wfm_d, D, nfm, "wfm", stage=stg) if nfm else None
    if mla:
        wuk = load_w_bf16(P, mla["wukv_d"], 128, 1024, "wuk", stage=stg)
        kvn = P.sb("kvn", [128, 1]); P.dma("sp", kvn.v, mla["kvn_d"])
        ckn = [P.sb("ckn%d" % i, [128, 128], BF16) for i in range(2)]
        ckT = [P.sb("ckT%d" % i, [128, 512], BF16) for i in range(2)]
        kst = [P.sb("kst%d" % i, [128, 512], BF16) for i in range(2)]; vst = [P.sb("vst%d" % i, [128, 512], BF16) for i in range(2)]
        pck = P.ps("pck", [128, 128], BF16)
    xs = [P.sb("x%d" % i, [128, D]) for i in range(2)]; junk = P.sb("junk", [128, D]); st = [P.sb("st%d" % i, [128, 8]) for i in range(2)]
    xn = [P.sb("xn%d" % i, [128, D], BF16) for i in range(2)]
    hT = [P.sb("hT%d" % i, [128, KC, 512], BF16) for i in range(2)]
    yt = [P.sb("yt%d" % i, [128, max(ntm, 1)]) for i in range(2)]
    ytb = [P.sb("ytb%d" % i, [128, max(ntm, 1)], BF16) for i in range(2)]
    fo = [P.sb("fo%d" % i, [128, 512]) for i in range(2)]; fob = [P.sb("fob%d" % i, [128, 512], BF16) for i in range(2)]
    pT = [P.ps("pT%d" % i, [128, KC, 128], BF16) for i in range(2)]
    py = [P.ps("py%d" % i, [128, 512]) for i in range(2)]; pf = [P.ps("pf%d" % i, [128, 512]) for i in range(2)]
    if rope:
        Ct = [P.sb("Ct%d" % i, [64, 512]) for i in range(2)]; St = [P.sb("St%d" % i, [64, 512]) for i in range(2)]
        r1 = P.sb("r1", [64, 512]); r2 = P.sb("r2", [64, 512]); rb_ = [P.sb("rb%d" % i, [64, 512], BF16) for i in range(2)]
    ti = 0; yi = 0; fi = 0
    for gi, (tok0, gt) in enumerate(groups):
        v = gvar[gi]; W = gt * 128; hg = hT[gi % 2]
        for t in range(gt):
            b = ti % 2; ti += 1
            r0 = tok0 + t * 128
            P.dma("sp", xs[b].v, x_d[r0:r0 + 128, :])
            P.op("act", "activation", out=junk.v, in_=xs[b].v, func=AF.Square, accum_out=st[b][:, 0:1])
            rstd_of(P, st[b][:, 0:1], st[b][:, 2:3], st[b][:, 1:2], D)
            P.op("dve", "tensor_scalar", out=xn[b].v, in0=xs[b].v, scalar1=st[b][:, 2:3], scalar2=None, op0=ALU.mult)
            for k in range(KC):
                P.op("pe", "transpose", out=pT[b][:, k, :], in_=xn[b][:, k * 128:(k + 1) * 128], identity=idb.v)
            for k in range(KC):
                P.op("dve", "tensor_scalar", out=hg[:, k, t * 128:(t + 1) * 128], in0=pT[b][:, k, :], scalar1=gsc[:, v, k:k + 1], scalar2=gsh[:, v, k:k + 1], op0=ALU.mult, op1=ALU.add)
            if ntm:
                for c0 in range(0, ntm, 512):
                    cn = min(512, ntm - c0); pb = py[yi % 2]; yi += 1
                    for k in range(KC):
                        P.op("pe", "matmul", track=(k == KC - 1), out=pb[:, 0:cn], lhsT=hg[:, k, t * 128:(t + 1) * 128], rhs=wtm[:, k, c0:c0 + cn], start=(k == 0), stop=(k == KC - 1))
                    P.op("act", "activation", out=yt[b][:, c0:c0 + cn], in_=pb[:, 0:cn], func=AF.Copy)
                for (c0, n, ap, dt) in tm_outs:
                    if dt == BF16:
                        P.op("pool", "tensor_copy", out=ytb[b][:, c0:c0 + n], in_=yt[b][:, c0:c0 + n])
                        P.dma("sp", ap[r0:r0 + 128, :], ytb[b][:, c0:c0 + n])
                    else:
                        P.dma("sp", ap[r0:r0 + 128, :], yt[b][:, c0:c0 + n])
            if mla:
                cc = mla["col"]
                P.op("act", "activation", out=junk[:, 0:128], in_=yt[b][:, cc:cc + 128], func=AF.Square, accum_out=st[b][:, 4:5])
                rstd_of(P, st[b][:, 4:5], st[b][:, 6:7], st[b][:, 5:6], 128)
                P.op("dve", "tensor_scalar", out=ckn[b].v, in0=yt[b][:, cc:cc + 128], scalar1=st[b][:, 6:7], scalar2=None, op0=ALU.mult)
                P.op("pe", "transpose", out=pck.v, in_=ckn[b].v, identity=idb.v)
                P.op("dve", "tensor_scalar", out=ckT[gi % 2][:, t * 128:(t + 1) * 128], in0=pck.v, scalar1=kvn[:, 0:1], scalar2=None, op0=ALU.mult)
                pb = py[yi % 2]; yi += 1
                P.op("pe", "matmul", out=pb.v, lhsT=ckT[gi % 2][:, t * 128:(t + 1) * 128], rhs=wuk[:, 0, 512:1024], start=True, stop=True)
                vb_ = vst[ti % 2]
                P.op("act", "activation", out=vb_.v, in_=pb.v, func=AF.Copy)
                P.dma("sp", mla["V_s"][r0:r0 + 128, :], vb_.v)
        for (c0, m, ap, dt, scale) in fm_specs:
            pb = pf[fi % 2]; f32t = fo[fi % 2]; bft = fob[fi % 2]; fi += 1
            for k in range(KC):
                P.op("pe", "matmul", track=(k == KC - 1), out=pb[0:m, 0:W], lhsT=wfm[:, k, c0:c0 + m], rhs=hg[:, k, 0:W], start=(k == 0), stop=(k == KC - 1))
            dst = bft if dt == BF16 else f32t
            P.op("act", "activation", out=dst[0:m, 0:W], in_=pb[0:m, 0:W], func=AF.Copy, scale=float(scale))
            P.dma("sp", ap[:, tok0:tok0 + W], dst[0:m, 0:W])
        if rope:
            ckr, ckrs, ap, C_d, S_d = rope
            u = gi % 2
            P.dma("sp", Ct[u][:, 0:W], C_d[:, tok0:tok0 + W]); P.dma("sp", St[u][:, 0:W], S_d[:, tok0:tok0 + W])
            pa = pf[fi % 2]; fi += 1; pb = pf[fi % 2]; fi += 1
            for k in range(KC):
                P.op("pe", "matmul", track=(k == KC - 1), out=pa[0:64, 0:W], lhsT=wfm[:, k, ckr:ckr + 64], rhs=hg[:, k, 0:W], start=(k == 0), stop=(k == KC - 1))
            for k in range(KC):
                P.op("pe", "matmul", track=(k == KC - 1), out=pb[0:64, 0:W], lhsT=wfm[:, k, ckrs:ckrs + 64], rhs=hg[:, k, 0:W], start=(k == 0), stop=(k == KC - 1))
            P.op("dve", "tensor_tensor", out=r1[:, 0:W], in0=pa[0:64, 0:W], in1=Ct[u][:, 0:W], op=ALU.mult)
            P.op("dve", "tensor_tensor", out=r2[:, 0:W], in0=pb[0:64, 0:W], in1=St[u][:, 0:W], op=ALU.mult)
            P.op("pool", "tensor_tensor", out=rb_[u][:, 0:W], in0=r1[:, 0:W], in1=r2[:, 0:W], op=ALU.add)
            P.dma("sp", ap[:, tok0:tok0 + W], rb_[u][:, 0:W])
        if mla:
            for h in range(4):
                pb = pf[fi % 2]; kb_ = kst[fi % 2]; fi += 1
                P.op("pe", "matmul", out=pb[:, 0:W], lhsT=wuk[:, 0, h * 128:(h + 1) * 128], rhs=ckT[gi % 2][:, 0:W], start=True, stop=True)
                P.op("act", "activation", out=kb_[:, 0:W], in_=pb[:, 0:W], func=AF.Copy)
                P.dma("sp", mla["kT_s"][h, :, tok0:tok0 + W], kb_[:, 0:W])
    P.end()


def conv_items(w1_d, w2_d, b1_d, w1bf, w2bf, b1bf, l):
    items = [(b1_d[l], b1bf[l], 32, 2048)]
    for e in range(32):
        for k in range(8):
            ks = slice(k * 128, (k + 1) * 128)
            items.append((w1_d[l, e, ks, :], w1bf[l, e, ks, :], 128, 2048))
            items.append((w2_d[l, e, ks, :], w2bf[l, e, ks, :], 128, 1024))
    return items


def conv_steps(P, items):
    R = 6
    pools = {2048: ([P.sb("cstA%d" % i, [128, 2048]) for i in range(R)], [P.sb("cbfA%d" % i, [128, 2048], BF16) for i in range(R)]),
             1024: ([P.sb("cstB%d" % i, [128, 1024]) for i in range(R)], [P.sb("cbfB%d" % i, [128, 1024], BF16) for i in range(R)])}
    pcnt = {2048: 0, 1024: 0}
    state = dict(nxt=0, loaded=[], cast=[])

    def tick(k):
        for (i, dst, rows, n) in state["cast"]:
            P.dma("pool", dst, pools[n][1][i][0:rows, :])
        state["cast"] = []
        for (i, dst, rows, n) in state["loaded"]:
            P.op("act", "activation", out=pools[n][1][i][0:rows, :], in_=pools[n][0][i][0:rows, :], func=AF.Copy)
            state["cast"].append((i, dst, rows, n))
        state["loaded"] = []
        for _ in range(k):
            if state["nxt"] < len(items):
                src, dst, rows, n = items[state["nxt"]]; state["nxt"] += 1
                i = pcnt[n] % R; pcnt[n] += 1
                P.dma("pool", pools[n][0][i][0:rows, :], src)
                state["loaded"].append((i, dst, rows, n))
        return state["nxt"] < len(items) or state["loaded"] or state["cast"]
    return tick


def stage_gla(P, scans, wa_d, tri_d, mu_d, extra=None, per_block=4):
    P.begin()
    CB = 8
    tri = P.sb("tri", [64, 64]); P.dma("sp", tri.v, tri_d)
    mu = P.sb("mu", [64, CB, 64]); P.dma("sp", mu.v, mu_d)
    one = P.sb("one", [64, 1]); P.op("dve", "memset", ap=one.v, constant=1.0)
    wa = P.sb("wa", [17, 8, 64]); P.dma("sp", wa.v, wa_d)
    S = [P.sb("S%d" % i, [64, 128]) for i in range(2)]
    tmp = [P.sb("tmp%d" % i, [64, 128]) for i in range(2)]
    L = lambda nm, shp: [P.sb("%s%d" % (nm, i), shp) for i in range(2)]
    qTb = L("qTb", [64, CB * 64]); kTb = L("kTb", [64, CB * 64]); aTb = L("aTb", [17, CB * 64]); kb = L("kb", [64, CB, 64]); vb = L("vb", [64, CB, 128])
    for a in aTb:
        P.op("dve", "memset", ap=a.v, constant=1.0)
    le = L("le", [64, CB, 64]); E1 = L("E1", [64, CB * 64]); E2 = L("E2", [64, CB * 64]); E3 = L("E3", [64, CB, 64])
    qs = L("qs", [64, CB * 64]); ks = L("ks", [64, CB * 64]); kd = L("kd", [64, CB, 64]); ATm = L("ATm", [64, CB, 64]); ob = L("ob", [64, CB, 128])
    pXA = P.ps("pXA", [64, CB, 64]); pb = P.ps("pb", [64, CB, 64]); pbT = P.ps("pbT", [64, CB, 64]); pA = P.ps("pA", [64, CB, 64])
    pKV = P.ps("pKV", [64, CB, 128]); po = P.ps("po", [64, CB, 128])
    blocks = [(0, 4)] + [(4 + 8 * i, 8) for i in range(32)]
    bi = 0; ti = 0
    tick = extra(P) if extra else None
    for (qT_d, kT_d, k_d, v_d, aT_d, wi, o_d) in scans:
        cur = 0
        P.op("dve", "memset", ap=S[0].v, constant=0.0)
        for (c0, n) in blocks:
            u = bi % 2; bi += 1; t0 = c0 * 64; W = n * 64
            if tick:
                tick(per_block)
            P.dma("sp", qTb[u][:, 0:W], qT_d[:, t0:t0 + W]); P.dma("sp", kTb[u][:, 0:W], kT_d[:, t0:t0 + W]); P.dma("sp", aTb[u][0:16, 0:W], aT_d[:, t0:t0 + W])
            P.dma("sp", kb[u][:, 0:n, :], k_d[t0:t0 + W, :].rearrange("(c p) d -> p c d", p=64))
            P.dma("sp", vb[u][:, 0:n, :], v_d[t0:t0 + W, :].rearrange("(c p) d -> p c d", p=64))
            for c in range(n):
                P.op("pe", "matmul", out=pXA[:, c, :], lhsT=aTb[u][:, c * 64:(c + 1) * 64], rhs=wa[:, wi, :], start=True, stop=True, track=(c == n - 1))
            P.op("act", "activation", out=le[u][:, 0:n, :], in_=pXA[:, 0:n, :], func=AF.Exp, scale=-1.0)
            P.op("act", "activation", out=le[u][:, 0:n, :], in_=le[u][:, 0:n, :], func=AF.Ln, bias=one[:, 0:1])
            for c in range(n):
                P.op("pe", "matmul", out=pb[:, c, :], lhsT=tri.v, rhs=le[u][:, c, :], start=True, stop=True, track=False)
                P.op("pe", "matmul", out=pbT[:, c, :], lhsT=le[u][:, c, :], rhs=tri.v, start=True, stop=True, track=(c == n - 1))
            pbTf = pbT.v.rearrange("p c t -> p (c t)")
            P.op("act", "activation", out=E1[u][:, 0:W], in_=pbTf[:, 0:W], func=AF.Exp)
            P.op("act", "activation", out=E2[u][:, 0:W], in_=pbTf[:, 0:W], func=AF.Exp, scale=-1.0)
            P.op("act", "activation", out=E3[u][:, 0:n, :], in_=pb[:, 0:n, :], func=AF.Exp, scale=-1.0)
            P.op("dve", "scalar_tensor_tensor", out=qs[u][:, 0:W], in0=qTb[u][:, 0:W], scalar=0.125, in1=E1[u][:, 0:W], op0=ALU.mult, op1=ALU.mult)
            P.op("dve", "tensor_tensor", out=ks[u][:, 0:W], in0=kTb[u][:, 0:W], in1=E2[u][:, 0:W], op=ALU.mult)
            P.op("dve", "tensor_tensor", out=kd[u][:, 0:n, :], in0=kb[u][:, 0:n, :], in1=E3[u][:, 0:n, :], op=ALU.mult)
            for c in range(n):
                cs_ = slice(c * 64, (c + 1) * 64)
                P.op("pe", "matmul", out=pA[:, c, :], lhsT=ks[u][:, cs_], rhs=qs[u][:, cs_], start=True, stop=True, track=(c == n - 1))
            P.op("dve", "tensor_tensor", out=ATm[u][:, 0:n, :], in0=pA[:, 0:n, :], in1=mu[:, 0:n, :], op=ALU.mult)
            for c in range(n):
                P.op("pe", "matmul", out=pKV[:, c, :], lhsT=kd[u][:, c, :], rhs=vb[u][:, c, :], start=True, stop=True, track=(c == n - 1))
            for c in range(n):
                cs_ = slice(c * 64, (c + 1) * 64)
                P.op("pe", "matmul", out=po[:, c, :], lhsT=qs[u][:, cs_], rhs=S[cur].v, start=True, stop=False, track=False)
                P.op("pe", "matmul", out=po[:, c, :], lhsT=ATm[u][:, c, :], rhs=vb[u][:, c, :], start=False, stop=True)
                tb = tmp[ti % 2]; ti += 1
                P.op("dve", "tensor_tensor", out=tb.v, in0=S[cur].v, in1=pKV[:, c, :], op=ALU.add)
                P.op("dve", "tensor_scalar", out=S[1 - cur].v, in0=tb.v, scalar1=E1[u][:, c * 64 + 63:c * 64 + 64], scalar2=None, op0=ALU.mult)
                cur = 1 - cur
            P.op("act", "activation", out=ob[u][:, 0:n, :], in_=po[:, 0:n, :], func=AF.Copy)
            P.dma("sp", o_d[t0:t0 + W, :].rearrange("(c p) d -> p c d", p=64), ob[u][:, 0:n, :])
    while tick and tick(per_block):
        pass
    P.end()


def stage_mla_full(P, idf, idb, cq_s, qn_d, wuq_d, wuqs_d, Cq_d, Sq_d, kT_s, krT_s, V_s, om_s):
    P.begin()
    NK = TALL; NB = NK // 128; NQ = TOWN
    scale = 192.0 ** -0.5
    qa = P.sb("qa", [128, 4, NQ], BF16); qb = P.sb("qb", [65, 4, NQ], BF16)
    stg = [P.sb("wst%d" % i, [128, 768]) for i in range(2)]
    wuq = load_w_bf16(P, wuq_d, 256, 768, "wuq", stage=stg)
    wuqs = load_w_bf16(P, wuqs_d, 256, 256, "wuqs", stage=stg)
    qn = P.sb("qn", [128, 2]); P.dma("sp", qn.v, qn_d)
    cqT = P.sb("cqT", [128, 2, NQ], BF16)
    xs = [P.sb("x%d" % i, [128, 256]) for i in range(2)]; junk = P.sb("junk", [128, 256]); st = [P.sb("st%d" % i, [128, 4]) for i in range(2)]
    xn = [P.sb("xn%d" % i, [128, 256], BF16) for i in range(2)]
    ps1 = [P.ps("ps1_%d" % i, [128, 512]) for i in range(2)]; pst = [P.ps("pst%d" % i, [128, 512]) for i in range(2)]
    pO = [P.ps("pO%d" % i, [128, 512]) for i in range(4)]
    for t in range(NOWN):
        b = t % 2
        pT = pO[b].v.bitcast(BF16)
        P.dma("sp", xs[b].v, cq_s[t * 128:(t + 1) * 128, :])
        P.op("act", "activation", out=junk.v, in_=xs[b].v, func=AF.Square, accum_out=st[b][:, 0:1])
        rstd_of(P, st[b][:, 0:1], st[b][:, 2:3], st[b][:, 1:2], 256)
        P.op("dve", "tensor_scalar", out=xn[b].v, in0=xs[b].v, scalar1=st[b][:, 2:3], scalar2=None, op0=ALU.mult)
        for k in range(2):
            P.op("pe", "transpose", out=pT[:, k * 128:(k + 1) * 128], in_=xn[b][:, k * 128:(k + 1) * 128], identity=idb.v)
        for k in range(2):
            P.op("dve", "tensor_scalar", out=cqT[:, k, t * 128:(t + 1) * 128], in0=pT[:, k * 128:(k + 1) * 128], scalar1=qn[:, k:k + 1], scalar2=None, op0=ALU.mult)
    Ct = P.sb("Ct", [64, 512]); St = P.sb("St", [64, 512]); r1 = P.sb("r1", [64, 512]); r2 = P.sb("r2", [64, 512])
    for c0 in range(0, NQ, 512):
        cn = min(512, NQ - c0)
        P.dma("sp", Ct[:, 0:cn], Cq_d[:, c0:c0 + cn]); P.dma("sp", St[:, 0:cn], Sq_d[:, c0:c0 + cn])
        for h in range(4):
            for k in range(2):
                P.op("pe", "matmul", track=(k == 1), out=ps1[0][:, 0:cn], lhsT=wuq[:, k, h * 192:h * 192 + 128], rhs=cqT[:, k, c0:c0 + cn], start=(k == 0), stop=(k == 1))
            P.op("act", "activation", out=qa[:, h, c0:c0 + cn], in_=ps1[0][:, 0:cn], func=AF.Copy, scale=scale)
            for k in range(2):
                P.op("pe", "matmul", track=(k == 1), out=ps1[1][0:64, 0:cn], lhsT=wuq[:, k, h * 192 + 128:h * 192 + 192], rhs=cqT[:, k, c0:c0 + cn], start=(k == 0), stop=(k == 1))
            for k in range(2):
                P.op("pe", "matmul", track=(k == 1), out=pst[0][0:64, 0:cn], lhsT=wuqs[:, k, h * 64:(h + 1) * 64], rhs=cqT[:, k, c0:c0 + cn], start=(k == 0), stop=(k == 1))
            P.op("dve", "tensor_tensor", out=r1[:, 0:cn], in0=ps1[1][0:64, 0:cn], in1=Ct[:, 0:cn], op=ALU.mult)
            P.op("dve", "tensor_tensor", out=r2[:, 0:cn], in0=pst[0][0:64, 0:cn], in1=St[:, 0:cn], op=ALU.mult)
            P.op("dve", "tensor_tensor", out=r1[:, 0:cn], in0=r1[:, 0:cn], in1=r2[:, 0:cn], op=ALU.add)
            P.op("act", "activation", out=qb[0:64, h, c0:c0 + cn], in_=r1[:, 0:cn], func=AF.Copy, scale=scale)
    kTa = P.sb("kTa", [128, NK], BF16); kTb = P.sb("kTb", [65, NK], BF16); Vb = P.sb("Vb", [128, NB, 129], BF16)
    P.op("pool", "memset", ap=kTb.v, constant=1.0)
    P.op("pool", "memset", ap=Vb.v, constant=1.0)
    P.dma("sp", kTb[0:64, :], krT_s)
    mx = P.sb("mx", [128, 4, 40]); negm = P.sb("negm", [128, 4]); Z = P.sb("Z", [128, 65])
    P.op("dve", "memset", ap=Z.v, constant=0.0)
    pt = [P.sb("pt%d" % i, [128, 512], BF16) for i in range(2)]
    rl = P.sb("rl", [128, 4]); ot = [P.sb("ot%d" % i, [128, 4, 128]) for i in range(2)]
    groups = [(g * 512, 512, NK) for g in range(5)] + [(2560, 128, 256), (2688, 128, 256)]
    c1 = 0; c2 = 0; gi = 0
    for h in range(4):
        P.dma("sp", kTa.v, kT_s[h])
        for b0 in range(0, NB, 26):
            bn = min(26, NB - b0)
            P.dma("sp", Vb[:, b0:b0 + bn, 0:128], V_s[b0 * 128:(b0 + bn) * 128, h * 128:(h + 1) * 128].rearrange("(b p) f -> p b f", p=128))
        for (q0, nq, nkeys) in groups:
            u = gi % 2; gi += 1; nqt = nq // 128
            kblocks = [(k0, min(512, nkeys - k0)) for k0 in range(0, nkeys, 512)]
            for qt in range(nqt):
                qs_ = slice(q0 + qt * 128, q0 + (qt + 1) * 128)
                for bi, (k0, kn) in enumerate(kblocks):
                    pb = ps1[c1 % 2]; c1 += 1
                    P.op("pe", "matmul", track=False, out=pb[:, 0:kn], lhsT=qa[:, h, qs_], rhs=kTa[:, k0:k0 + kn], start=True, stop=False)
                    P.op("pe", "matmul", out=pb[:, 0:kn], lhsT=qb[0:64, h, qs_], rhs=kTb[0:64, k0:k0 + kn], start=False, stop=True)
                    P.op("dve", "tensor_reduce", out=mx[:, qt, bi:bi + 1], in_=pb[:, 0:kn], axis=AX.X, op=ALU.max)
                P.op("dve", "tensor_reduce", out=negm[:, qt:qt + 1], in_=mx[:, qt, 0:len(kblocks)], axis=AX.X, op=ALU.max)
                P.op("dve", "tensor_scalar", out=Z[:, 64:65], in0=negm[:, qt:qt + 1], scalar1=-1.0, scalar2=None, op0=ALU.mult)
                pz = ps1[c1 % 2]; c1 += 1
                P.op("pe", "matmul", out=pz[0:65, 0:128], lhsT=Z.v, rhs=idf.v, start=True, stop=True)
                P.op("dve", "tensor_copy", out=qb[64:65, h, qs_], in_=pz[64:65, 0:128])
            nkb = nkeys // 128

            def qk(kb_, slot):
                ks_ = slice(kb_ * 128, (kb_ + 1) * 128)
                pb = pst[slot % 2]
                P.op("pe", "matmul", track=False, out=pb[:, 0:nq], lhsT=kTa[:, ks_], rhs=qa[:, h, q0:q0 + nq], start=True, stop=False)
                P.op("pe", "matmul", out=pb[:, 0:nq], lhsT=kTb[0:65, ks_], rhs=qb[0:65, h, q0:q0 + nq], start=False, stop=True)
            qk(0, c2)
            for kb_ in range(nkb):
                pb = pst[c2 % 2]; ptb = pt[c2 % 2]
                P.op("act", "activation", out=ptb[:, 0:nq], in_=pb[:, 0:nq], func=AF.Exp)
                if kb_ + 1 < nkb:
                    qk(kb_ + 1, c2 + 1)
                c2 += 1
                for qt in range(nqt):
                    P.op("pe", "matmul", track=(kb_ == nkb - 1), out=pO[qt][:, 0:129], lhsT=ptb[:, qt * 128:(qt + 1) * 128], rhs=Vb[:, kb_, :], start=(kb_ == 0), stop=(kb_ == nkb - 1))
            for qt in range(nqt):
                P.op("dve", "reciprocal", out=rl[:, qt:qt + 1], in_=pO[qt][:, 128:129])
                P.op("dve", "tensor_scalar", out=ot[u][:, qt, :], in0=pO[qt][:, 0:128], scalar1=rl[:, qt:qt + 1], scalar2=None, op0=ALU.mult)
            P.dma("sp", om_s[q0:q0 + nq, h * 128:(h + 1) * 128].rearrange("(t p) d -> p t d", p=128), ot[u][:, 0:nqt, :])
    P.end()


def stage_post(P, idf, idb, mode, variants, x_ap, ow_d, g1_d, gates_s, l, modcol, g2col_d, rw_d, rb_d, x1_s, hT_s, G_s, GT_s, gla=None, aT_ap=None, hTok_s=None):
    P.begin()
    nt = len(variants); nv = 2; T = nt * 128; D = 1024; KC = 8
    R2 = lambda nm, shp, dt=F32: [P.sb("%s_%d" % (nm, i), shp, dt) for i in range(2)]
    stage = [P.sb("wst%d" % i, [128, D]) for i in range(2)]
    wb = load_w_bf16(P, ow_d, D, D, "owb", stage=stage)
    rw = P.sb("rw", [128, KC, 32]); P.dma("sp", rw.v, rw_d.rearrange("(k p) n -> p k n", p=128))
    rb = P.sb("rb", [128, 32]); P.dma("sp", rb.v, rb_d)
    g2 = P.sb("g2", [128, KC]); P.dma("sp", g2.v, g2col_d)
    sc = modcols(P, modcol, l, 4, g2.v, "scm")
    sh = lambda v, k: modcol[:, l, 24 + k, v:v + 1]
    g1 = P.sb("g1", [128, D]); P.dma("sp", g1.v, g1_d)
    GG = []
    for v in range(nv):
        gg = P.sb("GG%d" % v, [128, D]); P.dma("sp", gg.v, gates_s[l, 0, v])
        P.op("dve", "tensor_tensor", out=gg.v, in0=gg.v, in1=g1.v, op=ALU.mult)
        GG.append(gg)
    if mode == "gla":
        onc = P.sb("onc", [128, 1]); P.dma("sp", onc.v, gla["on_d"])
        idxf = P.sb("idxf", [128, nt], I32); idxb = P.sb("idxb", [128, nt], I32)
        P.dma("sp", idxf.v, gla["idxf_d"]); P.dma("sp", idxb.v, gla["idxb_d"])
        tof_ = R2("tof", [128, 512]); tob_ = R2("tob", [128, 512]); tr_ = R2("tr", [128, 512]); tom_ = R2("tom", [128, 512])
        cat_ = R2("cat", [128, D], BF16)
        pT = P.ps("pT", [128, KC, 128], BF16)
    else:
        a32_ = R2("a32", [128, KC, 128])
    aT_ = R2("aT", [128, KC, 128], BF16)
    xt_ = R2("xt", [128, D]); x1_ = R2("x1", [128, D]); junk = P.sb("junk", [128, D]); st_ = R2("st", [128, 16])
    xn2_ = R2("xn2", [128, D]); h2T_ = R2("h2T", [128, KC, 128]); h2Tb_ = R2("h2Tb", [128, KC, 128], BF16)
    lg_ = R2("lg", [128, 32]); t8_ = R2("t8", [128, 8]); msk_ = R2("msk", [128, 32]); ex_ = R2("ex", [128, 32]); Gt_ = R2("Gt", [128, 32]); GTt_ = R2("GTt", [32, 128])
    py = [P.ps("py%d" % i, [128, 512]) for i in range(2)]
    pT32 = P.ps("pT32", [128, KC, 128])
    plg = P.ps("plg", [128, 128])
    pTt = P.ps("pTt", [128, D], BF16); htok_ = R2("htok", [128, D], BF16)
    for t in range(nt):
        v = variants[t]; sl = slice(t * 128, (t + 1) * 128); u_ = t % 2
        if mode == "gla":
            tof = tof_[u_]; tob = tob_[u_]; tr = tr_[u_]; tom = tom_[u_]; cat = cat_[u_]
        else:
            a32 = a32_[u_]
        aT = aT_[u_]; xt = xt_[u_]; x1 = x1_[u_]; st = st_[u_]; xn2 = xn2_[u_]; h2T = h2T_[u_]; h2Tb = h2Tb_[u_]
        lg = lg_[u_]; t8 = t8_[u_]; msk = msk_[u_]; ex = ex_[u_]; Gt = Gt_[u_]; GTt = GTt_[u_]
        P.dma("sp", xt.v, x_ap[sl, :])
        if mode == "gla":
            P.gather(tof.v, gla["of_s"], idxf[:, t:t + 1]); P.gather(tob.v, gla["ob_s"], idxb[:, t:t + 1])
            P.dma("sp", tr.v, gla["r_s"][sl, :]); P.dma("sp", tom.v, gla["om_s"][sl, :])
            P.op("dve", "tensor_tensor", out=tof.v, in0=tof.v, in1=tob.v, op=ALU.add)
            for h in range(4):
                P.op("act", "activation", out=junk[:, 0:128], in_=tof[:, h * 128:(h + 1) * 128], func=AF.Square, accum_out=st[:, h:h + 1])
            rstd_of(P, st[:, 0:4], st[:, 8:12], st[:, 4:8], 128)
            P.op("act", "activation", out=tr.v, in_=tr.v, func=AF.Silu)
            for h in range(4):
                hs = slice(h * 128, (h + 1) * 128)
                P.op("dve", "scalar_tensor_tensor", out=cat[:, hs], in0=tof[:, hs], scalar=st[:, 8 + h:9 + h], in1=tr[:, hs], op0=ALU.mult, op1=ALU.mult)
            P.op("pool", "tensor_copy", out=cat[:, 512:1024], in_=tom.v)
            for k in range(KC):
                P.op("pe", "transpose", out=pT[:, k, :], in_=cat[:, k * 128:(k + 1) * 128], identity=idb.v)
            P.op("dve", "tensor_scalar", out=aT[:, 0:4, :], in0=pT[:, 0:4, :], scalar1=onc[:, 0:1], scalar2=None, op0=ALU.mult)
            P.op("act", "activation", out=aT[:, 4:8, :], in_=pT[:, 4:8, :], func=AF.Copy)
        else:
            P.dma("sp", a32.v, aT_ap[:, sl].rearrange("(k p) t -> p k t", p=128))
            P.op("pool", "tensor_copy", out=aT.v, in_=a32.v)
        for n in range(2):
            for k in range(KC):
                P.op("pe", "matmul", track=(k == KC - 1), out=py[n].v, lhsT=aT[:, k, :], rhs=wb[:, k, n * 512:(n + 1) * 512], start=(k == 0), stop=(k == KC - 1))
            P.op("act", "activation", out=junk[:, 0:512], in_=py[n].v, func=AF.Square, accum_out=st[:, 12 + n:13 + n])
        P.op("dve", "tensor_tensor", out=st[:, 12:13], in0=st[:, 12:13], in1=st[:, 13:14], op=ALU.add)
        rstd_of(P, st[:, 12:13], st[:, 14:15], st[:, 13:14], D)
        for n in range(2):
            ns = slice(n * 512, (n + 1) * 512)
            P.op("dve", "scalar_tensor_tensor", out=x1[:, ns], in0=py[n].v, scalar=st[:, 14:15], in1=GG[v][:, ns], op0=ALU.mult, op1=ALU.mult)
        P.op("pool", "tensor_tensor", out=x1.v, in0=x1.v, in1=xt.v, op=ALU.add)
        P.dma("sp", x1_s[sl, :], x1.v)
        P.op("act", "activation", out=junk.v, in_=x1.v, func=AF.Square, accum_out=st[:, 15:16])
        rstd_of(P, st[:, 15:16], st[:, 4:5], st[:, 5:6], D)
        P.op("dve", "tensor_scalar", out=xn2.v, in0=x1.v, scalar1=st[:, 4:5], scalar2=None, op0=ALU.mult)
        for k in range(KC):
            P.op("pe", "transpose", out=pT32[:, k, :], in_=xn2[:, k * 128:(k + 1) * 128], identity=idf.v)
        for k in range(KC):
            P.op("dve", "tensor_scalar", out=h2T[:, k, :], in0=pT32[:, k, :], scalar1=sc[:, v, k:k + 1], scalar2=sh(v, k), op0=ALU.mult, op1=ALU.add)
        P.op("pool", "tensor_copy", out=h2Tb.v, in_=h2T.v)
        P.dma("sp", hT_s[:, sl].rearrange("(k p) t -> p k t", p=128), h2Tb.v)
        if hTok_s is not None:
            for k in range(KC):
                P.op("pe", "transpose", out=pTt[:, k * 128:(k + 1) * 128], in_=h2Tb[:, k, :], identity=idb.v, track=(k == KC - 1))
            P.op("act", "activation", out=htok_[u_].v, in_=pTt.v, func=AF.Copy)
            P.dma("sp", hTok_s[sl, :], htok_[u_].v)
        for k in range(KC):
            P.op("pe", "matmul", track=(k == KC - 1), out=plg[:, 0:32], lhsT=h2T[:, k, :], rhs=rw[:, k, :], start=(k == 0), stop=(k == KC - 1))
        P.op("dve", "tensor_tensor", out=lg.v, in0=plg[:, 0:32], in1=rb.v, op=ALU.add)
        P.op("dve", "max", out=t8.v, in_=lg.v)
        P.op("dve", "tensor_scalar", out=msk.v, in0=lg.v, scalar1=t8[:, 3:4], scalar2=None, op0=ALU.is_ge)
        P.op("dve", "tensor_scalar", out=t8[:, 7:8], in0=t8[:, 0:1], scalar1=-1.0, scalar2=None, op0=ALU.mult)
        P.op("act", "activation", out=ex.v, in_=lg.v, func=AF.Exp, bias=t8[:, 7:8])
        P.op("dve", "tensor_tensor", out=ex.v, in0=ex.v, in1=msk.v, op=ALU.mult)
        P.op("dve", "tensor_reduce", out=t8[:, 6:7], in_=ex.v, axis=AX.X, op=ALU.add)
        P.op("dve", "reciprocal", out=t8[:, 5:6], in_=t8[:, 6:7])
        P.op("dve", "tensor_scalar", out=Gt.v, in0=ex.v, scalar1=t8[:, 5:6], scalar2=None, op0=ALU.mult)
        P.dma("sp", G_s[sl, :], Gt.v)
        P.op("pe", "transpose", out=plg[0:32, :], in_=Gt.v, identity=idf.v)
        P.op("act", "activation", out=GTt.v, in_=plg[0:32, :], func=AF.Copy)
        P.dma("sp", GT_s[:, sl], GTt.v)
    P.end()


def stage_moe(P, variants, groups, hT_s, G_s, GT_s, x1_s, w1_d, w2_d, b1g_d, b1l_d, b2_d, g3_d, gates_s, l, x2_ap, NE=32):
    P.begin()
    nt = len(variants); nv = 2; T = nt * 128; D = 1024; KC = 8; F = 1024
    mg = max(len(g) for g in groups)
    g3 = P.sb("g3", [128, D]); P.dma("sp", g3.v, g3_d)
    GG = []
    for v in range(nv):
        gg = P.sb("GG%d" % v, [128, D]); P.dma("sp", gg.v, gates_s[l, 1, v])
        P.op("dve", "tensor_tensor", out=gg.v, in0=gg.v, in1=g3.v, op=ALU.mult)
        GG.append(gg)
    b1g = P.sb("b1g", [128, 32, KC]); b1l = P.sb("b1l", [128, 32, KC]); P.dma("sp", b1g.v, b1g_d); P.dma("sp", b1l.v, b1l_d)
    b2 = P.sb("b2", [32, D]); P.dma("sp", b2.v, b2_d)
    stg = [P.sb("stg%d" % i, [128, 2 * F]) for i in range(2)]
    w1g = [P.sb("w1g%d" % i, [128, KC, F], BF16) for i in range(2)]
    w1l = [P.sb("w1l%d" % i, [128, KC, F], BF16) for i in range(2)]
    w2b = [P.sb("w2b%d" % i, [128, KC, D], BF16) for i in range(2)]
    hT = P.sb("hT", [128, KC, mg * 128], BF16)
    Gs = P.sb("Gs", [128, mg, 32]); GTs = P.sb("GTs", [32, mg * 128])
    yacc = P.sb("yacc", [128, mg, D])
    actT = P.sb("actT", [128, KC, 512], BF16)
    tg = [P.sb("tg%d" % i, [128, 512]) for i in range(2)]; tsg = [P.sb("tsg%d" % i, [128, 512]) for i in range(2)]
    tl = [P.sb("tl%d" % i, [128, 512]) for i in range(2)]
    xt = g3; junk = P.sb("junk", [128, D]); st = P.sb("st", [128, 4])
    pg = [P.ps("pg%d" % i, [128, 512]) for i in range(2)]; pl = [P.ps("pl%d" % i, [128, 512]) for i in range(2)]
    py = [P.ps("py%d" % i, [128, 512]) for i in range(2)]
    sti = [0]

    def load_expert_steps(e, buf):
        steps = []
        for k in range(KC):
            def step(k=k):
                s = stg[sti[0] % 2]; sti[0] += 1
                P.dma("sp", s.v, w1_d[e, k * 128:(k + 1) * 128, :])
                sv = s.v.rearrange("p (f two) -> p f two", two=2)
                P.op("act", "activation", out=w1g[buf][:, k, :], in_=sv[:, :, 0], func=AF.Copy)
                P.op("act", "activation", out=w1l[buf][:, k, :], in_=sv[:, :, 1], func=AF.Copy)
                s = stg[sti[0] % 2]; sti[0] += 1
                P.dma("sp", s[:, 0:D], w2_d[e, k * 128:(k + 1) * 128, :])
                P.op("act", "activation", out=w2b[buf][:, k, :], in_=s[:, 0:D], func=AF.Copy)
            steps.append(step)
        return steps

    for grp in groups:
        ng = len(grp)
        t0 = grp[0]; tok0 = t0 * 128; ntok = ng * 128
        P.dma("sp", hT[:, :, 0:ntok], hT_s[:, tok0:tok0 + ntok].rearrange("(k p) t -> p k t", p=128))
        P.dma("sp", Gs[:, 0:ng, :], G_s[tok0:tok0 + ntok, :].rearrange("(g p) e -> p g e", p=128))
        P.dma("sp", GTs[:, 0:ntok], GT_s[:, tok0:tok0 + ntok])
        for i in range(ng):
            for n in range(2):
                P.op("pe", "matmul", out=py[n].v, lhsT=GTs[:, i * 128:(i + 1) * 128], rhs=b2[:, n * 512:(n + 1) * 512], start=True, stop=True)
                P.op("act", "activation", out=yacc[:, i, n * 512:(n + 1) * 512], in_=py[n].v, func=AF.Copy)
        for s in load_expert_steps(0, 0):
            s()
        blocks = [(b0, min(512, ntok - b0)) for b0 in range(0, ntok, 512)]
        ci = 0
        for e in range(NE):
            buf = e % 2
            nxt = load_expert_steps(e + 1, 1 - buf) if e + 1 < NE else []
            for bi, (b0, bn) in enumerate(blocks):
                for j in range(KC):
                    if bi == 0 and nxt:
                        nxt[j]()
                    c = ci % 2; ci += 1
                    for k in range(KC):
                        P.op("pe", "matmul", track=(k == KC - 1), out=pg[c][:, 0:bn], lhsT=w1g[buf][:, k, j * 128:(j + 1) * 128], rhs=hT[:, k, b0:b0 + bn], start=(k == 0), stop=(k == KC - 1))
                    for k in range(KC):
                        P.op("pe", "matmul", track=(k == KC - 1), out=pl[c][:, 0:bn], lhsT=w1l[buf][:, k, j * 128:(j + 1) * 128], rhs=hT[:, k, b0:b0 + bn], start=(k == 0), stop=(k == KC - 1))
                    P.op("dve", "tensor_scalar", out=tg[c][:, 0:bn], in0=pg[c][:, 0:bn], scalar1=b1g[:, e, j:j + 1], scalar2=7.0, op0=ALU.add, op1=ALU.min)
                    P.op("act", "activation", out=tsg[c][:, 0:bn], in_=tg[c][:, 0:bn], func=AF.Sigmoid, scale=1.702)
                    P.op("dve", "tensor_scalar", out=tl[c][:, 0:bn], in0=pl[c][:, 0:bn], scalar1=b1l[:, e, j:j + 1], scalar2=-7.0, op0=ALU.add, op1=ALU.max)
                    P.op("dve", "tensor_scalar", out=tl[c][:, 0:bn], in0=tl[c][:, 0:bn], scalar1=7.0, scalar2=1.0, op0=ALU.min, op1=ALU.add)
                    P.op("pool", "tensor_tensor", out=tg[c][:, 0:bn], in0=tg[c][:, 0:bn], in1=tsg[c][:, 0:bn], op=ALU.mult)
                    P.op("pool", "tensor_tensor", out=actT[:, j, 0:bn], in0=tg[c][:, 0:bn], in1=tl[c][:, 0:bn], op=ALU.mult)
                for i in range(b0 // 128, (b0 + bn) // 128):
                    for n in range(2):
                        ns = slice(n * 512, (n + 1) * 512)
                        for j in range(KC):
                            P.op("pe", "matmul", track=(j == KC - 1), out=py[n].v, lhsT=actT[:, j, i * 128 - b0:(i + 1) * 128 - b0], rhs=w2b[buf][:, j, ns], start=(j == 0), stop=(j == KC - 1))
                        P.op("dve", "scalar_tensor_tensor", out=yacc[:, i, ns], in0=py[n].v, scalar=Gs[:, i, e:e + 1], in1=yacc[:, i, ns], op0=ALU.mult, op1=ALU.add)
        for i in range(ng):
            t = grp[i]; v = variants[t]; sl = slice(t * 128, (t + 1) * 128)
            P.dma("sp", xt.v, x1_s[sl, :])
            P.op("act", "activation", out=junk.v, in_=yacc[:, i, :], func=AF.Square, accum_out=st[:, 0:1])
            rstd_of(P, st[:, 0:1], st[:, 2:3], st[:, 1:2], D)
            P.op("dve", "scalar_tensor_tensor", out=junk.v, in0=yacc[:, i, :], scalar=st[:, 2:3], in1=GG[v].v, op0=ALU.mult, op1=ALU.mult)
            P.op("pool", "tensor_tensor", out=xt.v, in0=junk.v, in1=xt.v, op=ALU.add)
            P.dma("sp", x2_ap[sl, :], xt.v)
    P.end()


def stage_na(P, idb, qT_s, kT_s, V_s, bi_d, be_d, oT_s):
    P.begin()
    ones = P.sb("ones", [128, 128], BF16); P.op("pool", "memset", ap=ones.v, constant=1.0)
    kb = [P.sb("kb%d" % i, [128, 8, 768], BF16) for i in range(2)]; vb = [P.sb("vb%d" % i, [128, 6, 1024], BF16) for i in range(2)]
    kcb = P.sb("kcb", [128, 8, 256], BF16); vcb = P.sb("vcb", [128, 2, 1024], BF16)
    P.dma("sp", kcb.v, kT_s[:, 2560:2816].rearrange("(k p) n -> p k n", p=128))
    P.dma("sp", vcb.v, V_s[2560:2816, :].rearrange("(b p) f -> p b f", p=128))
    Bi = P.sb("Bi", [128, 8, 512]); P.dma("sp", Bi.v, bi_d)
    Be = [P.sb("Be%d" % i, [128, 8, 768]) for i in range(2)]
    qrow = [P.sb("qrow%d" % i, [128, 8, 64], BF16) for i in range(2)]
    QBD = [P.sb("QBD%d" % i, [128, 128], BF16) for i in range(2)]
    for z in QBD:
        P.op("pool", "memset", ap=z.v, constant=0.0)
    Sb = [P.sb("Sb%d" % i, [128, 1024]) for i in range(2)]; Pm = [P.sb("Pm%d" % i, [128, 1024], BF16) for i in range(2)]
    PT = [P.sb("PT%d" % i, [128, 8, 128], BF16) for i in range(2)]
    sm = P.sb("sm", [128, 4]); rl = P.sb("rl", [128, 128]); orow = [P.sb("orow%d" % i, [128, 8, 64]) for i in range(2)]
    pS = [P.ps("pS%d" % i, [128, 1024]) for i in range(2)]
    pPT = P.ps("pPT", [128, 8, 128], BF16); pO = P.ps("pO", [128, 128]); pL = P.ps("pL", [128, 128])
    it = 0; nbe = 0
    for i in range(32):
        u = i % 2
        if i < 4:
            e0, nr, edge = 0, 12, i
        elif i >= 28:
            e0, nr, edge = 28, 12, i - 24
        else:
            e0, nr, edge = i, 8, None
        nw = nr * 64; nbk = nw // 128
        P.dma("sp", kb[u][:, :, 0:nw], kT_s[:, e0 * 64:e0 * 64 + nw].rearrange("(k p) n -> p k n", p=128))
        P.dma("sp", vb[u][:, 0:nbk, :], V_s[e0 * 64:e0 * 64 + nw, :].rearrange("(b p) f -> p b f", p=128))
        P.dma("sp", qrow[u].v, qT_s[:, (i + 4) * 64:(i + 5) * 64].rearrange("(k p) t -> p k t", p=128))
        if edge is not None:
            B = Be[nbe % 2]; nbe += 1
            P.dma("sp", B.v, be_d[edge])
        else:
            B = Bi
        ntot = nw + 256; nblk = nbk + 2
        def scores(hp, w):
            P.op("pool", "tensor_copy", out=QBD[w][0:64, 0:64], in_=qrow[u][0:64, hp, :])
            P.op("pool", "tensor_copy", out=QBD[w][64:128, 64:128], in_=qrow[u][64:128, hp, :])
            for c0 in range(0, nw, 512):
                cn = min(512, nw - c0)
                P.op("pe", "matmul", out=pS[w][:, c0:c0 + cn], lhsT=QBD[w].v, rhs=kb[u][:, hp, c0:c0 + cn], start=True, stop=True, track=False)
            P.op("pe", "matmul", out=pS[w][:, nw:ntot], lhsT=QBD[w].v, rhs=kcb[:, hp, :], start=True, stop=True)
        scores(0, it % 2)
        for hp in range(8):
            w = it % 2; it += 1
            if hp + 1 < 8:
                scores(hp + 1, it % 2)
            P.op("dve", "tensor_tensor", out=Sb[w][:, 0:nw], in0=pS[w][:, 0:nw], in1=B[:, hp, 0:nw], op=ALU.add)
            P.op("act", "activation", out=Sb[w][:, nw:ntot], in_=pS[w][:, nw:ntot], func=AF.Copy)
            P.op("dve", "tensor_reduce", out=sm[:, 0:1], in_=Sb[w][:, 0:ntot], axis=AX.X, op=ALU.max)
            P.op("dve", "tensor_scalar", out=sm[:, 1:2], in0=sm[:, 0:1], scalar1=-1.0, scalar2=None, op0=ALU.mult)
            P.op("act", "activation", out=Pm[w][:, 0:ntot], in_=Sb[w][:, 0:ntot], func=AF.Exp, bias=sm[:, 1:2])
            for b in range(nblk):
                P.op("pe", "transpose", out=pPT[:, b, :], in_=Pm[w][:, b * 128:(b + 1) * 128], identity=idb.v, track=(b == nblk - 1))
            P.op("dve", "tensor_copy", out=PT[w][:, 0:nblk, :], in_=pPT[:, 0:nblk, :])
            hs = slice(hp * 128, (hp + 1) * 128)
            for b in range(nblk):
                vblk = vb[u][:, b, hs] if b < nbk else vcb[:, b - nbk, hs]
                P.op("pe", "matmul", out=pO.v, lhsT=vblk, rhs=PT[w][:, b, :], start=(b == 0), stop=(b == nblk - 1), track=(b == nblk - 1))
            for b in range(nblk):
                P.op("pe", "matmul", out=pL.v, lhsT=ones.v, rhs=PT[w][:, b, :], start=(b == 0), stop=(b == nblk - 1), track=(b == nblk - 1))
            P.op("dve", "reciprocal", out=rl.v, in_=pL.v)
            P.op("dve", "tensor_tensor", out=orow[u][0:64, hp, :], in0=pO[0:64, 0:64], in1=rl[0:64, 0:64], op=ALU.mult)
            P.op("dve", "tensor_tensor", out=orow[u][64:128, hp, :], in0=pO[64:128, 64:128], in1=rl[64:128, 64:128], op=ALU.mult)
        P.dma("sp", oT_s[:, i * 64:(i + 1) * 64].rearrange("(k p) t -> p k t", p=128), orow[u].v)
    P.end()


MOE_PASSES_F0 = [[[0, 1, 2, 3], [4, 5, 6, 7]], [[8, 9, 10, 11], [12, 13, 14, 15]], [[16, 17, 18, 19], [20, 21]]]
MOE_PASSES_F1 = [[[0, 1, 2, 3], [4, 5, 6, 7]], [[8, 9, 10, 11], [12, 13, 14, 15]]]
MOE_GROUPS_F0 = [list(range(0, 6)), list(range(6, 12)), list(range(12, 17)), list(range(17, 22))]
MOE_GROUPS_F1 = [list(range(0, 6)), list(range(6, 11)), list(range(11, 16))]


def build_fused(dbg=False, upto=99):
    P = Prog("fused")
    D = 1024
    dr = P.dram
    x_all = dr("x_all", [TALL, D]); x_rev = dr("x_rev", [TALL, D]); x_own = dr("x_own", [TOWN, D])
    adaw = dr("adaw", [2, D, 6144]); bcol = dr("bcol", [128, 2, 48]); brow = dr("brow", [2, 128, 6144]); cT = dr("cT", [128, 8, 2])
    ng = dr("ng", [2, 4, 128, 8]); ngrow = dr("ngrow", [2, 4, 128, D])
    w_tmA = dr("w_tmA", [D, 896]); w_fmA = dr("w_fmA", [D, 672]); w_tmB = dr("w_tmB", [D, 768]); w_fmB = dr("w_fmB", [D, 528]); w_own = dr("w_own", [D, 768])
    Ca = dr("Ca", [64, TALL]); Sa = dr("Sa", [64, TALL]); Cq = dr("Cq", [64, TOWN]); Sq = dr("Sq", [64, TOWN])
    wukv = dr("wukv", [128, 1024]); kvn = dr("kvn", [128, 1]); qn = dr("qn", [128, 2]); wuq = dr("wuq", [256, 768]); wuqs = dr("wuqs", [256, 256])
    wa = dr("wa", [17, 8, 64]); tri = dr("tri", [64, 64]); mu = dr("mu", [64, 8, 64])
    idxf = dr("idxf", [128, NOWN], I32); idxb = dr("idxb", [128, NOWN], I32)
    ow0 = dr("ow0", [D, D]); ow1 = dr("ow1", [D, D]); onc = dr("onc", [128, 1])
    rw = dr("rw", [2, D, 32]); rb = dr("rb", [2, 128, 32])
    if upto >= 7:
        w1 = dr("w1", [2, 32, D, 2048]); w2 = dr("w2", [2, 32, D, D])
    b1 = dr("b1", [2, 32, 2048]); b2 = dr("b2", [2, 32, D]); iota_d = dr("iota", [128, 128]); lst_d = dr("lst", [128, 128])
    wqk = dr("wqk", [D, 2048]); wv = dr("wv", [D, D]); Bint = dr("Bint", [128, 8, 512]); Bedge = dr("Bedge", [8, 128, 8, 768])
    out = dr("out", [2048, D], out=True)
    S = lambda n, s, dt=F32: P.scratch(n, s, dt, dbg=(dbg is True or (dbg and n in dbg)))
    gates = S("gates", [2, 2, 2, 128, D])
    kA = S("kA", [TALL, 256]); vA = S("vA", [TALL, 512]); qTA = S("qTA", [256, TALL]); kTA = S("kTA", [256, TALL]); aTA = S("aTA", [16, TALL])
    kB = S("kB", [TALL, 256]); vB = S("vB", [TALL, 512]); qTB = S("qTB", [256, TALL]); kTB = S("kTB", [256, TALL]); aTB = S("aTB", [16, TALL])
    kTm = S("kTm", [4, 128, TALL], BF16); krT = S("krT", [64, TALL], BF16); Vm = S("Vm", [TALL, 512], BF16)
    r_own = S("r_own", [TOWN, 512]); cq_own = S("cq_own", [TOWN, 256])
    oF = S("oF", [TALL, 512]); oB = S("oB", [TALL, 512]); om = S("om", [TOWN, 512])
    w1bf = S("w1bf", [2, 32, D, 2048], BF16); w2bf = S("w2bf", [2, 32, D, D], BF16); b1bf = S("b1bf", [2, 32, 2048], BF16)
    hka = S("hka", [TOWN, D], BF16); hkb = S("hkb", [2048, D], BF16)
    x1a = S("x1a", [TOWN, D]); hTa = S("hTa", [D, TOWN], BF16); Ga = S("Ga", [TOWN, 32]); GTa = S("GTa", [32, TOWN]); x2a = S("x2a", [TOWN, D])
    qT1 = S("qT1", [D, TOWN], BF16); kT1 = S("kT1", [D, TOWN], BF16); V1 = S("V1", [TOWN, D], BF16); oT1 = S("oT1", [D, 2048])
    x1b = S("x1b", [2048, D]); hTb = S("hTb", [D, 2048], BF16); Gb = S("Gb", [2048, 32]); GTb = S("GTb", [32, 2048])
    idf, idb = ident(P)
    modcol = P.sb("modcol", [128, 2, 48, 2])
    stage_ada(P, adaw, bcol, brow, cT, modcol, gates)
    if upto < 1:
        P.finish(); return P
    gall = [(0, 2)] + [(256 + 512 * i, 4) for i in range(32)]; gv = [1] + [0] * 32
    gown = [(512 * i, 4) for i in range(5)] + [(2560, 2)]; gvo = [0] * 5 + [1]

    def n1(l):
        g = P.sb("gn%d" % P.nscope, [128, 8]); P.dma("sp", g.v, ng[l, 0])
        sc = modcols(P, modcol, l, 1, g.v, "sc%d" % P.nscope)
        sh = P.sb("sh%d" % P.nscope, [128, 2, 8])
        for s in range(2):
            P.op("dve", "tensor_copy", out=sh[:, s, :], in_=modcol[:, l, 0:8, s])
        return sc, sh
    sc0, sh0 = n1(0)
    if upto >= 7:
        it0 = conv_items(w1, w2, b1, w1bf, w2bf, b1bf, 0); it1 = conv_items(w1, w2, b1, w1bf, w2bf, b1bf, 1)
        xA = xB = None
    else:
        xA = xB = None
    stage_pro2(P, idb, x_all, gall, gv, sc0, sh0, D, w_tmA, 896, [(0, 256, kA, F32), (256, 512, vA, F32)], w_fmA, 672,
              [(0, 128, qTA[0:128], F32, 1.0), (128, 128, qTA[128:256], F32, 1.0), (256, 128, kTA[0:128], F32, 1.0), (384, 128, kTA[128:256], F32, 1.0), (512, 16, aTA, F32, 1.0)],
              rope=(544, 608, krT, Ca, Sa), mla=dict(col=768, wukv_d=wukv, kvn_d=kvn, kT_s=kTm, V_s=Vm), tag="proA", extra=xA)
    stage_pro2(P, idb, x_rev, gall, gv, sc0, sh0, D, w_tmB, 768, [(0, 256, kB, F32), (256, 512, vB, F32)], w_fmB, 528,
              [(0, 128, qTB[0:128], F32, 1.0), (128, 128, qTB[128:256], F32, 1.0), (256, 128, kTB[0:128], F32, 1.0), (384, 128, kTB[128:256], F32, 1.0), (512, 16, aTB, F32, 1.0)], tag="proB", extra=xB)
    if upto < 2:
        P.finish(); return P
    stage_pro2(P, idb, x_own, gown, gvo, sc0, sh0, D, w_own, 768, [(0, 512, r_own, F32), (512, 256, cq_own, F32)], None, 0, [], tag="proO")
    if upto < 3:
        P.finish(); return P
    scans = []
    for h in range(4):
        hs = slice(h * 64, (h + 1) * 64)
        scans.append((qTA[hs], kTA[hs], kA[:, hs], vA[:, h * 128:(h + 1) * 128], aTA, h, oF[:, h * 128:(h + 1) * 128]))
    for h in range(4):
        hs = slice(h * 64, (h + 1) * 64)
        scans.append((qTB[hs], kTB[hs], kB[:, hs], vB[:, h * 128:(h + 1) * 128], aTB, 4 + h, oB[:, h * 128:(h + 1) * 128]))
    stage_gla(P, scans, wa, tri, mu, extra=(lambda P_: conv_steps(P_, it0 + it1)) if upto >= 7 else None, per_block=4)
    if upto < 4:
        P.finish(); return P
    stage_mla_full(P, idf, idb, cq_own, qn, wuq, wuqs, Cq, Sq, kTm, krT, Vm, om)
    if upto < 5:
        P.finish(); return P
    stage_post(P, idf, idb, "gla", VOWN, x_own, ow0, ngrow[0, 1], gates, 0, modcol, ng[0, 2], rw[0], rb[0], x1a, hTa, Ga, GTa,
               gla=dict(on_d=onc, idxf_d=idxf, idxb_d=idxb, of_s=oF, ob_s=oB, r_s=r_own, om_s=om), hTok_s=hka)
    if upto < 7:
        P.finish(); return P
    stage_moe_sp(P, idb, VOWN, MOE_PASSES_F0, hka, Ga, GTa, x1a, w1bf[0], w2bf[0], b1bf[0], b2[0], ngrow[0, 3], gates, 0, x2a, iota_d, lst_d)
    sc1, sh1 = n1(1)
    stage_pro2(P, idb, x2a, gown, gvo, sc1, sh1, D, wv, 1024, [(0, 1024, V1, BF16)], wqk, 2048,
              [(j * 128, 128, qT1[j * 128:(j + 1) * 128], BF16, 0.125) for j in range(8)] + [(1024 + j * 128, 128, kT1[j * 128:(j + 1) * 128], BF16, 1.0) for j in range(8)], tag="proQ")
    stage_na(P, idb, qT1, kT1, V1, Bint, Bedge, oT1)
    stage_post(P, idf, idb, "fm", [0] * 16, x2a[256:2304], ow1, ngrow[1, 1], gates, 1, modcol, ng[1, 2], rw[1], rb[1], x1b, hTb, Gb, GTb, aT_ap=oT1, hTok_s=hkb)
    stage_moe_sp(P, idb, [0] * 16, MOE_PASSES_F1, hkb, Gb, GTb, x1b, w1bf[1], w2bf[1], b1bf[1], b2[1], ngrow[1, 3], gates, 1, out, iota_d, lst_d)
    P.finish()
    return P


def _c(a):
    return np.ascontiguousarray(a, dtype=np.float32)


def _col(a):
    return _c(a.reshape(-1, 128).T)


def _bc(a):
    return _c(np.broadcast_to(a, (128,) + a.shape))


def _rope_tables():
    t = np.arange(16384)
    row = (t // 64).astype(np.float32); colp = (t % 64).astype(np.float32)
    inv = (np.float32(10000.0) ** (-np.arange(16, dtype=np.float32) / np.float32(16))).astype(np.float32)
    ang = np.concatenate([row[:, None] * inv, colp[:, None] * inv], -1).astype(np.float32)
    return np.cos(ang).astype(np.float32), np.sin(ang).astype(np.float32)


def na_bias_edge(rpb, r, grows):
    r0 = int(np.clip(r - 4, 0, 248))
    col = np.arange(64); c0 = np.clip(col - 8, 0, 48); kcol = np.arange(64)
    inwin = (kcol[None, :] >= c0[:, None]) & (kcol[None, :] < c0[:, None] + 16)
    cidx = np.clip(kcol[None, :] - col[:, None] + 15, 0, 30)
    out = np.full((16, 64, len(grows), 64), -30000.0, np.float32)
    for w, g in enumerate(grows):
        if 0 <= g <= 255 and r0 <= g < r0 + 8:
            t = rpb[:, g - r + 7][:, cidx]
            out[:, :, w, :] = np.where(inwin[None], t, np.float32(-30000.0))
    t = out.reshape(8, 2, 64, len(grows) * 64)
    return np.ascontiguousarray(t.transpose(1, 2, 0, 3).reshape(128, 8, len(grows) * 64))


_FP = {}


def fused_inputs(x, c, ctx, c_ctx, ada_w, ada_b, norm_g, router_w, router_b, moe_w1, moe_b1, moe_w2, moe_b2,
                 ab_in_w, gla_wa_f, gla_ba_f, gla_wa_b, gla_ba_b, gla_onorm, mla_qnorm, mla_wuq, mla_kvnorm, mla_wukv,
                 ab_out_w, na_qkv_w, na_rpb, na_out_w):
    f32 = np.float32
    A = lambda a: np.asarray(a, dtype=f32)
    x = A(x)[0]; ctx = A(ctx)[0]; ada_w = A(ada_w); ada_b = A(ada_b); norm_g = A(norm_g); inw = A(ab_in_w)[0]
    cos, sin = _rope_tables()
    com = {}
    com["x_all"] = _c(np.concatenate([ctx, x], 0)); com["x_rev"] = _c(np.concatenate([ctx[::-1], x[::-1]], 0))
    com["adaw"] = ada_w; com["bcol"] = _c(ada_b.reshape(2, 48, 128).transpose(2, 0, 1)); com["brow"] = _c(np.stack([_bc(ada_b[0]), _bc(ada_b[1])]))
    com["cT"] = _c(np.stack([A(c)[0], A(c_ctx)], 0).reshape(2, 8, 128).transpose(2, 1, 0))
    com["ng"] = _c(np.stack([np.stack([_col(norm_g[l, i]) for i in range(4)]) for l in range(2)]))
    com["ngrow"] = _c(np.stack([np.stack([_bc(norm_g[l, i]) for i in range(4)]) for l in range(2)]))
    kr = inw[:, 1952:2016]; krs = np.concatenate([kr[:, 32:], kr[:, :32]], 1)
    z16 = np.zeros((1024, 16), f32)
    com["w_tmA"] = _c(np.concatenate([inw[:, 256:512], inw[:, 512:1024], inw[:, 1824:1952]], 1))
    com["w_fmA"] = _c(np.concatenate([inw[:, 0:256], inw[:, 256:512], inw[:, 1536:1552], z16, kr, krs], 1))
    com["w_tmB"] = _c(np.concatenate([inw[:, 256:512], inw[:, 512:1024]], 1))
    com["w_fmB"] = _c(np.concatenate([inw[:, 0:256], inw[:, 256:512], inw[:, 1552:1568]], 1))
    com["w_own"] = _c(np.concatenate([inw[:, 1024:1536], inw[:, 1568:1824]], 1))
    Ca = np.ones((64, TALL), f32); Sa = np.zeros((64, TALL), f32)
    Ca[:, 256:] = np.concatenate([cos.T, cos.T], 0); Sa[:, 256:] = np.concatenate([-sin.T, sin.T], 0)
    com["Ca"] = Ca; com["Sa"] = Sa
    wk = A(mla_wukv)[0].reshape(128, 4, 256)
    com["wukv"] = _c(np.concatenate([wk[:, :, :128].reshape(128, 512), wk[:, :, 128:].reshape(128, 512)], 1))
    com["kvn"] = _c(A(mla_kvnorm)[0].reshape(128, 1)); com["qn"] = _col(A(mla_qnorm)[0])
    wq = A(mla_wuq)[0]; com["wuq"] = _c(wq)
    com["wuqs"] = _c(np.concatenate([np.concatenate([wq[:, h * 192 + 160:h * 192 + 192], wq[:, h * 192 + 128:h * 192 + 160]], 1) for h in range(4)], 1))
    wa = np.zeros((17, 8, 64), f32)
    for d, (w_, b_) in enumerate(((A(gla_wa_f)[0], A(gla_ba_f)[0]), (A(gla_wa_b)[0], A(gla_ba_b)[0]))):
        for h in range(4):
            wa[:16, d * 4 + h] = w_[:, h * 64:(h + 1) * 64]; wa[16, d * 4 + h] = b_[h * 64:(h + 1) * 64]
    com["wa"] = wa
    com["tri"] = np.where(np.arange(64)[:, None] <= np.arange(64)[None, :], -1.0 / 16, 0.0).astype(f32)
    com["mu"] = _c(np.repeat((np.arange(64)[:, None] <= np.arange(64)[None, :]).astype(f32)[:, None, :], 8, 1))
    com["ow0"] = _c(A(ab_out_w)[0]); com["ow1"] = _c(A(na_out_w)[0]); com["onc"] = _c(A(gla_onorm)[0].reshape(128, 1))
    com["rw"] = _c(A(router_w)); com["rb"] = _c(np.stack([_bc(A(router_b)[0]), _bc(A(router_b)[1])]))
    com["w1"] = A(moe_w1); com["w2"] = A(moe_w2)
    b1 = A(moe_b1)
    colE = lambda a: _c(a.reshape(32, 8, 128).transpose(2, 0, 1))
    com["b1"] = _c(b1); com["iota"] = _bc(np.arange(128, dtype=f32)); com["lst"] = (np.arange(128)[:, None] < np.arange(128)[None, :]).astype(f32)
    com["b2"] = _c(A(moe_b2))
    qkv = A(na_qkv_w)[0]; com["wqk"] = _c(qkv[:, :2048]); com["wv"] = _c(qkv[:, 2048:])
    rpb = A(na_rpb)[0]
    com["Bint"] = na_bias_table(rpb, 4)
    maps = []
    for cc in range(8):
        m = dict(com)
        er = np.arange(32 * cc - 4, 32 * cc + 36)
        erc = np.clip(er, 0, 255)
        tok = (erc[:, None] * 64 + np.arange(64)[None, :]).ravel()
        m["x_own"] = _c(np.concatenate([x[tok], ctx], 0))
        Cq = np.ones((64, TOWN), f32); Sq = np.zeros((64, TOWN), f32)
        Cq[:, :2560] = np.concatenate([cos[tok].T, cos[tok].T], 0); Sq[:, :2560] = np.concatenate([-sin[tok].T, sin[tok].T], 0)
        m["Cq"] = Cq; m["Sq"] = Sq
        posf = np.concatenate([256 + tok, np.arange(256)]); posb = np.concatenate([256 + (16383 - tok), 255 - np.arange(256)])
        m["idxf"] = np.ascontiguousarray(posf.reshape(NOWN, 128).T.astype(np.int32)); m["idxb"] = np.ascontiguousarray(posb.reshape(NOWN, 128).T.astype(np.int32))
        be = []
        for e in range(8):
            i = e if e < 4 else 24 + e
            e0 = 0 if e < 4 else 28
            be.append(na_bias_edge(rpb, 32 * cc + i, [int(er[e0 + w]) for w in range(12)]))
        m["Bedge"] = _c(np.stack(be))
        maps.append(m)
    return maps


def kernel(**inputs):
    if "P" not in _FP:
        _FP["P"] = build_fused()
    P = _FP["P"]
    maps = fused_inputs(**inputs)
    maps = [{k: m[k] for k in P.in_names} for m in maps]
    res = run_bass_kernel_spmd(P.nc, maps, core_ids=list(range(8)))
    return np.concatenate([r["out"] for r in res.results], 0)[None].astype(np.float32)


def stage_pro2(P, idb, x_d, groups, gvar, gsc, gsh, D, wtm_d, ntm, tm_outs, wfm_d, nfm, fm_specs, rope=None, mla=None, tag="pro", extra=None, per_tile=2):
    P.begin()
    KC = D // 128
    tiles = []
    for gi, (tok0, gt) in enumerate(groups):
        for t in range(gt):
            tiles.append((tok0 + t * 128, gvar[gi]))
    nt = len(tiles)
    stg = [P.sb("wst%d" % i, [128, max(ntm, nfm, 1024)]) for i in range(2)]
    wtm = load_w_bf16(P, wtm_d, D, ntm, "wtm", stage=stg) if ntm else None
    wfm = load_w_bf16(P, wfm_d, D, nfm, "wfm", stage=stg) if nfm else None
    R = 3
    ring = lambda nm, shp, dt=F32, n=R: [P.sb("%s%d" % (nm, i), shp, dt) for i in range(n)]
    if mla:
        wuk = load_w_bf16(P, mla["wukv_d"], 128, 1024, "wuk", stage=stg)
        kvn = P.sb("kvn", [128, 1]); P.dma("sp", kvn.v, mla["kvn_d"])
        ckn = ring("ckn", [128, 128], BF16); ckT = ring("ckT", [128, 128], BF16)
        kst = ring("kst", [128, 4, 128], BF16); vst = ring("vst", [128, 512], BF16)
        pck = P.ps("pck", [128, 128], BF16)
    xs = ring("x", [128, D]); junk = P.sb("junk", [128, D]); st = ring("st", [128, 8])
    xn = ring("xn", [128, D], BF16); hT = ring("hT", [128, KC, 128], BF16)
    yt = ring("yt", [128, max(ntm, 1)]); ytb = ring("ytb", [128, max(ntm, 1)], BF16)
    nspec = max(len(fm_specs), 1)
    fo = [[P.sb("fo%d_%d" % (i, j), [128, 128], (BF16 if fm_specs[j][3] == BF16 else F32)) for j in range(len(fm_specs))] for i in range(R)]
    pT = [P.ps("pT%d" % i, [128, KC, 128], BF16) for i in range(2)]
    py = [P.ps("py%d" % i, [128, 512]) for i in range(2)]; pf = [P.ps("pf%d" % i, [128, 4, 128]) for i in range(2)]
    if rope:
        Ct = ring("Ct", [64, 128]); St = ring("St", [64, 128]); r1 = ring("r1", [64, 128]); r2 = ring("r2", [64, 128]); rb_ = ring("rb", [64, 128], BF16)
    cnt = dict(y=0, f=0)

    def A(t):
        r0, v = tiles[t]; a = t % R; b = t % 2
        P.dma("sp", xs[a].v, x_d[r0:r0 + 128, :])
        P.op("act", "activation", out=junk.v, in_=xs[a].v, func=AF.Square, accum_out=st[a][:, 0:1])
        rstd_of(P, st[a][:, 0:1], st[a][:, 2:3], st[a][:, 1:2], D)
        P.op("dve", "tensor_scalar", out=xn[a].v, in0=xs[a].v, scalar1=st[a][:, 2:3], scalar2=None, op0=ALU.mult)
        for k in range(KC):
            P.op("pe", "transpose", out=pT[b][:, k, :], in_=xn[a][:, k * 128:(k + 1) * 128], identity=idb.v, track=(k == KC - 1))
        for k in range(KC):
            P.op("dve", "tensor_scalar", out=hT[a][:, k, :], in0=pT[b][:, k, :], scalar1=gsc[:, v, k:k + 1], scalar2=gsh[:, v, k:k + 1], op0=ALU.mult, op1=ALU.add)

    def B1(t):
        r0, v = tiles[t]; a = t % R
        if ntm:
            for c0 in range(0, ntm, 512):
                cn = min(512, ntm - c0); pb = py[cnt["y"] % 2]; cnt["y"] += 1
                for k in range(KC):
                    P.op("pe", "matmul", track=(k == KC - 1), out=pb[:, 0:cn], lhsT=hT[a][:, k, :], rhs=wtm[:, k, c0:c0 + cn], start=(k == 0), stop=(k == KC - 1))
                P.op("act", "activation", out=yt[a][:, c0:c0 + cn], in_=pb[:, 0:cn], func=AF.Copy)
            for (c0, n, ap, dt) in tm_outs:
                if dt == BF16:
                    P.op("pool", "tensor_copy", out=ytb[a][:, c0:c0 + n], in_=yt[a][:, c0:c0 + n])
                    P.dma("sp", ap[r0:r0 + 128, :], ytb[a][:, c0:c0 + n])
                else:
                    P.dma("sp", ap[r0:r0 + 128, :], yt[a][:, c0:c0 + n])
        if mla:
            cc = mla["col"]
            P.op("act", "activation", out=junk[:, 0:128], in_=yt[a][:, cc:cc + 128], func=AF.Square, accum_out=st[a][:, 4:5])
            rstd_of(P, st[a][:, 4:5], st[a][:, 6:7], st[a][:, 5:6], 128)
            P.op("dve", "tensor_scalar", out=ckn[a].v, in0=yt[a][:, cc:cc + 128], scalar1=st[a][:, 6:7], scalar2=None, op0=ALU.mult)
            P.op("pe", "transpose", out=pck.v, in_=ckn[a].v, identity=idb.v)
            P.op("dve", "tensor_scalar", out=ckT[a].v, in0=pck.v, scalar1=kvn[:, 0:1], scalar2=None, op0=ALU.mult)

    def B2(t):
        r0, v = tiles[t]; a = t % R
        for si in range(0, len(fm_specs), 4):
            chunk = fm_specs[si:si + 4]
            pb = pf[cnt["f"] % 2]; cnt["f"] += 1
            for j, (c0, m, ap, dt, scale) in enumerate(chunk):
                for k in range(KC):
                    P.op("pe", "matmul", track=(k == KC - 1), out=pb[0:m, j, :], lhsT=wfm[:, k, c0:c0 + m], rhs=hT[a][:, k, :], start=(k == 0), stop=(k == KC - 1))
            for j, (c0, m, ap, dt, scale) in enumerate(chunk):
                dst = fo[a][si + j]
                P.op("act", "activation", out=dst[0:m, :], in_=pb[0:m, j, :], func=AF.Copy, scale=float(scale))
                P.dma("sp", ap[:, r0:r0 + 128], dst[0:m, :])
        if rope:
            ckr, ckrs, ap, C_d, S_d = rope
            P.dma("sp", Ct[a].v, C_d[:, r0:r0 + 128]); P.dma("sp", St[a].v, S_d[:, r0:r0 + 128])
            pb = pf[cnt["f"] % 2]; cnt["f"] += 1
            for k in range(KC):
                P.op("pe", "matmul", track=(k == KC - 1), out=pb[0:64, 0, :], lhsT=wfm[:, k, ckr:ckr + 64], rhs=hT[a][:, k, :], start=(k == 0), stop=(k == KC - 1))
            for k in range(KC):
                P.op("pe", "matmul", track=(k == KC - 1), out=pb[0:64, 1, :], lhsT=wfm[:, k, ckrs:ckrs + 64], rhs=hT[a][:, k, :], start=(k == 0), stop=(k == KC - 1))
            P.op("dve", "tensor_tensor", out=r1[a].v, in0=pb[0:64, 0, :], in1=Ct[a].v, op=ALU.mult)
            P.op("dve", "tensor_tensor", out=r2[a].v, in0=pb[0:64, 1, :], in1=St[a].v, op=ALU.mult)
            P.op("pool", "tensor_tensor", out=rb_[a].v, in0=r1[a].v, in1=r2[a].v, op=ALU.add)
            P.dma("sp", ap[:, r0:r0 + 128], rb_[a].v)
        if mla:
            pb = py[cnt["y"] % 2]; cnt["y"] += 1
            P.op("pe", "matmul", out=pb.v, lhsT=ckT[a].v, rhs=wuk[:, 0, 512:1024], start=True, stop=True)
            P.op("act", "activation", out=vst[a].v, in_=pb.v, func=AF.Copy)
            P.dma("sp", mla["V_s"][r0:r0 + 128, :], vst[a].v)
            pb = pf[cnt["f"] % 2]; cnt["f"] += 1
            for h in range(4):
                P.op("pe", "matmul", out=pb[:, h, :], lhsT=wuk[:, 0, h * 128:(h + 1) * 128], rhs=ckT[a].v, start=True, stop=True, track=(h == 3))
            P.op("act", "activation", out=kst[a].v, in_=pb.v, func=AF.Copy)
            P.dma("sp", mla["kT_s"][:, :, r0:r0 + 128].rearrange("h p t -> p h t"), kst[a].v)

    xsteps = extra(P) if extra else []
    xi = 0
    for step in range(nt + 2):
        for _ in range(per_tile):
            if xi < len(xsteps):
                xsteps[xi](); xi += 1
        if step < nt:
            A(step)
        if 0 <= step - 1 < nt:
            B1(step - 1)
        if 0 <= step - 2 < nt:
            B2(step - 2)
    while xi < len(xsteps):
        xsteps[xi](); xi += 1
    P.end()


def stage_moe_sp(P, idb, variants, passes, hTok_s, G_s, GT_s, x1_s, w1_d, w2_d, b1_d, b2_d, g3_d, gates_s, l, x2_ap, iota_d, lst_d, NE=32):
    P.begin()
    nt = len(variants); nv = 2; D = 1024; KC = 8; F = 1024
    mp = max(sum(len(g) for g in ps_) for ps_ in passes)
    b2 = P.sb("b2", [32, D]); P.dma("sp", b2.v, b2_d)
    iota = P.sb("iota", [128, 128]); P.dma("sp", iota.v, iota_d)
    lst32 = P.sb("lst32", [128, 128]); P.dma("sp", lst32.v, lst_d)
    lst = P.sb("lst", [128, 128], BF16); P.op("dve", "tensor_copy", out=lst.v, in_=lst32.v)
    onesb = P.sb("onesb", [128, 128], BF16); P.op("dve", "memset", ap=onesb.v, constant=1.0)
    stg = [P.sb("stg%d" % i, [128, 2 * F]) for i in range(2)]
    w1b = [P.sb("w1b%d" % i, [128, KC, 2 * F], BF16) for i in range(2)]
    w2b = [P.sb("w2b%d" % i, [128, KC, D], BF16) for i in range(2)]
    b1b = [P.sb("b1b%d" % i, [1, 2 * F], BF16) for i in range(2)]
    Gs = P.sb("Gs", [128, mp, 32]); GTs = P.sb("GTs", [32, mp * 128]); msk = P.sb("msk", [128, mp, 32]); mskb = P.sb("mskb", [128, mp, 32], BF16)
    pos = P.sb("pos", [128, mp, 32])
    yacc = P.sb("yacc", [128, mp, D])
    htm = [P.sb("htm%d" % i, [128, 4, D], BF16) for i in range(2)]
    Sel = [[P.sb("Sel%d_%d" % (j, i), [128, 128], BF16) for i in range(4)] for j in range(2)]
    SelG = [[P.sb("SelG%d_%d" % (j, i), [128, 128], BF16) for i in range(4)] for j in range(2)]
    XeT = [P.sb("XeT%d" % i, [128, KC, 128], BF16) for i in range(2)]
    tgh = [P.sb("tg%d" % i, [128, 512]) for i in range(2)]; tlh = [P.sb("tl%d" % i, [128, 512], BF16) for i in range(2)]
    tsgh = [P.sb("tsg%d" % i, [128, 512], BF16) for i in range(2)]; actbh = [P.sb("actb%d" % i, [128, 512], BF16) for i in range(2)]
    actT = P.sb("actT", [128, KC, 128], BF16); Ye = P.sb("Ye", [128, D], BF16); SGT = P.sb("SGT", [128, 4, 128], BF16)
    st = P.sb("st", [128, 4])
    pX = P.ps("pX", [128, 1024]); pUa = P.ps("pUa", [128, 1024]); pUb = P.ps("pUb", [128, 1024])
    pT = P.ps("pT", [128, KC, 128], BF16); pST = P.ps("pST", [128, 4, 128], BF16)
    pXv = pX.v.rearrange("p (f q) -> p f q", q=128)

    def load_expert_steps(e, buf):
        steps = []
        for k in range(KC):
            def step(k=k):
                P.dma("sp", w1b[buf][:, k, :], w1_d[e, k * 128:(k + 1) * 128, :])
                P.dma("sp", w2b[buf][:, k, :], w2_d[e, k * 128:(k + 1) * 128, :])
                if k == KC - 1:
                    P.dma("sp", b1b[buf].v, b1_d[e:e + 1, :])
            steps.append(step)
        return steps

    for groups in passes:
        ptiles = [t for g in groups for t in g]
        t0 = ptiles[0]; npt = len(ptiles); tok0 = t0 * 128; ntok = npt * 128
        P.dma("sp", Gs[:, 0:npt, :], G_s[tok0:tok0 + ntok, :].rearrange("(g p) e -> p g e", p=128))
        P.dma("sp", GTs[:, 0:ntok], GT_s[:, tok0:tok0 + ntok])
        P.op("dve", "tensor_scalar", out=msk[:, 0:npt, :], in0=Gs[:, 0:npt, :], scalar1=0.0, scalar2=None, op0=ALU.is_gt)
        P.op("dve", "tensor_copy", out=mskb[:, 0:npt, :], in_=msk[:, 0:npt, :])
        for i in range(npt):
            for n in range(2):
                P.op("pe", "matmul", out=pX[:, n * 512:(n + 1) * 512], lhsT=GTs[:, i * 128:(i + 1) * 128], rhs=b2[:, n * 512:(n + 1) * 512], start=True, stop=True)
            P.op("act", "activation", out=yacc[:, i, :], in_=pX.v, func=AF.Copy)
        for g in groups:
            for gi_, t in enumerate(g):
                i = t - t0
                for j_ in range(gi_):
                    P.op("pe", "matmul", track=False, out=pUa[:, 0:32], lhsT=onesb.v, rhs=mskb[:, g[j_] - t0, :], start=(j_ == 0), stop=False)
                P.op("pe", "matmul", out=pUa[:, 0:32], lhsT=lst.v, rhs=mskb[:, i, :], start=(gi_ == 0), stop=True)
                P.op("dve", "tensor_copy", out=pos[:, i, :], in_=pUa[:, 0:32])
        for s_ in load_expert_steps(0, 0):
            s_()
        its = [(e, gidx) for e in range(NE) for gidx in range(len(groups))]
        nxt_cache = {}

        def prep(n):
            e, gidx = its[n]; g = groups[gidx]; ng = len(g); i0 = g[0] - t0; s2 = n % 2
            hb = htm[s2]
            P.dma("sp", hb[:, 0:ng, :], hTok_s[g[0] * 128:(g[0] + ng) * 128, :].rearrange("(g p) f -> p g f", p=128))
            for ii in range(ng):
                P.op("dve", "tensor_scalar", out=Sel[s2][ii].v, in0=iota.v, scalar1=pos[:, i0 + ii, e:e + 1], scalar2=msk[:, i0 + ii, e:e + 1], op0=ALU.is_equal, op1=ALU.mult)
                P.op("dve", "tensor_scalar", out=SelG[s2][ii].v, in0=iota.v, scalar1=pos[:, i0 + ii, e:e + 1], scalar2=Gs[:, i0 + ii, e:e + 1], op0=ALU.is_equal, op1=ALU.mult)
            for f in range(KC):
                for ii in range(ng):
                    P.op("pe", "matmul", track=(f == KC - 1 and ii == ng - 1), out=pXv[:, f, :], lhsT=hb[:, ii, f * 128:(f + 1) * 128], rhs=Sel[s2][ii].v, start=(ii == 0), stop=(ii == ng - 1))
            P.op("act", "activation", out=XeT[s2].v, in_=pXv, func=AF.Copy)

        def ffn1(n):
            e, gidx = its[n]; buf = e % 2; s2 = n % 2
            for h in range(2):
                pu = (pUa, pUb)[h]
                for c in range(2):
                    col = h * 1024 + c * 512
                    for k in range(KC):
                        P.op("pe", "matmul", track=False, out=pu[:, c * 512:(c + 1) * 512], lhsT=XeT[s2][:, k, :], rhs=w1b[buf][:, k, col:col + 512], start=(k == 0), stop=False)
                    P.op("pe", "matmul", track=(c == 1), out=pu[:, c * 512:(c + 1) * 512], lhsT=onesb[0:1, :], rhs=b1b[buf][0:1, col:col + 512], start=False, stop=True)
                uv = pu.v.rearrange("p (f two) -> p f two", two=2)
                P.op("dve", "tensor_scalar", out=tgh[h].v, in0=uv[:, :, 0], scalar1=7.0, scalar2=None, op0=ALU.min)
                P.op("act", "activation", out=tsgh[h].v, in_=tgh[h].v, func=AF.Sigmoid, scale=1.702)
                P.op("dve", "tensor_scalar", out=tlh[h].v, in0=uv[:, :, 1], scalar1=-7.0, scalar2=7.0, op0=ALU.max, op1=ALU.min)
                P.op("pool", "tensor_tensor", out=tgh[h].v, in0=tgh[h].v, in1=tsgh[h].v, op=ALU.mult)
                P.op("pool", "tensor_scalar", out=tlh[h].v, in0=tlh[h].v, scalar1=1.0, scalar2=None, op0=ALU.add)
                P.op("pool", "tensor_tensor", out=actbh[h].v, in0=tlh[h].v, in1=tgh[h].v, op=ALU.mult)

        def stage3(n):
            e, gidx = its[n]; g = groups[gidx]; ng = len(g); i0 = g[0] - t0; buf = e % 2; s2 = n % 2
            for f in range(KC):
                P.op("pe", "transpose", out=pT[:, f, :], in_=actbh[f // 4][:, (f % 4) * 128:(f % 4 + 1) * 128], identity=idb.v, track=(f == KC - 1))
            P.op("act", "activation", out=actT.v, in_=pT.v, func=AF.Copy)
            for ii in range(ng):
                P.op("pe", "transpose", out=pST[:, ii, :], in_=SelG[s2][ii].v, identity=idb.v, track=(ii == ng - 1))
            P.op("dve", "tensor_copy", out=SGT[:, 0:ng, :], in_=pST[:, 0:ng, :])
            for half in range(2):
                hs = slice(half * 512, (half + 1) * 512)
                for k in range(KC):
                    P.op("pe", "matmul", track=(k == KC - 1), out=pUa[:, hs], lhsT=actT[:, k, :], rhs=w2b[buf][:, k, hs], start=(k == 0), stop=(k == KC - 1))
            P.op("act", "activation", out=Ye.v, in_=pUa.v, func=AF.Copy)
            ci = 0
            for ii in range(ng):
                for half in range(2):
                    hs = slice(half * 512, (half + 1) * 512); pc = pUb[:, (ci % 2) * 512:(ci % 2 + 1) * 512]; ci += 1
                    P.op("pe", "matmul", out=pc, lhsT=SGT[:, ii, :], rhs=Ye[:, hs], start=True, stop=True)
                    P.op("dve", "tensor_tensor", out=yacc[:, i0 + ii, hs], in0=pc, in1=yacc[:, i0 + ii, hs], op=ALU.add)
            if e + 1 < NE:
                if e not in nxt_cache:
                    nxt_cache[e] = load_expert_steps(e + 1, 1 - buf)
                nxt = nxt_cache[e]
                per = (len(nxt) + len(groups) - 1) // len(groups)
                for s_ in nxt[gidx * per:(gidx + 1) * per]:
                    s_()

        prep(0)
        for n in range(len(its)):
            ffn1(n)
            if n + 1 < len(its):
                prep(n + 1)
            stage3(n)
        P.dma("sp", stg[1][:, 0:D], g3_d)
        for v in range(nv):
            P.dma("sp", stg[0][:, v * D:(v + 1) * D], gates_s[l, 1, v])
            P.op("dve", "tensor_tensor", out=stg[0][:, v * D:(v + 1) * D], in0=stg[0][:, v * D:(v + 1) * D], in1=stg[1][:, 0:D], op=ALU.mult)
        hf = htm[0].v.rearrange("p a b -> p (a b)").bitcast(F32)
        for i in range(npt):
            t = ptiles[i]; v = variants[t]; sl = slice(t * 128, (t + 1) * 128)
            xt = hf[:, 0:D]; junk = hf[:, D:2 * D]; tmp = stg[1][:, D:2 * D]
            P.dma("sp", xt, x1_s[sl, :])
            P.op("act", "activation", out=junk, in_=yacc[:, i, :], func=AF.Square, accum_out=st[:, 0:1])
            rstd_of(P, st[:, 0:1], st[:, 2:3], st[:, 1:2], D)
            P.op("dve", "scalar_tensor_tensor", out=tmp, in0=yacc[:, i, :], scalar=st[:, 2:3], in1=stg[0][:, v * D:(v + 1) * D], op0=ALU.mult, op1=ALU.mult)
            P.op("pool", "tensor_tensor", out=xt, in0=tmp, in1=xt, op=ALU.add)
            P.dma("sp", x2_ap[sl, :], xt)
    P.end()
```

```python
import numpy as np
import ml_dtypes
import concourse.bass as bass
import concourse.mybir as mybir
from concourse.bass_utils import run_bass_kernel_spmd

F32 = mybir.dt.float32
BF16 = mybir.dt.bfloat16
I32 = mybir.dt.int32
ALU = mybir.AluOpType
AF = mybir.ActivationFunctionType
AX = mybir.AxisListType

COMPUTE = ("pe", "act", "dve", "pool")


class View:
    __slots__ = ("b", "ap")

    def __init__(self, b, ap):
        self.b = b
        self.ap = ap

    def __getitem__(self, k):
        return View(self.b, self.ap[k])

    def bitcast(self, dt):
        return View(self.b, self.ap.bitcast(dt))

    def rearrange(self, s, **kw):
        return View(self.b, self.ap.rearrange(s, **kw))

    def to_broadcast(self, shape):
        return View(self.b, self.ap.to_broadcast(shape))


class Buf:
    def __init__(self, prog, t, name):
        self.p = prog
        self.t = t
        self.name = name
        self.w = None
        self.r = {}
        self.wsem = None
        self.wcnt = 0
        self.rsem = None
        self.rcnt = 0

    def __getitem__(self, k):
        return View(self, self.t[k])

    @property
    def v(self):
        return View(self, self.t.ap())


class Prog:
    def __init__(self, name="k"):
        self.nc = bass.Bass("TRN2", target_bir_lowering=False, name=name)
        nc = self.nc
        self.E = dict(pe=nc.tensor, act=nc.scalar, dve=nc.vector, pool=nc.gpsimd, sp=nc.sync)
        self.sem = {e: nc.alloc_semaphore("sem_" + e) for e in COMPUTE}
        self.cnt = {e: 0 for e in COMPUTE}
        self.pending = {e: False for e in COMPUTE}
        self.seen = {e: {} for e in self.E}
        self.bufs = []
        self.nsem = 4
        self.ninst = 0
        self.sempool = []
        self.semid = {}
        self.stack = None
        self.scope_bufs = None
        self.nscope = 0

    def sb(self, name, shape, dt=F32):
        if self.stack is not None:
            t = self.stack.enter_context(self.nc.sbuf_tensor("sb%d_%s" % (self.nscope, name), list(shape), dt))
        else:
            t = self.nc.alloc_sbuf_tensor("sb_" + name, list(shape), dt)
        b = Buf(self, t, name)
        self.bufs.append(b)
        if self.scope_bufs is not None:
            self.scope_bufs.append(b)
        return b

    def ps(self, name, shape, dt=F32):
        if self.stack is not None:
            t = self.stack.enter_context(self.nc.psum_tensor("ps%d_%s" % (self.nscope, name), list(shape), dt))
        else:
            t = self.nc.alloc_psum_tensor("ps_" + name, list(shape), dt)
        b = Buf(self, t, name)
        self.bufs.append(b)
        if self.scope_bufs is not None:
            self.scope_bufs.append(b)
        return b

    def scratch(self, name, shape, dt=F32, dbg=False):
        return self.nc.dram_tensor(name, list(shape), dt, kind="ExternalOutput" if dbg else "Internal").ap()

    def begin(self):
        from contextlib import ExitStack
        self.nscope += 1
        self.stack = ExitStack()
        self.scope_bufs = []

    def barrier(self):
        for e in self.E:
            for f in COMPUTE:
                if self.cnt[f]:
                    self._wait(e, f, self.sem[f], self.cnt[f])
            for b in self.bufs:
                if b.wsem is not None and b.wcnt:
                    self._wait(e, ("s", id(b.wsem)), b.wsem, 16 * b.wcnt)
                if b.rsem is not None and b.rcnt:
                    self._wait(e, ("s", id(b.rsem)), b.rsem, 16 * b.rcnt)

    def end(self):
        self.barrier()
        for b in self.scope_bufs:
            if b.wsem is not None:
                self.sempool.append([b.wsem, b.wcnt]); b.wsem = None
            if b.rsem is not None:
                self.sempool.append([b.rsem, b.rcnt]); b.rsem = None
            self.bufs.remove(b)
        self.stack.close()
        self.stack = None
        self.scope_bufs = None

    def _getsem(self, name):
        if self.sempool:
            s, c = self.sempool.pop()
            return s, c
        return self._newsem(name), 0

    def dram(self, name, shape, dt=F32, out=False):
        if not out:
            self.__dict__.setdefault("in_names", []).append(name)
        return self.nc.dram_tensor(name, list(shape), dt, kind="ExternalOutput" if out else "ExternalInput").ap()

    def _newsem(self, name):
        self.nsem += 1
        return self.nc.alloc_semaphore(name)

    def _wait(self, e, key, sem, val):
        if self.seen[e].get(key, 0) >= val:
            return
        self.E[e].wait_ge(sem, val)
        self.seen[e][key] = val

    def _w_done(self, e, b):
        if b.w is not None:
            f, n = b.w
            if not (f == "pe" and e == "pe"):
                self._wait(e, f, self.sem[f], n)
        if b.wcnt:
            self._wait(e, ("s", id(b.wsem)), b.wsem, 16 * b.wcnt)

    def _r_done(self, e, b):
        for f, n in b.r.items():
            if f == "pe" and e == "pe":
                continue
            self._wait(e, f, self.sem[f], n)
        if b.rcnt:
            self._wait(e, ("s", id(b.rsem)), b.rsem, 16 * b.rcnt)

    def op(self, e, fn, track=True, **kw):
        wkeys = ("out", "accum_out", "ap")
        reads = [v.b for k, v in kw.items() if isinstance(v, View) and k not in wkeys]
        writes = [kw[k].b for k in wkeys if k in kw and isinstance(kw[k], View)]
        for b in reads:
            self._w_done(e, b)
        for b in writes:
            self._w_done(e, b)
            self._r_done(e, b)
        args = {k: (v.ap if isinstance(v, View) else v) for k, v in kw.items()}
        ins = getattr(self.E[e], fn)(**args)
        self.ninst += 1
        seq = self.cnt[e] + 1
        if track:
            ins.then_inc(self.sem[e], 1)
            self.cnt[e] = seq
            self.pending[e] = False
        else:
            self.pending[e] = True
        for b in reads:
            b.r[e] = seq
        for b in writes:
            b.w = (e, seq)
            b.r = {}
        return ins

    def dma(self, q, out, in_, **kw):
        if isinstance(out, View):
            b = out.b
            self._w_done(q, b)
            self._r_done(q, b)
            if b.wsem is None:
                b.wsem, b.wcnt = self._getsem("dw%d_%s" % (self.nscope, b.name))
            ins = self.E[q].dma_start(out=out.ap, in_=in_, **kw)
            ins.then_inc(b.wsem, 16)
            b.wcnt += 1
            b.w = None
            b.r = {}
        else:
            b = in_.b
            self._w_done(q, b)
            if b.rsem is None:
                b.rsem, b.rcnt = self._getsem("dr%d_%s" % (self.nscope, b.name))
            ins = self.E[q].dma_start(out=out, in_=in_.ap, **kw)
            ins.then_inc(b.rsem, 16)
            b.rcnt += 1
        self.ninst += 1
        return ins

    def gather(self, out, src_ap, idx):
        q = "pool"
        b = out.b
        self._w_done(q, b); self._r_done(q, b); self._w_done(q, idx.b)
        if b.wsem is None:
            b.wsem, b.wcnt = self._getsem("dw%d_%s" % (self.nscope, b.name))
        ins = self.nc.gpsimd.indirect_dma_start(out=out.ap, out_offset=None, in_=src_ap,
                                                in_offset=bass.IndirectOffsetOnAxis(ap=idx.ap, axis=0))
        ins.then_inc(b.wsem, 16)
        b.wcnt += 1; b.w = None; b.r = {}
        idx.b.r["pool"] = self.cnt["pool"] + 1
        self.ninst += 1
        return ins

    def finish(self, q="sp"):
        for b in self.bufs:
            if b.rcnt:
                self._wait(q, ("s", id(b.rsem)), b.rsem, 16 * b.rcnt)
        for e in COMPUTE:
            if self.cnt[e]:
                self._wait(q, e, self.sem[e], self.cnt[e])
        return self.nc


def run(prog, in_maps, ncores=8, trace=False):
    res = run_bass_kernel_spmd(prog.nc, in_maps, core_ids=list(range(ncores)), trace=trace)
    return res


EPS = 1e-6


def ident(P):
    idf = P.sb("idf", [128, 128]); idb = P.sb("idb", [128, 128], BF16)
    P.op("pool", "memset", ap=idf.v, constant=1.0)
    P.op("pool", "affine_select", out=idf.v, in_=idf.v, pattern=[[-1, 128]], compare_op=ALU.is_equal, fill=0.0, base=0, channel_multiplier=1)
    P.op("pool", "tensor_copy", out=idb.v, in_=idf.v)
    return idf, idb


def load_w_bf16(P, w_d, K, N, name, q="sp", cast_eng="pool", stage=None):
    KC = (K + 127) // 128
    wb = P.sb(name, [128, KC, N], BF16)
    for k in range(KC):
        rows = min(128, K - k * 128)
        st = stage[k % len(stage)]
        P.dma(q, st[0:rows, 0:N], w_d[k * 128:k * 128 + rows, :])
        P.op(cast_eng, "tensor_copy", out=wb[0:rows, k, :], in_=st[0:rows, 0:N])
    return wb


def rstd_of(P, ss_view, out_view, tmp_view, D):
    P.op("dve", "tensor_scalar", out=tmp_view, in0=ss_view, scalar1=1.0 / D, scalar2=EPS, op0=ALU.mult, op1=ALU.add)
    P.op("act", "activation", out=tmp_view, in_=tmp_view, func=AF.Sqrt)
    P.op("dve", "reciprocal", out=out_view, in_=tmp_view)


def build_proj(D, N, variants, rope=None, name="proj"):
    P = Prog(name)
    nt = len(variants); nv = max(variants) + 1
    KC = D // 128
    x_d = P.dram("x", [nt * 128, D]); w_d = P.dram("w", [D, N]); y_d = P.dram("y", [nt * 128, N], out=True)
    g_d = P.dram("gcol", [128, KC]); sc_d = P.dram("sc", [128, nv, KC]); sh_d = P.dram("sh", [128, nv, KC])
    if rope:
        cs_d = P.dram("cs", [nt * 128, 2, rope[1], 32])
    idf, idb = ident(P)
    stage = [P.sb("wst%d" % i, [128, N]) for i in range(2)]
    wb = load_w_bf16(P, w_d, D, N, "wb", stage=stage)
    g = P.sb("g", [128, KC]); sc = P.sb("scm", [128, nv, KC]); sh = P.sb("shm", [128, nv, KC])
    P.dma("sp", g.v, g_d); P.dma("sp", sc.v, sc_d); P.dma("sp", sh.v, sh_d)
    for v in range(nv):
        P.op("dve", "scalar_tensor_tensor", out=sc[:, v, :], in0=sc[:, v, :], scalar=1.0, in1=g.v, op0=ALU.add, op1=ALU.mult)
    xs = [P.sb("x%d" % i, [128, D]) for i in range(2)]
    junk = P.sb("junk", [128, D]); st = [P.sb("st%d" % i, [128, 4]) for i in range(2)]
    xn = [P.sb("xn%d" % i, [128, D], BF16) for i in range(2)]
    hT = [P.sb("hT%d" % i, [128, KC, 128], BF16) for i in range(2)]
    ys = [P.sb("y%d" % i, [128, N]) for i in range(2)]
    pT = [P.ps("pT%d" % i, [128, KC, 128], BF16) for i in range(2)]
    py = [P.ps("py%d" % i, [128, 512]) for i in range(4)]
    if rope:
        cs = [P.sb("cs%d" % i, [128, 2, rope[1], 32]) for i in range(2)]
        rt = [P.sb("rt%d" % i, [128, rope[1], 32]) for i in range(4)]
    nchunks = [(c, min(512, N - c)) for c in range(0, N, 512)]
    ci = 0
    for t in range(nt):
        b = t % 2; v = variants[t]
        P.dma("sp", xs[b].v, x_d[t * 128:(t + 1) * 128, :])
        if rope and v == 0:
            P.dma("sp", cs[b].v, cs_d[t * 128:(t + 1) * 128])
        P.op("act", "activation", out=junk.v, in_=xs[b].v, func=AF.Square, accum_out=st[b][:, 0:1])
        rstd_of(P, st[b][:, 0:1], st[b][:, 2:3], st[b][:, 1:2], D)
        P.op("dve", "tensor_scalar", out=xn[b].v, in0=xs[b].v, scalar1=st[b][:, 2:3], scalar2=None, op0=ALU.mult)
        for k in range(KC):
            P.op("pe", "transpose", out=pT[b][:, k, :], in_=xn[b][:, k * 128:(k + 1) * 128], identity=idb.v)
        for k in range(KC):
            P.op("dve", "tensor_scalar", out=hT[b][:, k, :], in0=pT[b][:, k, :], scalar1=sc[:, v, k:k + 1], scalar2=sh[:, v, k:k + 1], op0=ALU.mult, op1=ALU.add)
        for (c0, cn) in nchunks:
            pb = py[ci % 4]; ci += 1
            for k in range(KC):
                P.op("pe", "matmul", track=(k == KC - 1), out=pb[:, 0:cn], lhsT=hT[b][:, k, :], rhs=wb[:, k, c0:c0 + cn], start=(k == 0), stop=(k == KC - 1))
            P.op("act", "activation", out=ys[b][:, c0:c0 + cn], in_=pb[:, 0:cn], func=AF.Copy)
        if rope and v == 0:
            col0, ns, stride, off = rope
            seg = ys[b][:, col0:col0 + ns * stride].rearrange("p (h d) -> p h d", d=stride)
            x1 = seg[:, :, off:off + 32]; x2 = seg[:, :, off + 32:off + 64]
            co = cs[b][:, 0]; si = cs[b][:, 1]
            P.op("pool", "tensor_tensor", out=rt[0].v, in0=x1, in1=co, op=ALU.mult)
            P.op("pool", "tensor_tensor", out=rt[1].v, in0=x2, in1=si, op=ALU.mult)
            P.op("pool", "tensor_tensor", out=rt[2].v, in0=x1, in1=si, op=ALU.mult)
            P.op("pool", "tensor_tensor", out=rt[3].v, in0=x2, in1=co, op=ALU.mult)
            P.op("pool", "tensor_tensor", out=x1, in0=rt[0].v, in1=rt[1].v, op=ALU.subtract)
            P.op("pool", "tensor_tensor", out=x2, in0=rt[2].v, in1=rt[3].v, op=ALU.add)
        P.dma("sp", y_d[t * 128:(t + 1) * 128, :], ys[b].v)
    P.finish()
    return P


def build_post(mode, variants, name="post"):
    P = Prog(name)
    nt = len(variants); nv = max(variants) + 1; T = nt * 128; D = 1024; KC = 8
    x_d = P.dram("x", [T, D]); ow_d = P.dram("ow", [D, D])
    g1_d = P.dram("g1g", [128, D]); gate_d = P.dram("gate", [nv, 128, D])
    g2_d = P.dram("gcol", [128, KC]); sc_d = P.dram("sc", [128, nv, KC]); sh_d = P.dram("sh", [128, nv, KC])
    rw_d = P.dram("rw", [D, 32]); rb_d = P.dram("rb", [128, 32])
    x1_d = P.dram("x1", [T, D], out=True); hT_d = P.dram("hT", [D, T], BF16, out=True); G_d = P.dram("G", [T, 32], out=True)
    if mode == "gla":
        of_d = P.dram("of", [T, 512]); ob_d = P.dram("ob", [T, 512]); r_d = P.dram("r", [T, 512]); om_d = P.dram("om", [T, 512])
        on_d = P.dram("oncol", [128, 1])
    else:
        aT_d = P.dram("aT", [D, T])
    idf, idb = ident(P)
    stage = [P.sb("wst%d" % i, [128, D]) for i in range(2)]
    wb = load_w_bf16(P, ow_d, D, D, "owb", stage=stage)
    rw = P.sb("rw", [128, KC, 32]); P.dma("sp", rw.v, rw_d.rearrange("(k p) n -> p k n", p=128))
    rb = P.sb("rb", [128, 32]); P.dma("sp", rb.v, rb_d)
    g2 = P.sb("g2", [128, KC]); sc = P.sb("scm", [128, nv, KC]); sh = P.sb("shm", [128, nv, KC])
    P.dma("sp", g2.v, g2_d); P.dma("sp", sc.v, sc_d); P.dma("sp", sh.v, sh_d)
    for v in range(nv):
        P.op("dve", "scalar_tensor_tensor", out=sc[:, v, :], in0=sc[:, v, :], scalar=1.0, in1=g2.v, op0=ALU.add, op1=ALU.mult)
    g1 = P.sb("g1", [128, D]); P.dma("sp", g1.v, g1_d)
    GG = []
    for v in range(nv):
        gg = P.sb("GG%d" % v, [128, D]); P.dma("sp", gg.v, gate_d[v])
        P.op("dve", "tensor_tensor", out=gg.v, in0=gg.v, in1=g1.v, op=ALU.mult)
        GG.append(gg)
    if mode == "gla":
        onc = P.sb("onc", [128, 1]); P.dma("sp", onc.v, on_d)
        tof = P.sb("tof", [128, 512]); tob = P.sb("tob", [128, 512]); tr = P.sb("tr", [128, 512]); tom = P.sb("tom", [128, 512])
        cat = P.sb("cat", [128, D], BF16)
        pT = P.ps("pT", [128, KC, 128], BF16)
    else:
        a32 = P.sb("a32", [128, KC, 128])
    aT = P.sb("aT", [128, KC, 128], BF16)
    xt = P.sb("xt", [128, D]); x1 = P.sb("x1", [128, D]); junk = P.sb("junk", [128, D]); st = P.sb("st", [128, 16])
    xn2 = P.sb("xn2", [128, D]); h2T = P.sb("h2T", [128, KC, 128]); h2Tb = P.sb("h2Tb", [128, KC, 128], BF16)
    lg = P.sb("lg", [128, 32]); t8 = P.sb("t8", [128, 8]); msk = P.sb("msk", [128, 32]); ex = P.sb("ex", [128, 32]); Gt = P.sb("Gt", [128, 32])
    py = [P.ps("py%d" % i, [128, 512]) for i in range(2)]
    pT32 = P.ps("pT32", [128, KC, 128])
    plg = P.ps("plg", [128, 32])
    for t in range(nt):
        v = variants[t]; sl = slice(t * 128, (t + 1) * 128)
        P.dma("sp", xt.v, x_d[sl, :])
        if mode == "gla":
            P.dma("sp", tof.v, of_d[sl, :]); P.dma("sp", tob.v, ob_d[sl, :]); P.dma("sp", tr.v, r_d[sl, :]); P.dma("sp", tom.v, om_d[sl, :])
            P.op("dve", "tensor_tensor", out=tof.v, in0=tof.v, in1=tob.v, op=ALU.add)
            for h in range(4):
                P.op("act", "activation", out=junk[:, 0:128], in_=tof[:, h * 128:(h + 1) * 128], func=AF.Square, accum_out=st[:, h:h + 1])
            rstd_of(P, st[:, 0:4], st[:, 8:12], st[:, 4:8], 128)
            P.op("act", "activation", out=tr.v, in_=tr.v, func=AF.Silu)
            for h in range(4):
                hs = slice(h * 128, (h + 1) * 128)
                P.op("dve", "scalar_tensor_tensor", out=cat[:, hs], in0=tof[:, hs], scalar=st[:, 8 + h:9 + h], in1=tr[:, hs], op0=ALU.mult, op1=ALU.mult)
            P.op("pool", "tensor_copy", out=cat[:, 512:1024], in_=tom.v)
            for k in range(KC):
                P.op("pe", "transpose", out=pT[:, k, :], in_=cat[:, k * 128:(k + 1) * 128], identity=idb.v)
            P.op("dve", "tensor_scalar", out=aT[:, 0:4, :], in0=pT[:, 0:4, :], scalar1=onc[:, 0:1], scalar2=None, op0=ALU.mult)
            P.op("act", "activation", out=aT[:, 4:8, :], in_=pT[:, 4:8, :], func=AF.Copy)
        else:
            P.dma("sp", a32.v, aT_d[:, sl].rearrange("(k p) t -> p k t", p=128))
            P.op("pool", "tensor_copy", out=aT.v, in_=a32.v)
        for n in range(2):
            for k in range(KC):
                P.op("pe", "matmul", track=(k == KC - 1), out=py[n].v, lhsT=aT[:, k, :], rhs=wb[:, k, n * 512:(n + 1) * 512], start=(k == 0), stop=(k == KC - 1))
            P.op("act", "activation", out=junk[:, 0:512], in_=py[n].v, func=AF.Square, accum_out=st[:, 12 + n:13 + n])
        P.op("dve", "tensor_tensor", out=st[:, 12:13], in0=st[:, 12:13], in1=st[:, 13:14], op=ALU.add)
        rstd_of(P, st[:, 12:13], st[:, 14:15], st[:, 13:14], D)
        for n in range(2):
            ns = slice(n * 512, (n + 1) * 512)
            P.op("dve", "scalar_tensor_tensor", out=x1[:, ns], in0=py[n].v, scalar=st[:, 14:15], in1=GG[v][:, ns], op0=ALU.mult, op1=ALU.mult)
        P.op("pool", "tensor_tensor", out=x1.v, in0=x1.v, in1=xt.v, op=ALU.add)
        P.dma("sp", x1_d[sl, :], x1.v)
        P.op("act", "activation", out=junk.v, in_=x1.v, func=AF.Square, accum_out=st[:, 15:16])
        rstd_of(P, st[:, 15:16], st[:, 4:5], st[:, 5:6], D)
        P.op("dve", "tensor_scalar", out=xn2.v, in0=x1.v, scalar1=st[:, 4:5], scalar2=None, op0=ALU.mult)
        for k in range(KC):
            P.op("pe", "transpose", out=pT32[:, k, :], in_=xn2[:, k * 128:(k + 1) * 128], identity=idf.v)
        for k in range(KC):
            P.op("dve", "tensor_scalar", out=h2T[:, k, :], in0=pT32[:, k, :], scalar1=sc[:, v, k:k + 1], scalar2=sh[:, v, k:k + 1], op0=ALU.mult, op1=ALU.add)
        P.op("pool", "tensor_copy", out=h2Tb.v, in_=h2T.v)
        P.dma("sp", hT_d[:, sl].rearrange("(k p) t -> p k t", p=128), h2Tb.v)
        for k in range(KC):
            P.op("pe", "matmul", track=(k == KC - 1), out=plg.v, lhsT=h2T[:, k, :], rhs=rw[:, k, :], start=(k == 0), stop=(k == KC - 1))
        P.op("dve", "tensor_tensor", out=lg.v, in0=plg.v, in1=rb.v, op=ALU.add)
        P.op("dve", "max", out=t8.v, in_=lg.v)
        P.op("dve", "tensor_scalar", out=msk.v, in0=lg.v, scalar1=t8[:, 3:4], scalar2=None, op0=ALU.is_ge)
        P.op("dve", "tensor_scalar", out=t8[:, 7:8], in0=t8[:, 0:1], scalar1=-1.0, scalar2=None, op0=ALU.mult)
        P.op("act", "activation", out=ex.v, in_=lg.v, func=AF.Exp, bias=t8[:, 7:8])
        P.op("dve", "tensor_tensor", out=ex.v, in0=ex.v, in1=msk.v, op=ALU.mult)
        P.op("dve", "tensor_reduce", out=t8[:, 6:7], in_=ex.v, axis=AX.X, op=ALU.add)
        P.op("dve", "reciprocal", out=t8[:, 5:6], in_=t8[:, 6:7])
        P.op("dve", "tensor_scalar", out=Gt.v, in0=ex.v, scalar1=t8[:, 5:6], scalar2=None, op0=ALU.mult)
        P.dma("sp", G_d[sl, :], Gt.v)
    P.finish()
    return P


def build_moe(variants, groups, NE=32, name="moe"):
    P = Prog(name)
    nt = len(variants); nv = max(variants) + 1; T = nt * 128; D = 1024; KC = 8; F = 1024
    hT_d = P.dram("hT", [D, T], BF16); G_d = P.dram("G", [T, 32]); GT_d = P.dram("GT", [32, T]); x1_d = P.dram("x1", [T, D])
    w1_d = P.dram("w1", [32, D, 2 * F]); w2_d = P.dram("w2", [32, F, D])
    b1g_d = P.dram("b1g", [128, 32, KC]); b1l_d = P.dram("b1l", [128, 32, KC]); b2_d = P.dram("b2", [32, D])
    g3_d = P.dram("g3g", [128, D]); gate_d = P.dram("gate", [nv, 128, D])
    x2_d = P.dram("x2", [T, D], out=True)
    mg = max(len(g) for g in groups)
    g3 = P.sb("g3", [128, D]); P.dma("sp", g3.v, g3_d)
    GG = []
    for v in range(nv):
        gg = P.sb("GG%d" % v, [128, D]); P.dma("sp", gg.v, gate_d[v])
        P.op("dve", "tensor_tensor", out=gg.v, in0=gg.v, in1=g3.v, op=ALU.mult)
        GG.append(gg)
    b1g = P.sb("b1g", [128, 32, KC]); b1l = P.sb("b1l", [128, 32, KC]); P.dma("sp", b1g.v, b1g_d); P.dma("sp", b1l.v, b1l_d)
    b2 = P.sb("b2", [32, D]); P.dma("sp", b2.v, b2_d)
    stg = [P.sb("stg%d" % i, [128, 2 * F]) for i in range(2)]
    w1g = [P.sb("w1g%d" % i, [128, KC, F], BF16) for i in range(2)]
    w1l = [P.sb("w1l%d" % i, [128, KC, F], BF16) for i in range(2)]
    w2b = [P.sb("w2b%d" % i, [128, KC, D], BF16) for i in range(2)]
    hT = P.sb("hT", [128, KC, mg * 128], BF16)
    Gs = P.sb("Gs", [128, mg, 32]); GTs = P.sb("GTs", [32, mg * 128])
    yacc = P.sb("yacc", [128, mg, D])
    actT = P.sb("actT", [128, KC, 512], BF16)
    tg = [P.sb("tg%d" % i, [128, 512]) for i in range(2)]; tsg = [P.sb("tsg%d" % i, [128, 512]) for i in range(2)]
    tl = [P.sb("tl%d" % i, [128, 512]) for i in range(2)]
    xt = P.sb("xt", [128, D]); junk = P.sb("junk", [128, D]); st = P.sb("st", [128, 4])
    pg = [P.ps("pg%d" % i, [128, 512]) for i in range(2)]; pl = [P.ps("pl%d" % i, [128, 512]) for i in range(2)]
    py = [P.ps("py%d" % i, [128, 512]) for i in range(2)]
    sti = [0]

    def load_expert_steps(e, buf):
        steps = []
        for k in range(KC):
            def step(k=k):
                s = stg[sti[0] % 2]; sti[0] += 1
                P.dma("sp", s.v, w1_d[e, k * 128:(k + 1) * 128, :])
                sv = s.v.rearrange("p (f two) -> p f two", two=2)
                P.op("act", "activation", out=w1g[buf][:, k, :], in_=sv[:, :, 0], func=AF.Copy)
                P.op("act", "activation", out=w1l[buf][:, k, :], in_=sv[:, :, 1], func=AF.Copy)
                s = stg[sti[0] % 2]; sti[0] += 1
                P.dma("sp", s[:, 0:D], w2_d[e, k * 128:(k + 1) * 128, :])
                P.op("act", "activation", out=w2b[buf][:, k, :], in_=s[:, 0:D], func=AF.Copy)
            steps.append(step)
        return steps

    for grp in groups:
        ng = len(grp)
        t0 = grp[0]; tok0 = t0 * 128; ntok = ng * 128
        P.dma("sp", hT[:, :, 0:ntok], hT_d[:, tok0:tok0 + ntok].rearrange("(k p) t -> p k t", p=128))
        P.dma("sp", Gs[:, 0:ng, :], G_d[tok0:tok0 + ntok, :].rearrange("(g p) e -> p g e", p=128))
        P.dma("sp", GTs[:, 0:ntok], GT_d[:, tok0:tok0 + ntok])
        for i in range(ng):
            for n in range(2):
                P.op("pe", "matmul", out=py[n].v, lhsT=GTs[:, i * 128:(i + 1) * 128], rhs=b2[:, n * 512:(n + 1) * 512], start=True, stop=True)
                P.op("act", "activation", out=yacc[:, i, n * 512:(n + 1) * 512], in_=py[n].v, func=AF.Copy)
        for s in load_expert_steps(0, 0):
            s()
        blocks = [(b0, min(512, ntok - b0)) for b0 in range(0, ntok, 512)]
        ci = 0
        for e in range(NE):
            buf = e % 2
            nxt = load_expert_steps(e + 1, 1 - buf) if e + 1 < NE else []
            for bi, (b0, bn) in enumerate(blocks):
                for j in range(KC):
                    if bi == 0 and nxt:
                        nxt[j]()
                    c = ci % 2; ci += 1
                    for k in range(KC):
                        P.op("pe", "matmul", track=(k == KC - 1), out=pg[c][:, 0:bn], lhsT=w1g[buf][:, k, j * 128:(j + 1) * 128], rhs=hT[:, k, b0:b0 + bn], start=(k == 0), stop=(k == KC - 1))
                    for k in range(KC):
                        P.op("pe", "matmul", track=(k == KC - 1), out=pl[c][:, 0:bn], lhsT=w1l[buf][:, k, j * 128:(j + 1) * 128], rhs=hT[:, k, b0:b0 + bn], start=(k == 0), stop=(k == KC - 1))
                    P.op("dve", "tensor_scalar", out=tg[c][:, 0:bn], in0=pg[c][:, 0:bn], scalar1=b1g[:, e, j:j + 1], scalar2=7.0, op0=ALU.add, op1=ALU.min)
                    P.op("act", "activation", out=tsg[c][:, 0:bn], in_=tg[c][:, 0:bn], func=AF.Sigmoid, scale=1.702)
                    P.op("dve", "tensor_scalar", out=tl[c][:, 0:bn], in0=pl[c][:, 0:bn], scalar1=b1l[:, e, j:j + 1], scalar2=-7.0, op0=ALU.add, op1=ALU.max)
                    P.op("dve", "tensor_scalar", out=tl[c][:, 0:bn], in0=tl[c][:, 0:bn], scalar1=7.0, scalar2=1.0, op0=ALU.min, op1=ALU.add)
                    P.op("pool", "tensor_tensor", out=tg[c][:, 0:bn], in0=tg[c][:, 0:bn], in1=tsg[c][:, 0:bn], op=ALU.mult)
                    P.op("pool", "tensor_tensor", out=actT[:, j, 0:bn], in0=tg[c][:, 0:bn], in1=tl[c][:, 0:bn], op=ALU.mult)
                for i in range(b0 // 128, (b0 + bn) // 128):
                    for n in range(2):
                        ns = slice(n * 512, (n + 1) * 512)
                        for j in range(KC):
                            P.op("pe", "matmul", track=(j == KC - 1), out=py[n].v, lhsT=actT[:, j, i * 128 - b0:(i + 1) * 128 - b0], rhs=w2b[buf][:, j, ns], start=(j == 0), stop=(j == KC - 1))
                        P.op("dve", "scalar_tensor_tensor", out=yacc[:, i, ns], in0=py[n].v, scalar=Gs[:, i, e:e + 1], in1=yacc[:, i, ns], op0=ALU.mult, op1=ALU.add)
        for i in range(ng):
            t = grp[i]; v = variants[t]; sl = slice(t * 128, (t + 1) * 128)
            P.dma("sp", xt.v, x1_d[sl, :])
            P.op("act", "activation", out=junk.v, in_=yacc[:, i, :], func=AF.Square, accum_out=st[:, 0:1])
            rstd_of(P, st[:, 0:1], st[:, 2:3], st[:, 1:2], D)
            P.op("dve", "scalar_tensor_tensor", out=junk.v, in0=yacc[:, i, :], scalar=st[:, 2:3], in1=GG[v].v, op0=ALU.mult, op1=ALU.mult)
            P.op("pool", "tensor_tensor", out=xt.v, in0=junk.v, in1=xt.v, op=ALU.add)
            P.dma("sp", x2_d[sl, :], xt.v)
    P.finish()
    return P


def build_ada(name="ada"):
    P = Prog(name)
    NC_ = 1536
    w_d = P.dram("w", [1024, NC_]); b_d = P.dram("b", [2, NC_]); c_d = P.dram("cT", [128, 8, 2]); o_d = P.dram("mod", [2, NC_], out=True)
    w = P.sb("w", [128, 8, NC_]); P.dma("sp", w.v, w_d.rearrange("(k p) n -> p k n", p=128))
    b = P.sb("b", [2, NC_]); P.dma("sp", b.v, b_d)
    c = P.sb("c", [128, 8, 2]); P.dma("sp", c.v, c_d)
    o = P.sb("o", [2, NC_])
    P.op("act", "activation", out=c.v, in_=c.v, func=AF.Silu)
    pp = [P.ps("pp%d" % i, [2, 512]) for i in range(3)]
    for n in range(3):
        for k in range(8):
            P.op("pe", "matmul", track=(k == 7), out=pp[n].v, lhsT=c[:, k, :], rhs=w[:, k, n * 512:(n + 1) * 512], start=(k == 0), stop=(k == 7))
        P.op("dve", "tensor_tensor", out=o[:, n * 512:(n + 1) * 512], in0=pp[n].v, in1=b[:, n * 512:(n + 1) * 512], op=ALU.add)
    P.dma("sp", o_d, o.v)
    P.finish()
    return P


def build_gla(nchunks, name="gla"):
    P = Prog(name)
    Tt = nchunks * 64; CB = 8
    qT_d = P.dram("qT", [64, Tt]); kT_d = P.dram("kT", [64, Tt]); k_d = P.dram("k", [Tt, 64]); v_d = P.dram("v", [Tt, 128])
    aT_d = P.dram("aT", [17, Tt]); wa_d = P.dram("wa", [17, 64]); tri_d = P.dram("tri", [64, 64]); mu_d = P.dram("mu", [64, CB, 64])
    o_d = P.dram("o", [Tt, 128], out=True)
    wa = P.sb("wa", [17, 64]); P.dma("sp", wa.v, wa_d)
    tri = P.sb("tri", [64, 64]); P.dma("sp", tri.v, tri_d)
    mu = P.sb("mu", [64, CB, 64]); P.dma("sp", mu.v, mu_d)
    one = P.sb("one", [64, 1]); P.op("dve", "memset", ap=one.v, constant=1.0)
    S = [P.sb("S%d" % i, [64, 128]) for i in range(2)]
    P.op("dve", "memset", ap=S[0].v, constant=0.0)
    tmp = [P.sb("tmp%d" % i, [64, 128]) for i in range(2)]
    L = lambda nm, shp: [P.sb("%s%d" % (nm, i), shp) for i in range(2)]
    qTb = L("qTb", [64, CB * 64]); kTb = L("kTb", [64, CB * 64]); aTb = L("aTb", [17, CB * 64]); kb = L("kb", [64, CB, 64]); vb = L("vb", [64, CB, 128])
    le = L("le", [64, CB, 64]); E1 = L("E1", [64, CB * 64]); E2 = L("E2", [64, CB * 64]); E3 = L("E3", [64, CB, 64])
    qs = L("qs", [64, CB * 64]); ks = L("ks", [64, CB * 64]); kd = L("kd", [64, CB, 64]); ATm = L("ATm", [64, CB, 64]); ob = L("ob", [64, CB, 128])
    pXA = P.ps("pXA", [64, CB, 64]); pb = P.ps("pb", [64, CB, 64]); pbT = P.ps("pbT", [64, CB, 64]); pA = P.ps("pA", [64, CB, 64])
    pKV = P.ps("pKV", [64, CB, 128]); po = P.ps("po", [64, CB, 128])
    cur = 0; ti = 0
    blocks = [(c0, min(CB, nchunks - c0)) for c0 in range(0, nchunks, CB)]
    for bi, (c0, n) in enumerate(blocks):
        u = bi % 2; t0 = c0 * 64; W = n * 64
        P.dma("sp", qTb[u][:, 0:W], qT_d[:, t0:t0 + W]); P.dma("sp", kTb[u][:, 0:W], kT_d[:, t0:t0 + W]); P.dma("sp", aTb[u][:, 0:W], aT_d[:, t0:t0 + W])
        P.dma("sp", kb[u][:, 0:n, :], k_d[t0:t0 + W, :].rearrange("(c p) d -> p c d", p=64))
        P.dma("sp", vb[u][:, 0:n, :], v_d[t0:t0 + W, :].rearrange("(c p) d -> p c d", p=64))
        for c in range(n):
            P.op("pe", "matmul", out=pXA[:, c, :], lhsT=aTb[u][:, c * 64:(c + 1) * 64], rhs=wa.v, start=True, stop=True, track=(c == n - 1))
        P.op("act", "activation", out=le[u][:, 0:n, :], in_=pXA[:, 0:n, :], func=AF.Exp, scale=-1.0)
        P.op("act", "activation", out=le[u][:, 0:n, :], in_=le[u][:, 0:n, :], func=AF.Ln, bias=one[:, 0:1])
        for c in range(n):
            P.op("pe", "matmul", out=pb[:, c, :], lhsT=tri.v, rhs=le[u][:, c, :], start=True, stop=True, track=False)
            P.op("pe", "matmul", out=pbT[:, c, :], lhsT=le[u][:, c, :], rhs=tri.v, start=True, stop=True, track=(c == n - 1))
        pbTf = pbT.v.rearrange("p c t -> p (c t)")
        P.op("act", "activation", out=E1[u][:, 0:W], in_=pbTf[:, 0:W], func=AF.Exp)
        P.op("act", "activation", out=E2[u][:, 0:W], in_=pbTf[:, 0:W], func=AF.Exp, scale=-1.0)
        P.op("act", "activation", out=E3[u][:, 0:n, :], in_=pb[:, 0:n, :], func=AF.Exp, scale=-1.0)
        P.op("dve", "scalar_tensor_tensor", out=qs[u][:, 0:W], in0=qTb[u][:, 0:W], scalar=0.125, in1=E1[u][:, 0:W], op0=ALU.mult, op1=ALU.mult)
        P.op("dve", "tensor_tensor", out=ks[u][:, 0:W], in0=kTb[u][:, 0:W], in1=E2[u][:, 0:W], op=ALU.mult)
        P.op("pool", "tensor_tensor", out=kd[u][:, 0:n, :], in0=kb[u][:, 0:n, :], in1=E3[u][:, 0:n, :], op=ALU.mult)
        for c in range(n):
            cs_ = slice(c * 64, (c + 1) * 64)
            P.op("pe", "matmul", out=pA[:, c, :], lhsT=ks[u][:, cs_], rhs=qs[u][:, cs_], start=True, stop=True, track=(c == n - 1))
        P.op("dve", "tensor_tensor", out=ATm[u][:, 0:n, :], in0=pA[:, 0:n, :], in1=mu[:, 0:n, :], op=ALU.mult)
        for c in range(n):
            P.op("pe", "matmul", out=pKV[:, c, :], lhsT=kd[u][:, c, :], rhs=vb[u][:, c, :], start=True, stop=True, track=(c == n - 1))
        for c in range(n):
            cs_ = slice(c * 64, (c + 1) * 64)
            P.op("pe", "matmul", out=po[:, c, :], lhsT=qs[u][:, cs_], rhs=S[cur].v, start=True, stop=False, track=False)
            P.op("pe", "matmul", out=po[:, c, :], lhsT=ATm[u][:, c, :], rhs=vb[u][:, c, :], start=False, stop=True)
            tb = tmp[ti % 2]; ti += 1
            P.op("dve", "tensor_tensor", out=tb.v, in0=S[cur].v, in1=pKV[:, c, :], op=ALU.add)
            P.op("dve", "tensor_scalar", out=S[1 - cur].v, in0=tb.v, scalar1=E1[u][:, c * 64 + 63:c * 64 + 64], scalar2=None, op0=ALU.mult)
            cur = 1 - cur
        P.op("act", "activation", out=ob[u][:, 0:n, :], in_=po[:, 0:n, :], func=AF.Copy)
        P.dma("sp", o_d[t0:t0 + W, :].rearrange("(c p) d -> p c d", p=64), ob[u][:, 0:n, :])
    P.finish()
    return P


def build_mla(groups, NK, name="mla"):
    P = Prog(name)
    NQ = max(q0 + nq for q0, nq, _ in groups)
    NB = NK // 128
    qT_d = P.dram("qT", [193, NQ]); kT_d = P.dram("kT", [193, NK]); v_d = P.dram("V", [NK, 129]); o_d = P.dram("O", [NQ, 128], out=True)
    idf, idb = ident(P)
    kTa = P.sb("kTa", [128, NK], BF16); kTb = P.sb("kTb", [65, NK], BF16); Vb = P.sb("Vb", [128, NB, 129], BF16)
    stg = [P.sb("stg%d" % i, [128, 2048]) for i in range(2)]
    si = 0
    for c0 in range(0, NK, 2048):
        cn = min(2048, NK - c0)
        for (r0, rn, dst) in ((0, 128, kTa), (128, 65, kTb)):
            s = stg[si % 2]; si += 1
            P.dma("sp", s[0:rn, 0:cn], kT_d[r0:r0 + rn, c0:c0 + cn])
            P.op("pool" if si % 2 else "act", "tensor_copy" if si % 2 else "copy", out=dst[0:rn, c0:c0 + cn], in_=s[0:rn, 0:cn])
    VB = 15
    for b0 in range(0, NB, VB):
        bn = min(VB, NB - b0)
        s = stg[si % 2]; si += 1
        sv = s[:, 0:bn * 129].rearrange("p (b f) -> p b f", f=129)
        P.dma("sp", sv, v_d[b0 * 128:(b0 + bn) * 128, :].rearrange("(b p) f -> p b f", p=128))
        P.op("pool" if si % 2 else "act", "tensor_copy" if si % 2 else "copy", out=Vb[:, b0:b0 + bn, :], in_=sv)
    qst = [P.sb("qst%d" % i, [128, 512]) for i in range(2)]
    qa = [P.sb("qa%d" % i, [128, 512], BF16) for i in range(2)]; qb = [P.sb("qb%d" % i, [65, 512], BF16) for i in range(2)]
    mx = P.sb("mx", [128, 4, 40]); negm = P.sb("negm", [128, 4]); Z = P.sb("Z", [128, 65])
    P.op("dve", "memset", ap=Z.v, constant=0.0)
    pt = [P.sb("pt%d" % i, [128, 512], BF16) for i in range(2)]
    rl = P.sb("rl", [128, 4]); ot = [P.sb("ot%d" % i, [128, 4, 128]) for i in range(2)]
    ps1 = [P.ps("ps1_%d" % i, [128, 512]) for i in range(2)]; pst = [P.ps("pst%d" % i, [128, 512]) for i in range(2)]
    pO = [P.ps("pO%d" % i, [128, 129]) for i in range(4)]
    scale = 192.0 ** -0.5
    c1 = 0; c2 = 0
    for gi, (q0, nq, nkeys) in enumerate(groups):
        u = gi % 2; nqt = nq // 128
        P.dma("sp", qst[0][:, 0:nq], qT_d[0:128, q0:q0 + nq]); P.dma("sp", qst[1][0:64, 0:nq], qT_d[128:192, q0:q0 + nq])
        P.op("pool", "tensor_scalar", out=qa[u][:, 0:nq], in0=qst[0][:, 0:nq], scalar1=scale, scalar2=None, op0=ALU.mult)
        P.op("pool", "tensor_scalar", out=qb[u][0:64, 0:nq], in0=qst[1][0:64, 0:nq], scalar1=scale, scalar2=None, op0=ALU.mult)
        kblocks = [(k0, min(512, nkeys - k0)) for k0 in range(0, nkeys, 512)]
        for qt in range(nqt):
            qs_ = slice(qt * 128, (qt + 1) * 128)
            for bi, (k0, kn) in enumerate(kblocks):
                pb = ps1[c1 % 2]; c1 += 1
                P.op("pe", "matmul", track=False, out=pb[:, 0:kn], lhsT=qa[u][:, qs_], rhs=kTa[:, k0:k0 + kn], start=True, stop=False)
                P.op("pe", "matmul", out=pb[:, 0:kn], lhsT=qb[u][0:64, qs_], rhs=kTb[0:64, k0:k0 + kn], start=False, stop=True)
                P.op("dve", "tensor_reduce", out=mx[:, qt, bi:bi + 1], in_=pb[:, 0:kn], axis=AX.X, op=ALU.max)
            P.op("dve", "tensor_reduce", out=negm[:, qt:qt + 1], in_=mx[:, qt, 0:len(kblocks)], axis=AX.X, op=ALU.max)
            P.op("dve", "tensor_scalar", out=Z[:, 64:65], in0=negm[:, qt:qt + 1], scalar1=-1.0, scalar2=None, op0=ALU.mult)
            pz = ps1[c1 % 2]; c1 += 1
            P.op("pe", "matmul", out=pz[0:65, 0:128], lhsT=Z.v, rhs=idf.v, start=True, stop=True)
            P.op("dve", "tensor_copy", out=qb[u][64:65, qs_], in_=pz[64:65, 0:128])
        nkb = nkeys // 128
        for kb_ in range(nkb):
            ks_ = slice(kb_ * 128, (kb_ + 1) * 128)
            pb = pst[c2 % 2]; ptb = pt[c2 % 2]; c2 += 1
            P.op("pe", "matmul", track=False, out=pb[:, 0:nq], lhsT=kTa[:, ks_], rhs=qa[u][:, 0:nq], start=True, stop=False)
            P.op("pe", "matmul", out=pb[:, 0:nq], lhsT=kTb[0:65, ks_], rhs=qb[u][0:65, 0:nq], start=False, stop=True)
            P.op("act", "activation", out=ptb[:, 0:nq], in_=pb[:, 0:nq], func=AF.Exp)
            for qt in range(nqt):
                P.op("pe", "matmul", track=(kb_ == nkb - 1), out=pO[qt].v, lhsT=ptb[:, qt * 128:(qt + 1) * 128], rhs=Vb[:, kb_, :], start=(kb_ == 0), stop=(kb_ == nkb - 1))
        for qt in range(nqt):
            P.op("dve", "reciprocal", out=rl[:, qt:qt + 1], in_=pO[qt][:, 128:129])
            P.op("dve", "tensor_scalar", out=ot[u][:, qt, :], in0=pO[qt][:, 0:128], scalar1=rl[:, qt:qt + 1], scalar2=None, op0=ALU.mult)
        P.dma("sp", o_d[q0:q0 + nq, :].rearrange("(t p) d -> p t d", p=128), ot[u][:, 0:nqt, :])
    P.finish()
    return P


def build_na(nrows, edge_rows, name="na"):
    P = Prog(name)
    T = nrows * 64; ne = max(1, len(edge_rows))
    qT_d = P.dram("qT", [1024, T]); kw_d = P.dram("kwin", [nrows, 1024, 512]); vw_d = P.dram("vwin", [nrows, 512, 1024])
    kc_d = P.dram("kcT", [1024, 256]); vc_d = P.dram("vc", [256, 1024])
    bi_d = P.dram("Bint", [128, 8, 512]); be_d = P.dram("Bedge", [ne, 128, 8, 512])
    oT_d = P.dram("oT", [1024, T], out=True)
    idf, idb = ident(P)
    ones = P.sb("ones", [128, 128], BF16); P.op("pool", "memset", ap=ones.v, constant=1.0)
    kst = P.sb("kst", [128, 8, 512]); vst = P.sb("vst", [128, 4, 1024])
    kb = [P.sb("kb%d" % i, [128, 8, 512], BF16) for i in range(2)]; vb = [P.sb("vb%d" % i, [128, 4, 1024], BF16) for i in range(2)]
    kcb = P.sb("kcb", [128, 8, 256], BF16); vcb = P.sb("vcb", [128, 2, 1024], BF16)
    P.dma("sp", kst[:, :, 0:256], kc_d.rearrange("(k p) n -> p k n", p=128)); P.op("pool", "tensor_copy", out=kcb.v, in_=kst[:, :, 0:256])
    P.dma("sp", vst[:, 0:2, :], vc_d.rearrange("(b p) f -> p b f", p=128)); P.op("pool", "tensor_copy", out=vcb.v, in_=vst[:, 0:2, :])
    Bi = P.sb("Bi", [128, 8, 512]); P.dma("sp", Bi.v, bi_d)
    Be = [P.sb("Be%d" % i, [128, 8, 512]) for i in range(2)]
    qrow = [P.sb("qrow%d" % i, [128, 8, 64]) for i in range(2)]
    QBD = [P.sb("QBD%d" % i, [128, 128], BF16) for i in range(2)]
    for z in QBD:
        P.op("pool", "memset", ap=z.v, constant=0.0)
    Sb = [P.sb("Sb%d" % i, [128, 768]) for i in range(2)]; Pm = [P.sb("Pm%d" % i, [128, 768], BF16) for i in range(2)]
    PT = [P.sb("PT%d" % i, [128, 6, 128], BF16) for i in range(2)]
    sm = P.sb("sm", [128, 4]); rl = P.sb("rl", [128, 128]); orow = [P.sb("orow%d" % i, [128, 8, 64]) for i in range(2)]
    pS = [P.ps("pS%d" % i, [128, 1024]) for i in range(2)]
    pPT = P.ps("pPT", [128, 6, 128], BF16); pO = P.ps("pO", [128, 128]); pL = P.ps("pL", [128, 128])
    it = 0; nbe = 0
    for i in range(nrows):
        u = i % 2
        P.dma("sp", kst.v, kw_d[i].rearrange("(k p) n -> p k n", p=128)); P.op("pool", "tensor_copy", out=kb[u].v, in_=kst.v)
        P.dma("sp", vst.v, vw_d[i].rearrange("(b p) f -> p b f", p=128)); P.op("pool", "tensor_copy", out=vb[u].v, in_=vst.v)
        P.dma("sp", qrow[u].v, qT_d[:, i * 64:(i + 1) * 64].rearrange("(k p) t -> p k t", p=128))
        if i in edge_rows:
            B = Be[nbe % 2]; nbe += 1
            P.dma("sp", B.v, be_d[edge_rows[i]])
        else:
            B = Bi
        for hp in range(8):
            w = it % 2; it += 1
            P.op("pool", "tensor_scalar", out=QBD[w][0:64, 0:64], in0=qrow[u][0:64, hp, :], scalar1=0.125, scalar2=None, op0=ALU.mult)
            P.op("pool", "tensor_scalar", out=QBD[w][64:128, 64:128], in0=qrow[u][64:128, hp, :], scalar1=0.125, scalar2=None, op0=ALU.mult)
            P.op("pe", "matmul", out=pS[w][:, 0:512], lhsT=QBD[w].v, rhs=kb[u][:, hp, :], start=True, stop=True, track=False)
            P.op("pe", "matmul", out=pS[w][:, 512:768], lhsT=QBD[w].v, rhs=kcb[:, hp, :], start=True, stop=True)
            P.op("dve", "tensor_tensor", out=Sb[w][:, 0:512], in0=pS[w][:, 0:512], in1=B[:, hp, :], op=ALU.add)
            P.op("act", "activation", out=Sb[w][:, 512:768], in_=pS[w][:, 512:768], func=AF.Copy)
            P.op("dve", "tensor_reduce", out=sm[:, 0:1], in_=Sb[w].v, axis=AX.X, op=ALU.max)
            P.op("dve", "tensor_scalar", out=sm[:, 1:2], in0=sm[:, 0:1], scalar1=-1.0, scalar2=None, op0=ALU.mult)
            P.op("act", "activation", out=Pm[w].v, in_=Sb[w].v, func=AF.Exp, bias=sm[:, 1:2])
            for b in range(6):
                P.op("pe", "transpose", out=pPT[:, b, :], in_=Pm[w][:, b * 128:(b + 1) * 128], identity=idb.v, track=(b == 5))
            P.op("dve", "tensor_copy", out=PT[w].v, in_=pPT.v)
            hs = slice(hp * 128, (hp + 1) * 128)
            for b in range(6):
                vblk = vb[u][:, b, hs] if b < 4 else vcb[:, b - 4, hs]
                P.op("pe", "matmul", out=pO.v, lhsT=vblk, rhs=PT[w][:, b, :], start=(b == 0), stop=(b == 5), track=(b == 5))
            for b in range(6):
                P.op("pe", "matmul", out=pL.v, lhsT=ones.v, rhs=PT[w][:, b, :], start=(b == 0), stop=(b == 5), track=(b == 5))
            P.op("dve", "reciprocal", out=rl.v, in_=pL.v)
            P.op("dve", "tensor_tensor", out=orow[u][0:64, hp, :], in0=pO[0:64, 0:64], in1=rl[0:64, 0:64], op=ALU.mult)
            P.op("dve", "tensor_tensor", out=orow[u][64:128, hp, :], in0=pO[64:128, 64:128], in1=rl[64:128, 64:128], op=ALU.mult)
        P.dma("sp", oT_d[:, i * 64:(i + 1) * 64].rearrange("(k p) t -> p k t", p=128), orow[u].v)
    P.finish()
    return P


def na_bias_table(rpb, d):
    col = np.arange(64)
    c0 = np.clip(col - 8, 0, 48)
    kcol = np.arange(64)
    inwin = (kcol[None, :] >= c0[:, None]) & (kcol[None, :] < c0[:, None] + 16)
    cidx = np.clip(kcol[None, :] - col[:, None] + 15, 0, 30)
    ridx = np.arange(8) - d + 7
    t = rpb[:, ridx][:, :, cidx]
    t = np.where(inwin[None, None], t, np.float32(-30000.0)).astype(np.float32)
    t = t.transpose(0, 2, 1, 3).reshape(8, 2, 64, 512)
    return np.ascontiguousarray(t.transpose(1, 2, 0, 3).reshape(128, 8, 512))


TALL = 16640
NOWN = 22
VOWN = [0] * 20 + [1] * 2
TOWN = NOWN * 128


def modcols(P, modcol, l, i, g_d_view, name):
    t = P.sb(name, [128, 2, 8])
    for s in range(2):
        P.op("dve", "scalar_tensor_tensor", out=t[:, s, :], in0=modcol[:, l, i * 8:(i + 1) * 8, s], scalar=1.0, in1=g_d_view, op0=ALU.add, op1=ALU.mult)
    return t


def stage_ada(P, adaw_d, bcol_d, brow_d, cT_d, modcol, gates_s):
    P.begin()
    c = P.sb("c", [128, 8, 2]); P.dma("sp", c.v, cT_d)
    P.op("act", "activation", out=c.v, in_=c.v, func=AF.Silu)
    ones = P.sb("ones", [128, 128]); P.op("dve", "memset", ap=ones.v, constant=1.0)
    crep = P.sb("crep", [128, 2, 8, 128])
    for s in range(2):
        for k in range(8):
            P.op("dve", "tensor_scalar", out=crep[:, s, k, :], in0=ones.v, scalar1=c[:, k, s:s + 1], scalar2=None, op0=ALU.mult)
    bcol = P.sb("bcol", [128, 2, 48]); P.dma("sp", bcol.v, bcol_d)
    wp = [P.sb("wp%d" % i, [128, 8, 512]) for i in range(2)]
    brow = P.sb("brow", [128, 512]); grow = [P.sb("grow%d" % i, [128, 512]) for i in range(2)]
    pc = [P.ps("pc%d" % i, [128, 2]) for i in range(2)]; pr = [P.ps("pr%d" % i, [128, 512]) for i in range(2)]
    ci = 0; ri = 0
    for l in range(2):
        for piece in range(12):
            w = wp[piece % 2]
            P.dma("sp", w.v, adaw_d[l][:, piece * 512:(piece + 1) * 512].rearrange("(k p) n -> p k n", p=128))
            for jj in range(4):
                j = piece * 4 + jj
                p_ = pc[ci % 2]; ci += 1
                for k in range(8):
                    P.op("pe", "matmul", track=(k == 7), out=p_.v, lhsT=w[:, k, jj * 128:(jj + 1) * 128], rhs=c[:, k, :], start=(k == 0), stop=(k == 7))
                P.op("dve", "tensor_scalar", out=modcol[:, l, j, :], in0=p_.v, scalar1=bcol[:, l, j:j + 1], scalar2=None, op0=ALU.add)
            if piece in (4, 5, 10, 11):
                g = 0 if piece < 6 else 1; half = piece % 2
                P.dma("sp", brow.v, brow_d[l, :, piece * 512:(piece + 1) * 512])
                for s in range(2):
                    p_ = pr[ri % 2]; gr = grow[ri % 2]; ri += 1
                    for k in range(8):
                        P.op("pe", "matmul", track=(k == 7), out=p_.v, lhsT=crep[:, s, k, :], rhs=w[:, k, :], start=(k == 0), stop=(k == 7))
                    P.op("dve", "tensor_tensor", out=gr.v, in0=p_.v, in1=brow.v, op=ALU.add)
                    P.dma("sp", gates_s[l, g, s, :, half * 512:(half + 1) * 512], gr.v)
    P.end()


def stage_pro(P, idb, x_d, groups, gvar, gsc, gsh, D, wtm_d, ntm, tm_outs, wfm_d, nfm, fm_specs, rope=None, mla=None, tag="pro"):
    P.begin()
    KC = D // 128
    stg = [P.sb("wst%d" % i, [128, max(ntm, nfm, 1024)]) for i in range(2)]
    wtm = load_w_bf16(P, wtm_d, D, ntm, "wtm", stage=stg) if ntm else None
    wfm = load_w_bf16(P, wfm_d, D, nfm, "wfm", stage=stg) if nfm else None
    if mla:
        wuk = load_w_bf16(P, mla["wukv_d"], 128, 1024, "wuk", stage=stg)
        kvn = P.sb("kvn", [128, 1]); P.dma("sp", kvn.v, mla["kvn_d"])
        ckn = [P.sb("ckn%d" % i, [128, 128], BF16) for i in range(2)]
        ckT = [P.sb("ckT%d" % i, [128, 512], BF16) for i in range(2)]
        kst = [P.sb("kst%d" % i, [128, 512], BF16) for i in range(2)]; vst = [P.sb("vst%d" % i, [128, 512], BF16) for i in range(2)]
        pck = P.ps("pck", [128, 128], BF16)
    xs = [P.sb("x%d" % i, [128, D]) for i in range(2)]; junk = P.sb("junk", [128, D]); st = [P.sb("st%d" % i, [128, 8]) for i in range(2)]
    xn = [P.sb("xn%d" % i, [128, D], BF16) for i in range(2)]
    hT = [P.sb("hT%d" % i, [128, KC, 512], BF16) for i in range(2)]
    yt = [P.sb("yt%d" % i, [128, max(ntm, 1)]) for i in range(2)]
    ytb = [P.sb("ytb%d" % i, [128, max(ntm, 1)], BF16) for i in range(2)]
    fo = [P.sb("fo%d" % i, [128, 512]) for i in range(2)]; fob = [P.sb("fob%d" % i, [128, 512], BF16) for i in range(2)]
    pT = [P.ps("pT%d" % i, [128, KC, 128], BF16) for i in range(2)]
    py = [P.ps("py%d" % i, [128, 512]) for i in range(2)]; pf = [P.ps("pf%d" % i, [128, 512]) for i in range(2)]
    if rope:
        Ct = [P.sb("Ct%d" % i, [64, 512]) for i in range(2)]; St = [P.sb("St%d" % i, [64, 512]) for i in range(2)]
        r1 = P.sb("r1", [64, 512]); r2 = P.sb("r2", [64, 512]); rb_ = [P.sb("rb%d" % i, [64, 512], BF16) for i in range(2)]
    ti = 0; yi = 0; fi = 0
    for gi, (tok0, gt) in enumerate(groups):
        v = gvar[gi]; W = gt * 128; hg = hT[gi % 2]
        for t in range(gt):
            b = ti % 2; ti += 1
            r0 = tok0 + t * 128
            P.dma("sp", xs[b].v, x_d[r0:r0 + 128, :])
            P.op("act", "activation", out=junk.v, in_=xs[b].v, func=AF.Square, accum_out=st[b][:, 0:1])
            rstd_of(P, st[b][:, 0:1], st[b][:, 2:3], st[b][:, 1:2], D)
            P.op("dve", "tensor_scalar", out=xn[b].v, in0=xs[b].v, scalar1=st[b][:, 2:3], scalar2=None, op0=ALU.mult)
            for k in range(KC):
                P.op("pe", "transpose", out=pT[b][:, k, :], in_=xn[b][:, k * 128:(k + 1) * 128], identity=idb.v)
            for k in range(KC):
                P.op("dve", "tensor_scalar", out=hg[:, k, t * 128:(t + 1) * 128], in0=pT[b][:, k, :], scalar1=gsc[:, v, k:k + 1], scalar2=gsh[:, v, k:k + 1], op0=ALU.mult, op1=ALU.add)
            if ntm:
                for c0 in range(0, ntm, 512):
                    cn = min(512, ntm - c0); pb = py[yi % 2]; yi += 1
                    for k in range(KC):
                        P.op("pe", "matmul", track=(k == KC - 1), out=pb[:, 0:cn], lhsT=hg[:, k, t * 128:(t + 1) * 128], rhs=wtm[:, k, c0:c0 + cn], start=(k == 0), stop=(k == KC - 1))
                    P.op("act", "activation", out=yt[b][:, c0:c0 + cn], in_=pb[:, 0:cn], func=AF.Copy)
                for (c0, n, ap, dt) in tm_outs:
                    if dt == BF16:
                        P.op("pool", "tensor_copy", out=ytb[b][:, c0:c0 + n], in_=yt[b][:, c0:c0 + n])
                        P.dma("sp", ap[r0:r0 + 128, :], ytb[b][:, c0:c0 + n])
                    else:
                        P.dma("sp", ap[r0:r0 + 128, :], yt[b][:, c0:c0 + n])
            if mla:
                cc = mla["col"]
                P.op("act", "activation", out=junk[:, 0:128], in_=yt[b][:, cc:cc + 128], func=AF.Square, accum_out=st[b][:, 4:5])
                rstd_of(P, st[b][:, 4:5], st[b][:, 6:7], st[b][:, 5:6], 128)
                P.op("dve", "tensor_scalar", out=ckn[b].v, in0=yt[b][:, cc:cc + 128], scalar1=st[b][:, 6:7], scalar2=None, op0=ALU.mult)
                P.op("pe", "transpose", out=pck.v, in_=ckn[b].v, identity=idb.v)
                P.op("dve", "tensor_scalar", out=ckT[gi % 2][:, t * 128:(t + 1) * 128], in0=pck.v, scalar1=kvn[:, 0:1], scalar2=None, op0=ALU.mult)
                pb = py[yi % 2]; yi += 1
                P.op("pe", "matmul", out=pb.v, lhsT=ckT[gi % 2][:, t * 128:(t + 1) * 128], rhs=wuk[:, 0, 512:1024], start=True, stop=True)
                vb_ = vst[ti % 2]
                P.op("act", "activation", out=vb_.v, in_=pb.v, func=AF.Copy)
                P.dma("sp", mla["V_s"][r0:r0 + 128, :], vb_.v)
        for (c0, m, ap, dt, scale) in fm_specs:
            pb = pf[fi % 2]; f32t = fo[fi % 2]; bft = fob[fi % 2]; fi += 1
            for k in range(KC):
                P.op("pe", "matmul", track=(k == KC - 1), out=pb[0:m, 0:W], lhsT=wfm[:, k, c0:c0 + m], rhs=hg[:, k, 0:W], start=(k == 0), stop=(k == KC - 1))
            dst = bft if dt == BF16 else f32t
            P.op("act", "activation", out=dst[0:m, 0:W], in_=pb[0:m, 0:W], func=AF.Copy, scale=float(scale))
            P.dma("sp", ap[:, tok0:tok0 + W], dst[0:m, 0:W])
        if rope:
            ckr, ckrs, ap, C_d, S_d = rope
            u = gi % 2
            P.dma("sp", Ct[u][:, 0:W], C_d[:, tok0:tok0 + W]); P.dma("sp", St[u][:, 0:W], S_d[:, tok0:tok0 + W])
            pa = pf[fi % 2]; fi += 1; pb = pf[fi % 2]; fi += 1
            for k in range(KC):
                P.op("pe", "matmul", track=(k == KC - 1), out=pa[0:64, 0:W], lhsT=wfm[:, k, ckr:ckr + 64], rhs=hg[:, k, 0:W], start=(k == 0), stop=(k == KC - 1))
            for k in range(KC):
                P.op("pe", "matmul", track=(k == KC - 1), out=pb[0:64, 0:W], lhsT=wfm[:, k, ckrs:ckrs + 64], rhs=hg[:, k, 0:W], start=(k == 0), stop=(k == KC - 1))
            P.op("dve", "tensor_tensor", out=r1[:, 0:W], in0=pa[0:64, 0:W], in1=Ct[u][:, 0:W], op=ALU.mult)
            P.op("dve", "tensor_tensor", out=r2[:, 0:W], in0=pb[0:64, 0:W], in1=St[u][:, 0:W], op=ALU.mult)
            P.op("pool", "tensor_tensor", out=rb_[u][:, 0:W], in0=r1[:, 0:W], in1=r2[:, 0:W], op=ALU.add)
            P.dma("sp", ap[:, tok0:tok0 + W], rb_[u][:, 0:W])
        if mla:
            for h in range(4):
                pb = pf[fi % 2]; kb_ = kst[fi % 2]; fi += 1
                P.op("pe", "matmul", out=pb[:, 0:W], lhsT=wuk[:, 0, h * 128:(h + 1) * 128], rhs=ckT[gi % 2][:, 0:W], start=True, stop=True)
                P.op("act", "activation", out=kb_[:, 0:W], in_=pb[:, 0:W], func=AF.Copy)
                P.dma("sp", mla["kT_s"][h, :, tok0:tok0 + W], kb_[:, 0:W])
    P.end()


def conv_items(w1_d, w2_d, b1_d, w1bf, w2bf, b1bf, l):
    items = [(b1_d[l], b1bf[l], 32, 2048, False)]
    for e in range(32):
        for k in range(8):
            ks = slice(k * 128, (k + 1) * 128)
            items.append((w1_d[l, e, ks, :], w1bf[l, e, ks, :], 128, 2048, True))
            items.append((w2_d[l, e, ks, :], w2bf[l, e, ks, :], 128, 1024, False))
    return items


def conv_steps(P, items):
    R = 6
    pools = {2048: ([P.sb("cstA%d" % i, [128, 2048]) for i in range(R)], [P.sb("cbfA%d" % i, [128, 2048], BF16) for i in range(R)]),
             1024: ([P.sb("cstB%d" % i, [128, 1024]) for i in range(R)], [P.sb("cbfB%d" % i, [128, 1024], BF16) for i in range(R)])}
    pcnt = {2048: 0, 1024: 0}
    state = dict(nxt=0, loaded=[], cast=[])

    def tick(k):
        for (i, dst, rows, n, de) in state["cast"]:
            P.dma("pool", dst, pools[n][1][i][0:rows, :])
        state["cast"] = []
        for (i, dst, rows, n, de) in state["loaded"]:
            if de:
                sv = pools[n][0][i].v.rearrange("p (f two) -> p f two", two=2)
                P.op("act", "activation", out=pools[n][1][i][0:rows, 0:n // 2], in_=sv[0:rows, :, 0], func=AF.Copy)
                P.op("act", "activation", out=pools[n][1][i][0:rows, n // 2:n], in_=sv[0:rows, :, 1], func=AF.Copy)
            else:
                P.op("act", "activation", out=pools[n][1][i][0:rows, :], in_=pools[n][0][i][0:rows, :], func=AF.Copy)
            state["cast"].append((i, dst, rows, n, de))
        state["loaded"] = []
        for _ in range(k):
            if state["nxt"] < len(items):
                src, dst, rows, n, de = items[state["nxt"]]; state["nxt"] += 1
                i = pcnt[n] % R; pcnt[n] += 1
                P.dma("pool", pools[n][0][i][0:rows, :], src)
                state["loaded"].append((i, dst, rows, n, de))
        return state["nxt"] < len(items) or state["loaded"] or state["cast"]
    return tick


def stage_gla(P, scans, wa_d, tri_d, mu_d, extra=None, per_block=4):
    P.begin()
    CB = 8
    tri = P.sb("tri", [64, 64]); P.dma("sp", tri.v, tri_d)
    mu = P.sb("mu", [64, CB, 64]); P.dma("sp", mu.v, mu_d)
    one = P.sb("one", [64, 1]); P.op("dve", "memset", ap=one.v, constant=1.0)
    wa = P.sb("wa", [17, 8, 64]); P.dma("sp", wa.v, wa_d)
    S = [P.sb("S%d" % i, [64, 128]) for i in range(2)]
    tmp = [P.sb("tmp%d" % i, [64, 128]) for i in range(2)]
    L = lambda nm, shp: [P.sb("%s%d" % (nm, i), shp) for i in range(2)]
    qTb = L("qTb", [64, CB * 64]); kTb = L("kTb", [64, CB * 64]); aTb = L("aTb", [17, CB * 64]); kb = L("kb", [64, CB, 64]); vb = L("vb", [64, CB, 128])
    for a in aTb:
        P.op("dve", "memset", ap=a.v, constant=1.0)
    le = L("le", [64, CB, 64]); E1 = L("E1", [64, CB * 64]); E2 = L("E2", [64, CB * 64]); E3 = L("E3", [64, CB, 64])
    qs = L("qs", [64, CB * 64]); ks = L("ks", [64, CB * 64]); kd = L("kd", [64, CB, 64]); ATm = L("ATm", [64, CB, 64]); ob = L("ob", [64, CB, 128])
    pXA = P.ps("pXA", [64, CB, 64]); pb = P.ps("pb", [64, CB, 64]); pbT = P.ps("pbT", [64, CB, 64]); pA = P.ps("pA", [64, CB, 64])
    pKV = P.ps("pKV", [64, CB, 128]); po = P.ps("po", [64, CB, 128])
    blocks = [(0, 4)] + [(4 + 8 * i, 8) for i in range(32)]
    bi = 0; ti = 0
    tick = extra(P) if extra else None
    for (qT_d, kT_d, k_d, v_d, aT_d, wi, o_d) in scans:
        cur = 0
        P.op("dve", "memset", ap=S[0].v, constant=0.0)
        for (c0, n) in blocks:
            u = bi % 2; bi += 1; t0 = c0 * 64; W = n * 64
            if tick:
                tick(per_block)
            P.dma("sp", qTb[u][:, 0:W], qT_d[:, t0:t0 + W]); P.dma("sp", kTb[u][:, 0:W], kT_d[:, t0:t0 + W]); P.dma("sp", aTb[u][0:16, 0:W], aT_d[:, t0:t0 + W])
            P.dma("sp", kb[u][:, 0:n, :], k_d[t0:t0 + W, :].rearrange("(c p) d -> p c d", p=64))
            P.dma("sp", vb[u][:, 0:n, :], v_d[t0:t0 + W, :].rearrange("(c p) d -> p c d", p=64))
            for c in range(n):
                P.op("pe", "matmul", out=pXA[:, c, :], lhsT=aTb[u][:, c * 64:(c + 1) * 64], rhs=wa[:, wi, :], start=True, stop=True, track=(c == n - 1))
            P.op("act", "activation", out=le[u][:, 0:n, :], in_=pXA[:, 0:n, :], func=AF.Exp, scale=-1.0)
            P.op("act", "activation", out=le[u][:, 0:n, :], in_=le[u][:, 0:n, :], func=AF.Ln, bias=one[:, 0:1])
            for c in range(n):
                P.op("pe", "matmul", out=pb[:, c, :], lhsT=tri.v, rhs=le[u][:, c, :], start=True, stop=True, track=False)
                P.op("pe", "matmul", out=pbT[:, c, :], lhsT=le[u][:, c, :], rhs=tri.v, start=True, stop=True, track=(c == n - 1))
            pbTf = pbT.v.rearrange("p c t -> p (c t)")
            P.op("act", "activation", out=E1[u][:, 0:W], in_=pbTf[:, 0:W], func=AF.Exp)
            P.op("act", "activation", out=E2[u][:, 0:W], in_=pbTf[:, 0:W], func=AF.Exp, scale=-1.0)
            P.op("act", "activation", out=E3[u][:, 0:n, :], in_=pb[:, 0:n, :], func=AF.Exp, scale=-1.0)
            P.op("dve", "scalar_tensor_tensor", out=qs[u][:, 0:W], in0=qTb[u][:, 0:W], scalar=0.125, in1=E1[u][:, 0:W], op0=ALU.mult, op1=ALU.mult)
            P.op("dve", "tensor_tensor", out=ks[u][:, 0:W], in0=kTb[u][:, 0:W], in1=E2[u][:, 0:W], op=ALU.mult)
            P.op("dve", "tensor_tensor", out=kd[u][:, 0:n, :], in0=kb[u][:, 0:n, :], in1=E3[u][:, 0:n, :], op=ALU.mult)
            for c in range(n):
                cs_ = slice(c * 64, (c + 1) * 64)
                P.op("pe", "matmul", out=pA[:, c, :], lhsT=ks[u][:, cs_], rhs=qs[u][:, cs_], start=True, stop=True, track=(c == n - 1))
            P.op("dve", "tensor_tensor", out=ATm[u][:, 0:n, :], in0=pA[:, 0:n, :], in1=mu[:, 0:n, :], op=ALU.mult)
            for c in range(n):
                P.op("pe", "matmul", out=pKV[:, c, :], lhsT=kd[u][:, c, :], rhs=vb[u][:, c, :], start=True, stop=True, track=(c == n - 1))
            for c in range(n):
                cs_ = slice(c * 64, (c + 1) * 64)
                P.op("pe", "matmul", out=po[:, c, :], lhsT=qs[u][:, cs_], rhs=S[cur].v, start=True, stop=False, track=False)
                P.op("pe", "matmul", out=po[:, c, :], lhsT=ATm[u][:, c, :], rhs=vb[u][:, c, :], start=False, stop=True)
                tb = tmp[ti % 2]; ti += 1
                P.op("dve", "tensor_tensor", out=tb.v, in0=S[cur].v, in1=pKV[:, c, :], op=ALU.add)
                P.op("dve", "tensor_scalar", out=S[1 - cur].v, in0=tb.v, scalar1=E1[u][:, c * 64 + 63:c * 64 + 64], scalar2=None, op0=ALU.mult)
                cur = 1 - cur
            P.op("act", "activation", out=ob[u][:, 0:n, :], in_=po[:, 0:n, :], func=AF.Copy)
            P.dma("sp", o_d[t0:t0 + W, :].rearrange("(c p) d -> p c d", p=64), ob[u][:, 0:n, :])
    while tick and tick(per_block):
        pass
    P.end()


def stage_mla_full(P, idf, idb, cq_s, qn_d, wuq_d, wuqs_d, Cq_d, Sq_d, kT_s, krT_s, V_s, om_s):
    P.begin()
    NK = TALL; NB = NK // 128; NQ = TOWN
    scale = 192.0 ** -0.5
    qa = P.sb("qa", [128, 4, NQ], BF16); qb = P.sb("qb", [65, 4, NQ], BF16)
    stg = [P.sb("wst%d" % i, [128, 768]) for i in range(2)]
    wuq = load_w_bf16(P, wuq_d, 256, 768, "wuq", stage=stg)
    wuqs = load_w_bf16(P, wuqs_d, 256, 256, "wuqs", stage=stg)
    qn = P.sb("qn", [128, 2]); P.dma("sp", qn.v, qn_d)
    cqT = P.sb("cqT", [128, 2, NQ], BF16)
    xs = [P.sb("x%d" % i, [128, 256]) for i in range(2)]; junk = P.sb("junk", [128, 256]); st = [P.sb("st%d" % i, [128, 4]) for i in range(2)]
    xn = [P.sb("xn%d" % i, [128, 256], BF16) for i in range(2)]
    ps1 = [P.ps("ps1_%d" % i, [128, 512]) for i in range(2)]; pst = [P.ps("pst%d" % i, [128, 512]) for i in range(2)]
    pO = [P.ps("pO%d" % i, [128, 512]) for i in range(4)]
    for t in range(NOWN):
        b = t % 2
        pT = pO[b].v.bitcast(BF16)
        P.dma("sp", xs[b].v, cq_s[t * 128:(t + 1) * 128, :])
        P.op("act", "activation", out=junk.v, in_=xs[b].v, func=AF.Square, accum_out=st[b][:, 0:1])
        rstd_of(P, st[b][:, 0:1], st[b][:, 2:3], st[b][:, 1:2], 256)
        P.op("dve", "tensor_scalar", out=xn[b].v, in0=xs[b].v, scalar1=st[b][:, 2:3], scalar2=None, op0=ALU.mult)
        for k in range(2):
            P.op("pe", "transpose", out=pT[:, k * 128:(k + 1) * 128], in_=xn[b][:, k * 128:(k + 1) * 128], identity=idb.v)
        for k in range(2):
            P.op("dve", "tensor_scalar", out=cqT[:, k, t * 128:(t + 1) * 128], in0=pT[:, k * 128:(k + 1) * 128], scalar1=qn[:, k:k + 1], scalar2=None, op0=ALU.mult)
    Ct = P.sb("Ct", [64, 512]); St = P.sb("St", [64, 512]); r1 = P.sb("r1", [64, 512]); r2 = P.sb("r2", [64, 512])
    for c0 in range(0, NQ, 512):
        cn = min(512, NQ - c0)
        P.dma("sp", Ct[:, 0:cn], Cq_d[:, c0:c0 + cn]); P.dma("sp", St[:, 0:cn], Sq_d[:, c0:c0 + cn])
        for h in range(4):
            for k in range(2):
                P.op("pe", "matmul", track=(k == 1), out=ps1[0][:, 0:cn], lhsT=wuq[:, k, h * 192:h * 192 + 128], rhs=cqT[:, k, c0:c0 + cn], start=(k == 0), stop=(k == 1))
            P.op("act", "activation", out=qa[:, h, c0:c0 + cn], in_=ps1[0][:, 0:cn], func=AF.Copy, scale=scale)
            for k in range(2):
                P.op("pe", "matmul", track=(k == 1), out=ps1[1][0:64, 0:cn], lhsT=wuq[:, k, h * 192 + 128:h * 192 + 192], rhs=cqT[:, k, c0:c0 + cn], start=(k == 0), stop=(k == 1))
            for k in range(2):
                P.op("pe", "matmul", track=(k == 1), out=pst[0][0:64, 0:cn], lhsT=wuqs[:, k, h * 64:(h + 1) * 64], rhs=cqT[:, k, c0:c0 + cn], start=(k == 0), stop=(k == 1))
            P.op("dve", "tensor_tensor", out=r1[:, 0:cn], in0=ps1[1][0:64, 0:cn], in1=Ct[:, 0:cn], op=ALU.mult)
            P.op("dve", "tensor_tensor", out=r2[:, 0:cn], in0=pst[0][0:64, 0:cn], in1=St[:, 0:cn], op=ALU.mult)
            P.op("dve", "tensor_tensor", out=r1[:, 0:cn], in0=r1[:, 0:cn], in1=r2[:, 0:cn], op=ALU.add)
            P.op("act", "activation", out=qb[0:64, h, c0:c0 + cn], in_=r1[:, 0:cn], func=AF.Copy, scale=scale)
    kTa = P.sb("kTa", [128, NK], BF16); kTb = P.sb("kTb", [65, NK], BF16); Vb = P.sb("Vb", [128, NB, 129], BF16)
    P.op("pool", "memset", ap=kTb.v, constant=1.0)
    P.op("pool", "memset", ap=Vb.v, constant=1.0)
    P.dma("sp", kTb[0:64, :], krT_s)
    mx = P.sb("mx", [128, 4, 40]); negm = P.sb("negm", [128, 4]); Z = P.sb("Z", [128, 65])
    P.op("dve", "memset", ap=Z.v, constant=0.0)
    pt = [P.sb("pt%d" % i, [128, 512], BF16) for i in range(2)]
    rl = P.sb("rl", [128, 4]); ot = [P.sb("ot%d" % i, [128, 4, 128]) for i in range(2)]
    groups = [(g * 512, 512, NK) for g in range(5)] + [(2560, 128, 256), (2688, 128, 256)]
    c1 = 0; c2 = 0; gi = 0
    for h in range(4):
        P.dma("sp", kTa.v, kT_s[h])
        for b0 in range(0, NB, 26):
            bn = min(26, NB - b0)
            P.dma("sp", Vb[:, b0:b0 + bn, 0:128], V_s[b0 * 128:(b0 + bn) * 128, h * 128:(h + 1) * 128].rearrange("(b p) f -> p b f", p=128))
        for (q0, nq, nkeys) in groups:
            u = gi % 2; gi += 1; nqt = nq // 128
            kblocks = [(k0, min(512, nkeys - k0)) for k0 in range(0, nkeys, 512)]
            for qt in range(nqt):
                qs_ = slice(q0 + qt * 128, q0 + (qt + 1) * 128)
                for bi, (k0, kn) in enumerate(kblocks):
                    pb = ps1[c1 % 2]; c1 += 1
                    P.op("pe", "matmul", track=False, out=pb[:, 0:kn], lhsT=qa[:, h, qs_], rhs=kTa[:, k0:k0 + kn], start=True, stop=False)
                    P.op("pe", "matmul", out=pb[:, 0:kn], lhsT=qb[0:64, h, qs_], rhs=kTb[0:64, k0:k0 + kn], start=False, stop=True)
                    P.op("dve", "tensor_reduce", out=mx[:, qt, bi:bi + 1], in_=pb[:, 0:kn], axis=AX.X, op=ALU.max)
                P.op("dve", "tensor_reduce", out=negm[:, qt:qt + 1], in_=mx[:, qt, 0:len(kblocks)], axis=AX.X, op=ALU.max)
                P.op("dve", "tensor_scalar", out=Z[:, 64:65], in0=negm[:, qt:qt + 1], scalar1=-1.0, scalar2=None, op0=ALU.mult)
                pz = ps1[c1 % 2]; c1 += 1
                P.op("pe", "matmul", out=pz[0:65, 0:128], lhsT=Z.v, rhs=idf.v, start=True, stop=True)
                P.op("dve", "tensor_copy", out=qb[64:65, h, qs_], in_=pz[64:65, 0:128])
            nkb = nkeys // 128

            def qk(kb_, slot):
                ks_ = slice(kb_ * 128, (kb_ + 1) * 128)
                pb = pst[slot % 2]
                P.op("pe", "matmul", track=False, out=pb[:, 0:nq], lhsT=kTa[:, ks_], rhs=qa[:, h, q0:q0 + nq], start=True, stop=False)
                P.op("pe", "matmul", out=pb[:, 0:nq], lhsT=kTb[0:65, ks_], rhs=qb[0:65, h, q0:q0 + nq], start=False, stop=True)
            qk(0, c2)
            for kb_ in range(nkb):
                pb = pst[c2 % 2]; ptb = pt[c2 % 2]
                P.op("act", "activation", out=ptb[:, 0:nq], in_=pb[:, 0:nq], func=AF.Exp)
                if kb_ + 1 < nkb:
                    qk(kb_ + 1, c2 + 1)
                c2 += 1
                for qt in range(nqt):
                    P.op("pe", "matmul", track=(kb_ == nkb - 1), out=pO[qt][:, 0:129], lhsT=ptb[:, qt * 128:(qt + 1) * 128], rhs=Vb[:, kb_, :], start=(kb_ == 0), stop=(kb_ == nkb - 1))
            for qt in range(nqt):
                P.op("dve", "reciprocal", out=rl[:, qt:qt + 1], in_=pO[qt][:, 128:129])
                P.op("dve", "tensor_scalar", out=ot[u][:, qt, :], in0=pO[qt][:, 0:128], scalar1=rl[:, qt:qt + 1], scalar2=None, op0=ALU.mult)
            P.dma("sp", om_s[q0:q0 + nq, h * 128:(h + 1) * 128].rearrange("(t p) d -> p t d", p=128), ot[u][:, 0:nqt, :])
    P.end()


def stage_post(P, idf, idb, mode, variants, x_ap, ow_d, g1_d, gates_s, l, modcol, g2col_d, rw_d, rb_d, x1_s, hT_s, G_s, GT_s, gla=None, aT_ap=None, hTok_s=None):
    P.begin()
    nt = len(variants); nv = 2; T = nt * 128; D = 1024; KC = 8
    R2 = lambda nm, shp, dt=F32: [P.sb("%s_%d" % (nm, i), shp, dt) for i in range(2)]
    stage = [P.sb("wst%d" % i, [128, D]) for i in range(2)]
    wb = load_w_bf16(P, ow_d, D, D, "owb", stage=stage)
    rw = P.sb("rw", [128, KC, 32]); P.dma("sp", rw.v, rw_d.rearrange("(k p) n -> p k n", p=128))
    rb = P.sb("rb", [128, 32]); P.dma("sp", rb.v, rb_d)
    g2 = P.sb("g2", [128, KC]); P.dma("sp", g2.v, g2col_d)
    sc = modcols(P, modcol, l, 4, g2.v, "scm")
    sh = lambda v, k: modcol[:, l, 24 + k, v:v + 1]
    g1 = P.sb("g1", [128, D]); P.dma("sp", g1.v, g1_d)
    GG = []
    for v in range(nv):
        gg = P.sb("GG%d" % v, [128, D]); P.dma("sp", gg.v, gates_s[l, 0, v])
        P.op("dve", "tensor_tensor", out=gg.v, in0=gg.v, in1=g1.v, op=ALU.mult)
        GG.append(gg)
    if mode == "gla":
        onc = P.sb("onc", [128, 1]); P.dma("sp", onc.v, gla["on_d"])
        idxf = P.sb("idxf", [128, nt], I32); idxb = P.sb("idxb", [128, nt], I32)
        P.dma("sp", idxf.v, gla["idxf_d"]); P.dma("sp", idxb.v, gla["idxb_d"])
        tof_ = R2("tof", [128, 512]); tob_ = R2("tob", [128, 512]); tr_ = R2("tr", [128, 512]); tom_ = R2("tom", [128, 512])
        cat_ = R2("cat", [128, D], BF16)
        pT = P.ps("pT", [128, KC, 128], BF16)
    else:
        a32_ = R2("a32", [128, KC, 128])
    aT_ = R2("aT", [128, KC, 128], BF16)
    xt_ = R2("xt", [128, D]); x1_ = R2("x1", [128, D]); junk = P.sb("junk", [128, D]); st_ = R2("st", [128, 16])
    xn2_ = R2("xn2", [128, D]); h2T_ = R2("h2T", [128, KC, 128]); h2Tb_ = R2("h2Tb", [128, KC, 128], BF16)
    lg_ = R2("lg", [128, 32]); t8_ = R2("t8", [128, 8]); msk_ = R2("msk", [128, 32]); ex_ = R2("ex", [128, 32]); Gt_ = R2("Gt", [128, 32]); GTt_ = R2("GTt", [32, 128])
    py = [P.ps("py%d" % i, [128, 512]) for i in range(2)]
    pT32 = P.ps("pT32", [128, KC, 128])
    plg = P.ps("plg", [128, 128])
    pTt = P.ps("pTt", [128, D], BF16); htok_ = R2("htok", [128, D], BF16)
    for t in range(nt):
        v = variants[t]; sl = slice(t * 128, (t + 1) * 128); u_ = t % 2
        if mode == "gla":
            tof = tof_[u_]; tob = tob_[u_]; tr = tr_[u_]; tom = tom_[u_]; cat = cat_[u_]
        else:
            a32 = a32_[u_]
        aT = aT_[u_]; xt = xt_[u_]; x1 = x1_[u_]; st = st_[u_]; xn2 = xn2_[u_]; h2T = h2T_[u_]; h2Tb = h2Tb_[u_]
        lg = lg_[u_]; t8 = t8_[u_]; msk = msk_[u_]; ex = ex_[u_]; Gt = Gt_[u_]; GTt = GTt_[u_]
        P.dma("sp", xt.v, x_ap[sl, :])
        if mode == "gla":
            P.gather(tof.v, gla["of_s"], idxf[:, t:t + 1]); P.gather(tob.v, gla["ob_s"], idxb[:, t:t + 1])
            P.dma("sp", tr.v, gla["r_s"][sl, :]); P.dma("sp", tom.v, gla["om_s"][sl, :])
            P.op("dve", "tensor_tensor", out=tof.v, in0=tof.v, in1=tob.v, op=ALU.add)
            for h in range(4):
                P.op("act", "activation", out=junk[:, 0:128], in_=tof[:, h * 128:(h + 1) * 128], func=AF.Square, accum_out=st[:, h:h + 1])
            rstd_of(P, st[:, 0:4], st[:, 8:12], st[:, 4:8], 128)
            P.op("act", "activation", out=tr.v, in_=tr.v, func=AF.Silu)
            for h in range(4):
                hs = slice(h * 128, (h + 1) * 128)
                P.op("dve", "scalar_tensor_tensor", out=cat[:, hs], in0=tof[:, hs], scalar=st[:, 8 + h:9 + h], in1=tr[:, hs], op0=ALU.mult, op1=ALU.mult)
            P.op("pool", "tensor_copy", out=cat[:, 512:1024], in_=tom.v)
            for k in range(KC):
                P.op("pe", "transpose", out=pT[:, k, :], in_=cat[:, k * 128:(k + 1) * 128], identity=idb.v)
            P.op("dve", "tensor_scalar", out=aT[:, 0:4, :], in0=pT[:, 0:4, :], scalar1=onc[:, 0:1], scalar2=None, op0=ALU.mult)
            P.op("act", "activation", out=aT[:, 4:8, :], in_=pT[:, 4:8, :], func=AF.Copy)
        else:
            P.dma("sp", a32.v, aT_ap[:, sl].rearrange("(k p) t -> p k t", p=128))
            P.op("pool", "tensor_copy", out=aT.v, in_=a32.v)
        for n in range(2):
            for k in range(KC):
                P.op("pe", "matmul", track=(k == KC - 1), out=py[n].v, lhsT=aT[:, k, :], rhs=wb[:, k, n * 512:(n + 1) * 512], start=(k == 0), stop=(k == KC - 1))
            P.op("act", "activation", out=junk[:, 0:512], in_=py[n].v, func=AF.Square, accum_out=st[:, 12 + n:13 + n])
        P.op("dve", "tensor_tensor", out=st[:, 12:13], in0=st[:, 12:13], in1=st[:, 13:14], op=ALU.add)
        rstd_of(P, st[:, 12:13], st[:, 14:15], st[:, 13:14], D)
        for n in range(2):
            ns = slice(n * 512, (n + 1) * 512)
            P.op("dve", "scalar_tensor_tensor", out=x1[:, ns], in0=py[n].v, scalar=st[:, 14:15], in1=GG[v][:, ns], op0=ALU.mult, op1=ALU.mult)
        P.op("pool", "tensor_tensor", out=x1.v, in0=x1.v, in1=xt.v, op=ALU.add)
        P.dma("sp", x1_s[sl, :], x1.v)
        P.op("act", "activation", out=junk.v, in_=x1.v, func=AF.Square, accum_out=st[:, 15:16])
        rstd_of(P, st[:, 15:16], st[:, 4:5], st[:, 5:6], D)
        P.op("dve", "tensor_scalar", out=xn2.v, in0=x1.v, scalar1=st[:, 4:5], scalar2=None, op0=ALU.mult)
        for k in range(KC):
            P.op("pe", "transpose", out=pT32[:, k, :], in_=xn2[:, k * 128:(k + 1) * 128], identity=idf.v)
        for k in range(KC):
            P.op("dve", "tensor_scalar", out=h2T[:, k, :], in0=pT32[:, k, :], scalar1=sc[:, v, k:k + 1], scalar2=sh(v, k), op0=ALU.mult, op1=ALU.add)
        P.op("pool", "tensor_copy", out=h2Tb.v, in_=h2T.v)
        P.dma("sp", hT_s[:, sl].rearrange("(k p) t -> p k t", p=128), h2Tb.v)
        if hTok_s is not None:
            for k in range(KC):
                P.op("pe", "transpose", out=pTt[:, k * 128:(k + 1) * 128], in_=h2Tb[:, k, :], identity=idb.v, track=(k == KC - 1))
            P.op("act", "activation", out=htok_[u_].v, in_=pTt.v, func=AF.Copy)
            P.dma("sp", hTok_s[sl, :], htok_[u_].v)
        for k in range(KC):
            P.op("pe", "matmul", track=(k == KC - 1), out=plg[:, 0:32], lhsT=h2T[:, k, :], rhs=rw[:, k, :], start=(k == 0), stop=(k == KC - 1))
        P.op("dve", "tensor_tensor", out=lg.v, in0=plg[:, 0:32], in1=rb.v, op=ALU.add)
        P.op("dve", "max", out=t8.v, in_=lg.v)
        P.op("dve", "tensor_scalar", out=msk.v, in0=lg.v, scalar1=t8[:, 3:4], scalar2=None, op0=ALU.is_ge)
        P.op("dve", "tensor_scalar", out=t8[:, 7:8], in0=t8[:, 0:1], scalar1=-1.0, scalar2=None, op0=ALU.mult)
        P.op("act", "activation", out=ex.v, in_=lg.v, func=AF.Exp, bias=t8[:, 7:8])
        P.op("dve", "tensor_tensor", out=ex.v, in0=ex.v, in1=msk.v, op=ALU.mult)
        P.op("dve", "tensor_reduce", out=t8[:, 6:7], in_=ex.v, axis=AX.X, op=ALU.add)
        P.op("dve", "reciprocal", out=t8[:, 5:6], in_=t8[:, 6:7])
        P.op("dve", "tensor_scalar", out=Gt.v, in0=ex.v, scalar1=t8[:, 5:6], scalar2=None, op0=ALU.mult)
        P.dma("sp", G_s[sl, :], Gt.v)
        P.op("pe", "transpose", out=plg[0:32, :], in_=Gt.v, identity=idf.v)
        P.op("act", "activation", out=GTt.v, in_=plg[0:32, :], func=AF.Copy)
        P.dma("sp", GT_s[:, sl], GTt.v)
    P.end()


def stage_moe(P, variants, groups, hT_s, G_s, GT_s, x1_s, w1_d, w2_d, b1g_d, b1l_d, b2_d, g3_d, gates_s, l, x2_ap, NE=32):
    P.begin()
    nt = len(variants); nv = 2; T = nt * 128; D = 1024; KC = 8; F = 1024
    mg = max(len(g) for g in groups)
    g3 = P.sb("g3", [128, D]); P.dma("sp", g3.v, g3_d)
    GG = []
    for v in range(nv):
        gg = P.sb("GG%d" % v, [128, D]); P.dma("sp", gg.v, gates_s[l, 1, v])
        P.op("dve", "tensor_tensor", out=gg.v, in0=gg.v, in1=g3.v, op=ALU.mult)
        GG.append(gg)
    b1g = P.sb("b1g", [128, 32, KC]); b1l = P.sb("b1l", [128, 32, KC]); P.dma("sp", b1g.v, b1g_d); P.dma("sp", b1l.v, b1l_d)
    b2 = P.sb("b2", [32, D]); P.dma("sp", b2.v, b2_d)
    w1g = [P.sb("w1g%d" % i, [128, KC, F], BF16) for i in range(2)]
    w1l = [P.sb("w1l%d" % i, [128, KC, F], BF16) for i in range(2)]
    w2b = [P.sb("w2b%d" % i, [128, KC, D], BF16) for i in range(2)]
    hT = P.sb("hT", [128, KC, mg * 128], BF16)
    Gs = P.sb("Gs", [128, mg, 32]); GTs = P.sb("GTs", [32, mg * 128])
    yacc = P.sb("yacc", [128, mg, D])
    actT = P.sb("actT", [128, KC, 512], BF16)
    tg = [P.sb("tg%d" % i, [128, 512]) for i in range(2)]; tsg = [P.sb("tsg%d" % i, [128, 512]) for i in range(2)]
    tl = [P.sb("tl%d" % i, [128, 512]) for i in range(2)]
    xt = g3; junk = P.sb("junk", [128, D]); st = P.sb("st", [128, 4])
    pg = [P.ps("pg%d" % i, [128, 512]) for i in range(2)]; pl = [P.ps("pl%d" % i, [128, 512]) for i in range(2)]
    py = [P.ps("py%d" % i, [128, 512]) for i in range(2)]
    sti = [0]

    def load_expert_steps(e, buf):
        steps = []
        for k in range(KC):
            def step(k=k):
                P.dma("sp", w1g[buf][:, k, :], w1_d[e, k * 128:(k + 1) * 128, 0:F])
                P.dma("sp", w1l[buf][:, k, :], w1_d[e, k * 128:(k + 1) * 128, F:2 * F])
                P.dma("sp", w2b[buf][:, k, :], w2_d[e, k * 128:(k + 1) * 128, :])
            steps.append(step)
        return steps

    for grp in groups:
        ng = len(grp)
        t0 = grp[0]; tok0 = t0 * 128; ntok = ng * 128
        P.dma("sp", hT[:, :, 0:ntok], hT_s[:, tok0:tok0 + ntok].rearrange("(k p) t -> p k t", p=128))
        P.dma("sp", Gs[:, 0:ng, :], G_s[tok0:tok0 + ntok, :].rearrange("(g p) e -> p g e", p=128))
        P.dma("sp", GTs[:, 0:ntok], GT_s[:, tok0:tok0 + ntok])
        for i in range(ng):
            for n in range(2):
                P.op("pe", "matmul", out=py[n].v, lhsT=GTs[:, i * 128:(i + 1) * 128], rhs=b2[:, n * 512:(n + 1) * 512], start=True, stop=True)
                P.op("act", "activation", out=yacc[:, i, n * 512:(n + 1) * 512], in_=py[n].v, func=AF.Copy)
        for s in load_expert_steps(0, 0):
            s()
        blocks = [(b0, min(512, ntok - b0)) for b0 in range(0, ntok, 512)]
        ci = 0
        for e in range(NE):
            buf = e % 2
            nxt = load_expert_steps(e + 1, 1 - buf) if e + 1 < NE else []
            for bi, (b0, bn) in enumerate(blocks):
                for j in range(KC):
                    if bi == 0 and nxt:
                        nxt[j]()
                    c = ci % 2; ci += 1
                    for k in range(KC):
                        P.op("pe", "matmul", track=(k == KC - 1), out=pg[c][:, 0:bn], lhsT=w1g[buf][:, k, j * 128:(j + 1) * 128], rhs=hT[:, k, b0:b0 + bn], start=(k == 0), stop=(k == KC - 1))
                    for k in range(KC):
                        P.op("pe", "matmul", track=(k == KC - 1), out=pl[c][:, 0:bn], lhsT=w1l[buf][:, k, j * 128:(j + 1) * 128], rhs=hT[:, k, b0:b0 + bn], start=(k == 0), stop=(k == KC - 1))
                    P.op("dve", "tensor_scalar", out=tg[c][:, 0:bn], in0=pg[c][:, 0:bn], scalar1=b1g[:, e, j:j + 1], scalar2=7.0, op0=ALU.add, op1=ALU.min)
                    P.op("act", "activation", out=tsg[c][:, 0:bn], in_=tg[c][:, 0:bn], func=AF.Sigmoid, scale=1.702)
                    P.op("dve", "tensor_scalar", out=tl[c][:, 0:bn], in0=pl[c][:, 0:bn], scalar1=b1l[:, e, j:j + 1], scalar2=-7.0, op0=ALU.add, op1=ALU.max)
                    P.op("dve", "tensor_scalar", out=tl[c][:, 0:bn], in0=tl[c][:, 0:bn], scalar1=7.0, scalar2=1.0, op0=ALU.min, op1=ALU.add)
                    P.op("pool", "tensor_tensor", out=tg[c][:, 0:bn], in0=tg[c][:, 0:bn], in1=tsg[c][:, 0:bn], op=ALU.mult)
                    P.op("pool", "tensor_tensor", out=actT[:, j, 0:bn], in0=tg[c][:, 0:bn], in1=tl[c][:, 0:bn], op=ALU.mult)
                for i in range(b0 // 128, (b0 + bn) // 128):
                    for n in range(2):
                        ns = slice(n * 512, (n + 1) * 512)
                        for j in range(KC):
                            P.op("pe", "matmul", track=(j == KC - 1), out=py[n].v, lhsT=actT[:, j, i * 128 - b0:(i + 1) * 128 - b0], rhs=w2b[buf][:, j, ns], start=(j == 0), stop=(j == KC - 1))
                        P.op("dve", "scalar_tensor_tensor", out=yacc[:, i, ns], in0=py[n].v, scalar=Gs[:, i, e:e + 1], in1=yacc[:, i, ns], op0=ALU.mult, op1=ALU.add)
        for i in range(ng):
            t = grp[i]; v = variants[t]; sl = slice(t * 128, (t + 1) * 128)
            P.dma("sp", xt.v, x1_s[sl, :])
            P.op("act", "activation", out=junk.v, in_=yacc[:, i, :], func=AF.Square, accum_out=st[:, 0:1])
            rstd_of(P, st[:, 0:1], st[:, 2:3], st[:, 1:2], D)
            P.op("dve", "scalar_tensor_tensor", out=junk.v, in0=yacc[:, i, :], scalar=st[:, 2:3], in1=GG[v].v, op0=ALU.mult, op1=ALU.mult)
            P.op("pool", "tensor_tensor", out=xt.v, in0=junk.v, in1=xt.v, op=ALU.add)
            P.dma("sp", x2_ap[sl, :], xt.v)
    P.end()


def stage_na(P, idb, qT_s, kT_s, V_s, bi_d, be_d, oT_s):
    P.begin()
    ones = P.sb("ones", [128, 128], BF16); P.op("pool", "memset", ap=ones.v, constant=1.0)
    kb = [P.sb("kb%d" % i, [128, 8, 768], BF16) for i in range(2)]; vb = [P.sb("vb%d" % i, [128, 6, 1024], BF16) for i in range(2)]
    kcb = P.sb("kcb", [128, 8, 256], BF16); vcb = P.sb("vcb", [128, 2, 1024], BF16)
    P.dma("sp", kcb.v, kT_s[:, 2560:2816].rearrange("(k p) n -> p k n", p=128))
    P.dma("sp", vcb.v, V_s[2560:2816, :].rearrange("(b p) f -> p b f", p=128))
    Bi = P.sb("Bi", [128, 8, 512]); P.dma("sp", Bi.v, bi_d)
    Be = [P.sb("Be%d" % i, [128, 8, 768]) for i in range(2)]
    qrow = [P.sb("qrow%d" % i, [128, 8, 64], BF16) for i in range(2)]
    QBD = [P.sb("QBD%d" % i, [128, 128], BF16) for i in range(2)]
    for z in QBD:
        P.op("pool", "memset", ap=z.v, constant=0.0)
    Sb = [P.sb("Sb%d" % i, [128, 1024]) for i in range(2)]; Pm = [P.sb("Pm%d" % i, [128, 1024], BF16) for i in range(2)]
    PT = [P.sb("PT%d" % i, [128, 8, 128], BF16) for i in range(2)]
    sm = P.sb("sm", [128, 4]); rl = P.sb("rl", [128, 128]); orow = [P.sb("orow%d" % i, [128, 8, 64]) for i in range(2)]
    pS = [P.ps("pS%d" % i, [128, 1024]) for i in range(2)]
    pPT = P.ps("pPT", [128, 8, 128], BF16); pO = P.ps("pO", [128, 128]); pL = P.ps("pL", [128, 128])
    it = 0; nbe = 0
    for i in range(32):
        u = i % 2
        if i < 4:
            e0, nr, edge = 0, 12, i
        elif i >= 28:
            e0, nr, edge = 28, 12, i - 24
        else:
            e0, nr, edge = i, 8, None
        nw = nr * 64; nbk = nw // 128
        P.dma("sp", kb[u][:, :, 0:nw], kT_s[:, e0 * 64:e0 * 64 + nw].rearrange("(k p) n -> p k n", p=128))
        P.dma("sp", vb[u][:, 0:nbk, :], V_s[e0 * 64:e0 * 64 + nw, :].rearrange("(b p) f -> p b f", p=128))
        P.dma("sp", qrow[u].v, qT_s[:, (i + 4) * 64:(i + 5) * 64].rearrange("(k p) t -> p k t", p=128))
        if edge is not None:
            B = Be[nbe % 2]; nbe += 1
            P.dma("sp", B.v, be_d[edge])
        else:
            B = Bi
        ntot = nw + 256; nblk = nbk + 2
        def scores(hp, w):
            P.op("pool", "tensor_copy", out=QBD[w][0:64, 0:64], in_=qrow[u][0:64, hp, :])
            P.op("pool", "tensor_copy", out=QBD[w][64:128, 64:128], in_=qrow[u][64:128, hp, :])
            for c0 in range(0, nw, 512):
                cn = min(512, nw - c0)
                P.op("pe", "matmul", out=pS[w][:, c0:c0 + cn], lhsT=QBD[w].v, rhs=kb[u][:, hp, c0:c0 + cn], start=True, stop=True, track=False)
            P.op("pe", "matmul", out=pS[w][:, nw:ntot], lhsT=QBD[w].v, rhs=kcb[:, hp, :], start=True, stop=True)
        scores(0, it % 2)
        for hp in range(8):
            w = it % 2; it += 1
            if hp + 1 < 8:
                scores(hp + 1, it % 2)
            P.op("dve", "tensor_tensor", out=Sb[w][:, 0:nw], in0=pS[w][:, 0:nw], in1=B[:, hp, 0:nw], op=ALU.add)
            P.op("act", "activation", out=Sb[w][:, nw:ntot], in_=pS[w][:, nw:ntot], func=AF.Copy)
            P.op("dve", "tensor_reduce", out=sm[:, 0:1], in_=Sb[w][:, 0:ntot], axis=AX.X, op=ALU.max)
            P.op("dve", "tensor_scalar", out=sm[:, 1:2], in0=sm[:, 0:1], scalar1=-1.0, scalar2=None, op0=ALU.mult)
            P.op("act", "activation", out=Pm[w][:, 0:ntot], in_=Sb[w][:, 0:ntot], func=AF.Exp, bias=sm[:, 1:2])
            for b in range(nblk):
                P.op("pe", "transpose", out=pPT[:, b, :], in_=Pm[w][:, b * 128:(b + 1) * 128], identity=idb.v, track=(b == nblk - 1))
            P.op("dve", "tensor_copy", out=PT[w][:, 0:nblk, :], in_=pPT[:, 0:nblk, :])
            hs = slice(hp * 128, (hp + 1) * 128)
            for b in range(nblk):
                vblk = vb[u][:, b, hs] if b < nbk else vcb[:, b - nbk, hs]
                P.op("pe", "matmul", out=pO.v, lhsT=vblk, rhs=PT[w][:, b, :], start=(b == 0), stop=(b == nblk - 1), track=(b == nblk - 1))
            for b in range(nblk):
                P.op("pe", "matmul", out=pL.v, lhsT=ones.v, rhs=PT[w][:, b, :], start=(b == 0), stop=(b == nblk - 1), track=(b == nblk - 1))
            P.op("dve", "reciprocal", out=rl.v, in_=pL.v)
            P.op("dve", "tensor_tensor", out=orow[u][0:64, hp, :], in0=pO[0:64, 0:64], in1=rl[0:64, 0:64], op=ALU.mult)
            P.op("dve", "tensor_tensor", out=orow[u][64:128, hp, :], in0=pO[64:128, 64:128], in1=rl[64:128, 64:128], op=ALU.mult)
        P.dma("sp", oT_s[:, i * 64:(i + 1) * 64].rearrange("(k p) t -> p k t", p=128), orow[u].v)
    P.end()


MOE_GROUPS_D0 = [list(range(0, 8)), list(range(8, 15)), list(range(15, 22))]
MOE_GROUPS_D1 = [list(range(0, 8)), list(range(8, 16))]
MOE_PASSES_F0 = [[[0, 1, 2, 3], [4, 5, 6, 7]], [[8, 9, 10, 11], [12, 13, 14, 15]], [[16, 17, 18, 19], [20, 21]]]
MOE_PASSES_F1 = [[[0, 1, 2, 3], [4, 5, 6, 7]], [[8, 9, 10, 11], [12, 13, 14, 15]]]
MOE_GROUPS_F0 = [list(range(0, 6)), list(range(6, 12)), list(range(12, 17)), list(range(17, 22))]
MOE_GROUPS_F1 = [list(range(0, 6)), list(range(6, 11)), list(range(11, 16))]


def build_fused(dbg=False, upto=99):
    P = Prog("fused")
    D = 1024
    dr = P.dram
    x_all = dr("x_all", [TALL, D]); x_rev = dr("x_rev", [TALL, D]); x_own = dr("x_own", [TOWN, D])
    adaw = dr("adaw", [2, D, 6144]); bcol = dr("bcol", [128, 2, 48]); brow = dr("brow", [2, 128, 6144]); cT = dr("cT", [128, 8, 2])
    ng = dr("ng", [2, 4, 128, 8]); ngrow = dr("ngrow", [2, 4, 128, D])
    w_tmA = dr("w_tmA", [D, 896]); w_fmA = dr("w_fmA", [D, 672]); w_tmB = dr("w_tmB", [D, 768]); w_fmB = dr("w_fmB", [D, 528]); w_own = dr("w_own", [D, 768])
    Ca = dr("Ca", [64, TALL]); Sa = dr("Sa", [64, TALL]); Cq = dr("Cq", [64, TOWN]); Sq = dr("Sq", [64, TOWN])
    wukv = dr("wukv", [128, 1024]); kvn = dr("kvn", [128, 1]); qn = dr("qn", [128, 2]); wuq = dr("wuq", [256, 768]); wuqs = dr("wuqs", [256, 256])
    wa = dr("wa", [17, 8, 64]); tri = dr("tri", [64, 64]); mu = dr("mu", [64, 8, 64])
    idxf = dr("idxf", [128, NOWN], I32); idxb = dr("idxb", [128, NOWN], I32)
    ow0 = dr("ow0", [D, D]); ow1 = dr("ow1", [D, D]); onc = dr("onc", [128, 1])
    rw = dr("rw", [2, D, 32]); rb = dr("rb", [2, 128, 32])
    if upto >= 7:
        w1 = dr("w1", [2, 32, D, 2048]); w2 = dr("w2", [2, 32, D, D])
    b1 = dr("b1", [2, 32, 2048]); b1g = dr("b1g", [2, 128, 32, 8]); b1l = dr("b1l", [2, 128, 32, 8]); b2 = dr("b2", [2, 32, D]); iota_d = dr("iota", [128, 128]); lst_d = dr("lst", [128, 128])
    wqk = dr("wqk", [D, 2048]); wv = dr("wv", [D, D]); Bint = dr("Bint", [128, 8, 512]); Bedge = dr("Bedge", [8, 128, 8, 768])
    out = dr("out", [2048, D], out=True)
    S = lambda n, s, dt=F32: P.scratch(n, s, dt, dbg=(dbg is True or (dbg and n in dbg)))
    gates = S("gates", [2, 2, 2, 128, D])
    kA = S("kA", [TALL, 256]); vA = S("vA", [TALL, 512]); qTA = S("qTA", [256, TALL]); kTA = S("kTA", [256, TALL]); aTA = S("aTA", [16, TALL])
    kB = S("kB", [TALL, 256]); vB = S("vB", [TALL, 512]); qTB = S("qTB", [256, TALL]); kTB = S("kTB", [256, TALL]); aTB = S("aTB", [16, TALL])
    kTm = S("kTm", [4, 128, TALL], BF16); krT = S("krT", [64, TALL], BF16); Vm = S("Vm", [TALL, 512], BF16)
    r_own = S("r_own", [TOWN, 512]); cq_own = S("cq_own", [TOWN, 256])
    oF = S("oF", [TALL, 512]); oB = S("oB", [TALL, 512]); om = S("om", [TOWN, 512])
    w1bf = S("w1bf", [2, 32, D, 2048], BF16); w2bf = S("w2bf", [2, 32, D, D], BF16); b1bf = S("b1bf", [2, 32, 2048], BF16)
    hka = S("hka", [TOWN, D], BF16); hkb = S("hkb", [2048, D], BF16)
    x1a = S("x1a", [TOWN, D]); hTa = S("hTa", [D, TOWN], BF16); Ga = S("Ga", [TOWN, 32]); GTa = S("GTa", [32, TOWN]); x2a = S("x2a", [TOWN, D])
    qT1 = S("qT1", [D, TOWN], BF16); kT1 = S("kT1", [D, TOWN], BF16); V1 = S("V1", [TOWN, D], BF16); oT1 = S("oT1", [D, 2048])
    x1b = S("x1b", [2048, D]); hTb = S("hTb", [D, 2048], BF16); Gb = S("Gb", [2048, 32]); GTb = S("GTb", [32, 2048])
    idf, idb = ident(P)
    modcol = P.sb("modcol", [128, 2, 48, 2])
    stage_ada(P, adaw, bcol, brow, cT, modcol, gates)
    if upto < 1:
        P.finish(); return P
    gall = [(0, 2)] + [(256 + 512 * i, 4) for i in range(32)]; gv = [1] + [0] * 32
    gown = [(512 * i, 4) for i in range(5)] + [(2560, 2)]; gvo = [0] * 5 + [1]

    def n1(l):
        g = P.sb("gn%d" % P.nscope, [128, 8]); P.dma("sp", g.v, ng[l, 0])
        sc = modcols(P, modcol, l, 1, g.v, "sc%d" % P.nscope)
        sh = P.sb("sh%d" % P.nscope, [128, 2, 8])
        for s in range(2):
            P.op("dve", "tensor_copy", out=sh[:, s, :], in_=modcol[:, l, 0:8, s])
        return sc, sh
    sc0, sh0 = n1(0)
    if upto >= 7:
        it0 = conv_items(w1, w2, b1, w1bf, w2bf, b1bf, 0); it1 = conv_items(w1, w2, b1, w1bf, w2bf, b1bf, 1)
        xA = xB = None
    else:
        xA = xB = None
    stage_pro2(P, idb, x_all, gall, gv, sc0, sh0, D, w_tmA, 896, [(0, 256, kA, F32), (256, 512, vA, F32)], w_fmA, 672,
              [(0, 128, qTA[0:128], F32, 1.0), (128, 128, qTA[128:256], F32, 1.0), (256, 128, kTA[0:128], F32, 1.0), (384, 128, kTA[128:256], F32, 1.0), (512, 16, aTA, F32, 1.0)],
              rope=(544, 608, krT, Ca, Sa), mla=dict(col=768, wukv_d=wukv, kvn_d=kvn, kT_s=kTm, V_s=Vm), tag="proA", extra=xA)
    stage_pro2(P, idb, x_rev, gall, gv, sc0, sh0, D, w_tmB, 768, [(0, 256, kB, F32), (256, 512, vB, F32)], w_fmB, 528,
              [(0, 128, qTB[0:128], F32, 1.0), (128, 128, qTB[128:256], F32, 1.0), (256, 128, kTB[0:128], F32, 1.0), (384, 128, kTB[128:256], F32, 1.0), (512, 16, aTB, F32, 1.0)], tag="proB", extra=xB)
    if upto < 2:
        P.finish(); return P
    stage_pro2(P, idb, x_own, gown, gvo, sc0, sh0, D, w_own, 768, [(0, 512, r_own, F32), (512, 256, cq_own, F32)], None, 0, [], tag="proO")
    if upto < 3:
        P.finish(); return P
    scans = []
    for h in range(4):
        hs = slice(h * 64, (h + 1) * 64)
        scans.append((qTA[hs], kTA[hs], kA[:, hs], vA[:, h * 128:(h + 1) * 128], aTA, h, oF[:, h * 128:(h + 1) * 128]))
    for h in range(4):
        hs = slice(h * 64, (h + 1) * 64)
        scans.append((qTB[hs], kTB[hs], kB[:, hs], vB[:, h * 128:(h + 1) * 128], aTB, 4 + h, oB[:, h * 128:(h + 1) * 128]))
    stage_gla(P, scans, wa, tri, mu, extra=(lambda P_: conv_steps(P_, it0 + it1)) if upto >= 7 else None, per_block=4)
    if upto < 4:
        P.finish(); return P
    stage_mla_full(P, idf, idb, cq_own, qn, wuq, wuqs, Cq, Sq, kTm, krT, Vm, om)
    if upto < 5:
        P.finish(); return P
    stage_post(P, idf, idb, "gla", VOWN, x_own, ow0, ngrow[0, 1], gates, 0, modcol, ng[0, 2], rw[0], rb[0], x1a, hTa, Ga, GTa,
               gla=dict(on_d=onc, idxf_d=idxf, idxb_d=idxb, of_s=oF, ob_s=oB, r_s=r_own, om_s=om))
    if upto < 7:
        P.finish(); return P
    stage_moe(P, VOWN, MOE_GROUPS_D0, hTa, Ga, GTa, x1a, w1bf[0], w2bf[0], b1g[0], b1l[0], b2[0], ngrow[0, 3], gates, 0, x2a)
    sc1, sh1 = n1(1)
    stage_pro2(P, idb, x2a, gown, gvo, sc1, sh1, D, wv, 1024, [(0, 1024, V1, BF16)], wqk, 2048,
              [(j * 128, 128, qT1[j * 128:(j + 1) * 128], BF16, 0.125) for j in range(8)] + [(1024 + j * 128, 128, kT1[j * 128:(j + 1) * 128], BF16, 1.0) for j in range(8)], tag="proQ")
    stage_na(P, idb, qT1, kT1, V1, Bint, Bedge, oT1)
    stage_post(P, idf, idb, "fm", [0] * 16, x2a[256:2304], ow1, ngrow[1, 1], gates, 1, modcol, ng[1, 2], rw[1], rb[1], x1b, hTb, Gb, GTb, aT_ap=oT1)
    stage_moe(P, [0] * 16, MOE_GROUPS_D1, hTb, Gb, GTb, x1b, w1bf[1], w2bf[1], b1g[1], b1l[1], b2[1], ngrow[1, 3], gates, 1, out)
    P.finish()
    return P


def _c(a):
    return np.ascontiguousarray(a, dtype=np.float32)


def _col(a):
    return _c(a.reshape(-1, 128).T)


def _bc(a):
    return _c(np.broadcast_to(a, (128,) + a.shape))


def _rope_tables():
    t = np.arange(16384)
    row = (t // 64).astype(np.float32); colp = (t % 64).astype(np.float32)
    inv = (np.float32(10000.0) ** (-np.arange(16, dtype=np.float32) / np.float32(16))).astype(np.float32)
    ang = np.concatenate([row[:, None] * inv, colp[:, None] * inv], -1).astype(np.float32)
    return np.cos(ang).astype(np.float32), np.sin(ang).astype(np.float32)


def na_bias_edge(rpb, r, grows):
    r0 = int(np.clip(r - 4, 0, 248))
    col = np.arange(64); c0 = np.clip(col - 8, 0, 48); kcol = np.arange(64)
    inwin = (kcol[None, :] >= c0[:, None]) & (kcol[None, :] < c0[:, None] + 16)
    cidx = np.clip(kcol[None, :] - col[:, None] + 15, 0, 30)
    out = np.full((16, 64, len(grows), 64), -30000.0, np.float32)
    for w, g in enumerate(grows):
        if 0 <= g <= 255 and r0 <= g < r0 + 8:
            t = rpb[:, g - r + 7][:, cidx]
            out[:, :, w, :] = np.where(inwin[None], t, np.float32(-30000.0))
    t = out.reshape(8, 2, 64, len(grows) * 64)
    return np.ascontiguousarray(t.transpose(1, 2, 0, 3).reshape(128, 8, len(grows) * 64))


_FP = {}


def fused_inputs(x, c, ctx, c_ctx, ada_w, ada_b, norm_g, router_w, router_b, moe_w1, moe_b1, moe_w2, moe_b2,
                 ab_in_w, gla_wa_f, gla_ba_f, gla_wa_b, gla_ba_b, gla_onorm, mla_qnorm, mla_wuq, mla_kvnorm, mla_wukv,
                 ab_out_w, na_qkv_w, na_rpb, na_out_w):
    f32 = np.float32
    A = lambda a: np.asarray(a, dtype=f32)
    x = A(x)[0]; ctx = A(ctx)[0]; ada_w = A(ada_w); ada_b = A(ada_b); norm_g = A(norm_g); inw = A(ab_in_w)[0]
    cos, sin = _rope_tables()
    com = {}
    com["x_all"] = _c(np.concatenate([ctx, x], 0)); com["x_rev"] = _c(np.concatenate([ctx[::-1], x[::-1]], 0))
    com["adaw"] = ada_w; com["bcol"] = _c(ada_b.reshape(2, 48, 128).transpose(2, 0, 1)); com["brow"] = _c(np.stack([_bc(ada_b[0]), _bc(ada_b[1])]))
    com["cT"] = _c(np.stack([A(c)[0], A(c_ctx)], 0).reshape(2, 8, 128).transpose(2, 1, 0))
    com["ng"] = _c(np.stack([np.stack([_col(norm_g[l, i]) for i in range(4)]) for l in range(2)]))
    com["ngrow"] = _c(np.stack([np.stack([_bc(norm_g[l, i]) for i in range(4)]) for l in range(2)]))
    kr = inw[:, 1952:2016]; krs = np.concatenate([kr[:, 32:], kr[:, :32]], 1)
    z16 = np.zeros((1024, 16), f32)
    com["w_tmA"] = _c(np.concatenate([inw[:, 256:512], inw[:, 512:1024], inw[:, 1824:1952]], 1))
    com["w_fmA"] = _c(np.concatenate([inw[:, 0:256], inw[:, 256:512], inw[:, 1536:1552], z16, kr, krs], 1))
    com["w_tmB"] = _c(np.concatenate([inw[:, 256:512], inw[:, 512:1024]], 1))
    com["w_fmB"] = _c(np.concatenate([inw[:, 0:256], inw[:, 256:512], inw[:, 1552:1568]], 1))
    com["w_own"] = _c(np.concatenate([inw[:, 1024:1536], inw[:, 1568:1824]], 1))
    Ca = np.ones((64, TALL), f32); Sa = np.zeros((64, TALL), f32)
    Ca[:, 256:] = np.concatenate([cos.T, cos.T], 0); Sa[:, 256:] = np.concatenate([-sin.T, sin.T], 0)
    com["Ca"] = Ca; com["Sa"] = Sa
    wk = A(mla_wukv)[0].reshape(128, 4, 256)
    com["wukv"] = _c(np.concatenate([wk[:, :, :128].reshape(128, 512), wk[:, :, 128:].reshape(128, 512)], 1))
    com["kvn"] = _c(A(mla_kvnorm)[0].reshape(128, 1)); com["qn"] = _col(A(mla_qnorm)[0])
    wq = A(mla_wuq)[0]; com["wuq"] = _c(wq)
    com["wuqs"] = _c(np.concatenate([np.concatenate([wq[:, h * 192 + 160:h * 192 + 192], wq[:, h * 192 + 128:h * 192 + 160]], 1) for h in range(4)], 1))
    wa = np.zeros((17, 8, 64), f32)
    for d, (w_, b_) in enumerate(((A(gla_wa_f)[0], A(gla_ba_f)[0]), (A(gla_wa_b)[0], A(gla_ba_b)[0]))):
        for h in range(4):
            wa[:16, d * 4 + h] = w_[:, h * 64:(h + 1) * 64]; wa[16, d * 4 + h] = b_[h * 64:(h + 1) * 64]
    com["wa"] = wa
    com["tri"] = np.where(np.arange(64)[:, None] <= np.arange(64)[None, :], -1.0 / 16, 0.0).astype(f32)
    com["mu"] = _c(np.repeat((np.arange(64)[:, None] <= np.arange(64)[None, :]).astype(f32)[:, None, :], 8, 1))
    com["ow0"] = _c(A(ab_out_w)[0]); com["ow1"] = _c(A(na_out_w)[0]); com["onc"] = _c(A(gla_onorm)[0].reshape(128, 1))
    com["rw"] = _c(A(router_w)); com["rb"] = _c(np.stack([_bc(A(router_b)[0]), _bc(A(router_b)[1])]))
    com["w1"] = A(moe_w1); com["w2"] = A(moe_w2)
    b1 = A(moe_b1)
    colE = lambda a: _c(a.reshape(32, 8, 128).transpose(2, 0, 1))
    com["b1g"] = _c(np.stack([colE(b1[l][:, 0::2]) for l in range(2)])); com["b1l"] = _c(np.stack([colE(b1[l][:, 1::2]) for l in range(2)]))
    com["b1"] = _c(b1); com["iota"] = _bc(np.arange(128, dtype=f32)); com["lst"] = (np.arange(128)[:, None] < np.arange(128)[None, :]).astype(f32)
    com["b2"] = _c(A(moe_b2))
    qkv = A(na_qkv_w)[0]; com["wqk"] = _c(qkv[:, :2048]); com["wv"] = _c(qkv[:, 2048:])
    rpb = A(na_rpb)[0]
    com["Bint"] = na_bias_table(rpb, 4)
    maps = []
    for cc in range(8):
        m = dict(com)
        er = np.arange(32 * cc - 4, 32 * cc + 36)
        erc = np.clip(er, 0, 255)
        tok = (erc[:, None] * 64 + np.arange(64)[None, :]).ravel()
        m["x_own"] = _c(np.concatenate([x[tok], ctx], 0))
        Cq = np.ones((64, TOWN), f32); Sq = np.zeros((64, TOWN), f32)
        Cq[:, :2560] = np.concatenate([cos[tok].T, cos[tok].T], 0); Sq[:, :2560] = np.concatenate([-sin[tok].T, sin[tok].T], 0)
        m["Cq"] = Cq; m["Sq"] = Sq
        posf = np.concatenate([256 + tok, np.arange(256)]); posb = np.concatenate([256 + (16383 - tok), 255 - np.arange(256)])
        m["idxf"] = np.ascontiguousarray(posf.reshape(NOWN, 128).T.astype(np.int32)); m["idxb"] = np.ascontiguousarray(posb.reshape(NOWN, 128).T.astype(np.int32))
        be = []
        for e in range(8):
            i = e if e < 4 else 24 + e
            e0 = 0 if e < 4 else 28
            be.append(na_bias_edge(rpb, 32 * cc + i, [int(er[e0 + w]) for w in range(12)]))
        m["Bedge"] = _c(np.stack(be))
        maps.append(m)
    return maps


def kernel(**inputs):
    if "P" not in _FP:
        _FP["P"] = build_fused()
    P = _FP["P"]
    maps = fused_inputs(**inputs)
    maps = [{k: m[k] for k in P.in_names} for m in maps]
    res = run_bass_kernel_spmd(P.nc, maps, core_ids=list(range(8)))
    return np.concatenate([r["out"] for r in res.results], 0)[None].astype(np.float32)


def stage_pro2(P, idb, x_d, groups, gvar, gsc, gsh, D, wtm_d, ntm, tm_outs, wfm_d, nfm, fm_specs, rope=None, mla=None, tag="pro", extra=None, per_tile=2):
    P.begin()
    KC = D // 128
    tiles = []
    for gi, (tok0, gt) in enumerate(groups):
        for t in range(gt):
            tiles.append((tok0 + t * 128, gvar[gi]))
    nt = len(tiles)
    stg = [P.sb("wst%d" % i, [128, max(ntm, nfm, 1024)]) for i in range(2)]
    wtm = load_w_bf16(P, wtm_d, D, ntm, "wtm", stage=stg) if ntm else None
    wfm = load_w_bf16(P, wfm_d, D, nfm, "wfm", stage=stg) if nfm else None
    R = 3
    ring = lambda nm, shp, dt=F32, n=R: [P.sb("%s%d" % (nm, i), shp, dt) for i in range(n)]
    if mla:
        wuk = load_w_bf16(P, mla["wukv_d"], 128, 1024, "wuk", stage=stg)
        kvn = P.sb("kvn", [128, 1]); P.dma("sp", kvn.v, mla["kvn_d"])
        ckn = ring("ckn", [128, 128], BF16); ckT = ring("ckT", [128, 128], BF16)
        kst = ring("kst", [128, 4, 128], BF16); vst = ring("vst", [128, 512], BF16)
        pck = P.ps("pck", [128, 128], BF16)
    xs = ring("x", [128, D]); junk = P.sb("junk", [128, D]); st = ring("st", [128, 8])
    xn = ring("xn", [128, D], BF16); hT = ring("hT", [128, KC, 128], BF16)
    yt = ring("yt", [128, max(ntm, 1)]); ytb = ring("ytb", [128, max(ntm, 1)], BF16)
    nspec = max(len(fm_specs), 1)
    fo = [[P.sb("fo%d_%d" % (i, j), [128, 128], (BF16 if fm_specs[j][3] == BF16 else F32)) for j in range(len(fm_specs))] for i in range(R)]
    pT = [P.ps("pT%d" % i, [128, KC, 128], BF16) for i in range(2)]
    py = [P.ps("py%d" % i, [128, 512]) for i in range(2)]; pf = [P.ps("pf%d" % i, [128, 4, 128]) for i in range(2)]
    if rope:
        Ct = ring("Ct", [64, 128]); St = ring("St", [64, 128]); r1 = ring("r1", [64, 128]); r2 = ring("r2", [64, 128]); rb_ = ring("rb", [64, 128], BF16)
    cnt = dict(y=0, f=0)

    def A(t):
        r0, v = tiles[t]; a = t % R; b = t % 2
        P.dma("sp", xs[a].v, x_d[r0:r0 + 128, :])
        P.op("act", "activation", out=junk.v, in_=xs[a].v, func=AF.Square, accum_out=st[a][:, 0:1])
        rstd_of(P, st[a][:, 0:1], st[a][:, 2:3], st[a][:, 1:2], D)
        P.op("dve", "tensor_scalar", out=xn[a].v, in0=xs[a].v, scalar1=st[a][:, 2:3], scalar2=None, op0=ALU.mult)
        for k in range(KC):
            P.op("pe", "transpose", out=pT[b][:, k, :], in_=xn[a][:, k * 128:(k + 1) * 128], identity=idb.v, track=(k == KC - 1))
        for k in range(KC):
            P.op("dve", "tensor_scalar", out=hT[a][:, k, :], in0=pT[b][:, k, :], scalar1=gsc[:, v, k:k + 1], scalar2=gsh[:, v, k:k + 1], op0=ALU.mult, op1=ALU.add)

    def B1(t):
        r0, v = tiles[t]; a = t % R
        if ntm:
            for c0 in range(0, ntm, 512):
                cn = min(512, ntm - c0); pb = py[cnt["y"] % 2]; cnt["y"] += 1
                for k in range(KC):
                    P.op("pe", "matmul", track=(k == KC - 1), out=pb[:, 0:cn], lhsT=hT[a][:, k, :], rhs=wtm[:, k, c0:c0 + cn], start=(k == 0), stop=(k == KC - 1))
                P.op("act", "activation", out=yt[a][:, c0:c0 + cn], in_=pb[:, 0:cn], func=AF.Copy)
            for (c0, n, ap, dt) in tm_outs:
                if dt == BF16:
                    P.op("pool", "tensor_copy", out=ytb[a][:, c0:c0 + n], in_=yt[a][:, c0:c0 + n])
                    P.dma("sp", ap[r0:r0 + 128, :], ytb[a][:, c0:c0 + n])
                else:
                    P.dma("sp", ap[r0:r0 + 128, :], yt[a][:, c0:c0 + n])
        if mla:
            cc = mla["col"]
            P.op("act", "activation", out=junk[:, 0:128], in_=yt[a][:, cc:cc + 128], func=AF.Square, accum_out=st[a][:, 4:5])
            rstd_of(P, st[a][:, 4:5], st[a][:, 6:7], st[a][:, 5:6], 128)
            P.op("dve", "tensor_scalar", out=ckn[a].v, in0=yt[a][:, cc:cc + 128], scalar1=st[a][:, 6:7], scalar2=None, op0=ALU.mult)
            P.op("pe", "transpose", out=pck.v, in_=ckn[a].v, identity=idb.v)
            P.op("dve", "tensor_scalar", out=ckT[a].v, in0=pck.v, scalar1=kvn[:, 0:1], scalar2=None, op0=ALU.mult)

    def B2(t):
        r0, v = tiles[t]; a = t % R
        for si in range(0, len(fm_specs), 4):
            chunk = fm_specs[si:si + 4]
            pb = pf[cnt["f"] % 2]; cnt["f"] += 1
            for j, (c0, m, ap, dt, scale) in enumerate(chunk):
                for k in range(KC):
                    P.op("pe", "matmul", track=(k == KC - 1), out=pb[0:m, j, :], lhsT=wfm[:, k, c0:c0 + m], rhs=hT[a][:, k, :], start=(k == 0), stop=(k == KC - 1))
            for j, (c0, m, ap, dt, scale) in enumerate(chunk):
                dst = fo[a][si + j]
                P.op("act", "activation", out=dst[0:m, :], in_=pb[0:m, j, :], func=AF.Copy, scale=float(scale))
                P.dma("sp", ap[:, r0:r0 + 128], dst[0:m, :])
        if rope:
            ckr, ckrs, ap, C_d, S_d = rope
            P.dma("sp", Ct[a].v, C_d[:, r0:r0 + 128]); P.dma("sp", St[a].v, S_d[:, r0:r0 + 128])
            pb = pf[cnt["f"] % 2]; cnt["f"] += 1
            for k in range(KC):
                P.op("pe", "matmul", track=(k == KC - 1), out=pb[0:64, 0, :], lhsT=wfm[:, k, ckr:ckr + 64], rhs=hT[a][:, k, :], start=(k == 0), stop=(k == KC - 1))
            for k in range(KC):
                P.op("pe", "matmul", track=(k == KC - 1), out=pb[0:64, 1, :], lhsT=wfm[:, k, ckrs:ckrs + 64], rhs=hT[a][:, k, :], start=(k == 0), stop=(k == KC - 1))
            P.op("dve", "tensor_tensor", out=r1[a].v, in0=pb[0:64, 0, :], in1=Ct[a].v, op=ALU.mult)
            P.op("dve", "tensor_tensor", out=r2[a].v, in0=pb[0:64, 1, :], in1=St[a].v, op=ALU.mult)
            P.op("pool", "tensor_tensor", out=rb_[a].v, in0=r1[a].v, in1=r2[a].v, op=ALU.add)
            P.dma("sp", ap[:, r0:r0 + 128], rb_[a].v)
        if mla:
            pb = py[cnt["y"] % 2]; cnt["y"] += 1
            P.op("pe", "matmul", out=pb.v, lhsT=ckT[a].v, rhs=wuk[:, 0, 512:1024], start=True, stop=True)
            P.op("act", "activation", out=vst[a].v, in_=pb.v, func=AF.Copy)
            P.dma("sp", mla["V_s"][r0:r0 + 128, :], vst[a].v)
            pb = pf[cnt["f"] % 2]; cnt["f"] += 1
            for h in range(4):
                P.op("pe", "matmul", out=pb[:, h, :], lhsT=wuk[:, 0, h * 128:(h + 1) * 128], rhs=ckT[a].v, start=True, stop=True, track=(h == 3))
            P.op("act", "activation", out=kst[a].v, in_=pb.v, func=AF.Copy)
            P.dma("sp", mla["kT_s"][:, :, r0:r0 + 128].rearrange("h p t -> p h t"), kst[a].v)

    xsteps = extra(P) if extra else []
    xi = 0
    for step in range(nt + 2):
        for _ in range(per_tile):
            if xi < len(xsteps):
                xsteps[xi](); xi += 1
        if step < nt:
            A(step)
        if 0 <= step - 1 < nt:
            B1(step - 1)
        if 0 <= step - 2 < nt:
            B2(step - 2)
    while xi < len(xsteps):
        xsteps[xi](); xi += 1
    P.end()


def stage_moe_sp(P, idb, variants, passes, hTok_s, G_s, GT_s, x1_s, w1_d, w2_d, b1_d, b2_d, g3_d, gates_s, l, x2_ap, iota_d, lst_d, NE=32):
    P.begin()
    nt = len(variants); nv = 2; D = 1024; KC = 8; F = 1024
    mp = max(sum(len(g) for g in ps_) for ps_ in passes)
    b2 = P.sb("b2", [32, D]); P.dma("sp", b2.v, b2_d)
    iota = P.sb("iota", [128, 128]); P.dma("sp", iota.v, iota_d)
    lst32 = P.sb("lst32", [128, 128]); P.dma("sp", lst32.v, lst_d)
    lst = P.sb("lst", [128, 128], BF16); P.op("dve", "tensor_copy", out=lst.v, in_=lst32.v)
    onesb = P.sb("onesb", [128, 128], BF16); P.op("dve", "memset", ap=onesb.v, constant=1.0)
    stg = [P.sb("stg%d" % i, [128, 2 * F]) for i in range(2)]
    w1b = [P.sb("w1b%d" % i, [128, KC, 2 * F], BF16) for i in range(2)]
    w2b = [P.sb("w2b%d" % i, [128, KC, D], BF16) for i in range(2)]
    b1b = [P.sb("b1b%d" % i, [1, 2 * F], BF16) for i in range(2)]
    Gs = P.sb("Gs", [128, mp, 32]); GTs = P.sb("GTs", [32, mp * 128]); msk = P.sb("msk", [128, mp, 32]); mskb = P.sb("mskb", [128, mp, 32], BF16)
    pos = P.sb("pos", [128, mp, 32])
    yacc = P.sb("yacc", [128, mp, D])
    htm = [P.sb("htm%d" % i, [128, 4, D], BF16) for i in range(2)]
    Sel = [[P.sb("Sel%d_%d" % (j, i), [128, 128], BF16) for i in range(4)] for j in range(2)]
    SelG = [[P.sb("SelG%d_%d" % (j, i), [128, 128], BF16) for i in range(4)] for j in range(2)]
    XeT = [P.sb("XeT%d" % i, [128, KC, 128], BF16) for i in range(2)]
    tgh = [P.sb("tg%d" % i, [128, 512]) for i in range(2)]; tlh = [P.sb("tl%d" % i, [128, 512], BF16) for i in range(2)]
    tsgh = [P.sb("tsg%d" % i, [128, 512], BF16) for i in range(2)]; actbh = [P.sb("actb%d" % i, [128, 512], BF16) for i in range(2)]
    actT = P.sb("actT", [128, KC, 128], BF16); Ye = P.sb("Ye", [128, D], BF16); SGT = P.sb("SGT", [128, 4, 128], BF16)
    st = P.sb("st", [128, 4])
    pX = P.ps("pX", [128, 1024]); pUa = P.ps("pUa", [128, 1024]); pUb = P.ps("pUb", [128, 1024])
    pT = P.ps("pT", [128, KC, 128], BF16); pST = P.ps("pST", [128, 4, 128], BF16)
    pXv = pX.v.rearrange("p (f q) -> p f q", q=128)

    def load_expert_steps(e, buf):
        steps = []
        for k in range(KC):
            def step(k=k):
                P.dma("sp", w1b[buf][:, k, :], w1_d[e, k * 128:(k + 1) * 128, :])
                P.dma("sp", w2b[buf][:, k, :], w2_d[e, k * 128:(k + 1) * 128, :])
                if k == KC - 1:
                    P.dma("sp", b1b[buf].v, b1_d[e:e + 1, :])
            steps.append(step)
        return steps

    for groups in passes:
        ptiles = [t for g in groups for t in g]
        t0 = ptiles[0]; npt = len(ptiles); tok0 = t0 * 128; ntok = npt * 128
        P.dma("sp", Gs[:, 0:npt, :], G_s[tok0:tok0 + ntok, :].rearrange("(g p) e -> p g e", p=128))
        P.dma("sp", GTs[:, 0:ntok], GT_s[:, tok0:tok0 + ntok])
        P.op("dve", "tensor_scalar", out=msk[:, 0:npt, :], in0=Gs[:, 0:npt, :], scalar1=0.0, scalar2=None, op0=ALU.is_gt)
        P.op("dve", "tensor_copy", out=mskb[:, 0:npt, :], in_=msk[:, 0:npt, :])
        for i in range(npt):
            for n in range(2):
                P.op("pe", "matmul", out=pX[:, n * 512:(n + 1) * 512], lhsT=GTs[:, i * 128:(i + 1) * 128], rhs=b2[:, n * 512:(n + 1) * 512], start=True, stop=True)
            P.op("act", "activation", out=yacc[:, i, :], in_=pX.v, func=AF.Copy)
        for g in groups:
            for gi_, t in enumerate(g):
                i = t - t0
                for j_ in range(gi_):
                    P.op("pe", "matmul", track=False, out=pUa[:, 0:32], lhsT=onesb.v, rhs=mskb[:, g[j_] - t0, :], start=(j_ == 0), stop=False)
                P.op("pe", "matmul", out=pUa[:, 0:32], lhsT=lst.v, rhs=mskb[:, i, :], start=(gi_ == 0), stop=True)
                P.op("dve", "tensor_copy", out=pos[:, i, :], in_=pUa[:, 0:32])
        for s_ in load_expert_steps(0, 0):
            s_()
        its = [(e, gidx) for e in range(NE) for gidx in range(len(groups))]
        nxt_cache = {}

        def prep(n):
            e, gidx = its[n]; g = groups[gidx]; ng = len(g); i0 = g[0] - t0; s2 = n % 2
            hb = htm[s2]
            P.dma("sp", hb[:, 0:ng, :], hTok_s[g[0] * 128:(g[0] + ng) * 128, :].rearrange("(g p) f -> p g f", p=128))
            for ii in range(ng):
                P.op("dve", "tensor_scalar", out=Sel[s2][ii].v, in0=iota.v, scalar1=pos[:, i0 + ii, e:e + 1], scalar2=msk[:, i0 + ii, e:e + 1], op0=ALU.is_equal, op1=ALU.mult)
                P.op("dve", "tensor_scalar", out=SelG[s2][ii].v, in0=iota.v, scalar1=pos[:, i0 + ii, e:e + 1], scalar2=Gs[:, i0 + ii, e:e + 1], op0=ALU.is_equal, op1=ALU.mult)
            for f in range(KC):
                for ii in range(ng):
                    P.op("pe", "matmul", track=(f == KC - 1 and ii == ng - 1), out=pXv[:, f, :], lhsT=hb[:, ii, f * 128:(f + 1) * 128], rhs=Sel[s2][ii].v, start=(ii == 0), stop=(ii == ng - 1))
            P.op("act", "activation", out=XeT[s2].v, in_=pXv, func=AF.Copy)

        def ffn1(n):
            e, gidx = its[n]; buf = e % 2; s2 = n % 2
            for h in range(2):
                pu = (pUa, pUb)[h]
                for c in range(2):
                    col = h * 1024 + c * 512
                    for k in range(KC):
                        P.op("pe", "matmul", track=False, out=pu[:, c * 512:(c + 1) * 512], lhsT=XeT[s2][:, k, :], rhs=w1b[buf][:, k, col:col + 512], start=(k == 0), stop=False)
                    P.op("pe", "matmul", track=(c == 1), out=pu[:, c * 512:(c + 1) * 512], lhsT=onesb[0:1, :], rhs=b1b[buf][0:1, col:col + 512], start=False, stop=True)
                uv = pu.v.rearrange("p (f two) -> p f two", two=2)
                P.op("dve", "tensor_scalar", out=tgh[h].v, in0=uv[:, :, 0], scalar1=7.0, scalar2=None, op0=ALU.min)
                P.op("act", "activation", out=tsgh[h].v, in_=tgh[h].v, func=AF.Sigmoid, scale=1.702)
                P.op("dve", "tensor_scalar", out=tlh[h].v, in0=uv[:, :, 1], scalar1=-7.0, scalar2=7.0, op0=ALU.max, op1=ALU.min)
                P.op("pool", "tensor_tensor", out=tgh[h].v, in0=tgh[h].v, in1=tsgh[h].v, op=ALU.mult)
                P.op("pool", "tensor_scalar", out=tlh[h].v, in0=tlh[h].v, scalar1=1.0, scalar2=None, op0=ALU.add)
                P.op("pool", "tensor_tensor", out=actbh[h].v, in0=tlh[h].v, in1=tgh[h].v, op=ALU.mult)

        def stage3(n):
            e, gidx = its[n]; g = groups[gidx]; ng = len(g); i0 = g[0] - t0; buf = e % 2; s2 = n % 2
            for f in range(KC):
                P.op("pe", "transpose", out=pT[:, f, :], in_=actbh[f // 4][:, (f % 4) * 128:(f % 4 + 1) * 128], identity=idb.v, track=(f == KC - 1))
            P.op("act", "activation", out=actT.v, in_=pT.v, func=AF.Copy)
            for ii in range(ng):
                P.op("pe", "transpose", out=pST[:, ii, :], in_=SelG[s2][ii].v, identity=idb.v, track=(ii == ng - 1))
            P.op("dve", "tensor_copy", out=SGT[:, 0:ng, :], in_=pST[:, 0:ng, :])
            for half in range(2):
                hs = slice(half * 512, (half + 1) * 512)
                for k in range(KC):
                    P.op("pe", "matmul", track=(k == KC - 1), out=pUa[:, hs], lhsT=actT[:, k, :], rhs=w2b[buf][:, k, hs], start=(k == 0), stop=(k == KC - 1))
            P.op("act", "activation", out=Ye.v, in_=pUa.v, func=AF.Copy)
            ci = 0
            for ii in range(ng):
                for half in range(2):
                    hs = slice(half * 512, (half + 1) * 512); pc = pUb[:, (ci % 2) * 512:(ci % 2 + 1) * 512]; ci += 1
                    P.op("pe", "matmul", out=pc, lhsT=SGT[:, ii, :], rhs=Ye[:, hs], start=True, stop=True)
                    P.op("dve", "tensor_tensor", out=yacc[:, i0 + ii, hs], in0=pc, in1=yacc[:, i0 + ii, hs], op=ALU.add)
            if e + 1 < NE:
                if e not in nxt_cache:
                    nxt_cache[e] = load_expert_steps(e + 1, 1 - buf)
                nxt = nxt_cache[e]
                per = (len(nxt) + len(groups) - 1) // len(groups)
                for s_ in nxt[gidx * per:(gidx + 1) * per]:
                    s_()

        prep(0)
        for n in range(len(its)):
            ffn1(n)
            if n + 1 < len(its):
                prep(n + 1)
            stage3(n)
        P.dma("sp", stg[1][:, 0:D], g3_d)
        for v in range(nv):
            P.dma("sp", stg[0][:, v * D:(v + 1) * D], gates_s[l, 1, v])
            P.op("dve", "tensor_tensor", out=stg[0][:, v * D:(v + 1) * D], in0=stg[0][:, v * D:(v + 1) * D], in1=stg[1][:, 0:D], op=ALU.mult)
        hf = htm[0].v.rearrange("p a b -> p (a b)").bitcast(F32)
        for i in range(npt):
            t = ptiles[i]; v = variants[t]; sl = slice(t * 128, (t + 1) * 128)
            xt = hf[:, 0:D]; junk = hf[:, D:2 * D]; tmp = stg[1][:, D:2 * D]
            P.dma("sp", xt, x1_s[sl, :])
            P.op("act", "activation", out=junk, in_=yacc[:, i, :], func=AF.Square, accum_out=st[:, 0:1])
            rstd_of(P, st[:, 0:1], st[:, 2:3], st[:, 1:2], D)
            P.op("dve", "scalar_tensor_tensor", out=tmp, in0=yacc[:, i, :], scalar=st[:, 2:3], in1=stg[0][:, v * D:(v + 1) * D], op0=ALU.mult, op1=ALU.mult)
            P.op("pool", "tensor_tensor", out=xt, in0=tmp, in1=xt, op=ALU.add)
            P.dma("sp", x2_ap[sl, :], xt)
    P.end()
```

```python
import numpy as np
import ml_dtypes
import concourse.bass as bass
import concourse.mybir as mybir
from concourse.bass_utils import run_bass_kernel_spmd

F32 = mybir.dt.float32
BF16 = mybir.dt.bfloat16
I32 = mybir.dt.int32
ALU = mybir.AluOpType
AF = mybir.ActivationFunctionType
AX = mybir.AxisListType

COMPUTE = ("pe", "act", "dve", "pool")


class View:
    __slots__ = ("b", "ap")

    def __init__(self, b, ap):
        self.b = b
        self.ap = ap

    def __getitem__(self, k):
        return View(self.b, self.ap[k])

    def bitcast(self, dt):
        return View(self.b, self.ap.bitcast(dt))

    def rearrange(self, s, **kw):
        return View(self.b, self.ap.rearrange(s, **kw))

    def to_broadcast(self, shape):
        return View(self.b, self.ap.to_broadcast(shape))


class Buf:
    def __init__(self, prog, t, name):
        self.p = prog
        self.t = t
        self.name = name
        self.w = None
        self.r = {}
        self.wsem = None
        self.wcnt = 0
        self.rsem = None
        self.rcnt = 0

    def __getitem__(self, k):
        return View(self, self.t[k])

    @property
    def v(self):
        return View(self, self.t.ap())


class Prog:
    def __init__(self, name="k"):
        self.nc = bass.Bass("TRN2", target_bir_lowering=False, name=name)
        nc = self.nc
        self.E = dict(pe=nc.tensor, act=nc.scalar, dve=nc.vector, pool=nc.gpsimd, sp=nc.sync)
        self.sem = {e: nc.alloc_semaphore("sem_" + e) for e in COMPUTE}
        self.cnt = {e: 0 for e in COMPUTE}
        self.pending = {e: False for e in COMPUTE}
        self.seen = {e: {} for e in self.E}
        self.bufs = []
        self.nsem = 4
        self.ninst = 0
        self.sempool = []
        self.semid = {}
        self.stack = None
        self.scope_bufs = None
        self.nscope = 0

    def sb(self, name, shape, dt=F32):
        if self.stack is not None:
            t = self.stack.enter_context(self.nc.sbuf_tensor("sb%d_%s" % (self.nscope, name), list(shape), dt))
        else:
            t = self.nc.alloc_sbuf_tensor("sb_" + name, list(shape), dt)
        b = Buf(self, t, name)
        self.bufs.append(b)
        if self.scope_bufs is not None:
            self.scope_bufs.append(b)
        return b

    def ps(self, name, shape, dt=F32):
        if self.stack is not None:
            t = self.stack.enter_context(self.nc.psum_tensor("ps%d_%s" % (self.nscope, name), list(shape), dt))
        else:
            t = self.nc.alloc_psum_tensor("ps_" + name, list(shape), dt)
        b = Buf(self, t, name)
        self.bufs.append(b)
        if self.scope_bufs is not None:
            self.scope_bufs.append(b)
        return b

    def scratch(self, name, shape, dt=F32, dbg=False):
        return self.nc.dram_tensor(name, list(shape), dt, kind="ExternalOutput" if dbg else "Internal").ap()

    def begin(self):
        from contextlib import ExitStack
        self.nscope += 1
        self.stack = ExitStack()
        self.scope_bufs = []

    def barrier(self):
        for e in self.E:
            for f in COMPUTE:
                if self.cnt[f]:
                    self._wait(e, f, self.sem[f], self.cnt[f])
            for b in self.bufs:
                if b.wsem is not None and b.wcnt:
                    self._wait(e, ("s", id(b.wsem)), b.wsem, 16 * b.wcnt)
                if b.rsem is not None and b.rcnt:
                    self._wait(e, ("s", id(b.rsem)), b.rsem, 16 * b.rcnt)

    def end(self):
        self.barrier()
        for b in self.scope_bufs:
            if b.wsem is not None:
                self.sempool.append([b.wsem, b.wcnt]); b.wsem = None
            if b.rsem is not None:
                self.sempool.append([b.rsem, b.rcnt]); b.rsem = None
            self.bufs.remove(b)
        self.stack.close()
        self.stack = None
        self.scope_bufs = None

    def _getsem(self, name):
        if self.sempool:
            s, c = self.sempool.pop()
            return s, c
        return self._newsem(name), 0

    def dram(self, name, shape, dt=F32, out=False):
        if not out:
            self.__dict__.setdefault("in_names", []).append(name)
        return self.nc.dram_tensor(name, list(shape), dt, kind="ExternalOutput" if out else "ExternalInput").ap()

    def _newsem(self, name):
        self.nsem += 1
        return self.nc.alloc_semaphore(name)

    def _wait(self, e, key, sem, val):
        if self.seen[e].get(key, 0) >= val:
            return
        self.E[e].wait_ge(sem, val)
        self.seen[e][key] = val

    def _w_done(self, e, b):
        if b.w is not None:
            f, n = b.w
            if not (f == "pe" and e == "pe"):
                self._wait(e, f, self.sem[f], n)
        if b.wcnt:
            self._wait(e, ("s", id(b.wsem)), b.wsem, 16 * b.wcnt)

    def _r_done(self, e, b):
        for f, n in b.r.items():
            if f == "pe" and e == "pe":
                continue
            self._wait(e, f, self.sem[f], n)
        if b.rcnt:
            self._wait(e, ("s", id(b.rsem)), b.rsem, 16 * b.rcnt)

    def op(self, e, fn, track=True, **kw):
        wkeys = ("out", "accum_out", "ap")
        reads = [v.b for k, v in kw.items() if isinstance(v, View) and k not in wkeys]
        writes = [kw[k].b for k in wkeys if k in kw and isinstance(kw[k], View)]
        for b in reads:
            self._w_done(e, b)
        for b in writes:
            self._w_done(e, b)
            self._r_done(e, b)
        args = {k: (v.ap if isinstance(v, View) else v) for k, v in kw.items()}
        ins = getattr(self.E[e], fn)(**args)
        self.ninst += 1
        seq = self.cnt[e] + 1
        if track:
            ins.then_inc(self.sem[e], 1)
            self.cnt[e] = seq
            self.pending[e] = False
        else:
            self.pending[e] = True
        for b in reads:
            b.r[e] = seq
        for b in writes:
            b.w = (e, seq)
            b.r = {}
        return ins

    def dma(self, q, out, in_, **kw):
        if isinstance(out, View):
            b = out.b
            self._w_done(q, b)
            self._r_done(q, b)
            if b.wsem is None:
                b.wsem, b.wcnt = self._getsem("dw%d_%s" % (self.nscope, b.name))
            ins = self.E[q].dma_start(out=out.ap, in_=in_, **kw)
            ins.then_inc(b.wsem, 16)
            b.wcnt += 1
            b.w = None
            b.r = {}
        else:
            b = in_.b
            self._w_done(q, b)
            if b.rsem is None:
                b.rsem, b.rcnt = self._getsem("dr%d_%s" % (self.nscope, b.name))
            ins = self.E[q].dma_start(out=out, in_=in_.ap, **kw)
            ins.then_inc(b.rsem, 16)
            b.rcnt += 1
        self.ninst += 1
        return ins

    def gather(self, out, src_ap, idx):
        q = "pool"
        b = out.b
        self._w_done(q, b); self._r_done(q, b); self._w_done(q, idx.b)
        if b.wsem is None:
            b.wsem, b.wcnt = self._getsem("dw%d_%s" % (self.nscope, b.name))
        ins = self.nc.gpsimd.indirect_dma_start(out=out.ap, out_offset=None, in_=src_ap,
                                                in_offset=bass.IndirectOffsetOnAxis(ap=idx.ap, axis=0))
        ins.then_inc(b.wsem, 16)
        b.wcnt += 1; b.w = None; b.r = {}
        idx.b.r["pool"] = self.cnt["pool"] + 1
        self.ninst += 1
        return ins

    def finish(self, q="sp"):
        for b in self.bufs:
            if b.rcnt:
                self._wait(q, ("s", id(b.rsem)), b.rsem, 16 * b.rcnt)
        for e in COMPUTE:
            if self.cnt[e]:
                self._wait(q, e, self.sem[e], self.cnt[e])
        return self.nc


def run(prog, in_maps, ncores=8, trace=False):
    res = run_bass_kernel_spmd(prog.nc, in_maps, core_ids=list(range(ncores)), trace=trace)
    return res


EPS = 1e-6


def ident(P):
    idf = P.sb("idf", [128, 128]); idb = P.sb("idb", [128, 128], BF16)
    P.op("pool", "memset", ap=idf.v, constant=1.0)
    P.op("pool", "affine_select", out=idf.v, in_=idf.v, pattern=[[-1, 128]], compare_op=ALU.is_equal, fill=0.0, base=0, channel_multiplier=1)
    P.op("pool", "tensor_copy", out=idb.v, in_=idf.v)
    return idf, idb


def load_w_bf16(P, w_d, K, N, name, q="sp", cast_eng="pool", stage=None):
    KC = (K + 127) // 128
    wb = P.sb(name, [128, KC, N], BF16)
    for k in range(KC):
        rows = min(128, K - k * 128)
        st = stage[k % len(stage)]
        P.dma(q, st[0:rows, 0:N], w_d[k * 128:k * 128 + rows, :])
        P.op(cast_eng, "tensor_copy", out=wb[0:rows, k, :], in_=st[0:rows, 0:N])
    return wb


def rstd_of(P, ss_view, out_view, tmp_view, D):
    P.op("dve", "tensor_scalar", out=tmp_view, in0=ss_view, scalar1=1.0 / D, scalar2=EPS, op0=ALU.mult, op1=ALU.add)
    P.op("act", "activation", out=tmp_view, in_=tmp_view, func=AF.Sqrt)
    P.op("dve", "reciprocal", out=out_view, in_=tmp_view)


def build_proj(D, N, variants, rope=None, name="proj"):
    P = Prog(name)
    nt = len(variants); nv = max(variants) + 1
    KC = D // 128
    x_d = P.dram("x", [nt * 128, D]); w_d = P.dram("w", [D, N]); y_d = P.dram("y", [nt * 128, N], out=True)
    g_d = P.dram("gcol", [128, KC]); sc_d = P.dram("sc", [128, nv, KC]); sh_d = P.dram("sh", [128, nv, KC])
    if rope:
        cs_d = P.dram("cs", [nt * 128, 2, rope[1], 32])
    idf, idb = ident(P)
    stage = [P.sb("wst%d" % i, [128, N]) for i in range(2)]
    wb = load_w_bf16(P, w_d, D, N, "wb", stage=stage)
    g = P.sb("g", [128, KC]); sc = P.sb("scm", [128, nv, KC]); sh = P.sb("shm", [128, nv, KC])
    P.dma("sp", g.v, g_d); P.dma("sp", sc.v, sc_d); P.dma("sp", sh.v, sh_d)
    for v in range(nv):
        P.op("dve", "scalar_tensor_tensor", out=sc[:, v, :], in0=sc[:, v, :], scalar=1.0, in1=g.v, op0=ALU.add, op1=ALU.mult)
    xs = [P.sb("x%d" % i, [128, D]) for i in range(2)]
    junk = P.sb("junk", [128, D]); st = [P.sb("st%d" % i, [128, 4]) for i in range(2)]
    xn = [P.sb("xn%d" % i, [128, D], BF16) for i in range(2)]
    hT = [P.sb("hT%d" % i, [128, KC, 128], BF16) for i in range(2)]
    ys = [P.sb("y%d" % i, [128, N]) for i in range(2)]
    pT = [P.ps("pT%d" % i, [128, KC, 128], BF16) for i in range(2)]
    py = [P.ps("py%d" % i, [128, 512]) for i in range(4)]
    if rope:
        cs = [P.sb("cs%d" % i, [128, 2, rope[1], 32]) for i in range(2)]
        rt = [P.sb("rt%d" % i, [128, rope[1], 32]) for i in range(4)]
    nchunks = [(c, min(512, N - c)) for c in range(0, N, 512)]
    ci = 0
    for t in range(nt):
        b = t % 2; v = variants[t]
        P.dma("sp", xs[b].v, x_d[t * 128:(t + 1) * 128, :])
        if rope and v == 0:
            P.dma("sp", cs[b].v, cs_d[t * 128:(t + 1) * 128])
        P.op("act", "activation", out=junk.v, in_=xs[b].v, func=AF.Square, accum_out=st[b][:, 0:1])
        rstd_of(P, st[b][:, 0:1], st[b][:, 2:3], st[b][:, 1:2], D)
        P.op("dve", "tensor_scalar", out=xn[b].v, in0=xs[b].v, scalar1=st[b][:, 2:3], scalar2=None, op0=ALU.mult)
        for k in range(KC):
            P.op("pe", "transpose", out=pT[b][:, k, :], in_=xn[b][:, k * 128:(k + 1) * 128], identity=idb.v)
        for k in range(KC):
            P.op("dve", "tensor_scalar", out=hT[b][:, k, :], in0=pT[b][:, k, :], scalar1=sc[:, v, k:k + 1], scalar2=sh[:, v, k:k + 1], op0=ALU.mult, op1=ALU.add)
        for (c0, cn) in nchunks:
            pb = py[ci % 4]; ci += 1
            for k in range(KC):
                P.op("pe", "matmul", track=(k == KC - 1), out=pb[:, 0:cn], lhsT=hT[b][:, k, :], rhs=wb[:, k, c0:c0 + cn], start=(k == 0), stop=(k == KC - 1))
            P.op("act", "activation", out=ys[b][:, c0:c0 + cn], in_=pb[:, 0:cn], func=AF.Copy)
        if rope and v == 0:
            col0, ns, stride, off = rope
            seg = ys[b][:, col0:col0 + ns * stride].rearrange("p (h d) -> p h d", d=stride)
            x1 = seg[:, :, off:off + 32]; x2 = seg[:, :, off + 32:off + 64]
            co = cs[b][:, 0]; si = cs[b][:, 1]
            P.op("pool", "tensor_tensor", out=rt[0].v, in0=x1, in1=co, op=ALU.mult)
            P.op("pool", "tensor_tensor", out=rt[1].v, in0=x2, in1=si, op=ALU.mult)
            P.op("pool", "tensor_tensor", out=rt[2].v, in0=x1, in1=si, op=ALU.mult)
            P.op("pool", "tensor_tensor", out=rt[3].v, in0=x2, in1=co, op=ALU.mult)
            P.op("pool", "tensor_tensor", out=x1, in0=rt[0].v, in1=rt[1].v, op=ALU.subtract)
            P.op("pool", "tensor_tensor", out=x2, in0=rt[2].v, in1=rt[3].v, op=ALU.add)
        P.dma("sp", y_d[t * 128:(t + 1) * 128, :], ys[b].v)
    P.finish()
    return P


def build_post(mode, variants, name="post"):
    P = Prog(name)
    nt = len(variants); nv = max(variants) + 1; T = nt * 128; D = 1024; KC = 8
    x_d = P.dram("x", [T, D]); ow_d = P.dram("ow", [D, D])
    g1_d = P.dram("g1g", [128, D]); gate_d = P.dram("gate", [nv, 128, D])
    g2_d = P.dram("gcol", [128, KC]); sc_d = P.dram("sc", [128, nv, KC]); sh_d = P.dram("sh", [128, nv, KC])
    rw_d = P.dram("rw", [D, 32]); rb_d = P.dram("rb", [128, 32])
    x1_d = P.dram("x1", [T, D], out=True); hT_d = P.dram("hT", [D, T], BF16, out=True); G_d = P.dram("G", [T, 32], out=True)
    if mode == "gla":
        of_d = P.dram("of", [T, 512]); ob_d = P.dram("ob", [T, 512]); r_d = P.dram("r", [T, 512]); om_d = P.dram("om", [T, 512])
        on_d = P.dram("oncol", [128, 1])
    else:
        aT_d = P.dram("aT", [D, T])
    idf, idb = ident(P)
    stage = [P.sb("wst%d" % i, [128, D]) for i in range(2)]
    wb = load_w_bf16(P, ow_d, D, D, "owb", stage=stage)
    rw = P.sb("rw", [128, KC, 32]); P.dma("sp", rw.v, rw_d.rearrange("(k p) n -> p k n", p=128))
    rb = P.sb("rb", [128, 32]); P.dma("sp", rb.v, rb_d)
    g2 = P.sb("g2", [128, KC]); sc = P.sb("scm", [128, nv, KC]); sh = P.sb("shm", [128, nv, KC])
    P.dma("sp", g2.v, g2_d); P.dma("sp", sc.v, sc_d); P.dma("sp", sh.v, sh_d)
    for v in range(nv):
        P.op("dve", "scalar_tensor_tensor", out=sc[:, v, :], in0=sc[:, v, :], scalar=1.0, in1=g2.v, op0=ALU.add, op1=ALU.mult)
    g1 = P.sb("g1", [128, D]); P.dma("sp", g1.v, g1_d)
    GG = []
    for v in range(nv):
        gg = P.sb("GG%d" % v, [128, D]); P.dma("sp", gg.v, gate_d[v])
        P.op("dve", "tensor_tensor", out=gg.v, in0=gg.v, in1=g1.v, op=ALU.mult)
        GG.append(gg)
    if mode == "gla":
        onc = P.sb("onc", [128, 1]); P.dma("sp", onc.v, on_d)
        tof = P.sb("tof", [128, 512]); tob = P.sb("tob", [128, 512]); tr = P.sb("tr", [128, 512]); tom = P.sb("tom", [128, 512])
        cat = P.sb("cat", [128, D], BF16)
        pT = P.ps("pT", [128, KC, 128], BF16)
    else:
        a32 = P.sb("a32", [128, KC, 128])
    aT = P.sb("aT", [128, KC, 128], BF16)
    xt = P.sb("xt", [128, D]); x1 = P.sb("x1", [128, D]); junk = P.sb("junk", [128, D]); st = P.sb("st", [128, 16])
    xn2 = P.sb("xn2", [128, D]); h2T = P.sb("h2T", [128, KC, 128]); h2Tb = P.sb("h2Tb", [128, KC, 128], BF16)
    lg = P.sb("lg", [128, 32]); t8 = P.sb("t8", [128, 8]); msk = P.sb("msk", [128, 32]); ex = P.sb("ex", [128, 32]); Gt = P.sb("Gt", [128, 32])
    py = [P.ps("py%d" % i, [128, 512]) for i in range(2)]
    pT32 = P.ps("pT32", [128, KC, 128])
    plg = P.ps("plg", [128, 32])
    for t in range(nt):
        v = variants[t]; sl = slice(t * 128, (t + 1) * 128)
        P.dma("sp", xt.v, x_d[sl, :])
        if mode == "gla":
            P.dma("sp", tof.v, of_d[sl, :]); P.dma("sp", tob.v, ob_d[sl, :]); P.dma("sp", tr.v, r_d[sl, :]); P.dma("sp", tom.v, om_d[sl, :])
            P.op("dve", "tensor_tensor", out=tof.v, in0=tof.v, in1=tob.v, op=ALU.add)
            for h in range(4):
                P.op("act", "activation", out=junk[:, 0:128], in_=tof[:, h * 128:(h + 1) * 128], func=AF.Square, accum_out=st[:, h:h + 1])
            rstd_of(P, st[:, 0:4], st[:, 8:12], st[:, 4:8], 128)
            P.op("act", "activation", out=tr.v, in_=tr.v, func=AF.Silu)
            for h in range(4):
                hs = slice(h * 128, (h + 1) * 128)
                P.op("dve", "scalar_tensor_tensor", out=cat[:, hs], in0=tof[:, hs], scalar=st[:, 8 + h:9 + h], in1=tr[:, hs], op0=ALU.mult, op1=ALU.mult)
            P.op("pool", "tensor_copy", out=cat[:, 512:1024], in_=tom.v)
            for k in range(KC):
                P.op("pe", "transpose", out=pT[:, k, :], in_=cat[:, k * 128:(k + 1) * 128], identity=idb.v)
            P.op("dve", "tensor_scalar", out=aT[:, 0:4, :], in0=pT[:, 0:4, :], scalar1=onc[:, 0:1], scalar2=None, op0=ALU.mult)
            P.op("act", "activation", out=aT[:, 4:8, :], in_=pT[:, 4:8, :], func=AF.Copy)
        else:
            P.dma("sp", a32.v, aT_d[:, sl].rearrange("(k p) t -> p k t", p=128))
            P.op("pool", "tensor_copy", out=aT.v, in_=a32.v)
        for n in range(2):
            for k in range(KC):
                P.op("pe", "matmul", track=(k == KC - 1), out=py[n].v, lhsT=aT[:, k, :], rhs=wb[:, k, n * 512:(n + 1) * 512], start=(k == 0), stop=(k == KC - 1))
            P.op("act", "activation", out=junk[:, 0:512], in_=py[n].v, func=AF.Square, accum_out=st[:, 12 + n:13 + n])
        P.op("dve", "tensor_tensor", out=st[:, 12:13], in0=st[:, 12:13], in1=st[:, 13:14], op=ALU.add)
        rstd_of(P, st[:, 12:13], st[:, 14:15], st[:, 13:14], D)
        for n in range(2):
            ns = slice(n * 512, (n + 1) * 512)
            P.op("dve", "scalar_tensor_tensor", out=x1[:, ns], in0=py[n].v, scalar=st[:, 14:15], in1=GG[v][:, ns], op0=ALU.mult, op1=ALU.mult)
        P.op("pool", "tensor_tensor", out=x1.v, in0=x1.v, in1=xt.v, op=ALU.add)
        P.dma("sp", x1_d[sl, :], x1.v)
        P.op("act", "activation", out=junk.v, in_=x1.v, func=AF.Square, accum_out=st[:, 15:16])
        rstd_of(P, st[:, 15:16], st[:, 4:5], st[:, 5:6], D)
        P.op("dve", "tensor_scalar", out=xn2.v, in0=x1.v, scalar1=st[:, 4:5], scalar2=None, op0=ALU.mult)
        for k in range(KC):
            P.op("pe", "transpose", out=pT32[:, k, :], in_=xn2[:, k * 128:(k + 1) * 128], identity=idf.v)
        for k in range(KC):
            P.op("dve", "tensor_scalar", out=h2T[:, k, :], in0=pT32[:, k, :], scalar1=sc[:, v, k:k + 1], scalar2=sh[:, v, k:k + 1], op0=ALU.mult, op1=ALU.add)
        P.op("pool", "tensor_copy", out=h2Tb.v, in_=h2T.v)
        P.dma("sp", hT_d[:, sl].rearrange("(k p) t -> p k t", p=128), h2Tb.v)
        for k in range(KC):
            P.op("pe", "matmul", track=(k == KC - 1), out=plg.v, lhsT=h2T[:, k, :], rhs=rw[:, k, :], start=(k == 0), stop=(k == KC - 1))
        P.op("dve", "tensor_tensor", out=lg.v, in0=plg.v, in1=rb.v, op=ALU.add)
        P.op("dve", "max", out=t8.v, in_=lg.v)
        P.op("dve", "tensor_scalar", out=msk.v, in0=lg.v, scalar1=t8[:, 3:4], scalar2=None, op0=ALU.is_ge)
        P.op("dve", "tensor_scalar", out=t8[:, 7:8], in0=t8[:, 0:1], scalar1=-1.0, scalar2=None, op0=ALU.mult)
        P.op("act", "activation", out=ex.v, in_=lg.v, func=AF.Exp, bias=t8[:, 7:8])
        P.op("dve", "tensor_tensor", out=ex.v, in0=ex.v, in1=msk.v, op=ALU.mult)
        P.op("dve", "tensor_reduce", out=t8[:, 6:7], in_=ex.v, axis=AX.X, op=ALU.add)
        P.op("dve", "reciprocal", out=t8[:, 5:6], in_=t8[:, 6:7])
        P.op("dve", "tensor_scalar", out=Gt.v, in0=ex.v, scalar1=t8[:, 5:6], scalar2=None, op0=ALU.mult)
        P.dma("sp", G_d[sl, :], Gt.v)
    P.finish()
    return P


def build_moe(variants, groups, NE=32, name="moe"):
    P = Prog(name)
    nt = len(variants); nv = max(variants) + 1; T = nt * 128; D = 1024; KC = 8; F = 1024
    hT_d = P.dram("hT", [D, T], BF16); G_d = P.dram("G", [T, 32]); GT_d = P.dram("GT", [32, T]); x1_d = P.dram("x1", [T, D])
    w1_d = P.dram("w1", [32, D, 2 * F]); w2_d = P.dram("w2", [32, F, D])
    b1g_d = P.dram("b1g", [128, 32, KC]); b1l_d = P.dram("b1l", [128, 32, KC]); b2_d = P.dram("b2", [32, D])
    g3_d = P.dram("g3g", [128, D]); gate_d = P.dram("gate", [nv, 128, D])
    x2_d = P.dram("x2", [T, D], out=True)
    mg = max(len(g) for g in groups)
    g3 = P.sb("g3", [128, D]); P.dma("sp", g3.v, g3_d)
    GG = []
    for v in range(nv):
        gg = P.sb("GG%d" % v, [128, D]); P.dma("sp", gg.v, gate_d[v])
        P.op("dve", "tensor_tensor", out=gg.v, in0=gg.v, in1=g3.v, op=ALU.mult)
        GG.append(gg)
    b1g = P.sb("b1g", [128, 32, KC]); b1l = P.sb("b1l", [128, 32, KC]); P.dma("sp", b1g.v, b1g_d); P.dma("sp", b1l.v, b1l_d)
    b2 = P.sb("b2", [32, D]); P.dma("sp", b2.v, b2_d)
    stg = [P.sb("stg%d" % i, [128, 2 * F]) for i in range(2)]
    w1g = [P.sb("w1g%d" % i, [128, KC, F], BF16) for i in range(2)]
    w1l = [P.sb("w1l%d" % i, [128, KC, F], BF16) for i in range(2)]
    w2b = [P.sb("w2b%d" % i, [128, KC, D], BF16) for i in range(2)]
    hT = P.sb("hT", [128, KC, mg * 128], BF16)
    Gs = P.sb("Gs", [128, mg, 32]); GTs = P.sb("GTs", [32, mg * 128])
    yacc = P.sb("yacc", [128, mg, D])
    actT = P.sb("actT", [128, KC, 512], BF16)
    tg = [P.sb("tg%d" % i, [128, 512]) for i in range(2)]; tsg = [P.sb("tsg%d" % i, [128, 512]) for i in range(2)]
    tl = [P.sb("tl%d" % i, [128, 512]) for i in range(2)]
    xt = P.sb("xt", [128, D]); junk = P.sb("junk", [128, D]); st = P.sb("st", [128, 4])
    pg = [P.ps("pg%d" % i, [128, 512]) for i in range(2)]; pl = [P.ps("pl%d" % i, [128, 512]) for i in range(2)]
    py = [P.ps("py%d" % i, [128, 512]) for i in range(2)]
    sti = [0]

    def load_expert_steps(e, buf):
        steps = []
        for k in range(KC):
            def step(k=k):
                s = stg[sti[0] % 2]; sti[0] += 1
                P.dma("sp", s.v, w1_d[e, k * 128:(k + 1) * 128, :])
                sv = s.v.rearrange("p (f two) -> p f two", two=2)
                P.op("act", "activation", out=w1g[buf][:, k, :], in_=sv[:, :, 0], func=AF.Copy)
                P.op("act", "activation", out=w1l[buf][:, k, :], in_=sv[:, :, 1], func=AF.Copy)
                s = stg[sti[0] % 2]; sti[0] += 1
                P.dma("sp", s[:, 0:D], w2_d[e, k * 128:(k + 1) * 128, :])
                P.op("act", "activation", out=w2b[buf][:, k, :], in_=s[:, 0:D], func=AF.Copy)
            steps.append(step)
        return steps

    for grp in groups:
        ng = len(grp)
        t0 = grp[0]; tok0 = t0 * 128; ntok = ng * 128
        P.dma("sp", hT[:, :, 0:ntok], hT_d[:, tok0:tok0 + ntok].rearrange("(k p) t -> p k t", p=128))
        P.dma("sp", Gs[:, 0:ng, :], G_d[tok0:tok0 + ntok, :].rearrange("(g p) e -> p g e", p=128))
        P.dma("sp", GTs[:, 0:ntok], GT_d[:, tok0:tok0 + ntok])
        for i in range(ng):
            for n in range(2):
                P.op("pe", "matmul", out=py[n].v, lhsT=GTs[:, i * 128:(i + 1) * 128], rhs=b2[:, n * 512:(n + 1) * 512], start=True, stop=True)
                P.op("act", "activation", out=yacc[:, i, n * 512:(n + 1) * 512], in_=py[n].v, func=AF.Copy)
        for s in load_expert_steps(0, 0):
            s()
        blocks = [(b0, min(512, ntok - b0)) for b0 in range(0, ntok, 512)]
        ci = 0
        for e in range(NE):
            buf = e % 2
            nxt = load_expert_steps(e + 1, 1 - buf) if e + 1 < NE else []
            for bi, (b0, bn) in enumerate(blocks):
                for j in range(KC):
                    if bi == 0 and nxt:
                        nxt[j]()
                    c = ci % 2; ci += 1
                    for k in range(KC):
                        P.op("pe", "matmul", track=(k == KC - 1), out=pg[c][:, 0:bn], lhsT=w1g[buf][:, k, j * 128:(j + 1) * 128], rhs=hT[:, k, b0:b0 + bn], start=(k == 0), stop=(k == KC - 1))
                    for k in range(KC):
                        P.op("pe", "matmul", track=(k == KC - 1), out=pl[c][:, 0:bn], lhsT=w1l[buf][:, k, j * 128:(j + 1) * 128], rhs=hT[:, k, b0:b0 + bn], start=(k == 0), stop=(k == KC - 1))
                    P.op("dve", "tensor_scalar", out=tg[c][:, 0:bn], in0=pg[c][:, 0:bn], scalar1=b1g[:, e, j:j + 1], scalar2=7.0, op0=ALU.add, op1=ALU.min)
                    P.op("act", "activation", out=tsg[c][:, 0:bn], in_=tg[c][:, 0:bn], func=AF.Sigmoid, scale=1.702)
                    P.op("dve", "tensor_scalar", out=tl[c][:, 0:bn], in0=pl[c][:, 0:bn], scalar1=b1l[:, e, j:j + 1], scalar2=-7.0, op0=ALU.add, op1=ALU.max)
                    P.op("dve", "tensor_scalar", out=tl[c][:, 0:bn], in0=tl[c][:, 0:bn], scalar1=7.0, scalar2=1.0, op0=ALU.min, op1=ALU.add)
                    P.op("pool", "tensor_tensor", out=tg[c][:, 0:bn], in0=tg[c][:, 0:bn], in1=tsg[c][:, 0:bn], op=ALU.mult)
                    P.op("pool", "tensor_tensor", out=actT[:, j, 0:bn], in0=tg[c][:, 0:bn], in1=tl[c][:, 0:bn], op=ALU.mult)
                for i in range(b0 // 128, (b0 + bn) // 128):
                    for n in range(2):
                        ns = slice(n * 512, (n + 1) * 512)
                        for j in range(KC):
                            P.op("pe", "matmul", track=(j == KC - 1), out=py[n].v, lhsT=actT[:, j, i * 128 - b0:(i + 1) * 128 - b0], rhs=w2b[buf][:, j, ns], start=(j == 0), stop=(j == KC - 1))
                        P.op("dve", "scalar_tensor_tensor", out=yacc[:, i, ns], in0=py[n].v, scalar=Gs[:, i, e:e + 1], in1=yacc[:, i, ns], op0=ALU.mult, op1=ALU.add)
        for i in range(ng):
            t = grp[i]; v = variants[t]; sl = slice(t * 128, (t + 1) * 128)
            P.dma("sp", xt.v, x1_d[sl, :])
            P.op("act", "activation", out=junk.v, in_=yacc[:, i, :], func=AF.Square, accum_out=st[:, 0:1])
            rstd_of(P, st[:, 0:1], st[:, 2:3], st[:, 1:2], D)
            P.op("dve", "scalar_tensor_tensor", out=junk.v, in0=yacc[:, i, :], scalar=st[:, 2:3], in1=GG[v].v, op0=ALU.mult, op1=ALU.mult)
            P.op("pool", "tensor_tensor", out=xt.v, in0=junk.v, in1=xt.v, op=ALU.add)
            P.dma("sp", x2_d[sl, :], xt.v)
    P.finish()
    return P


def build_ada(name="ada"):
    P = Prog(name)
    NC_ = 1536
    w_d = P.dram("w", [1024, NC_]); b_d = P.dram("b", [2, NC_]); c_d = P.dram("cT", [128, 8, 2]); o_d = P.dram("mod", [2, NC_], out=True)
    w = P.sb("w", [128, 8, NC_]); P.dma("sp", w.v, w_d.rearrange("(k p) n -> p k n", p=128))
    b = P.sb("b", [2, NC_]); P.dma("sp", b.v, b_d)
    c = P.sb("c", [128, 8, 2]); P.dma("sp", c.v, c_d)
    o = P.sb("o", [2, NC_])
    P.op("act", "activation", out=c.v, in_=c.v, func=AF.Silu)
    pp = [P.ps("pp%d" % i, [2, 512]) for i in range(3)]
    for n in range(3):
        for k in range(8):
            P.op("pe", "matmul", track=(k == 7), out=pp[n].v, lhsT=c[:, k, :], rhs=w[:, k, n * 512:(n + 1) * 512], start=(k == 0), stop=(k == 7))
        P.op("dve", "tensor_tensor", out=o[:, n * 512:(n + 1) * 512], in0=pp[n].v, in1=b[:, n * 512:(n + 1) * 512], op=ALU.add)
    P.dma("sp", o_d, o.v)
    P.finish()
    return P


def build_gla(nchunks, name="gla"):
    P = Prog(name)
    Tt = nchunks * 64; CB = 8
    qT_d = P.dram("qT", [64, Tt]); kT_d = P.dram("kT", [64, Tt]); k_d = P.dram("k", [Tt, 64]); v_d = P.dram("v", [Tt, 128])
    aT_d = P.dram("aT", [17, Tt]); wa_d = P.dram("wa", [17, 64]); tri_d = P.dram("tri", [64, 64]); mu_d = P.dram("mu", [64, CB, 64])
    o_d = P.dram("o", [Tt, 128], out=True)
    wa = P.sb("wa", [17, 64]); P.dma("sp", wa.v, wa_d)
    tri = P.sb("tri", [64, 64]); P.dma("sp", tri.v, tri_d)
    mu = P.sb("mu", [64, CB, 64]); P.dma("sp", mu.v, mu_d)
    one = P.sb("one", [64, 1]); P.op("dve", "memset", ap=one.v, constant=1.0)
    S = [P.sb("S%d" % i, [64, 128]) for i in range(2)]
    P.op("dve", "memset", ap=S[0].v, constant=0.0)
    tmp = [P.sb("tmp%d" % i, [64, 128]) for i in range(2)]
    L = lambda nm, shp: [P.sb("%s%d" % (nm, i), shp) for i in range(2)]
    qTb = L("qTb", [64, CB * 64]); kTb = L("kTb", [64, CB * 64]); aTb = L("aTb", [17, CB * 64]); kb = L("kb", [64, CB, 64]); vb = L("vb", [64, CB, 128])
    le = L("le", [64, CB, 64]); E1 = L("E1", [64, CB * 64]); E2 = L("E2", [64, CB * 64]); E3 = L("E3", [64, CB, 64])
    qs = L("qs", [64, CB * 64]); ks = L("ks", [64, CB * 64]); kd = L("kd", [64, CB, 64]); ATm = L("ATm", [64, CB, 64]); ob = L("ob", [64, CB, 128])
    pXA = P.ps("pXA", [64, CB, 64]); pb = P.ps("pb", [64, CB, 64]); pbT = P.ps("pbT", [64, CB, 64]); pA = P.ps("pA", [64, CB, 64])
    pKV = P.ps("pKV", [64, CB, 128]); po = P.ps("po", [64, CB, 128])
    cur = 0; ti = 0
    blocks = [(c0, min(CB, nchunks - c0)) for c0 in range(0, nchunks, CB)]
    for bi, (c0, n) in enumerate(blocks):
        u = bi % 2; t0 = c0 * 64; W = n * 64
        P.dma("sp", qTb[u][:, 0:W], qT_d[:, t0:t0 + W]); P.dma("sp", kTb[u][:, 0:W], kT_d[:, t0:t0 + W]); P.dma("sp", aTb[u][:, 0:W], aT_d[:, t0:t0 + W])
        P.dma("sp", kb[u][:, 0:n, :], k_d[t0:t0 + W, :].rearrange("(c p) d -> p c d", p=64))
        P.dma("sp", vb[u][:, 0:n, :], v_d[t0:t0 + W, :].rearrange("(c p) d -> p c d", p=64))
        for c in range(n):
            P.op("pe", "matmul", out=pXA[:, c, :], lhsT=aTb[u][:, c * 64:(c + 1) * 64], rhs=wa.v, start=True, stop=True, track=(c == n - 1))
        P.op("act", "activation", out=le[u][:, 0:n, :], in_=pXA[:, 0:n, :], func=AF.Exp, scale=-1.0)
        P.op("act", "activation", out=le[u][:, 0:n, :], in_=le[u][:, 0:n, :], func=AF.Ln, bias=one[:, 0:1])
        for c in range(n):
            P.op("pe", "matmul", out=pb[:, c, :], lhsT=tri.v, rhs=le[u][:, c, :], start=True, stop=True, track=False)
            P.op("pe", "matmul", out=pbT[:, c, :], lhsT=le[u][:, c, :], rhs=tri.v, start=True, stop=True, track=(c == n - 1))
        pbTf = pbT.v.rearrange("p c t -> p (c t)")
        P.op("act", "activation", out=E1[u][:, 0:W], in_=pbTf[:, 0:W], func=AF.Exp)
        P.op("act", "activation", out=E2[u][:, 0:W], in_=pbTf[:, 0:W], func=AF.Exp, scale=-1.0)
        P.op("act", "activation", out=E3[u][:, 0:n, :], in_=pb[:, 0:n, :], func=AF.Exp, scale=-1.0)
        P.op("dve", "scalar_tensor_tensor", out=qs[u][:, 0:W], in0=qTb[u][:, 0:W], scalar=0.125, in1=E1[u][:, 0:W], op0=ALU.mult, op1=ALU.mult)
        P.op("dve", "tensor_tensor", out=ks[u][:, 0:W], in0=kTb[u][:, 0:W], in1=E2[u][:, 0:W], op=ALU.mult)
        P.op("pool", "tensor_tensor", out=kd[u][:, 0:n, :], in0=kb[u][:, 0:n, :], in1=E3[u][:, 0:n, :], op=ALU.mult)
        for c in range(n):
            cs_ = slice(c * 64, (c + 1) * 64)
            P.op("pe", "matmul", out=pA[:, c, :], lhsT=ks[u][:, cs_], rhs=qs[u][:, cs_], start=True, stop=True, track=(c == n - 1))
        P.op("dve", "tensor_tensor", out=ATm[u][:, 0:n, :], in0=pA[:, 0:n, :], in1=mu[:, 0:n, :], op=ALU.mult)
        for c in range(n):
            P.op("pe", "matmul", out=pKV[:, c, :], lhsT=kd[u][:, c, :], rhs=vb[u][:, c, :], start=True, stop=True, track=(c == n - 1))
        for c in range(n):
            cs_ = slice(c * 64, (c + 1) * 64)
            P.op("pe", "matmul", out=po[:, c, :], lhsT=qs[u][:, cs_], rhs=S[cur].v, start=True, stop=False, track=False)
            P.op("pe", "matmul", out=po[:, c, :], lhsT=ATm[u][:, c, :], rhs=vb[u][:, c, :], start=False, stop=True)
            tb = tmp[ti % 2]; ti += 1
            P.op("dve", "tensor_tensor", out=tb.v, in0=S[cur].v, in1=pKV[:, c, :], op=ALU.add)
            P.op("dve", "tensor_scalar", out=S[1 - cur].v, in0=tb.v, scalar1=E1[u][:, c * 64 + 63:c * 64 + 64], scalar2=None, op0=ALU.mult)
            cur = 1 - cur
        P.op("act", "activation", out=ob[u][:, 0:n, :], in_=po[:, 0:n, :], func=AF.Copy)
        P.dma("sp", o_d[t0:t0 + W, :].rearrange("(c p) d -> p c d", p=64), ob[u][:, 0:n, :])
    P.finish()
    return P


def build_mla(groups, NK, name="mla"):
    P = Prog(name)
    NQ = max(q0 + nq for q0, nq, _ in groups)
    NB = NK // 128
    qT_d = P.dram("qT", [193, NQ]); kT_d = P.dram("kT", [193, NK]); v_d = P.dram("V", [NK, 129]); o_d = P.dram("O", [NQ, 128], out=True)
    idf, idb = ident(P)
    kTa = P.sb("kTa", [128, NK], BF16); kTb = P.sb("kTb", [65, NK], BF16); Vb = P.sb("Vb", [128, NB, 129], BF16)
    stg = [P.sb("stg%d" % i, [128, 2048]) for i in range(2)]
    si = 0
    for c0 in range(0, NK, 2048):
        cn = min(2048, NK - c0)
        for (r0, rn, dst) in ((0, 128, kTa), (128, 65, kTb)):
            s = stg[si % 2]; si += 1
            P.dma("sp", s[0:rn, 0:cn], kT_d[r0:r0 + rn, c0:c0 + cn])
            P.op("pool" if si % 2 else "act", "tensor_copy" if si % 2 else "copy", out=dst[0:rn, c0:c0 + cn], in_=s[0:rn, 0:cn])
    VB = 15
    for b0 in range(0, NB, VB):
        bn = min(VB, NB - b0)
        s = stg[si % 2]; si += 1
        sv = s[:, 0:bn * 129].rearrange("p (b f) -> p b f", f=129)
        P.dma("sp", sv, v_d[b0 * 128:(b0 + bn) * 128, :].rearrange("(b p) f -> p b f", p=128))
        P.op("pool" if si % 2 else "act", "tensor_copy" if si % 2 else "copy", out=Vb[:, b0:b0 + bn, :], in_=sv)
    qst = [P.sb("qst%d" % i, [128, 512]) for i in range(2)]
    qa = [P.sb("qa%d" % i, [128, 512], BF16) for i in range(2)]; qb = [P.sb("qb%d" % i, [65, 512], BF16) for i in range(2)]
    mx = P.sb("mx", [128, 4, 40]); negm = P.sb("negm", [128, 4]); Z = P.sb("Z", [128, 65])
    P.op("dve", "memset", ap=Z.v, constant=0.0)
    pt = [P.sb("pt%d" % i, [128, 512], BF16) for i in range(2)]
    rl = P.sb("rl", [128, 4]); ot = [P.sb("ot%d" % i, [128, 4, 128]) for i in range(2)]
    ps1 = [P.ps("ps1_%d" % i, [128, 512]) for i in range(2)]; pst = [P.ps("pst%d" % i, [128, 512]) for i in range(2)]
    pO = [P.ps("pO%d" % i, [128, 129]) for i in range(4)]
    scale = 192.0 ** -0.5
    c1 = 0; c2 = 0
    for gi, (q0, nq, nkeys) in enumerate(groups):
        u = gi % 2; nqt = nq // 128
        P.dma("sp", qst[0][:, 0:nq], qT_d[0:128, q0:q0 + nq]); P.dma("sp", qst[1][0:64, 0:nq], qT_d[128:192, q0:q0 + nq])
        P.op("pool", "tensor_scalar", out=qa[u][:, 0:nq], in0=qst[0][:, 0:nq], scalar1=scale, scalar2=None, op0=ALU.mult)
        P.op("pool", "tensor_scalar", out=qb[u][0:64, 0:nq], in0=qst[1][0:64, 0:nq], scalar1=scale, scalar2=None, op0=ALU.mult)
        kblocks = [(k0, min(512, nkeys - k0)) for k0 in range(0, nkeys, 512)]
        for qt in range(nqt):
            qs_ = slice(qt * 128, (qt + 1) * 128)
            for bi, (k0, kn) in enumerate(kblocks):
                pb = ps1[c1 % 2]; c1 += 1
                P.op("pe", "matmul", track=False, out=pb[:, 0:kn], lhsT=qa[u][:, qs_], rhs=kTa[:, k0:k0 + kn], start=True, stop=False)
                P.op("pe", "matmul", out=pb[:, 0:kn], lhsT=qb[u][0:64, qs_], rhs=kTb[0:64, k0:k0 + kn], start=False, stop=True)
                P.op("dve", "tensor_reduce", out=mx[:, qt, bi:bi + 1], in_=pb[:, 0:kn], axis=AX.X, op=ALU.max)
            P.op("dve", "tensor_reduce", out=negm[:, qt:qt + 1], in_=mx[:, qt, 0:len(kblocks)], axis=AX.X, op=ALU.max)
            P.op("dve", "tensor_scalar", out=Z[:, 64:65], in0=negm[:, qt:qt + 1], scalar1=-1.0, scalar2=None, op0=ALU.mult)
            pz = ps1[c1 % 2]; c1 += 1
            P.op("pe", "matmul", out=pz[0:65, 0:128], lhsT=Z.v, rhs=idf.v, start=True, stop=True)
            P.op("dve", "tensor_copy", out=qb[u][64:65, qs_], in_=pz[64:65, 0:128])
        nkb = nkeys // 128
        for kb_ in range(nkb):
            ks_ = slice(kb_ * 128, (kb_ + 1) * 128)
            pb = pst[c2 % 2]; ptb = pt[c2 % 2]; c2 += 1
            P.op("pe", "matmul", track=False, out=pb[:, 0:nq], lhsT=kTa[:, ks_], rhs=qa[u][:, 0:nq], start=True, stop=False)
            P.op("pe", "matmul", out=pb[:, 0:nq], lhsT=kTb[0:65, ks_], rhs=qb[u][0:65, 0:nq], start=False, stop=True)
            P.op("act", "activation", out=ptb[:, 0:nq], in_=pb[:, 0:nq], func=AF.Exp)
            for qt in range(nqt):
                P.op("pe", "matmul", track=(kb_ == nkb - 1), out=pO[qt].v, lhsT=ptb[:, qt * 128:(qt + 1) * 128], rhs=Vb[:, kb_, :], start=(kb_ == 0), stop=(kb_ == nkb - 1))
        for qt in range(nqt):
            P.op("dve", "reciprocal", out=rl[:, qt:qt + 1], in_=pO[qt][:, 128:129])
            P.op("dve", "tensor_scalar", out=ot[u][:, qt, :], in0=pO[qt][:, 0:128], scalar1=rl[:, qt:qt + 1], scalar2=None, op0=ALU.mult)
        P.dma("sp", o_d[q0:q0 + nq, :].rearrange("(t p) d -> p t d", p=128), ot[u][:, 0:nqt, :])
    P.finish()
    return P


def build_na(nrows, edge_rows, name="na"):
    P = Prog(name)
    T = nrows * 64; ne = max(1, len(edge_rows))
    qT_d = P.dram("qT", [1024, T]); kw_d = P.dram("kwin", [nrows, 1024, 512]); vw_d = P.dram("vwin", [nrows, 512, 1024])
    kc_d = P.dram("kcT", [1024, 256]); vc_d = P.dram("vc", [256, 1024])
    bi_d = P.dram("Bint", [128, 8, 512]); be_d = P.dram("Bedge", [ne, 128, 8, 512])
    oT_d = P.dram("oT", [1024, T], out=True)
    idf, idb = ident(P)
    ones = P.sb("ones", [128, 128], BF16); P.op("pool", "memset", ap=ones.v, constant=1.0)
    kst = P.sb("kst", [128, 8, 512]); vst = P.sb("vst", [128, 4, 1024])
    kb = [P.sb("kb%d" % i, [128, 8, 512], BF16) for i in range(2)]; vb = [P.sb("vb%d" % i, [128, 4, 1024], BF16) for i in range(2)]
    kcb = P.sb("kcb", [128, 8, 256], BF16); vcb = P.sb("vcb", [128, 2, 1024], BF16)
    P.dma("sp", kst[:, :, 0:256], kc_d.rearrange("(k p) n -> p k n", p=128)); P.op("pool", "tensor_copy", out=kcb.v, in_=kst[:, :, 0:256])
    P.dma("sp", vst[:, 0:2, :], vc_d.rearrange("(b p) f -> p b f", p=128)); P.op("pool", "tensor_copy", out=vcb.v, in_=vst[:, 0:2, :])
    Bi = P.sb("Bi", [128, 8, 512]); P.dma("sp", Bi.v, bi_d)
    Be = [P.sb("Be%d" % i, [128, 8, 512]) for i in range(2)]
    qrow = [P.sb("qrow%d" % i, [128, 8, 64]) for i in range(2)]
    QBD = [P.sb("QBD%d" % i, [128, 128], BF16) for i in range(2)]
    for z in QBD:
        P.op("pool", "memset", ap=z.v, constant=0.0)
    Sb = [P.sb("Sb%d" % i, [128, 768]) for i in range(2)]; Pm = [P.sb("Pm%d" % i, [128, 768], BF16) for i in range(2)]
    PT = [P.sb("PT%d" % i, [128, 6, 128], BF16) for i in range(2)]
    sm = P.sb("sm", [128, 4]); rl = P.sb("rl", [128, 128]); orow = [P.sb("orow%d" % i, [128, 8, 64]) for i in range(2)]
    pS = [P.ps("pS%d" % i, [128, 1024]) for i in range(2)]
    pPT = P.ps("pPT", [128, 6, 128], BF16); pO = P.ps("pO", [128, 128]); pL = P.ps("pL", [128, 128])
    it = 0; nbe = 0
    for i in range(nrows):
        u = i % 2
        P.dma("sp", kst.v, kw_d[i].rearrange("(k p) n -> p k n", p=128)); P.op("pool", "tensor_copy", out=kb[u].v, in_=kst.v)
        P.dma("sp", vst.v, vw_d[i].rearrange("(b p) f -> p b f", p=128)); P.op("pool", "tensor_copy", out=vb[u].v, in_=vst.v)
        P.dma("sp", qrow[u].v, qT_d[:, i * 64:(i + 1) * 64].rearrange("(k p) t -> p k t", p=128))
        if i in edge_rows:
            B = Be[nbe % 2]; nbe += 1
            P.dma("sp", B.v, be_d[edge_rows[i]])
        else:
            B = Bi
        for hp in range(8):
            w = it % 2; it += 1
            P.op("pool", "tensor_scalar", out=QBD[w][0:64, 0:64], in0=qrow[u][0:64, hp, :], scalar1=0.125, scalar2=None, op0=ALU.mult)
            P.op("pool", "tensor_scalar", out=QBD[w][64:128, 64:128], in0=qrow[u][64:128, hp, :], scalar1=0.125, scalar2=None, op0=ALU.mult)
            P.op("pe", "matmul", out=pS[w][:, 0:512], lhsT=QBD[w].v, rhs=kb[u][:, hp, :], start=True, stop=True, track=False)
            P.op("pe", "matmul", out=pS[w][:, 512:768], lhsT=QBD[w].v, rhs=kcb[:, hp, :], start=True, stop=True)
            P.op("dve", "tensor_tensor", out=Sb[w][:, 0:512], in0=pS[w][:, 0:512], in1=B[:, hp, :], op=ALU.add)
            P.op("act", "activation", out=Sb[w][:, 512:768], in_=pS[w][:, 512:768], func=AF.Copy)
            P.op("dve", "tensor_reduce", out=sm[:, 0:1], in_=Sb[w].v, axis=AX.X, op=ALU.max)
            P.op("dve", "tensor_scalar", out=sm[:, 1:2], in0=sm[:, 0:1], scalar1=-1.0, scalar2=None, op0=ALU.mult)
            P.op("act", "activation", out=Pm[w].v, in_=Sb[w].v, func=AF.Exp, bias=sm[:, 1:2])
            for b in range(6):
                P.op("pe", "transpose", out=pPT[:, b, :], in_=Pm[w][:, b * 128:(b + 1) * 128], identity=idb.v, track=(b == 5))
            P.op("dve", "tensor_copy", out=PT[w].v, in_=pPT.v)
            hs = slice(hp * 128, (hp + 1) * 128)
            for b in range(6):
                vblk = vb[u][:, b, hs] if b < 4 else vcb[:, b - 4, hs]
                P.op("pe", "matmul", out=pO.v, lhsT=vblk, rhs=PT[w][:, b, :], start=(b == 0), stop=(b == 5), track=(b == 5))
            for b in range(6):
                P.op("pe", "matmul", out=pL.v, lhsT=ones.v, rhs=PT[w][:, b, :], start=(b == 0), stop=(b == 5), track=(b == 5))
            P.op("dve", "reciprocal", out=rl.v, in_=pL.v)
            P.op("dve", "tensor_tensor", out=orow[u][0:64, hp, :], in0=pO[0:64, 0:64], in1=rl[0:64, 0:64], op=ALU.mult)
            P.op("dve", "tensor_tensor", out=orow[u][64:128, hp, :], in0=pO[64:128, 64:128], in1=rl[64:128, 64:128], op=ALU.mult)
        P.dma("sp", oT_d[:, i * 64:(i + 1) * 64].rearrange("(k p) t -> p k t", p=128), orow[u].v)
    P.finish()
    return P


def na_bias_table(rpb, d):
    col = np.arange(64)
    c0 = np.clip(col - 8, 0, 48)
    kcol = np.arange(64)
    inwin = (kcol[None, :] >= c0[:, None]) & (kcol[None, :] < c0[:, None] + 16)
    cidx = np.clip(kcol[None, :] - col[:, None] + 15, 0, 30)
    ridx = np.arange(8) - d + 7
    t = rpb[:, ridx][:, :, cidx]
    t = np.where(inwin[None, None], t, np.float32(-30000.0)).astype(np.float32)
    t = t.transpose(0, 2, 1, 3).reshape(8, 2, 64, 512)
    return np.ascontiguousarray(t.transpose(1, 2, 0, 3).reshape(128, 8, 512))


TALL = 16640
NOWN = 22
VOWN = [0] * 20 + [1] * 2
TOWN = NOWN * 128


def modcols(P, modcol, l, i, g_d_view, name):
    t = P.sb(name, [128, 2, 8])
    for s in range(2):
        P.op("dve", "scalar_tensor_tensor", out=t[:, s, :], in0=modcol[:, l, i * 8:(i + 1) * 8, s], scalar=1.0, in1=g_d_view, op0=ALU.add, op1=ALU.mult)
    return t


def stage_ada(P, adaw_d, bcol_d, brow_d, cT_d, modcol, gates_s):
    P.begin()
    c = P.sb("c", [128, 8, 2]); P.dma("sp", c.v, cT_d)
    P.op("act", "activation", out=c.v, in_=c.v, func=AF.Silu)
    ones = P.sb("ones", [128, 128]); P.op("dve", "memset", ap=ones.v, constant=1.0)
    crep = P.sb("crep", [128, 2, 8, 128])
    for s in range(2):
        for k in range(8):
            P.op("dve", "tensor_scalar", out=crep[:, s, k, :], in0=ones.v, scalar1=c[:, k, s:s + 1], scalar2=None, op0=ALU.mult)
    bcol = P.sb("bcol", [128, 2, 48]); P.dma("sp", bcol.v, bcol_d)
    wp = [P.sb("wp%d" % i, [128, 8, 512]) for i in range(2)]
    brow = P.sb("brow", [128, 512]); grow = [P.sb("grow%d" % i, [128, 512]) for i in range(2)]
    pc = [P.ps("pc%d" % i, [128, 2]) for i in range(2)]; pr = [P.ps("pr%d" % i, [128, 512]) for i in range(2)]
    ci = 0; ri = 0
    for l in range(2):
        for piece in range(12):
            w = wp[piece % 2]
            P.dma("sp", w.v, adaw_d[l][:, piece * 512:(piece + 1) * 512].rearrange("(k p) n -> p k n", p=128))
            for jj in range(4):
                j = piece * 4 + jj
                p_ = pc[ci % 2]; ci += 1
                for k in range(8):
                    P.op("pe", "matmul", track=(k == 7), out=p_.v, lhsT=w[:, k, jj * 128:(jj + 1) * 128], rhs=c[:, k, :], start=(k == 0), stop=(k == 7))
                P.op("dve", "tensor_scalar", out=modcol[:, l, j, :], in0=p_.v, scalar1=bcol[:, l, j:j + 1], scalar2=None, op0=ALU.add)
            if piece in (4, 5, 10, 11):
                g = 0 if piece < 6 else 1; half = piece % 2
                P.dma("sp", brow.v, brow_d[l, :, piece * 512:(piece + 1) * 512])
                for s in range(2):
                    p_ = pr[ri % 2]; gr = grow[ri % 2]; ri += 1
                    for k in range(8):
                        P.op("pe", "matmul", track=(k == 7), out=p_.v, lhsT=crep[:, s, k, :], rhs=w[:, k, :], start=(k == 0), stop=(k == 7))
                    P.op("dve", "tensor_tensor", out=gr.v, in0=p_.v, in1=brow.v, op=ALU.add)
                    P.dma("sp", gates_s[l, g, s, :, half * 512:(half + 1) * 512], gr.v)
    P.end()


def stage_pro(P, idb, x_d, groups, gvar, gsc, gsh, D, wtm_d, ntm, tm_outs, wfm_d, nfm, fm_specs, rope=None, mla=None, tag="pro"):
    P.begin()
    KC = D // 128
    stg = [P.sb("wst%d" % i, [128, max(ntm, nfm, 1024)]) for i in range(2)]
    wtm = load_w_bf16(P, wtm_d, D, ntm, "wtm", stage=stg) if ntm else None
    wfm = load_w_bf16(P, wfm_d, D, nfm, "wfm", stage=stg) if nfm else None
    if mla:
        wuk = load_w_bf16(P, mla["wukv_d"], 128, 1024, "wuk", stage=stg)
        kvn = P.sb("kvn", [128, 1]); P.dma("sp", kvn.v, mla["kvn_d"])
        ckn = [P.sb("ckn%d" % i, [128, 128], BF16) for i in range(2)]
        ckT = [P.sb("ckT%d" % i, [128, 512], BF16) for i in range(2)]
        kst = [P.sb("kst%d" % i, [128, 512], BF16) for i in range(2)]; vst = [P.sb("vst%d" % i, [128, 512], BF16) for i in range(2)]
        pck = P.ps("pck", [128, 128], BF16)
    xs = [P.sb("x%d" % i, [128, D]) for i in range(2)]; junk = P.sb("junk", [128, D]); st = [P.sb("st%d" % i, [128, 8]) for i in range(2)]
    xn = [P.sb("xn%d" % i, [128, D], BF16) for i in range(2)]
    hT = [P.sb("hT%d" % i, [128, KC, 512], BF16) for i in range(2)]
    yt = [P.sb("yt%d" % i, [128, max(ntm, 1)]) for i in range(2)]
    ytb = [P.sb("ytb%d" % i, [128, max(ntm, 1)], BF16) for i in range(2)]
    fo = [P.sb("fo%d" % i, [128, 512]) for i in range(2)]; fob = [P.sb("fob%d" % i, [128, 512], BF16) for i in range(2)]
    pT = [P.ps("pT%d" % i, [128, KC, 128], BF16) for i in range(2)]
    py = [P.ps("py%d" % i, [128, 512]) for i in range(2)]; pf = [P.ps("pf%d" % i, [128, 512]) for i in range(2)]
    if rope:
        Ct = [P.sb("Ct%d" % i, [64, 512]) for i in range(2)]; St = [P.sb("St%d" % i, [64, 512]) for i in range(2)]
        r1 = P.sb("r1", [64, 512]); r2 = P.sb("r2", [64, 512]); rb_ = [P.sb("rb%d" % i, [64, 512], BF16) for i in range(2)]
    ti = 0; yi = 0; fi = 0
    for gi, (tok0, gt) in enumerate(groups):
        v = gvar[gi]; W = gt * 128; hg = hT[gi % 2]
        for t in range(gt):
            b = ti % 2; ti += 1
            r0 = tok0 + t * 128
            P.dma("sp", xs[b].v, x_d[r0:r0 + 128, :])
            P.op("act", "activation", out=junk.v, in_=xs[b].v, func=AF.Square, accum_out=st[b][:, 0:1])
            rstd_of(P, st[b][:, 0:1], st[b][:, 2:3], st[b][:, 1:2], D)
            P.op("dve", "tensor_scalar", out=xn[b].v, in0=xs[b].v, scalar1=st[b][:, 2:3], scalar2=None, op0=ALU.mult)
            for k in range(KC):
                P.op("pe", "transpose", out=pT[b][:, k, :], in_=xn[b][:, k * 128:(k + 1) * 128], identity=idb.v)
            for k in range(KC):
                P.op("dve", "tensor_scalar", out=hg[:, k, t * 128:(t + 1) * 128], in0=pT[b][:, k, :], scalar1=gsc[:, v, k:k + 1], scalar2=gsh[:, v, k:k + 1], op0=ALU.mult, op1=ALU.add)
            if ntm:
                for c0 in range(0, ntm, 512):
                    cn = min(512, ntm - c0); pb = py[yi % 2]; yi += 1
                    for k in range(KC):
                        P.op("pe", "matmul", track=(k == KC - 1), out=pb[:, 0:cn], lhsT=hg[:, k, t * 128:(t + 1) * 128], rhs=wtm[:, k, c0:c0 + cn], start=(k == 0), stop=(k == KC - 1))
                    P.op("act", "activation", out=yt[b][:, c0:c0 + cn], in_=pb[:, 0:cn], func=AF.Copy)
                for (c0, n, ap, dt) in tm_outs:
                    if dt == BF16:
                        P.op("pool", "tensor_copy", out=ytb[b][:, c0:c0 + n], in_=yt[b][:, c0:c0 + n])
                        P.dma("sp", ap[r0:r0 + 128, :], ytb[b][:, c0:c0 + n])
                    else:
                        P.dma("sp", ap[r0:r0 + 128, :], yt[b][:, c0:c0 + n])
            if mla:
                cc = mla["col"]
                P.op("act", "activation", out=junk[:, 0:128], in_=yt[b][:, cc:cc + 128], func=AF.Square, accum_out=st[b][:, 4:5])
                rstd_of(P, st[b][:, 4:5], st[b][:, 6:7], st[b][:, 5:6], 128)
                P.op("dve", "tensor_scalar", out=ckn[b].v, in0=yt[b][:, cc:cc + 128], scalar1=st[b][:, 6:7], scalar2=None, op0=ALU.mult)
                P.op("pe", "transpose", out=pck.v, in_=ckn[b].v, identity=idb.v)
                P.op("dve", "tensor_scalar", out=ckT[gi % 2][:, t * 128:(t + 1) * 128], in0=pck.v, scalar1=kvn[:, 0:1], scalar2=None, op0=ALU.mult)
                pb = py[yi % 2]; yi += 1
                P.op("pe", "matmul", out=pb.v, lhsT=ckT[gi % 2][:, t * 128:(t + 1) * 128], rhs=wuk[:, 0, 512:1024], start=True, stop=True)
                vb_ = vst[ti % 2]
                P.op("act", "activation", out=vb_.v, in_=pb.v, func=AF.Copy)
                P.dma("sp", mla["V_s"][r0:r0 + 128, :], vb_.v)
        for (c0, m, ap, dt, scale) in fm_specs:
            pb = pf[fi % 2]; f32t = fo[fi % 2]; bft = fob[fi % 2]; fi += 1
            for k in range(KC):
                P.op("pe", "matmul", track=(k == KC - 1), out=pb[0:m, 0:W], lhsT=wfm[:, k, c0:c0 + m], rhs=hg[:, k, 0:W], start=(k == 0), stop=(k == KC - 1))
            dst = bft if dt == BF16 else f32t
            P.op("act", "activation", out=dst[0:m, 0:W], in_=pb[0:m, 0:W], func=AF.Copy, scale=float(scale))
            P.dma("sp", ap[:, tok0:tok0 + W], dst[0:m, 0:W])
        if rope:
            ckr, ckrs, ap, C_d, S_d = rope
            u = gi % 2
            P.dma("sp", Ct[u][:, 0:W], C_d[:, tok0:tok0 + W]); P.dma("sp", St[u][:, 0:W], S_d[:, tok0:tok0 + W])
            pa = pf[fi % 2]; fi += 1; pb = pf[fi % 2]; fi += 1
            for k in range(KC):
                P.op("pe", "matmul", track=(k == KC - 1), out=pa[0:64, 0:W], lhsT=wfm[:, k, ckr:ckr + 64], rhs=hg[:, k, 0:W], start=(k == 0), stop=(k == KC - 1))
            for k in range(KC):
                P.op("pe", "matmul", track=(k == KC - 1), out=pb[0:64, 0:W], lhsT=wfm[:, k, ckrs:ckrs + 64], rhs=hg[:, k, 0:W], start=(k == 0), stop=(k == KC - 1))
            P.op("dve", "tensor_tensor", out=r1[:, 0:W], in0=pa[0:64, 0:W], in1=Ct[u][:, 0:W], op=ALU.mult)
            P.op("dve", "tensor_tensor", out=r2[:, 0:W], in0=pb[0:64, 0:W], in1=St[u][:, 0:W], op=ALU.mult)
            P.op("pool", "tensor_tensor", out=rb_[u][:, 0:W], in0=r1[:, 0:W], in1=r2[:, 0:W], op=ALU.add)
            P.dma("sp", ap[:, tok0:tok0 + W], rb_[u][:, 0:W])
        if mla:
            for h in range(4):
                pb = pf[fi % 2]; kb_ = kst[fi % 2]; fi += 1
                P.op("pe", "matmul", out=pb[:, 0:W], lhsT=wuk[:, 0, h * 128:(h + 1) * 128], rhs=ckT[gi % 2][:, 0:W], start=True, stop=True)
                P.op("act", "activation", out=kb_[:, 0:W], in_=pb[:, 0:W], func=AF.Copy)
                P.dma("sp", mla["kT_s"][h, :, tok0:tok0 + W], kb_[:, 0:W])
    P.end()


def conv_items(w1_d, w2_d, b1_d, w1bf, w2bf, b1bf, l):
    items = [(b1_d[l], b1bf[l], 32, 2048, False)]
    for e in range(32):
        for k in range(8):
            ks = slice(k * 128, (k + 1) * 128)
            items.append((w1_d[l, e, ks, :], w1bf[l, e, ks, :], 128, 2048, True))
            items.append((w2_d[l, e, ks, :], w2bf[l, e, ks, :], 128, 1024, False))
    return items


def conv_steps(P, items):
    R = 6
    pools = {2048: ([P.sb("cstA%d" % i, [128, 2048]) for i in range(R)], [P.sb("cbfA%d" % i, [128, 2048], BF16) for i in range(R)]),
             1024: ([P.sb("cstB%d" % i, [128, 1024]) for i in range(R)], [P.sb("cbfB%d" % i, [128, 1024], BF16) for i in range(R)])}
    pcnt = {2048: 0, 1024: 0}
    state = dict(nxt=0, loaded=[], cast=[])

    def tick(k):
        for (i, dst, rows, n, de) in state["cast"]:
            P.dma("pool", dst, pools[n][1][i][0:rows, :])
        state["cast"] = []
        for (i, dst, rows, n, de) in state["loaded"]:
            if de:
                sv = pools[n][0][i].v.rearrange("p (f two) -> p f two", two=2)
                P.op("act", "activation", out=pools[n][1][i][0:rows, 0:n // 2], in_=sv[0:rows, :, 0], func=AF.Copy)
                P.op("act", "activation", out=pools[n][1][i][0:rows, n // 2:n], in_=sv[0:rows, :, 1], func=AF.Copy)
            else:
                P.op("act", "activation", out=pools[n][1][i][0:rows, :], in_=pools[n][0][i][0:rows, :], func=AF.Copy)
            state["cast"].append((i, dst, rows, n, de))
        state["loaded"] = []
        for _ in range(k):
            if state["nxt"] < len(items):
                src, dst, rows, n, de = items[state["nxt"]]; state["nxt"] += 1
                i = pcnt[n] % R; pcnt[n] += 1
                P.dma("pool", pools[n][0][i][0:rows, :], src)
                state["loaded"].append((i, dst, rows, n, de))
        return state["nxt"] < len(items) or state["loaded"] or state["cast"]
    return tick


def stage_gla(P, scans, wa_d, tri_d, mu_d, extra=None, per_block=4):
    P.begin()
    CB = 8
    tri = P.sb("tri", [64, 64]); P.dma("sp", tri.v, tri_d)
    mu = P.sb("mu", [64, CB, 64]); P.dma("sp", mu.v, mu_d)
    one = P.sb("one", [64, 1]); P.op("dve", "memset", ap=one.v, constant=1.0)
    wa = P.sb("wa", [17, 8, 64]); P.dma("sp", wa.v, wa_d)
    S = [P.sb("S%d" % i, [64, 128]) for i in range(2)]
    tmp = [P.sb("tmp%d" % i, [64, 128]) for i in range(2)]
    L = lambda nm, shp: [P.sb("%s%d" % (nm, i), shp) for i in range(2)]
    qTb = L("qTb", [64, CB * 64]); kTb = L("kTb", [64, CB * 64]); aTb = L("aTb", [17, CB * 64]); kb = L("kb", [64, CB, 64]); vb = L("vb", [64, CB, 128])
    for a in aTb:
        P.op("dve", "memset", ap=a.v, constant=1.0)
    le = L("le", [64, CB, 64]); E1 = L("E1", [64, CB * 64]); E2 = L("E2", [64, CB * 64]); E3 = L("E3", [64, CB, 64])
    qs = L("qs", [64, CB * 64]); ks = L("ks", [64, CB * 64]); kd = L("kd", [64, CB, 64]); ATm = L("ATm", [64, CB, 64]); ob = L("ob", [64, CB, 128])
    pXA = P.ps("pXA", [64, CB, 64]); pb = P.ps("pb", [64, CB, 64]); pbT = P.ps("pbT", [64, CB, 64]); pA = P.ps("pA", [64, CB, 64])
    pKV = P.ps("pKV", [64, CB, 128]); po = P.ps("po", [64, CB, 128])
    blocks = [(0, 4)] + [(4 + 8 * i, 8) for i in range(32)]
    bi = 0; ti = 0
    tick = extra(P) if extra else None
    for (qT_d, kT_d, k_d, v_d, aT_d, wi, o_d) in scans:
        cur = 0
        P.op("dve", "memset", ap=S[0].v, constant=0.0)
        for (c0, n) in blocks:
            u = bi % 2; bi += 1; t0 = c0 * 64; W = n * 64
            if tick:
                tick(per_block)
            P.dma("sp", qTb[u][:, 0:W], qT_d[:, t0:t0 + W]); P.dma("sp", kTb[u][:, 0:W], kT_d[:, t0:t0 + W]); P.dma("sp", aTb[u][0:16, 0:W], aT_d[:, t0:t0 + W])
            P.dma("sp", kb[u][:, 0:n, :], k_d[t0:t0 + W, :].rearrange("(c p) d -> p c d", p=64))
            P.dma("sp", vb[u][:, 0:n, :], v_d[t0:t0 + W, :].rearrange("(c p) d -> p c d", p=64))
            for c in range(n):
                P.op("pe", "matmul", out=pXA[:, c, :], lhsT=aTb[u][:, c * 64:(c + 1) * 64], rhs=wa[:, wi, :], start=True, stop=True, track=(c == n - 1))
            P.op("act", "activation", out=le[u][:, 0:n, :], in_=pXA[:, 0:n, :], func=AF.Exp, scale=-1.0)
            P.op("act", "activation", out=le[u][:, 0:n, :], in_=le[u][:, 0:n, :], func=AF.Ln, bias=one[:, 0:1])
            for c in range(n):
                P.op("pe", "matmul", out=pb[:, c, :], lhsT=tri.v, rhs=le[u][:, c, :], start=True, stop=True, track=False)
                P.op("pe", "matmul", out=pbT[:, c, :], lhsT=le[u][:, c, :], rhs=tri.v, start=True, stop=True, track=(c == n - 1))
            pbTf = pbT.v.rearrange("p c t -> p (c t)")
            P.op("act", "activation", out=E1[u][:, 0:W], in_=pbTf[:, 0:W], func=AF.Exp)
            P.op("act", "activation", out=E2[u][:, 0:W], in_=pbTf[:, 0:W], func=AF.Exp, scale=-1.0)
            P.op("act", "activation", out=E3[u][:, 0:n, :], in_=pb[:, 0:n, :], func=AF.Exp, scale=-1.0)
            P.op("dve", "scalar_tensor_tensor", out=qs[u][:, 0:W], in0=qTb[u][:, 0:W], scalar=0.125, in1=E1[u][:, 0:W], op0=ALU.mult, op1=ALU.mult)
            P.op("dve", "tensor_tensor", out=ks[u][:, 0:W], in0=kTb[u][:, 0:W], in1=E2[u][:, 0:W], op=ALU.mult)
            P.op("dve", "tensor_tensor", out=kd[u][:, 0:n, :], in0=kb[u][:, 0:n, :], in1=E3[u][:, 0:n, :], op=ALU.mult)
            for c in range(n):
                cs_ = slice(c * 64, (c + 1) * 64)
                P.op("pe", "matmul", out=pA[:, c, :], lhsT=ks[u][:, cs_], rhs=qs[u][:, cs_], start=True, stop=True, track=(c == n - 1))
            P.op("dve", "tensor_tensor", out=ATm[u][:, 0:n, :], in0=pA[:, 0:n, :], in1=mu[:, 0:n, :], op=ALU.mult)
            for c in range(n):
                P.op("pe", "matmul", out=pKV[:, c, :], lhsT=kd[u][:, c, :], rhs=vb[u][:, c, :], start=True, stop=True, track=(c == n - 1))
            for c in range(n):
                cs_ = slice(c * 64, (c + 1) * 64)
                P.op("pe", "matmul", out=po[:, c, :], lhsT=qs[u][:, cs_], rhs=S[cur].v, start=True, stop=False, track=False)
                P.op("pe", "matmul", out=po[:, c, :], lhsT=ATm[u][:, c, :], rhs=vb[u][:, c, :], start=False, stop=True)
                tb = tmp[ti % 2]; ti += 1
                P.op("dve", "tensor_tensor", out=tb.v, in0=S[cur].v, in1=pKV[:, c, :], op=ALU.add)
                P.op("dve", "tensor_scalar", out=S[1 - cur].v, in0=tb.v, scalar1=E1[u][:, c * 64 + 63:c * 64 + 64], scalar2=None, op0=ALU.mult)
                cur = 1 - cur
            P.op("act", "activation", out=ob[u][:, 0:n, :], in_=po[:, 0:n, :], func=AF.Copy)
            P.dma("sp", o_d[t0:t0 + W, :].rearrange("(c p) d -> p c d", p=64), ob[u][:, 0:n, :])
    while tick and tick(per_block):
        pass
    P.end()


def stage_mla_full(P, idf, idb, cq_s, qn_d, wuq_d, wuqs_d, Cq_d, Sq_d, kT_s, krT_s, V_s, om_s):
    P.begin()
    NK = TALL; NB = NK // 128; NQ = TOWN
    scale = 192.0 ** -0.5
    qa = P.sb("qa", [128, 4, NQ], BF16); qb = P.sb("qb", [65, 4, NQ], BF16)
    stg = [P.sb("wst%d" % i, [128, 768]) for i in range(2)]
    wuq = load_w_bf16(P, wuq_d, 256, 768, "wuq", stage=stg)
    wuqs = load_w_bf16(P, wuqs_d, 256, 256, "wuqs", stage=stg)
    qn = P.sb("qn", [128, 2]); P.dma("sp", qn.v, qn_d)
    cqT = P.sb("cqT", [128, 2, NQ], BF16)
    xs = [P.sb("x%d" % i, [128, 256]) for i in range(2)]; junk = P.sb("junk", [128, 256]); st = [P.sb("st%d" % i, [128, 4]) for i in range(2)]
    xn = [P.sb("xn%d" % i, [128, 256], BF16) for i in range(2)]
    ps1 = [P.ps("ps1_%d" % i, [128, 512]) for i in range(2)]; pst = [P.ps("pst%d" % i, [128, 512]) for i in range(2)]
    pO = [P.ps("pO%d" % i, [128, 512]) for i in range(4)]
    for t in range(NOWN):
        b = t % 2
        pT = pO[b].v.bitcast(BF16)
        P.dma("sp", xs[b].v, cq_s[t * 128:(t + 1) * 128, :])
        P.op("act", "activation", out=junk.v, in_=xs[b].v, func=AF.Square, accum_out=st[b][:, 0:1])
        rstd_of(P, st[b][:, 0:1], st[b][:, 2:3], st[b][:, 1:2], 256)
        P.op("dve", "tensor_scalar", out=xn[b].v, in0=xs[b].v, scalar1=st[b][:, 2:3], scalar2=None, op0=ALU.mult)
        for k in range(2):
            P.op("pe", "transpose", out=pT[:, k * 128:(k + 1) * 128], in_=xn[b][:, k * 128:(k + 1) * 128], identity=idb.v)
        for k in range(2):
            P.op("dve", "tensor_scalar", out=cqT[:, k, t * 128:(t + 1) * 128], in0=pT[:, k * 128:(k + 1) * 128], scalar1=qn[:, k:k + 1], scalar2=None, op0=ALU.mult)
    Ct = P.sb("Ct", [64, 512]); St = P.sb("St", [64, 512]); r1 = P.sb("r1", [64, 512]); r2 = P.sb("r2", [64, 512])
    for c0 in range(0, NQ, 512):
        cn = min(512, NQ - c0)
        P.dma("sp", Ct[:, 0:cn], Cq_d[:, c0:c0 + cn]); P.dma("sp", St[:, 0:cn], Sq_d[:, c0:c0 + cn])
        for h in range(4):
            for k in range(2):
                P.op("pe", "matmul", track=(k == 1), out=ps1[0][:, 0:cn], lhsT=wuq[:, k, h * 192:h * 192 + 128], rhs=cqT[:, k, c0:c0 + cn], start=(k == 0), stop=(k == 1))
            P.op("act", "activation", out=qa[:, h, c0:c0 + cn], in_=ps1[0][:, 0:cn], func=AF.Copy, scale=scale)
            for k in range(2):
                P.op("pe", "matmul", track=(k == 1), out=ps1[1][0:64, 0:cn], lhsT=wuq[:, k, h * 192 + 128:h * 192 + 192], rhs=cqT[:, k, c0:c0 + cn], start=(k == 0), stop=(k == 1))
            for k in range(2):
                P.op("pe", "matmul", track=(k == 1), out=pst[0][0:64, 0:cn], lhsT=wuqs[:, k, h * 64:(h + 1) * 64], rhs=cqT[:, k, c0:c0 + cn], start=(k == 0), stop=(k == 1))
            P.op("dve", "tensor_tensor", out=r1[:, 0:cn], in0=ps1[1][0:64, 0:cn], in1=Ct[:, 0:cn], op=ALU.mult)
            P.op("dve", "tensor_tensor", out=r2[:, 0:cn], in0=pst[0][0:64, 0:cn], in1=St[:, 0:cn], op=ALU.mult)
            P.op("dve", "tensor_tensor", out=r1[:, 0:cn], in0=r1[:, 0:cn], in1=r2[:, 0:cn], op=ALU.add)
            P.op("act", "activation", out=qb[0:64, h, c0:c0 + cn], in_=r1[:, 0:cn], func=AF.Copy, scale=scale)
    kTa = P.sb("kTa", [128, NK], BF16); kTb = P.sb("kTb", [65, NK], BF16); Vb = P.sb("Vb", [128, NB, 129], BF16)
    P.op("pool", "memset", ap=kTb.v, constant=1.0)
    P.op("pool", "memset", ap=Vb.v, constant=1.0)
    P.dma("sp", kTb[0:64, :], krT_s)
    mx = P.sb("mx", [128, 4, 40]); negm = P.sb("negm", [128, 4]); Z = P.sb("Z", [128, 65])
    P.op("dve", "memset", ap=Z.v, constant=0.0)
    pt = [P.sb("pt%d" % i, [128, 512], BF16) for i in range(2)]
    rl = P.sb("rl", [128, 4]); ot = [P.sb("ot%d" % i, [128, 4, 128]) for i in range(2)]
    groups = [(g * 512, 512, NK) for g in range(5)] + [(2560, 128, 256), (2688, 128, 256)]
    c1 = 0; c2 = 0; gi = 0
    for h in range(4):
        P.dma("sp", kTa.v, kT_s[h])
        for b0 in range(0, NB, 26):
            bn = min(26, NB - b0)
            P.dma("sp", Vb[:, b0:b0 + bn, 0:128], V_s[b0 * 128:(b0 + bn) * 128, h * 128:(h + 1) * 128].rearrange("(b p) f -> p b f", p=128))
        for (q0, nq, nkeys) in groups:
            u = gi % 2; gi += 1; nqt = nq // 128
            kblocks = [(k0, min(512, nkeys - k0)) for k0 in range(0, nkeys, 512)]
            for qt in range(nqt):
                qs_ = slice(q0 + qt * 128, q0 + (qt + 1) * 128)
                for bi, (k0, kn) in enumerate(kblocks):
                    pb = ps1[c1 % 2]; c1 += 1
                    P.op("pe", "matmul", track=False, out=pb[:, 0:kn], lhsT=qa[:, h, qs_], rhs=kTa[:, k0:k0 + kn], start=True, stop=False)
                    P.op("pe", "matmul", out=pb[:, 0:kn], lhsT=qb[0:64, h, qs_], rhs=kTb[0:64, k0:k0 + kn], start=False, stop=True)
                    P.op("dve", "tensor_reduce", out=mx[:, qt, bi:bi + 1], in_=pb[:, 0:kn], axis=AX.X, op=ALU.max)
                P.op("dve", "tensor_reduce", out=negm[:, qt:qt + 1], in_=mx[:, qt, 0:len(kblocks)], axis=AX.X, op=ALU.max)
                P.op("dve", "tensor_scalar", out=Z[:, 64:65], in0=negm[:, qt:qt + 1], scalar1=-1.0, scalar2=None, op0=ALU.mult)
                pz = ps1[c1 % 2]; c1 += 1
                P.op("pe", "matmul", out=pz[0:65, 0:128], lhsT=Z.v, rhs=idf.v, start=True, stop=True)
                P.op("dve", "tensor_copy", out=qb[64:65, h, qs_], in_=pz[64:65, 0:128])
            nkb = nkeys // 128

            def qk(kb_, slot):
                ks_ = slice(kb_ * 128, (kb_ + 1) * 128)
                pb = pst[slot % 2]
                P.op("pe", "matmul", track=False, out=pb[:, 0:nq], lhsT=kTa[:, ks_], rhs=qa[:, h, q0:q0 + nq], start=True, stop=False)
                P.op("pe", "matmul", out=pb[:, 0:nq], lhsT=kTb[0:65, ks_], rhs=qb[0:65, h, q0:q0 + nq], start=False, stop=True)
            qk(0, c2)
            for kb_ in range(nkb):
                pb = pst[c2 % 2]; ptb = pt[c2 % 2]
                P.op("act", "activation", out=ptb[:, 0:nq], in_=pb[:, 0:nq], func=AF.Exp)
                if kb_ + 1 < nkb:
                    qk(kb_ + 1, c2 + 1)
                c2 += 1
                for qt in range(nqt):
                    P.op("pe", "matmul", track=(kb_ == nkb - 1), out=pO[qt][:, 0:129], lhsT=ptb[:, qt * 128:(qt + 1) * 128], rhs=Vb[:, kb_, :], start=(kb_ == 0), stop=(kb_ == nkb - 1))
            for qt in range(nqt):
                P.op("dve", "reciprocal", out=rl[:, qt:qt + 1], in_=pO[qt][:, 128:129])
                P.op("dve", "tensor_scalar", out=ot[u][:, qt, :], in0=pO[qt][:, 0:128], scalar1=rl[:, qt:qt + 1], scalar2=None, op0=ALU.mult)
            P.dma("sp", om_s[q0:q0 + nq, h * 128:(h + 1) * 128].rearrange("(t p) d -> p t d", p=128), ot[u][:, 0:nqt, :])
    P.end()


def stage_post(P, idf, idb, mode, variants, x_ap, ow_d, g1_d, gates_s, l, modcol, g2col_d, rw_d, rb_d, x1_s, hT_s, G_s, GT_s, gla=None, aT_ap=None, hTok_s=None):
    P.begin()
    nt = len(variants); nv = 2; T = nt * 128; D = 1024; KC = 8
    R2 = lambda nm, shp, dt=F32: [P.sb("%s_%d" % (nm, i), shp, dt) for i in range(2)]
    stage = [P.sb("wst%d" % i, [128, D]) for i in range(2)]
    wb = load_w_bf16(P, ow_d, D, D, "owb", stage=stage)
    rw = P.sb("rw", [128, KC, 32]); P.dma("sp", rw.v, rw_d.rearrange("(k p) n -> p k n", p=128))
    rb = P.sb("rb", [128, 32]); P.dma("sp", rb.v, rb_d)
    g2 = P.sb("g2", [128, KC]); P.dma("sp", g2.v, g2col_d)
    sc = modcols(P, modcol, l, 4, g2.v, "scm")
    sh = lambda v, k: modcol[:, l, 24 + k, v:v + 1]
    g1 = P.sb("g1", [128, D]); P.dma("sp", g1.v, g1_d)
    GG = []
    for v in range(nv):
        gg = P.sb("GG%d" % v, [128, D]); P.dma("sp", gg.v, gates_s[l, 0, v])
        P.op("dve", "tensor_tensor", out=gg.v, in0=gg.v, in1=g1.v, op=ALU.mult)
        GG.append(gg)
    if mode == "gla":
        onc = P.sb("onc", [128, 1]); P.dma("sp", onc.v, gla["on_d"])
        idxf = P.sb("idxf", [128, nt], I32); idxb = P.sb("idxb", [128, nt], I32)
        P.dma("sp", idxf.v, gla["idxf_d"]); P.dma("sp", idxb.v, gla["idxb_d"])
        tof_ = R2("tof", [128, 512]); tob_ = R2("tob", [128, 512]); tr_ = R2("tr", [128, 512]); tom_ = R2("tom", [128, 512])
        cat_ = R2("cat", [128, D], BF16)
        pT = P.ps("pT", [128, KC, 128], BF16)
    else:
        a32_ = R2("a32", [128, KC, 128])
    aT_ = R2("aT", [128, KC, 128], BF16)
    xt_ = R2("xt", [128, D]); x1_ = R2("x1", [128, D]); junk = P.sb("junk", [128, D]); st_ = R2("st", [128, 16])
    xn2_ = R2("xn2", [128, D]); h2T_ = R2("h2T", [128, KC, 128]); h2Tb_ = R2("h2Tb", [128, KC, 128], BF16)
    lg_ = R2("lg", [128, 32]); t8_ = R2("t8", [128, 8]); msk_ = R2("msk", [128, 32]); ex_ = R2("ex", [128, 32]); Gt_ = R2("Gt", [128, 32]); GTt_ = R2("GTt", [32, 128])
    py = [P.ps("py%d" % i, [128, 512]) for i in range(2)]
    pT32 = P.ps("pT32", [128, KC, 128])
    plg = P.ps("plg", [128, 128])
    pTt = P.ps("pTt", [128, D], BF16); htok_ = R2("htok", [128, D], BF16)
    def loads(t):
        sl = slice(t * 128, (t + 1) * 128); u_ = t % 2
        P.dma("sp", xt_[u_].v, x_ap[sl, :])
        if mode == "gla":
            P.gather(tof_[u_].v, gla["of_s"], idxf[:, t:t + 1]); P.gather(tob_[u_].v, gla["ob_s"], idxb[:, t:t + 1])
            P.dma("sp", tr_[u_].v, gla["r_s"][sl, :]); P.dma("sp", tom_[u_].v, gla["om_s"][sl, :])
        else:
            P.dma("sp", a32_[u_].v, aT_ap[:, sl].rearrange("(k p) t -> p k t", p=128))
    loads(0)
    for t in range(nt):
        v = variants[t]; sl = slice(t * 128, (t + 1) * 128); u_ = t % 2
        if mode == "gla":
            tof = tof_[u_]; tob = tob_[u_]; tr = tr_[u_]; tom = tom_[u_]; cat = cat_[u_]
        else:
            a32 = a32_[u_]
        aT = aT_[u_]; xt = xt_[u_]; x1 = x1_[u_]; st = st_[u_]; xn2 = xn2_[u_]; h2T = h2T_[u_]; h2Tb = h2Tb_[u_]
        lg = lg_[u_]; t8 = t8_[u_]; msk = msk_[u_]; ex = ex_[u_]; Gt = Gt_[u_]; GTt = GTt_[u_]
        if t + 1 < nt:
            loads(t + 1)
        if mode == "gla":
            P.op("dve", "tensor_tensor", out=tof.v, in0=tof.v, in1=tob.v, op=ALU.add)
            for h in range(4):
                P.op("act", "activation", out=junk[:, 0:128], in_=tof[:, h * 128:(h + 1) * 128], func=AF.Square, accum_out=st[:, h:h + 1])
            rstd_of(P, st[:, 0:4], st[:, 8:12], st[:, 4:8], 128)
            P.op("act", "activation", out=tr.v, in_=tr.v, func=AF.Silu)
            for h in range(4):
                hs = slice(h * 128, (h + 1) * 128)
                P.op("dve", "scalar_tensor_tensor", out=cat[:, hs], in0=tof[:, hs], scalar=st[:, 8 + h:9 + h], in1=tr[:, hs], op0=ALU.mult, op1=ALU.mult)
            P.op("pool", "tensor_copy", out=cat[:, 512:1024], in_=tom.v)
            for k in range(KC):
                P.op("pe", "transpose", out=pT[:, k, :], in_=cat[:, k * 128:(k + 1) * 128], identity=idb.v)
            P.op("dve", "tensor_scalar", out=aT[:, 0:4, :], in0=pT[:, 0:4, :], scalar1=onc[:, 0:1], scalar2=None, op0=ALU.mult)
            P.op("act", "activation", out=aT[:, 4:8, :], in_=pT[:, 4:8, :], func=AF.Copy)
        else:
            P.op("pool", "tensor_copy", out=aT.v, in_=a32.v)
        for n in range(2):
            for k in range(KC):
                P.op("pe", "matmul", track=(k == KC - 1), out=py[n].v, lhsT=aT[:, k, :], rhs=wb[:, k, n * 512:(n + 1) * 512], start=(k == 0), stop=(k == KC - 1))
            P.op("act", "activation", out=junk[:, 0:512], in_=py[n].v, func=AF.Square, accum_out=st[:, 12 + n:13 + n])
        P.op("dve", "tensor_tensor", out=st[:, 12:13], in0=st[:, 12:13], in1=st[:, 13:14], op=ALU.add)
        rstd_of(P, st[:, 12:13], st[:, 14:15], st[:, 13:14], D)
        for n in range(2):
            ns = slice(n * 512, (n + 1) * 512)
            P.op("dve", "scalar_tensor_tensor", out=x1[:, ns], in0=py[n].v, scalar=st[:, 14:15], in1=GG[v][:, ns], op0=ALU.mult, op1=ALU.mult)
        P.op("pool", "tensor_tensor", out=x1.v, in0=x1.v, in1=xt.v, op=ALU.add)
        P.dma("sp", x1_s[sl, :], x1.v)
        P.op("act", "activation", out=junk.v, in_=x1.v, func=AF.Square, accum_out=st[:, 15:16])
        rstd_of(P, st[:, 15:16], st[:, 4:5], st[:, 5:6], D)
        P.op("dve", "tensor_scalar", out=xn2.v, in0=x1.v, scalar1=st[:, 4:5], scalar2=None, op0=ALU.mult)
        for k in range(KC):
            P.op("pe", "transpose", out=pT32[:, k, :], in_=xn2[:, k * 128:(k + 1) * 128], identity=idf.v)
        for k in range(KC):
            P.op("dve", "tensor_scalar", out=h2T[:, k, :], in0=pT32[:, k, :], scalar1=sc[:, v, k:k + 1], scalar2=sh(v, k), op0=ALU.mult, op1=ALU.add)
        P.op("pool", "tensor_copy", out=h2Tb.v, in_=h2T.v)
        P.dma("sp", hT_s[:, sl].rearrange("(k p) t -> p k t", p=128), h2Tb.v)
        if hTok_s is not None:
            for k in range(KC):
                P.op("pe", "transpose", out=pTt[:, k * 128:(k + 1) * 128], in_=h2Tb[:, k, :], identity=idb.v, track=(k == KC - 1))
            P.op("act", "activation", out=htok_[u_].v, in_=pTt.v, func=AF.Copy)
            P.dma("sp", hTok_s[sl, :], htok_[u_].v)
        for k in range(KC):
            P.op("pe", "matmul", track=(k == KC - 1), out=plg[:, 0:32], lhsT=h2T[:, k, :], rhs=rw[:, k, :], start=(k == 0), stop=(k == KC - 1))
        P.op("dve", "tensor_tensor", out=lg.v, in0=plg[:, 0:32], in1=rb.v, op=ALU.add)
        P.op("dve", "max", out=t8.v, in_=lg.v)
        P.op("dve", "tensor_scalar", out=msk.v, in0=lg.v, scalar1=t8[:, 3:4], scalar2=None, op0=ALU.is_ge)
        P.op("dve", "tensor_scalar", out=t8[:, 7:8], in0=t8[:, 0:1], scalar1=-1.0, scalar2=None, op0=ALU.mult)
        P.op("act", "activation", out=ex.v, in_=lg.v, func=AF.Exp, bias=t8[:, 7:8])
        P.op("dve", "tensor_tensor", out=ex.v, in0=ex.v, in1=msk.v, op=ALU.mult)
        P.op("dve", "tensor_reduce", out=t8[:, 6:7], in_=ex.v, axis=AX.X, op=ALU.add)
        P.op("dve", "reciprocal", out=t8[:, 5:6], in_=t8[:, 6:7])
        P.op("dve", "tensor_scalar", out=Gt.v, in0=ex.v, scalar1=t8[:, 5:6], scalar2=None, op0=ALU.mult)
        P.dma("sp", G_s[sl, :], Gt.v)
        P.op("pe", "transpose", out=plg[0:32, :], in_=Gt.v, identity=idf.v)
        P.op("act", "activation", out=GTt.v, in_=plg[0:32, :], func=AF.Copy)
        P.dma("sp", GT_s[:, sl], GTt.v)
    P.end()


def stage_moe(P, variants, groups, hT_s, G_s, GT_s, x1_s, w1_d, w2_d, b1g_d, b1l_d, b2_d, g3_d, gates_s, l, x2_ap, NE=32):
    P.begin()
    nt = len(variants); nv = 2; T = nt * 128; D = 1024; KC = 8; F = 1024
    mg = max(len(g) for g in groups)
    g3 = P.sb("g3", [128, D]); P.dma("sp", g3.v, g3_d)
    GG = []
    for v in range(nv):
        gg = P.sb("GG%d" % v, [128, D]); P.dma("sp", gg.v, gates_s[l, 1, v])
        P.op("dve", "tensor_tensor", out=gg.v, in0=gg.v, in1=g3.v, op=ALU.mult)
        GG.append(gg)
    b1g = P.sb("b1g", [128, 32, KC]); b1l = P.sb("b1l", [128, 32, KC]); P.dma("sp", b1g.v, b1g_d); P.dma("sp", b1l.v, b1l_d)
    b2 = P.sb("b2", [32, D]); P.dma("sp", b2.v, b2_d)
    w1g = [P.sb("w1g%d" % i, [128, KC, F], BF16) for i in range(2)]
    w1l = [P.sb("w1l%d" % i, [128, KC, F], BF16) for i in range(2)]
    w2b = [P.sb("w2b%d" % i, [128, KC, D], BF16) for i in range(2)]
    hT = P.sb("hT", [128, KC, mg * 128], BF16)
    Gs = P.sb("Gs", [128, mg, 32]); GTs = P.sb("GTs", [32, mg * 128])
    yacc = P.sb("yacc", [128, mg, D])
    actT = P.sb("actT", [128, KC, 512], BF16)
    tg = [P.sb("tg%d" % i, [128, 512]) for i in range(2)]; tsg = [P.sb("tsg%d" % i, [128, 512]) for i in range(2)]
    tl = [P.sb("tl%d" % i, [128, 512]) for i in range(2)]
    xt = g3; junk = P.sb("junk", [128, D]); st = P.sb("st", [128, 4])
    pg = [P.ps("pg%d" % i, [128, 512]) for i in range(2)]; pl = [P.ps("pl%d" % i, [128, 512]) for i in range(2)]
    py = [P.ps("py%d" % i, [128, 512]) for i in range(2)]
    sti = [0]

    def load_expert_steps(e, buf):
        steps = []
        for k in range(KC):
            def step(k=k):
                P.dma("sp", w1g[buf][:, k, :], w1_d[e, k * 128:(k + 1) * 128, 0:F])
                P.dma("sp", w1l[buf][:, k, :], w1_d[e, k * 128:(k + 1) * 128, F:2 * F])
                P.dma("sp", w2b[buf][:, k, :], w2_d[e, k * 128:(k + 1) * 128, :])
            steps.append(step)
        return steps

    for grp in groups:
        ng = len(grp)
        t0 = grp[0]; tok0 = t0 * 128; ntok = ng * 128
        P.dma("sp", hT[:, :, 0:ntok], hT_s[:, tok0:tok0 + ntok].rearrange("(k p) t -> p k t", p=128))
        P.dma("sp", Gs[:, 0:ng, :], G_s[tok0:tok0 + ntok, :].rearrange("(g p) e -> p g e", p=128))
        P.dma("sp", GTs[:, 0:ntok], GT_s[:, tok0:tok0 + ntok])
        for i in range(ng):
            for n in range(2):
                P.op("pe", "matmul", out=py[n].v, lhsT=GTs[:, i * 128:(i + 1) * 128], rhs=b2[:, n * 512:(n + 1) * 512], start=True, stop=True)
                P.op("act", "activation", out=yacc[:, i, n * 512:(n + 1) * 512], in_=py[n].v, func=AF.Copy)
        for s in load_expert_steps(0, 0):
            s()
        blocks = [(b0, min(512, ntok - b0)) for b0 in range(0, ntok, 512)]
        ci = 0
        for e in range(NE):
            buf = e % 2
            nxt = load_expert_steps(e + 1, 1 - buf) if e + 1 < NE else []
            for bi, (b0, bn) in enumerate(blocks):
                for j in range(KC):
                    if bi == 0 and nxt:
                        nxt[j]()
                    c = ci % 2; ci += 1
                    for k in range(KC):
                        P.op("pe", "matmul", track=(k == KC - 1), out=pg[c][:, 0:bn], lhsT=w1g[buf][:, k, j * 128:(j + 1) * 128], rhs=hT[:, k, b0:b0 + bn], start=(k == 0), stop=(k == KC - 1))
                    for k in range(KC):
                        P.op("pe", "matmul", track=(k == KC - 1), out=pl[c][:, 0:bn], lhsT=w1l[buf][:, k, j * 128:(j + 1) * 128], rhs=hT[:, k, b0:b0 + bn], start=(k == 0), stop=(k == KC - 1))
                    P.op("dve", "tensor_scalar", out=tg[c][:, 0:bn], in0=pg[c][:, 0:bn], scalar1=b1g[:, e, j:j + 1], scalar2=7.0, op0=ALU.add, op1=ALU.min)
                    P.op("act", "activation", out=tsg[c][:, 0:bn], in_=tg[c][:, 0:bn], func=AF.Sigmoid, scale=1.702)
                    P.op("dve", "tensor_scalar", out=tl[c][:, 0:bn], in0=pl[c][:, 0:bn], scalar1=b1l[:, e, j:j + 1], scalar2=-7.0, op0=ALU.add, op1=ALU.max)
                    P.op("dve", "tensor_scalar", out=tl[c][:, 0:bn], in0=tl[c][:, 0:bn], scalar1=7.0, scalar2=1.0, op0=ALU.min, op1=ALU.add)
                    P.op("pool", "tensor_tensor", out=tg[c][:, 0:bn], in0=tg[c][:, 0:bn], in1=tsg[c][:, 0:bn], op=ALU.mult)
                    P.op("pool", "tensor_tensor", out=actT[:, j, 0:bn], in0=tg[c][:, 0:bn], in1=tl[c][:, 0:bn], op=ALU.mult)
                for i in range(b0 // 128, (b0 + bn) // 128):
                    for n in range(2):
                        ns = slice(n * 512, (n + 1) * 512)
                        for j in range(KC):
                            P.op("pe", "matmul", track=(j == KC - 1), out=py[n].v, lhsT=actT[:, j, i * 128 - b0:(i + 1) * 128 - b0], rhs=w2b[buf][:, j, ns], start=(j == 0), stop=(j == KC - 1))
                        P.op("dve", "scalar_tensor_tensor", out=yacc[:, i, ns], in0=py[n].v, scalar=Gs[:, i, e:e + 1], in1=yacc[:, i, ns], op0=ALU.mult, op1=ALU.add)
        for i in range(ng):
            t = grp[i]; v = variants[t]; sl = slice(t * 128, (t + 1) * 128)
            P.dma("sp", xt.v, x1_s[sl, :])
            P.op("act", "activation", out=junk.v, in_=yacc[:, i, :], func=AF.Square, accum_out=st[:, 0:1])
            rstd_of(P, st[:, 0:1], st[:, 2:3], st[:, 1:2], D)
            P.op("dve", "scalar_tensor_tensor", out=junk.v, in0=yacc[:, i, :], scalar=st[:, 2:3], in1=GG[v].v, op0=ALU.mult, op1=ALU.mult)
            P.op("pool", "tensor_tensor", out=xt.v, in0=junk.v, in1=xt.v, op=ALU.add)
            P.dma("sp", x2_ap[sl, :], xt.v)
    P.end()


def stage_na(P, idb, qT_s, kT_s, V_s, bi_d, be_d, oT_s):
    P.begin()
    ones = P.sb("ones", [128, 128], BF16); P.op("pool", "memset", ap=ones.v, constant=1.0)
    kb = [P.sb("kb%d" % i, [128, 8, 768], BF16) for i in range(2)]; vb = [P.sb("vb%d" % i, [128, 6, 1024], BF16) for i in range(2)]
    kcb = P.sb("kcb", [128, 8, 256], BF16); vcb = P.sb("vcb", [128, 2, 1024], BF16)
    P.dma("sp", kcb.v, kT_s[:, 2560:2816].rearrange("(k p) n -> p k n", p=128))
    P.dma("sp", vcb.v, V_s[2560:2816, :].rearrange("(b p) f -> p b f", p=128))
    Bi = P.sb("Bi", [128, 8, 512]); P.dma("sp", Bi.v, bi_d)
    Be = [P.sb("Be%d" % i, [128, 8, 768]) for i in range(2)]
    qrow = [P.sb("qrow%d" % i, [128, 8, 64], BF16) for i in range(2)]
    QBD = [P.sb("QBD%d" % i, [128, 128], BF16) for i in range(2)]
    for z in QBD:
        P.op("pool", "memset", ap=z.v, constant=0.0)
    Sb = [P.sb("Sb%d" % i, [128, 1024]) for i in range(2)]; Pm = [P.sb("Pm%d" % i, [128, 1024], BF16) for i in range(2)]
    PT = [P.sb("PT%d" % i, [128, 8, 128], BF16) for i in range(2)]
    sm = P.sb("sm", [128, 4]); rl = P.sb("rl", [128, 128]); orow = [P.sb("orow%d" % i, [128, 8, 64]) for i in range(2)]
    pS = [P.ps("pS%d" % i, [128, 1024]) for i in range(2)]
    pPT = P.ps("pPT", [128, 8, 128], BF16); pO = P.ps("pO", [128, 128]); pL = P.ps("pL", [128, 128])
    it = 0; nbe = 0
    for i in range(32):
        u = i % 2
        if i < 4:
            e0, nr, edge = 0, 12, i
        elif i >= 28:
            e0, nr, edge = 28, 12, i - 24
        else:
            e0, nr, edge = i, 8, None
        nw = nr * 64; nbk = nw // 128
        P.dma("sp", kb[u][:, :, 0:nw], kT_s[:, e0 * 64:e0 * 64 + nw].rearrange("(k p) n -> p k n", p=128))
        P.dma("sp", vb[u][:, 0:nbk, :], V_s[e0 * 64:e0 * 64 + nw, :].rearrange("(b p) f -> p b f", p=128))
        P.dma("sp", qrow[u].v, qT_s[:, (i + 4) * 64:(i + 5) * 64].rearrange("(k p) t -> p k t", p=128))
        if edge is not None:
            B = Be[nbe % 2]; nbe += 1
            P.dma("sp", B.v, be_d[edge])
        else:
            B = Bi
        ntot = nw + 256; nblk = nbk + 2
        def scores(hp, w):
            P.op("pool", "tensor_copy", out=QBD[w][0:64, 0:64], in_=qrow[u][0:64, hp, :])
            P.op("pool", "tensor_copy", out=QBD[w][64:128, 64:128], in_=qrow[u][64:128, hp, :])
            for c0 in range(0, nw, 512):
                cn = min(512, nw - c0)
                P.op("pe", "matmul", out=pS[w][:, c0:c0 + cn], lhsT=QBD[w].v, rhs=kb[u][:, hp, c0:c0 + cn], start=True, stop=True, track=False)
            P.op("pe", "matmul", out=pS[w][:, nw:ntot], lhsT=QBD[w].v, rhs=kcb[:, hp, :], start=True, stop=True)
        scores(0, it % 2)
        for hp in range(8):
            w = it % 2; it += 1
            if hp + 1 < 8:
                scores(hp + 1, it % 2)
            P.op("dve", "tensor_tensor", out=Sb[w][:, 0:nw], in0=pS[w][:, 0:nw], in1=B[:, hp, 0:nw], op=ALU.add)
            P.op("act", "activation", out=Sb[w][:, nw:ntot], in_=pS[w][:, nw:ntot], func=AF.Copy)
            P.op("dve", "tensor_reduce", out=sm[:, 0:1], in_=Sb[w][:, 0:ntot], axis=AX.X, op=ALU.max)
            P.op("dve", "tensor_scalar", out=sm[:, 1:2], in0=sm[:, 0:1], scalar1=-1.0, scalar2=None, op0=ALU.mult)
            P.op("act", "activation", out=Pm[w][:, 0:ntot], in_=Sb[w][:, 0:ntot], func=AF.Exp, bias=sm[:, 1:2])
            for b in range(nblk):
                P.op("pe", "transpose", out=pPT[:, b, :], in_=Pm[w][:, b * 128:(b + 1) * 128], identity=idb.v, track=(b == nblk - 1))
            P.op("dve", "tensor_copy", out=PT[w][:, 0:nblk, :], in_=pPT[:, 0:nblk, :])
            hs = slice(hp * 128, (hp + 1) * 128)
            for b in range(nblk):
                vblk = vb[u][:, b, hs] if b < nbk else vcb[:, b - nbk, hs]
                P.op("pe", "matmul", out=pO.v, lhsT=vblk, rhs=PT[w][:, b, :], start=(b == 0), stop=(b == nblk - 1), track=(b == nblk - 1))
            for b in range(nblk):
                P.op("pe", "matmul", out=pL.v, lhsT=ones.v, rhs=PT[w][:, b, :], start=(b == 0), stop=(b == nblk - 1), track=(b == nblk - 1))
            P.op("dve", "reciprocal", out=rl.v, in_=pL.v)
            P.op("dve", "tensor_tensor", out=orow[u][0:64, hp, :], in0=pO[0:64, 0:64], in1=rl[0:64, 0:64], op=ALU.mult)
            P.op("dve", "tensor_tensor", out=orow[u][64:128, hp, :], in0=pO[64:128, 64:128], in1=rl[64:128, 64:128], op=ALU.mult)
        P.dma("sp", oT_s[:, i * 64:(i + 1) * 64].rearrange("(k p) t -> p k t", p=128), orow[u].v)
    P.end()


MOE_GROUPS_D0 = [list(range(0, 8)), list(range(8, 15)), list(range(15, 22))]
MOE_GROUPS_D1 = [list(range(0, 8)), list(range(8, 16))]
MOE_PASSES_F0 = [[[0, 1, 2, 3], [4, 5, 6, 7]], [[8, 9, 10, 11], [12, 13, 14, 15]], [[16, 17, 18, 19], [20, 21]]]
MOE_PASSES_F1 = [[[0, 1, 2, 3], [4, 5, 6, 7]], [[8, 9, 10, 11], [12, 13, 14, 15]]]
MOE_GROUPS_F0 = [list(range(0, 6)), list(range(6, 12)), list(range(12, 17)), list(range(17, 22))]
MOE_GROUPS_F1 = [list(range(0, 6)), list(range(6, 11)), list(range(11, 16))]


def build_fused(dbg=False, upto=99):
    P = Prog("fused")
    D = 1024
    dr = P.dram
    x_all = dr("x_all", [TALL, D]); x_rev = dr("x_rev", [TALL, D]); x_own = dr("x_own", [TOWN, D])
    adaw = dr("adaw", [2, D, 6144]); bcol = dr("bcol", [128, 2, 48]); brow = dr("brow", [2, 128, 6144]); cT = dr("cT", [128, 8, 2])
    ng = dr("ng", [2, 4, 128, 8]); ngrow = dr("ngrow", [2, 4, 128, D])
    w_tmA = dr("w_tmA", [D, 896]); w_fmA = dr("w_fmA", [D, 672]); w_tmB = dr("w_tmB", [D, 768]); w_fmB = dr("w_fmB", [D, 528]); w_own = dr("w_own", [D, 768])
    Ca = dr("Ca", [64, TALL]); Sa = dr("Sa", [64, TALL]); Cq = dr("Cq", [64, TOWN]); Sq = dr("Sq", [64, TOWN])
    wukv = dr("wukv", [128, 1024]); kvn = dr("kvn", [128, 1]); qn = dr("qn", [128, 2]); wuq = dr("wuq", [256, 768]); wuqs = dr("wuqs", [256, 256])
    wa = dr("wa", [17, 8, 64]); tri = dr("tri", [64, 64]); mu = dr("mu", [64, 8, 64])
    idxf = dr("idxf", [128, NOWN], I32); idxb = dr("idxb", [128, NOWN], I32)
    ow0 = dr("ow0", [D, D]); ow1 = dr("ow1", [D, D]); onc = dr("onc", [128, 1])
    rw = dr("rw", [2, D, 32]); rb = dr("rb", [2, 128, 32])
    if upto >= 7:
        w1 = dr("w1", [2, 32, D, 2048]); w2 = dr("w2", [2, 32, D, D])
    b1 = dr("b1", [2, 32, 2048]); b1g = dr("b1g", [2, 128, 32, 8]); b1l = dr("b1l", [2, 128, 32, 8]); b2 = dr("b2", [2, 32, D]); iota_d = dr("iota", [128, 128]); lst_d = dr("lst", [128, 128])
    wqk = dr("wqk", [D, 2048]); wv = dr("wv", [D, D]); Bint = dr("Bint", [128, 8, 512]); Bedge = dr("Bedge", [8, 128, 8, 768])
    out = dr("out", [2048, D], out=True)
    S = lambda n, s, dt=F32: P.scratch(n, s, dt, dbg=(dbg is True or (dbg and n in dbg)))
    gates = S("gates", [2, 2, 2, 128, D])
    kA = S("kA", [TALL, 256]); vA = S("vA", [TALL, 512]); qTA = S("qTA", [256, TALL]); kTA = S("kTA", [256, TALL]); aTA = S("aTA", [16, TALL])
    kB = S("kB", [TALL, 256]); vB = S("vB", [TALL, 512]); qTB = S("qTB", [256, TALL]); kTB = S("kTB", [256, TALL]); aTB = S("aTB", [16, TALL])
    kTm = S("kTm", [4, 128, TALL], BF16); krT = S("krT", [64, TALL], BF16); Vm = S("Vm", [TALL, 512], BF16)
    r_own = S("r_own", [TOWN, 512]); cq_own = S("cq_own", [TOWN, 256])
    oF = S("oF", [TALL, 512]); oB = S("oB", [TALL, 512]); om = S("om", [TOWN, 512])
    w1bf = S("w1bf", [2, 32, D, 2048], BF16); w2bf = S("w2bf", [2, 32, D, D], BF16); b1bf = S("b1bf", [2, 32, 2048], BF16)
    hka = S("hka", [TOWN, D], BF16); hkb = S("hkb", [2048, D], BF16)
    x1a = S("x1a", [TOWN, D]); hTa = S("hTa", [D, TOWN], BF16); Ga = S("Ga", [TOWN, 32]); GTa = S("GTa", [32, TOWN]); x2a = S("x2a", [TOWN, D])
    qT1 = S("qT1", [D, TOWN], BF16); kT1 = S("kT1", [D, TOWN], BF16); V1 = S("V1", [TOWN, D], BF16); oT1 = S("oT1", [D, 2048])
    x1b = S("x1b", [2048, D]); hTb = S("hTb", [D, 2048], BF16); Gb = S("Gb", [2048, 32]); GTb = S("GTb", [32, 2048])
    idf, idb = ident(P)
    modcol = P.sb("modcol", [128, 2, 48, 2])
    stage_ada(P, adaw, bcol, brow, cT, modcol, gates)
    if upto < 1:
        P.finish(); return P
    gall = [(0, 2)] + [(256 + 512 * i, 4) for i in range(32)]; gv = [1] + [0] * 32
    gown = [(512 * i, 4) for i in range(5)] + [(2560, 2)]; gvo = [0] * 5 + [1]

    def n1(l):
        g = P.sb("gn%d" % P.nscope, [128, 8]); P.dma("sp", g.v, ng[l, 0])
        sc = modcols(P, modcol, l, 1, g.v, "sc%d" % P.nscope)
        sh = P.sb("sh%d" % P.nscope, [128, 2, 8])
        for s in range(2):
            P.op("dve", "tensor_copy", out=sh[:, s, :], in_=modcol[:, l, 0:8, s])
        return sc, sh
    sc0, sh0 = n1(0)
    if upto >= 7:
        it0 = conv_items(w1, w2, b1, w1bf, w2bf, b1bf, 0); it1 = conv_items(w1, w2, b1, w1bf, w2bf, b1bf, 1)
        xA = xB = None
    else:
        xA = xB = None
    stage_pro2(P, idb, x_all, gall, gv, sc0, sh0, D, w_tmA, 896, [(0, 256, kA, F32), (256, 512, vA, F32)], w_fmA, 672,
              [(0, 128, qTA[0:128], F32, 1.0), (128, 128, qTA[128:256], F32, 1.0), (256, 128, kTA[0:128], F32, 1.0), (384, 128, kTA[128:256], F32, 1.0), (512, 16, aTA, F32, 1.0)],
              rope=(544, 608, krT, Ca, Sa), mla=dict(col=768, wukv_d=wukv, kvn_d=kvn, kT_s=kTm, V_s=Vm), tag="proA", extra=xA)
    stage_pro2(P, idb, x_rev, gall, gv, sc0, sh0, D, w_tmB, 768, [(0, 256, kB, F32), (256, 512, vB, F32)], w_fmB, 528,
              [(0, 128, qTB[0:128], F32, 1.0), (128, 128, qTB[128:256], F32, 1.0), (256, 128, kTB[0:128], F32, 1.0), (384, 128, kTB[128:256], F32, 1.0), (512, 16, aTB, F32, 1.0)], tag="proB", extra=xB)
    if upto < 2:
        P.finish(); return P
    stage_pro2(P, idb, x_own, gown, gvo, sc0, sh0, D, w_own, 768, [(0, 512, r_own, F32), (512, 256, cq_own, F32)], None, 0, [], tag="proO")
    if upto < 3:
        P.finish(); return P
    scans = []
    for h in range(4):
        hs = slice(h * 64, (h + 1) * 64)
        scans.append((qTA[hs], kTA[hs], kA[:, hs], vA[:, h * 128:(h + 1) * 128], aTA, h, oF[:, h * 128:(h + 1) * 128]))
    for h in range(4):
        hs = slice(h * 64, (h + 1) * 64)
        scans.append((qTB[hs], kTB[hs], kB[:, hs], vB[:, h * 128:(h + 1) * 128], aTB, 4 + h, oB[:, h * 128:(h + 1) * 128]))
    stage_gla(P, scans, wa, tri, mu, extra=(lambda P_: conv_steps(P_, it0 + it1)) if upto >= 7 else None, per_block=4)
    if upto < 4:
        P.finish(); return P
    stage_mla_full(P, idf, idb, cq_own, qn, wuq, wuqs, Cq, Sq, kTm, krT, Vm, om)
    if upto < 5:
        P.finish(); return P
    stage_post(P, idf, idb, "gla", VOWN, x_own, ow0, ngrow[0, 1], gates, 0, modcol, ng[0, 2], rw[0], rb[0], x1a, hTa, Ga, GTa,
               gla=dict(on_d=onc, idxf_d=idxf, idxb_d=idxb, of_s=oF, ob_s=oB, r_s=r_own, om_s=om))
    if upto < 7:
        P.finish(); return P
    stage_moe(P, VOWN, MOE_GROUPS_D0, hTa, Ga, GTa, x1a, w1bf[0], w2bf[0], b1g[0], b1l[0], b2[0], ngrow[0, 3], gates, 0, x2a)
    sc1, sh1 = n1(1)
    stage_pro2(P, idb, x2a, gown, gvo, sc1, sh1, D, wv, 1024, [(0, 1024, V1, BF16)], wqk, 2048,
              [(j * 128, 128, qT1[j * 128:(j + 1) * 128], BF16, 0.125) for j in range(8)] + [(1024 + j * 128, 128, kT1[j * 128:(j + 1) * 128], BF16, 1.0) for j in range(8)], tag="proQ")
    stage_na(P, idb, qT1, kT1, V1, Bint, Bedge, oT1)
    stage_post(P, idf, idb, "fm", [0] * 16, x2a[256:2304], ow1, ngrow[1, 1], gates, 1, modcol, ng[1, 2], rw[1], rb[1], x1b, hTb, Gb, GTb, aT_ap=oT1)
    stage_moe(P, [0] * 16, MOE_GROUPS_D1, hTb, Gb, GTb, x1b, w1bf[1], w2bf[1], b1g[1], b1l[1], b2[1], ngrow[1, 3], gates, 1, out)
    P.finish()
    return P


def _c(a):
    return np.ascontiguousarray(a, dtype=np.float32)


def _col(a):
    return _c(a.reshape(-1, 128).T)


def _bc(a):
    return _c(np.broadcast_to(a, (128,) + a.shape))


def _rope_tables():
    t = np.arange(16384)
    row = (t // 64).astype(np.float32); colp = (t % 64).astype(np.float32)
    inv = (np.float32(10000.0) ** (-np.arange(16, dtype=np.float32) / np.float32(16))).astype(np.float32)
    ang = np.concatenate([row[:, None] * inv, colp[:, None] * inv], -1).astype(np.float32)
    return np.cos(ang).astype(np.float32), np.sin(ang).astype(np.float32)


def na_bias_edge(rpb, r, grows):
    r0 = int(np.clip(r - 4, 0, 248))
    col = np.arange(64); c0 = np.clip(col - 8, 0, 48); kcol = np.arange(64)
    inwin = (kcol[None, :] >= c0[:, None]) & (kcol[None, :] < c0[:, None] + 16)
    cidx = np.clip(kcol[None, :] - col[:, None] + 15, 0, 30)
    out = np.full((16, 64, len(grows), 64), -30000.0, np.float32)
    for w, g in enumerate(grows):
        if 0 <= g <= 255 and r0 <= g < r0 + 8:
            t = rpb[:, g - r + 7][:, cidx]
            out[:, :, w, :] = np.where(inwin[None], t, np.float32(-30000.0))
    t = out.reshape(8, 2, 64, len(grows) * 64)
    return np.ascontiguousarray(t.transpose(1, 2, 0, 3).reshape(128, 8, len(grows) * 64))


_FP = {}


def fused_inputs(x, c, ctx, c_ctx, ada_w, ada_b, norm_g, router_w, router_b, moe_w1, moe_b1, moe_w2, moe_b2,
                 ab_in_w, gla_wa_f, gla_ba_f, gla_wa_b, gla_ba_b, gla_onorm, mla_qnorm, mla_wuq, mla_kvnorm, mla_wukv,
                 ab_out_w, na_qkv_w, na_rpb, na_out_w):
    f32 = np.float32
    A = lambda a: np.asarray(a, dtype=f32)
    x = A(x)[0]; ctx = A(ctx)[0]; ada_w = A(ada_w); ada_b = A(ada_b); norm_g = A(norm_g); inw = A(ab_in_w)[0]
    cos, sin = _rope_tables()
    com = {}
    com["x_all"] = _c(np.concatenate([ctx, x], 0)); com["x_rev"] = _c(np.concatenate([ctx[::-1], x[::-1]], 0))
    com["adaw"] = ada_w; com["bcol"] = _c(ada_b.reshape(2, 48, 128).transpose(2, 0, 1)); com["brow"] = _c(np.stack([_bc(ada_b[0]), _bc(ada_b[1])]))
    com["cT"] = _c(np.stack([A(c)[0], A(c_ctx)], 0).reshape(2, 8, 128).transpose(2, 1, 0))
    com["ng"] = _c(np.stack([np.stack([_col(norm_g[l, i]) for i in range(4)]) for l in range(2)]))
    com["ngrow"] = _c(np.stack([np.stack([_bc(norm_g[l, i]) for i in range(4)]) for l in range(2)]))
    kr = inw[:, 1952:2016]; krs = np.concatenate([kr[:, 32:], kr[:, :32]], 1)
    z16 = np.zeros((1024, 16), f32)
    com["w_tmA"] = _c(np.concatenate([inw[:, 256:512], inw[:, 512:1024], inw[:, 1824:1952]], 1))
    com["w_fmA"] = _c(np.concatenate([inw[:, 0:256], inw[:, 256:512], inw[:, 1536:1552], z16, kr, krs], 1))
    com["w_tmB"] = _c(np.concatenate([inw[:, 256:512], inw[:, 512:1024]], 1))
    com["w_fmB"] = _c(np.concatenate([inw[:, 0:256], inw[:, 256:512], inw[:, 1552:1568]], 1))
    com["w_own"] = _c(np.concatenate([inw[:, 1024:1536], inw[:, 1568:1824]], 1))
    Ca = np.ones((64, TALL), f32); Sa = np.zeros((64, TALL), f32)
    Ca[:, 256:] = np.concatenate([cos.T, cos.T], 0); Sa[:, 256:] = np.concatenate([-sin.T, sin.T], 0)
    com["Ca"] = Ca; com["Sa"] = Sa
    wk = A(mla_wukv)[0].reshape(128, 4, 256)
    com["wukv"] = _c(np.concatenate([wk[:, :, :128].reshape(128, 512), wk[:, :, 128:].reshape(128, 512)], 1))
    com["kvn"] = _c(A(mla_kvnorm)[0].reshape(128, 1)); com["qn"] = _col(A(mla_qnorm)[0])
    wq = A(mla_wuq)[0]; com["wuq"] = _c(wq)
    com["wuqs"] = _c(np.concatenate([np.concatenate([wq[:, h * 192 + 160:h * 192 + 192], wq[:, h * 192 + 128:h * 192 + 160]], 1) for h in range(4)], 1))
    wa = np.zeros((17, 8, 64), f32)
    for d, (w_, b_) in enumerate(((A(gla_wa_f)[0], A(gla_ba_f)[0]), (A(gla_wa_b)[0], A(gla_ba_b)[0]))):
        for h in range(4):
            wa[:16, d * 4 + h] = w_[:, h * 64:(h + 1) * 64]; wa[16, d * 4 + h] = b_[h * 64:(h + 1) * 64]
    com["wa"] = wa
    com["tri"] = np.where(np.arange(64)[:, None] <= np.arange(64)[None, :], -1.0 / 16, 0.0).astype(f32)
    com["mu"] = _c(np.repeat((np.arange(64)[:, None] <= np.arange(64)[None, :]).astype(f32)[:, None, :], 8, 1))
    com["ow0"] = _c(A(ab_out_w)[0]); com["ow1"] = _c(A(na_out_w)[0]); com["onc"] = _c(A(gla_onorm)[0].reshape(128, 1))
    com["rw"] = _c(A(router_w)); com["rb"] = _c(np.stack([_bc(A(router_b)[0]), _bc(A(router_b)[1])]))
    com["w1"] = A(moe_w1); com["w2"] = A(moe_w2)
    b1 = A(moe_b1)
    colE = lambda a: _c(a.reshape(32, 8, 128).transpose(2, 0, 1))
    com["b1g"] = _c(np.stack([colE(b1[l][:, 0::2]) for l in range(2)])); com["b1l"] = _c(np.stack([colE(b1[l][:, 1::2]) for l in range(2)]))
    com["b1"] = _c(b1); com["iota"] = _bc(np.arange(128, dtype=f32)); com["lst"] = (np.arange(128)[:, None] < np.arange(128)[None, :]).astype(f32)
    com["b2"] = _c(A(moe_b2))
    qkv = A(na_qkv_w)[0]; com["wqk"] = _c(qkv[:, :2048]); com["wv"] = _c(qkv[:, 2048:])
    rpb = A(na_rpb)[0]
    com["Bint"] = na_bias_table(rpb, 4)
    maps = []
    for cc in range(8):
        m = dict(com)
        er = np.arange(32 * cc - 4, 32 * cc + 36)
        erc = np.clip(er, 0, 255)
        tok = (erc[:, None] * 64 + np.arange(64)[None, :]).ravel()
        m["x_own"] = _c(np.concatenate([x[tok], ctx], 0))
        Cq = np.ones((64, TOWN), f32); Sq = np.zeros((64, TOWN), f32)
        Cq[:, :2560] = np.concatenate([cos[tok].T, cos[tok].T], 0); Sq[:, :2560] = np.concatenate([-sin[tok].T, sin[tok].T], 0)
        m["Cq"] = Cq; m["Sq"] = Sq
        posf = np.concatenate([256 + tok, np.arange(256)]); posb = np.concatenate([256 + (16383 - tok), 255 - np.arange(256)])
        m["idxf"] = np.ascontiguousarray(posf.reshape(NOWN, 128).T.astype(np.int32)); m["idxb"] = np.ascontiguousarray(posb.reshape(NOWN, 128).T.astype(np.int32))
        be = []
        for e in range(8):
            i = e if e < 4 else 24 + e
            e0 = 0 if e < 4 else 28
            be.append(na_bias_edge(rpb, 32 * cc + i, [int(er[e0 + w]) for w in range(12)]))
        m["Bedge"] = _c(np.stack(be))
        maps.append(m)
    return maps


def kernel(**inputs):
    if "P" not in _FP:
        _FP["P"] = build_fused()
    P = _FP["P"]
    maps = fused_inputs(**inputs)
    maps = [{k: m[k] for k in P.in_names} for m in maps]
    res = run_bass_kernel_spmd(P.nc, maps, core_ids=list(range(8)))
    return np.concatenate([r["out"] for r in res.results], 0)[None].astype(np.float32)


def stage_pro2(P, idb, x_d, groups, gvar, gsc, gsh, D, wtm_d, ntm, tm_outs, wfm_d, nfm, fm_specs, rope=None, mla=None, tag="pro", extra=None, per_tile=2):
    P.begin()
    KC = D // 128
    tiles = []
    for gi, (tok0, gt) in enumerate(groups):
        for t in range(gt):
            tiles.append((tok0 + t * 128, gvar[gi]))
    nt = len(tiles)
    stg = [P.sb("wst%d" % i, [128, max(ntm, nfm, 1024)]) for i in range(2)]
    wtm = load_w_bf16(P, wtm_d, D, ntm, "wtm", stage=stg) if ntm else None
    wfm = load_w_bf16(P, wfm_d, D, nfm, "wfm", stage=stg) if nfm else None
    R = 3
    ring = lambda nm, shp, dt=F32, n=R: [P.sb("%s%d" % (nm, i), shp, dt) for i in range(n)]
    if mla:
        wuk = load_w_bf16(P, mla["wukv_d"], 128, 1024, "wuk", stage=stg)
        kvn = P.sb("kvn", [128, 1]); P.dma("sp", kvn.v, mla["kvn_d"])
        ckn = ring("ckn", [128, 128], BF16); ckT = ring("ckT", [128, 128], BF16)
        kst = ring("kst", [128, 4, 128], BF16); vst = ring("vst", [128, 512], BF16)
        pck = P.ps("pck", [128, 128], BF16)
    xs = ring("x", [128, D]); junk = P.sb("junk", [128, D]); st = ring("st", [128, 8])
    xn = ring("xn", [128, D], BF16); hT = ring("hT", [128, KC, 128], BF16)
    yt = ring("yt", [128, max(ntm, 1)]); ytb = ring("ytb", [128, max(ntm, 1)], BF16)
    nspec = max(len(fm_specs), 1)
    fo = [[P.sb("fo%d_%d" % (i, j), [128, 128], (BF16 if fm_specs[j][3] == BF16 else F32)) for j in range(len(fm_specs))] for i in range(R)]
    pT = [P.ps("pT%d" % i, [128, KC, 128], BF16) for i in range(2)]
    py = [P.ps("py%d" % i, [128, 512]) for i in range(2)]; pf = [P.ps("pf%d" % i, [128, 4, 128]) for i in range(2)]
    if rope:
        Ct = ring("Ct", [64, 128]); St = ring("St", [64, 128]); r1 = ring("r1", [64, 128]); r2 = ring("r2", [64, 128]); rb_ = ring("rb", [64, 128], BF16)
    cnt = dict(y=0, f=0)

    def A(t):
        r0, v = tiles[t]; a = t % R; b = t % 2
        P.dma("sp", xs[a].v, x_d[r0:r0 + 128, :])
        P.op("act", "activation", out=junk.v, in_=xs[a].v, func=AF.Square, accum_out=st[a][:, 0:1])
        rstd_of(P, st[a][:, 0:1], st[a][:, 2:3], st[a][:, 1:2], D)
        P.op("dve", "tensor_scalar", out=xn[a].v, in0=xs[a].v, scalar1=st[a][:, 2:3], scalar2=None, op0=ALU.mult)
        for k in range(KC):
            P.op("pe", "transpose", out=pT[b][:, k, :], in_=xn[a][:, k * 128:(k + 1) * 128], identity=idb.v, track=(k == KC - 1))
        for k in range(KC):
            P.op("dve", "tensor_scalar", out=hT[a][:, k, :], in0=pT[b][:, k, :], scalar1=gsc[:, v, k:k + 1], scalar2=gsh[:, v, k:k + 1], op0=ALU.mult, op1=ALU.add)

    def B1(t):
        r0, v = tiles[t]; a = t % R
        if ntm:
            for c0 in range(0, ntm, 512):
                cn = min(512, ntm - c0); pb = py[cnt["y"] % 2]; cnt["y"] += 1
                for k in range(KC):
                    P.op("pe", "matmul", track=(k == KC - 1), out=pb[:, 0:cn], lhsT=hT[a][:, k, :], rhs=wtm[:, k, c0:c0 + cn], start=(k == 0), stop=(k == KC - 1))
                P.op("act", "activation", out=yt[a][:, c0:c0 + cn], in_=pb[:, 0:cn], func=AF.Copy)
            for (c0, n, ap, dt) in tm_outs:
                if dt == BF16:
                    P.op("pool", "tensor_copy", out=ytb[a][:, c0:c0 + n], in_=yt[a][:, c0:c0 + n])
                    P.dma("sp", ap[r0:r0 + 128, :], ytb[a][:, c0:c0 + n])
                else:
                    P.dma("sp", ap[r0:r0 + 128, :], yt[a][:, c0:c0 + n])
        if mla:
            cc = mla["col"]
            P.op("act", "activation", out=junk[:, 0:128], in_=yt[a][:, cc:cc + 128], func=AF.Square, accum_out=st[a][:, 4:5])
            rstd_of(P, st[a][:, 4:5], st[a][:, 6:7], st[a][:, 5:6], 128)
            P.op("dve", "tensor_scalar", out=ckn[a].v, in0=yt[a][:, cc:cc + 128], scalar1=st[a][:, 6:7], scalar2=None, op0=ALU.mult)
            P.op("pe", "transpose", out=pck.v, in_=ckn[a].v, identity=idb.v)
            P.op("dve", "tensor_scalar", out=ckT[a].v, in0=pck.v, scalar1=kvn[:, 0:1], scalar2=None, op0=ALU.mult)

    def B2(t):
        r0, v = tiles[t]; a = t % R
        for si in range(0, len(fm_specs), 4):
            chunk = fm_specs[si:si + 4]
            pb = pf[cnt["f"] % 2]; cnt["f"] += 1
            for j, (c0, m, ap, dt, scale) in enumerate(chunk):
                for k in range(KC):
                    P.op("pe", "matmul", track=(k == KC - 1), out=pb[0:m, j, :], lhsT=wfm[:, k, c0:c0 + m], rhs=hT[a][:, k, :], start=(k == 0), stop=(k == KC - 1))
            for j, (c0, m, ap, dt, scale) in enumerate(chunk):
                dst = fo[a][si + j]
                P.op("act", "activation", out=dst[0:m, :], in_=pb[0:m, j, :], func=AF.Copy, scale=float(scale))
                P.dma("sp", ap[:, r0:r0 + 128], dst[0:m, :])
        if rope:
            ckr, ckrs, ap, C_d, S_d = rope
            P.dma("sp", Ct[a].v, C_d[:, r0:r0 + 128]); P.dma("sp", St[a].v, S_d[:, r0:r0 + 128])
            pb = pf[cnt["f"] % 2]; cnt["f"] += 1
            for k in range(KC):
                P.op("pe", "matmul", track=(k == KC - 1), out=pb[0:64, 0, :], lhsT=wfm[:, k, ckr:ckr + 64], rhs=hT[a][:, k, :], start=(k == 0), stop=(k == KC - 1))
            for k in range(KC):
                P.op("pe", "matmul", track=(k == KC - 1), out=pb[0:64, 1, :], lhsT=wfm[:, k, ckrs:ckrs + 64], rhs=hT[a][:, k, :], start=(k == 0), stop=(k == KC - 1))
            P.op("dve", "tensor_tensor", out=r1[a].v, in0=pb[0:64, 0, :], in1=Ct[a].v, op=ALU.mult)
            P.op("dve", "tensor_tensor", out=r2[a].v, in0=pb[0:64, 1, :], in1=St[a].v, op=ALU.mult)
            P.op("pool", "tensor_tensor", out=rb_[a].v, in0=r1[a].v, in1=r2[a].v, op=ALU.add)
            P.dma("sp", ap[:, r0:r0 + 128], rb_[a].v)
        if mla:
            pb = py[cnt["y"] % 2]; cnt["y"] += 1
            P.op("pe", "matmul", out=pb.v, lhsT=ckT[a].v, rhs=wuk[:, 0, 512:1024], start=True, stop=True)
            P.op("act", "activation", out=vst[a].v, in_=pb.v, func=AF.Copy)
            P.dma("sp", mla["V_s"][r0:r0 + 128, :], vst[a].v)
            pb = pf[cnt["f"] % 2]; cnt["f"] += 1
            for h in range(4):
                P.op("pe", "matmul", out=pb[:, h, :], lhsT=wuk[:, 0, h * 128:(h + 1) * 128], rhs=ckT[a].v, start=True, stop=True, track=(h == 3))
            P.op("act", "activation", out=kst[a].v, in_=pb.v, func=AF.Copy)
            P.dma("sp", mla["kT_s"][:, :, r0:r0 + 128].rearrange("h p t -> p h t"), kst[a].v)

    xsteps = extra(P) if extra else []
    xi = 0
    for step in range(nt + 2):
        for _ in range(per_tile):
            if xi < len(xsteps):
                xsteps[xi](); xi += 1
        if step < nt:
            A(step)
        if 0 <= step - 1 < nt:
            B1(step - 1)
        if 0 <= step - 2 < nt:
            B2(step - 2)
    while xi < len(xsteps):
        xsteps[xi](); xi += 1
    P.end()


def stage_moe_sp(P, idb, variants, passes, hTok_s, G_s, GT_s, x1_s, w1_d, w2_d, b1_d, b2_d, g3_d, gates_s, l, x2_ap, iota_d, lst_d, NE=32):
    P.begin()
    nt = len(variants); nv = 2; D = 1024; KC = 8; F = 1024
    mp = max(sum(len(g) for g in ps_) for ps_ in passes)
    b2 = P.sb("b2", [32, D]); P.dma("sp", b2.v, b2_d)
    iota = P.sb("iota", [128, 128]); P.dma("sp", iota.v, iota_d)
    lst32 = P.sb("lst32", [128, 128]); P.dma("sp", lst32.v, lst_d)
    lst = P.sb("lst", [128, 128], BF16); P.op("dve", "tensor_copy", out=lst.v, in_=lst32.v)
    onesb = P.sb("onesb", [128, 128], BF16); P.op("dve", "memset", ap=onesb.v, constant=1.0)
    stg = [P.sb("stg%d" % i, [128, 2 * F]) for i in range(2)]
    w1b = [P.sb("w1b%d" % i, [128, KC, 2 * F], BF16) for i in range(2)]
    w2b = [P.sb("w2b%d" % i, [128, KC, D], BF16) for i in range(2)]
    b1b = [P.sb("b1b%d" % i, [1, 2 * F], BF16) for i in range(2)]
    Gs = P.sb("Gs", [128, mp, 32]); GTs = P.sb("GTs", [32, mp * 128]); msk = P.sb("msk", [128, mp, 32]); mskb = P.sb("mskb", [128, mp, 32], BF16)
    pos = P.sb("pos", [128, mp, 32])
    yacc = P.sb("yacc", [128, mp, D])
    htm = [P.sb("htm%d" % i, [128, 4, D], BF16) for i in range(2)]
    Sel = [[P.sb("Sel%d_%d" % (j, i), [128, 128], BF16) for i in range(4)] for j in range(2)]
    SelG = [[P.sb("SelG%d_%d" % (j, i), [128, 128], BF16) for i in range(4)] for j in range(2)]
    XeT = [P.sb("XeT%d" % i, [128, KC, 128], BF16) for i in range(2)]
    tgh = [P.sb("tg%d" % i, [128, 512]) for i in range(2)]; tlh = [P.sb("tl%d" % i, [128, 512], BF16) for i in range(2)]
    tsgh = [P.sb("tsg%d" % i, [128, 512], BF16) for i in range(2)]; actbh = [P.sb("actb%d" % i, [128, 512], BF16) for i in range(2)]
    actT = P.sb("actT", [128, KC, 128], BF16); Ye = P.sb("Ye", [128, D], BF16); SGT = P.sb("SGT", [128, 4, 128], BF16)
    st = P.sb("st", [128, 4])
    pX = P.ps("pX", [128, 1024]); pUa = P.ps("pUa", [128, 1024]); pUb = P.ps("pUb", [128, 1024])
    pT = P.ps("pT", [128, KC, 128], BF16); pST = P.ps("pST", [128, 4, 128], BF16)
    pXv = pX.v.rearrange("p (f q) -> p f q", q=128)

    def load_expert_steps(e, buf):
        steps = []
        for k in range(KC):
            def step(k=k):
                P.dma("sp", w1b[buf][:, k, :], w1_d[e, k * 128:(k + 1) * 128, :])
                P.dma("sp", w2b[buf][:, k, :], w2_d[e, k * 128:(k + 1) * 128, :])
                if k == KC - 1:
                    P.dma("sp", b1b[buf].v, b1_d[e:e + 1, :])
            steps.append(step)
        return steps

    for groups in passes:
        ptiles = [t for g in groups for t in g]
        t0 = ptiles[0]; npt = len(ptiles); tok0 = t0 * 128; ntok = npt * 128
        P.dma("sp", Gs[:, 0:npt, :], G_s[tok0:tok0 + ntok, :].rearrange("(g p) e -> p g e", p=128))
        P.dma("sp", GTs[:, 0:ntok], GT_s[:, tok0:tok0 + ntok])
        P.op("dve", "tensor_scalar", out=msk[:, 0:npt, :], in0=Gs[:, 0:npt, :], scalar1=0.0, scalar2=None, op0=ALU.is_gt)
        P.op("dve", "tensor_copy", out=mskb[:, 0:npt, :], in_=msk[:, 0:npt, :])
        for i in range(npt):
            for n in range(2):
                P.op("pe", "matmul", out=pX[:, n * 512:(n + 1) * 512], lhsT=GTs[:, i * 128:(i + 1) * 128], rhs=b2[:, n * 512:(n + 1) * 512], start=True, stop=True)
            P.op("act", "activation", out=yacc[:, i, :], in_=pX.v, func=AF.Copy)
        for g in groups:
            for gi_, t in enumerate(g):
                i = t - t0
                for j_ in range(gi_):
                    P.op("pe", "matmul", track=False, out=pUa[:, 0:32], lhsT=onesb.v, rhs=mskb[:, g[j_] - t0, :], start=(j_ == 0), stop=False)
                P.op("pe", "matmul", out=pUa[:, 0:32], lhsT=lst.v, rhs=mskb[:, i, :], start=(gi_ == 0), stop=True)
                P.op("dve", "tensor_copy", out=pos[:, i, :], in_=pUa[:, 0:32])
        for s_ in load_expert_steps(0, 0):
            s_()
        its = [(e, gidx) for e in range(NE) for gidx in range(len(groups))]
        nxt_cache = {}

        def prep(n):
            e, gidx = its[n]; g = groups[gidx]; ng = len(g); i0 = g[0] - t0; s2 = n % 2
            hb = htm[s2]
            P.dma("sp", hb[:, 0:ng, :], hTok_s[g[0] * 128:(g[0] + ng) * 128, :].rearrange("(g p) f -> p g f", p=128))
            for ii in range(ng):
                P.op("dve", "tensor_scalar", out=Sel[s2][ii].v, in0=iota.v, scalar1=pos[:, i0 + ii, e:e + 1], scalar2=msk[:, i0 + ii, e:e + 1], op0=ALU.is_equal, op1=ALU.mult)
                P.op("dve", "tensor_scalar", out=SelG[s2][ii].v, in0=iota.v, scalar1=pos[:, i0 + ii, e:e + 1], scalar2=Gs[:, i0 + ii, e:e + 1], op0=ALU.is_equal, op1=ALU.mult)
            for f in range(KC):
                for ii in range(ng):
                    P.op("pe", "matmul", track=(f == KC - 1 and ii == ng - 1), out=pXv[:, f, :], lhsT=hb[:, ii, f * 128:(f + 1) * 128], rhs=Sel[s2][ii].v, start=(ii == 0), stop=(ii == ng - 1))
            P.op("act", "activation", out=XeT[s2].v, in_=pXv, func=AF.Copy)

        def ffn1(n):
            e, gidx = its[n]; buf = e % 2; s2 = n % 2
            for h in range(2):
                pu = (pUa, pUb)[h]
                for c in range(2):
                    col = h * 1024 + c * 512
                    for k in range(KC):
                        P.op("pe", "matmul", track=False, out=pu[:, c * 512:(c + 1) * 512], lhsT=XeT[s2][:, k, :], rhs=w1b[buf][:, k, col:col + 512], start=(k == 0), stop=False)
                    P.op("pe", "matmul", track=(c == 1), out=pu[:, c * 512:(c + 1) * 512], lhsT=onesb[0:1, :], rhs=b1b[buf][0:1, col:col + 512], start=False, stop=True)
                uv = pu.v.rearrange("p (f two) -> p f two", two=2)
                P.op("dve", "tensor_scalar", out=tgh[h].v, in0=uv[:, :, 0], scalar1=7.0, scalar2=None, op0=ALU.min)
                P.op("act", "activation", out=tsgh[h].v, in_=tgh[h].v, func=AF.Sigmoid, scale=1.702)
                P.op("dve", "tensor_scalar", out=tlh[h].v, in0=uv[:, :, 1], scalar1=-7.0, scalar2=7.0, op0=ALU.max, op1=ALU.min)
                P.op("pool", "tensor_tensor", out=tgh[h].v, in0=tgh[h].v, in1=tsgh[h].v, op=ALU.mult)
                P.op("pool", "tensor_scalar", out=tlh[h].v, in0=tlh[h].v, scalar1=1.0, scalar2=None, op0=ALU.add)
                P.op("pool", "tensor_tensor", out=actbh[h].v, in0=tlh[h].v, in1=tgh[h].v, op=ALU.mult)

        def stage3(n):
            e, gidx = its[n]; g = groups[gidx]; ng = len(g); i0 = g[0] - t0; buf = e % 2; s2 = n % 2
            for f in range(KC):
                P.op("pe", "transpose", out=pT[:, f, :], in_=actbh[f // 4][:, (f % 4) * 128:(f % 4 + 1) * 128], identity=idb.v, track=(f == KC - 1))
            P.op("act", "activation", out=actT.v, in_=pT.v, func=AF.Copy)
            for ii in range(ng):
                P.op("pe", "transpose", out=pST[:, ii, :], in_=SelG[s2][ii].v, identity=idb.v, track=(ii == ng - 1))
            P.op("dve", "tensor_copy", out=SGT[:, 0:ng, :], in_=pST[:, 0:ng, :])
            for half in range(2):
                hs = slice(half * 512, (half + 1) * 512)
                for k in range(KC):
                    P.op("pe", "matmul", track=(k == KC - 1), out=pUa[:, hs], lhsT=actT[:, k, :], rhs=w2b[buf][:, k, hs], start=(k == 0), stop=(k == KC - 1))
            P.op("act", "activation", out=Ye.v, in_=pUa.v, func=AF.Copy)
            ci = 0
            for ii in range(ng):
                for half in range(2):
                    hs = slice(half * 512, (half + 1) * 512); pc = pUb[:, (ci % 2) * 512:(ci % 2 + 1) * 512]; ci += 1
                    P.op("pe", "matmul", out=pc, lhsT=SGT[:, ii, :], rhs=Ye[:, hs], start=True, stop=True)
                    P.op("dve", "tensor_tensor", out=yacc[:, i0 + ii, hs], in0=pc, in1=yacc[:, i0 + ii, hs], op=ALU.add)
            if e + 1 < NE:
                if e not in nxt_cache:
                    nxt_cache[e] = load_expert_steps(e + 1, 1 - buf)
                nxt = nxt_cache[e]
                per = (len(nxt) + len(groups) - 1) // len(groups)
                for s_ in nxt[gidx * per:(gidx + 1) * per]:
                    s_()

        prep(0)
        for n in range(len(its)):
            ffn1(n)
            if n + 1 < len(its):
                prep(n + 1)
            stage3(n)
        P.dma("sp", stg[1][:, 0:D], g3_d)
        for v in range(nv):
            P.dma("sp", stg[0][:, v * D:(v + 1) * D], gates_s[l, 1, v])
            P.op("dve", "tensor_tensor", out=stg[0][:, v * D:(v + 1) * D], in0=stg[0][:, v * D:(v + 1) * D], in1=stg[1][:, 0:D], op=ALU.mult)
        hf = htm[0].v.rearrange("p a b -> p (a b)").bitcast(F32)
        for i in range(npt):
            t = ptiles[i]; v = variants[t]; sl = slice(t * 128, (t + 1) * 128)
            xt = hf[:, 0:D]; junk = hf[:, D:2 * D]; tmp = stg[1][:, D:2 * D]
            P.dma("sp", xt, x1_s[sl, :])
            P.op("act", "activation", out=junk, in_=yacc[:, i, :], func=AF.Square, accum_out=st[:, 0:1])
            rstd_of(P, st[:, 0:1], st[:, 2:3], st[:, 1:2], D)
            P.op("dve", "scalar_tensor_tensor", out=tmp, in0=yacc[:, i, :], scalar=st[:, 2:3], in1=stg[0][:, v * D:(v + 1) * D], op0=ALU.mult, op1=ALU.mult)
            P.op("pool", "tensor_tensor", out=xt, in0=tmp, in1=xt, op=ALU.add)
            P.dma("sp", x2_ap[sl, :], xt)
    P.end()
```
